# Optimizing a Trainium2 kernel written in Bass

```python
import jax, jax.numpy as jnp
from jax import lax
import numpy as np

D_MODEL = 1024
BATCH = 32
SEQ = 2048
DEPTH = 1

FOX_HEADS = 8
FOX_HEAD_DIM = 64
FOX_WIDTH = FOX_HEADS * FOX_HEAD_DIM
RET_HEADS = 4
RET_HEAD_DIM = 128
RET_WIDTH = RET_HEADS * RET_HEAD_DIM
MIX_WIDTH = FOX_WIDTH + RET_WIDTH
D_FF = 4 * D_MODEL
Q_BLOCK = 128
RET_CHUNK = 128
ROPE_BASE = 10000.0
EPS = 1e-6
N_MOD = 6
IN_COLS = 4 * FOX_WIDTH + FOX_HEADS + 4 * RET_WIDTH

kernel_name = "hybrid_fox_retention_adaln_block"


def _rms(x):
    x32 = x.astype(jnp.float32)
    return x32 * lax.rsqrt(jnp.mean(x32 * x32, axis=-1, keepdims=True) + EPS)


def _rope(x):
    s, d = x.shape[1], x.shape[-1]
    pos = jnp.arange(s, dtype=jnp.float32)
    inv_freq = ROPE_BASE ** (-jnp.arange(0, d, 2, dtype=jnp.float32) / d)
    ang = pos[:, None] * inv_freq[None, :]
    cos = jnp.cos(ang)[None, :, None, :]
    sin = jnp.sin(ang)[None, :, None, :]
    x32 = x.astype(jnp.float32)
    x1, x2 = x32[..., : d // 2], x32[..., d // 2:]
    return jnp.concatenate([x1 * cos - x2 * sin, x1 * sin + x2 * cos], axis=-1).astype(x.dtype)


def _forgetting_attention(q, k, v, log_f):
    b, s, h, d = q.shape
    scale = 1.0 / np.sqrt(d)
    cum = jnp.cumsum(log_f, axis=1).transpose(0, 2, 1)
    qh = q.transpose(0, 2, 1, 3)
    kh = k.transpose(0, 2, 1, 3)
    vh = v.transpose(0, 2, 1, 3)
    outs = []
    for blk in range(s // Q_BLOCK):
        s0, s1 = blk * Q_BLOCK, (blk + 1) * Q_BLOCK
        logits = jnp.einsum('bhqd,bhkd->bhqk', qh[:, :, s0:s1], kh[:, :, :s1]).astype(jnp.float32) * scale
        logits = logits + cum[:, :, s0:s1, None] - cum[:, :, None, :s1]
        q_pos = jnp.arange(s0, s1)[:, None]
        k_pos = jnp.arange(s1)[None, :]
        logits = jnp.where(q_pos >= k_pos, logits, -jnp.inf)
        p = jax.nn.softmax(logits, axis=-1).astype(v.dtype)
        outs.append(jnp.einsum('bhqk,bhkd->bhqd', p, vh[:, :, :s1]))
    out = jnp.concatenate(outs, axis=2)
    return out.transpose(0, 2, 1, 3)


def _retention(q, k, v):
    b, s, h, dk = q.shape
    dv = v.shape[-1]
    n_chunks = s // RET_CHUNK
    log_g = jnp.log(1.0 - 2.0 ** (-5.0 - jnp.arange(h, dtype=jnp.float32)))
    n = jnp.arange(RET_CHUNK, dtype=jnp.float32)
    diff = n[:, None] - n[None, :]
    decay_mask = jnp.where(diff[None] >= 0,
                           jnp.exp(jnp.maximum(diff, 0.0)[None] * log_g[:, None, None]), 0.0)
    xi = jnp.exp((n[None, :] + 1.0) * log_g[:, None])
    zeta = jnp.exp((RET_CHUNK - 1.0 - n[None, :]) * log_g[:, None])
    g_chunk = jnp.exp(RET_CHUNK * log_g)

    def to_chunks(t):
        return t.astype(jnp.float32).reshape(b, n_chunks, RET_CHUNK, h, t.shape[-1]).transpose(1, 0, 3, 2, 4)

    qc, kc, vc = to_chunks(q), to_chunks(k * (dk ** -0.5)), to_chunks(v)

    def step(state, inp):
        qi, ki, vi = inp
        inner = jnp.einsum('bhnd,bhmd->bhnm', qi, ki) * decay_mask[None]
        inner_out = jnp.einsum('bhnm,bhmv->bhnv', inner, vi)
        cross_out = jnp.einsum('bhnd,bhdv->bhnv', qi, state) * xi[None, :, :, None]
        new_state = state * g_chunk[None, :, None, None] + \
            jnp.einsum('bhmd,bhmv->bhdv', ki * zeta[None, :, :, None], vi)
        return new_state, inner_out + cross_out

    state0 = jnp.zeros((b, h, dk, dv), jnp.float32)
    _, out = lax.scan(step, state0, (qc, kc, vc))
    return out.transpose(1, 0, 3, 2, 4).reshape(b, s, h, dv)


def setup_inputs(seed: int = 0) -> dict:
    key = jax.random.key(seed)
    ks = jax.random.split(key, 16)
    nrm = jax.random.normal
    f32 = jnp.float32
    return {
        "x": nrm(ks[0], (BATCH, SEQ, D_MODEL), f32),
        "c": nrm(ks[1], (BATCH, D_MODEL), f32),
        "w_ada": nrm(ks[2], (DEPTH, D_MODEL, N_MOD * D_MODEL), f32) * (0.5 * D_MODEL ** -0.5),
        "b_ada": nrm(ks[3], (DEPTH, N_MOD * D_MODEL), f32) * 0.02,
        "w_in": nrm(ks[4], (DEPTH, D_MODEL, IN_COLS), f32) * D_MODEL ** -0.5,
        "b_forget": 2.0 + 0.5 * nrm(ks[5], (DEPTH, FOX_HEADS), f32),
        "q_norm_gain": 1.0 + 0.05 * nrm(ks[6], (DEPTH, FOX_HEAD_DIM), f32),
        "k_norm_gain": 1.0 + 0.05 * nrm(ks[7], (DEPTH, FOX_HEAD_DIM), f32),
        "fox_out_gain": 1.0 + 0.05 * nrm(ks[8], (DEPTH, FOX_HEADS, FOX_HEAD_DIM), f32),
        "ret_out_gain": 1.0 + 0.05 * nrm(ks[9], (DEPTH, RET_HEADS, RET_HEAD_DIM), f32),
        "w_out": nrm(ks[10], (DEPTH, MIX_WIDTH, D_MODEL), f32) * MIX_WIDTH ** -0.5,
        "w_mlp_in": nrm(ks[11], (DEPTH, D_MODEL, D_FF), f32) * D_MODEL ** -0.5,
        "w_mlp_out": nrm(ks[12], (DEPTH, D_FF, D_MODEL), f32) * D_FF ** -0.5,
    }


def reference(x, c, w_ada, b_ada, w_in, b_forget, q_norm_gain, k_norm_gain,
              fox_out_gain, ret_out_gain, w_out, w_mlp_in, w_mlp_out):
    b, s, _ = x.shape
    dt = x.dtype
    c_act = jax.nn.silu(c)
    o_fq, o_fk, o_fv, o_fog = 0, FOX_WIDTH, 2 * FOX_WIDTH, 3 * FOX_WIDTH
    o_ff = 4 * FOX_WIDTH
    o_rq = o_ff + FOX_HEADS
    o_rk, o_rv, o_rg = o_rq + RET_WIDTH, o_rq + 2 * RET_WIDTH, o_rq + 3 * RET_WIDTH
    for l in range(DEPTH):
        mod = jnp.einsum('bd,de->be', c_act, w_ada[l]) + b_ada[l]
        shift_m, scale_m, gate_m, shift_f, scale_f, gate_f = [m[:, None, :] for m in jnp.split(mod, N_MOD, axis=-1)]

        h = (_rms(x) * (1.0 + scale_m) + shift_m).astype(dt)
        proj = jnp.einsum('bsd,de->bse', h, w_in[l])

        fq = proj[..., o_fq:o_fk].reshape(b, s, FOX_HEADS, FOX_HEAD_DIM)
        fk = proj[..., o_fk:o_fv].reshape(b, s, FOX_HEADS, FOX_HEAD_DIM)
        fv = proj[..., o_fv:o_fog].reshape(b, s, FOX_HEADS, FOX_HEAD_DIM)
        f_og = proj[..., o_fog:o_ff]
        f_logit = proj[..., o_ff:o_rq]
        fq = (_rms(fq) * q_norm_gain[l]).astype(dt)
        fk = (_rms(fk) * k_norm_gain[l]).astype(dt)
        log_f = jax.nn.log_sigmoid(f_logit.astype(jnp.float32) + b_forget[l])
        fox = _forgetting_attention(fq, fk, fv, log_f)
        fox = (_rms(fox) * fox_out_gain[l]).reshape(b, s, FOX_WIDTH) * jax.nn.sigmoid(f_og.astype(jnp.float32))

        rq = _rope(proj[..., o_rq:o_rk].reshape(b, s, RET_HEADS, RET_HEAD_DIM))
        rk = _rope(proj[..., o_rk:o_rv].reshape(b, s, RET_HEADS, RET_HEAD_DIM))
        rv = proj[..., o_rv:o_rg].reshape(b, s, RET_HEADS, RET_HEAD_DIM)
        r_gate = proj[..., o_rg:]
        ret = _retention(rq, rk, rv)
        ret = (_rms(ret) * ret_out_gain[l]).reshape(b, s, RET_WIDTH) * jax.nn.silu(r_gate.astype(jnp.float32))

        mixed = jnp.concatenate([fox, ret], axis=-1).astype(dt)
        x = x + gate_m * jnp.einsum('bse,ed->bsd', mixed, w_out[l])

        h = (_rms(x) * (1.0 + scale_f) + shift_f).astype(dt)
        u = jnp.square(jax.nn.relu(jnp.einsum('bsd,df->bsf', h, w_mlp_in[l])))
        x = x + gate_f * jnp.einsum('bsf,fd->bsd', u, w_mlp_out[l])
    return x
```

```python
import numpy as np
import ml_dtypes
from contextlib import ExitStack
import concourse.bass as bass
import concourse.mybir as mybir
from concourse.bass_utils import run_bass_kernel_spmd

F32 = mybir.dt.float32
BF16 = mybir.dt.bfloat16
AF = mybir.ActivationFunctionType
ALU = mybir.AluOpType
AX = mybir.AxisListType

NCORES = 8
D = 1024
S = 2048
NSEQ = 4
NT = 16
G = 4
NG = NT // G
DFF = 4096
EPS = 1e-6
IN_COLS = 4104
NCHUNK = 26

ENGS = ("pe", "act", "dve", "pool", "sp")


class _Op:
    __slots__ = ("fn", "waits", "sig", "dma", "tok")


class Prog:
    def __init__(self):
        self.ops = {e: [] for e in ENGS}
        self.res = {}
        self.known = {e: {} for e in ENGS}
        self.clock = {}
        self.dma_count = {}
        self.needed = set()

    def _deps(self, eng, reads, writes):
        deps = set()
        for k in reads:
            r = self.res.get(k)
            if r is not None and r[0] is not None:
                deps.add(r[0])
        for k in writes:
            r = self.res.get(k)
            if r is not None:
                if r[0] is not None and r[0][0] != eng:
                    deps.add(r[0])
                for src, v in r[1].items():
                    if src != eng:
                        deps.add((src, v))
        return deps

    def _commit(self, tok, reads, writes):
        for k in reads:
            r = self.res.get(k)
            if r is None:
                r = [None, {}]
                self.res[k] = r
            if r[1].get(tok[0], 0) < tok[1]:
                r[1][tok[0]] = tok[1]
        for k in writes:
            self.res[k] = [tok, {}]

    def op(self, eng, fn, reads=(), writes=(), dma=None):
        deps = self._deps(eng if dma is None else "dma:" + dma, reads, writes)
        kn = self.known[eng]
        waits = []
        for (src, v) in sorted(deps, key=lambda t: (str(t[0]), t[1])):
            if kn.get(src, 0) >= v:
                continue
            waits.append((src, v))
            self.needed.add((src, v))
            for s2, v2 in self.clock[(src, v)].items():
                if kn.get(s2, 0) < v2:
                    kn[s2] = v2
        o = _Op()
        o.fn = fn
        o.waits = waits
        o.dma = dma
        self.ops[eng].append(o)
        if dma is None:
            tok = (eng, len(self.ops[eng]))
        else:
            src = "dma:" + dma
            self.dma_count[src] = self.dma_count.get(src, 0) + 16
            tok = (src, self.dma_count[src])
        o.tok = tok
        ck = dict(kn)
        ck[tok[0]] = tok[1]
        self.clock[tok] = ck
        self._commit(tok, reads, writes)
        return tok

    def wait_all(self, eng, toks):
        kn = self.known[eng]
        waits = []
        for (src, v) in toks:
            if kn.get(src, 0) >= v:
                continue
            waits.append((src, v))
            self.needed.add((src, v))
            kn[src] = v
        o = _Op()
        o.fn = None
        o.waits = waits
        o.dma = None
        o.tok = None
        self.ops[eng].append(o)

    def emit(self, nc, es):
        sems = {}
        for e in ("pe", "act", "dve", "pool"):
            sems[e] = es.enter_context(nc.semaphore("sem_" + e))
        for src in self.dma_count:
            sems[src] = es.enter_context(nc.semaphore("sem_" + src.replace(":", "_")))
        sigval = {}
        for e in ("pe", "act", "dve", "pool"):
            cnt = 0
            for i, o in enumerate(self.ops[e]):
                o.sig = False
                if o.fn is not None and o.dma is None and (e, i + 1) in self.needed:
                    cnt += 1
                    o.sig = True
                    sigval[(e, i + 1)] = cnt
        blk = es.enter_context(nc.Block())

        def run(e, name):
            for o in self.ops[name]:
                for (src, v) in o.waits:
                    val = v if src.startswith("dma:") else sigval[(src, v)]
                    e.wait_ge(sems[src], val)
                if o.fn is None:
                    continue
                ins = o.fn(e)
                if o.dma is not None:
                    ins.then_inc(sems["dma:" + o.dma], 16)
                elif o.sig:
                    ins.then_inc(sems[name], 1)

        @blk.tensor
        def _(e):
            run(e, "pe")

        @blk.scalar
        def _(e):
            run(e, "act")

        @blk.vector
        def _(e):
            run(e, "dve")

        @blk.gpsimd
        def _(e):
            run(e, "pool")

        @blk.sync
        def _(e):
            run(e, "sp")


def _constants():
    f = np.float32
    n = np.arange(128, dtype=f)
    ident = np.eye(128, dtype=f)
    tri = (n[:, None] <= n[None, :]).astype(f)
    ones = np.ones((128, 128), f)
    h = np.arange(4, dtype=f)
    log_g = np.log(f(1.0) - f(2.0) ** (f(-5.0) - h)).astype(f)
    diff = n[None, :] - n[:, None]
    maskT = np.where(diff[None] >= 0, np.exp(np.maximum(diff, 0.0)[None] * log_g[:, None, None]), 0.0).astype(f)
    retmaskT = np.ascontiguousarray(maskT.transpose(1, 0, 2))
    xi = np.exp((n[None, :] + 1.0) * log_g[:, None]).astype(f)
    xi_bc = np.ascontiguousarray(np.broadcast_to(xi[None], (128, 4, 128))).astype(f)
    zeta = np.exp((128 - 1.0 - n[None, :]) * log_g[:, None]).astype(f)
    zeta_t = np.ascontiguousarray(zeta.T)
    g_chunk = np.exp(f(128.0) * log_g).astype(f)
    pos = np.arange(S, dtype=f)
    inv_freq = (f(10000.0) ** (-np.arange(0, 128, 2, dtype=f) / f(128))).astype(f)
    ang = (pos[:, None] * inv_freq[None, :]).astype(f)
    cos = np.cos(ang).astype(f)
    sin = np.sin(ang).astype(f)
    ks = f(128.0 ** -0.5)

    def lay(a):
        return np.ascontiguousarray(a.reshape(16, 128, 64).transpose(1, 0, 2))

    rope = np.stack([lay(cos), lay(sin), lay(-sin), lay(cos * ks), lay(sin * ks), lay(-sin * ks)], 0)
    sel = np.zeros((4, 4, 128), f)
    for b in range(4):
        sel[b, b, :] = 1.0
    return dict(
        identf=ident, identb=ident.astype(ml_dtypes.bfloat16), trib=tri.astype(ml_dtypes.bfloat16),
        trif=tri, onesf=ones, retmaskT=retmaskT, xi_bc=xi_bc, zeta_t=zeta_t,
        rope=np.ascontiguousarray(rope), g_chunk=g_chunk,
    )


_CONST = None


def build_program():
    consts = _constants()
    g_chunk = [float(v) for v in consts["g_chunk"]]
    nc = bass.Bass("TRN2", target_bir_lowering=False)
    P = Prog()
    es = ExitStack()

    def din(name, shape, dt=F32):
        return nc.dram_tensor(name, list(shape), dt, kind="ExternalInput").ap()

    x_d = din("x", [NSEQ * S, D])
    c_d = din("c", [NSEQ, D])
    wada_d = din("w_ada", [D, 6 * D])
    bada_d = din("b_ada", [1, 6 * D])
    win_d = din("w_in", [D, IN_COLS])
    bfg_d = din("b_forget", [1, 8])
    qg_d = din("q_gain", [1, 64])
    kg_d = din("k_gain", [1, 64])
    fxg_d = din("fox_gain", [1, 512])
    rtg_d = din("ret_gain", [1, 512])
    wout_d = din("w_out", [D, D])
    w1_d = din("w1", [D, DFF])
    w2_d = din("w2", [DFF, D])
    identf_d = din("identf", [128, 128])
    identb_d = din("identb", [128, 128], BF16)
    trib_d = din("trib", [128, 128], BF16)
    trif_d = din("trif", [128, 128])
    onesf_d = din("onesf", [128, 128])
    retm_d = din("retmaskT", [128, 4, 128])
    xibc_d = din("xi_bc", [128, 4, 128])
    zeta_d = din("zeta_t", [128, 4])
    rope_d = din("rope", [6, 128, 16, 64])
    y_d = nc.dram_tensor("y", [NSEQ * S, D], F32, kind="ExternalOutput").ap()
    wbf_d = nc.dram_tensor("wbf", [NCHUNK, 128, 4096], BF16, kind="Internal").ap()
    gates_d = nc.dram_tensor("gates_scr", [4, 2048], F32, kind="Internal").ap()

    def sb(name, shape, dt=F32):
        return es.enter_context(nc.sbuf_tensor(name, list(shape), dt))

    wbuf = [sb(f"wbuf{i}", [128, 4096], BF16) for i in range(3)]
    xs = sb("xs", [128, G, D])
    hTA = sb("hTA", [128, 8, 512], BF16)
    hTB = sb("hTB", [128, 8, 512], BF16)
    U = sb("U", [128, 16384], BF16)
    KT = sb("KT", [70, 8, S], BF16)
    Vaug = sb("Vaug", [128, NT, 8, 65], BF16)
    qaug = [sb(f"qaug{i}", [128, 8, 70], BF16) for i in range(2)]
    kaug = [sb(f"kaug{i}", [128, 8, 70], BF16) for i in range(2)]
    state = sb("state", [128, 4, 128])
    state_bf = sb("state_bf", [128, 4, 128], BF16)
    mixed = sb("mixed", [128, G, D], BF16)
    gates_f = sb("gates_f", [128, G, 512], BF16)
    gates_r = sb("gates_r", [128, G, 512], BF16)
    PT = [sb(f"PT{i}", [128, 512], BF16) for i in range(3)]
    STr = [sb(f"STr{i}", [128, 4, 128], BF16) for i in range(2)]
    TA = [sb(f"TA{i}", [128, 512]) for i in range(2)]
    TB = [sb(f"TB{i}", [128, 512]) for i in range(2)]
    TE = [sb(f"TE{i}", [128, 4, 64]) for i in range(3)]
    ropeg = sb("ropeg", [128, 6, G, 64])
    gm_bc = sb("gm_bc", [128, D])
    gf_bc = sb("gf_bc", [128, D])
    opm_m = sb("opm_m", [128, 8, 4])
    sh_m = sb("sh_m", [128, 8, 4])
    opm_f = sb("opm_f", [128, 8, 4])
    sh_f = sb("sh_f", [128, 8, 4])
    identf = sb("identf_s", [128, 128])
    identb = sb("identb_s", [128, 128], BF16)
    trib = sb("trib_s", [128, 128], BF16)
    trif = sb("trif_s", [128, 128])
    onesf = sb("onesf_s", [128, 128])
    retm = sb("retm_s", [128, 4, 128])
    xibc = sb("xibc_s", [128, 4, 128])
    zeta = sb("zeta_s", [128, 4])
    qg_bc = sb("qg_bc", [128, 64])
    kg_bc = sb("kg_bc", [128, 64])
    fxg_bc = sb("fxg_bc", [128, 512])
    rtg_bc = sb("rtg_bc", [128, 512])
    bfg_bc = sb("bfg_bc", [128, 8])
    wfg = sb("wfg", [128, 8, 8], BF16)
    wfg32 = sb("wfg32", [128, 8, 8])
    nhalf = sb("nhalf", [128, 8])
    st = sb("st", [128, 64])
    rs_run = sb("rs_run", [128, 8])
    fz = sb("fz", [128, 3, 8])
    cr = sb("cr", [128, 2, 8])
    cumsp = sb("cumsp", [128, G, 8, 3], BF16)
    cactT = sb("cactT", [128, 8, 4])
    ctmp = sb("ctmp", [128, 8, 4])
    ones14 = sb("ones14", [1, 4])

    c4 = xs[0:4, 0, :]
    junk = hTB[:, 0:2, :].rearrange("p a n -> p (a n)")
    badar = [TB[0][0:1, :], TB[1][0:1, :]]
    grow = xs[0:4, 1:3, :].rearrange("p t d -> p (t d)")
    PS = [es.enter_context(nc.psum_tensor(f"P{i}", [128, 512], F32)) for i in range(8)]

    def UK(lo, hi):
        return [("U", i) for i in range(lo, hi)]

    def u_chunk(fc):
        return U[:, fc * 512:(fc + 1) * 512]

    def u_f32(lo_gran, n_gran):
        return U[:, lo_gran * 512:(lo_gran + n_gran) * 512].bitcast(F32)

    QT_v = U[0:70, 0:4096].rearrange("p (h n) -> p h n", n=512)
    QTr_v = U[:, 4096:6144].rearrange("p (h n) -> p h n", n=512)
    QxT_v = U[:, 6144:8192].rearrange("p (h n) -> p h n", n=512)
    KTr_v = U[:, 8192:10240].rearrange("p (h n) -> p h n", n=512)
    Kz_v = U[:, 10240:12288].rearrange("p (t n) -> p t n", n=512)
    Vr_v = U[:, 12288:14336].rearrange("p (t n) -> p t n", n=512)
    rqt_v = [U[:, 14336:14848], U[:, 14848:15360]]
    rkt_v = [U[:, 15360:15872], U[:, 15872:16384]]

    def PK(i):
        return [("P", i)]

    def Pb(i):
        return PS[i][:, :].bitcast(BF16)

    def dma(out, in_, sem, reads=(), writes=(), eng="sp"):
        return P.op(eng, lambda e, o=out, i=in_: e.dma_start(out=o, in_=i), reads=reads, writes=writes, dma=sem)

    dma(identf[:, :], identf_d, "c0", writes=["identf"])
    dma(identb[:, :], identb_d, "c0", writes=["identb"])
    dma(trib[:, :], trib_d, "c0", writes=["trib"])
    dma(trif[:, :], trif_d, "c0", writes=["trif"])
    dma(onesf[:, :], onesf_d, "c0", writes=["onesf"])
    dma(retm[:, :, :], retm_d, "c0", writes=["retm"])
    dma(xibc[:, :, :], xibc_d, "c0", writes=["xibc"])
    dma(zeta[:, :], zeta_d, "c0", writes=["zeta"])
    dma(qg_bc[:, :], qg_d.partition_broadcast(128), "c0", writes=["qg"])
    dma(kg_bc[:, :], kg_d.partition_broadcast(128), "c0", writes=["kg"])
    dma(fxg_bc[:, :], fxg_d.partition_broadcast(128), "c0", writes=["fxg"])
    dma(rtg_bc[:, :], rtg_d.partition_broadcast(128), "c0", writes=["rtg"])
    dma(bfg_bc[:, :], bfg_d.partition_broadcast(128), "c0", writes=["bfg"])
    dma(c4, c_d, "c0", writes=["c4"])
    dma(wfg32[:, :, :], win_d[:, 2048:2056].rearrange("(c p) n -> p c n", p=128), "c0", writes=["wfg32"])
    c0_total = P.dma_count["dma:c0"]
    for k in ["identf", "identb", "trib", "trif", "onesf", "retm", "xibc", "zeta", "qg", "kg", "fxg",
              "rtg", "bfg", "c4", "wfg32"]:
        P.res[k][0] = ("dma:c0", c0_total)
    P.clock[("dma:c0", c0_total)] = {"dma:c0": c0_total}

    P.op("pool", lambda e: e.memset(nhalf[:, :], -0.5), writes=["nhalf"])
    P.op("pool", lambda e: e.memset(ones14[:, :], 1.0), writes=["ones14"])
    P.op("pool", lambda e: e.memset(Vaug[:, :, :, 64:65], 1.0), writes=["Vaug_ones"])
    for i in range(2):
        P.op("pool", lambda e, i=i: e.memset(qaug[i][:, :, 67:70], 1.0), writes=[("qaug", i)])
        P.op("pool", lambda e, i=i: e.memset(kaug[i][:, :, 64:67], 1.0), writes=[("kaug", i)])
    P.op("dve", lambda e: e.tensor_copy(out=wfg[:, :, :], in_=wfg32[:, :, :]), reads=["wfg32"], writes=["wfg"])
    P.op("dve", lambda e: e.tensor_scalar(out=qg_bc[:, :], in0=qg_bc[:, :], scalar1=0.125, scalar2=None,
                                          op0=ALU.mult), reads=["qg"], writes=["qg"])

    def chunk_src(k):
        if k < 8:
            col0 = [0, 512, 1024, 1536, 2056, 2568, 3080, 3592][k]
            return win_d[:, col0:col0 + 512].rearrange("(c p) n -> p c n", p=128)
        if k < 10:
            q = k - 8
            return wout_d[:, q * 512:(q + 1) * 512].rearrange("(c p) n -> p c n", p=128)
        if k < 18:
            j = k - 10
            return w1_d[:, j * 512:(j + 1) * 512].rearrange("(c p) n -> p c n", p=128)
        j = k - 18
        return w2_d[j * 512:(j + 1) * 512, :].rearrange("(c p) n -> p c n", p=128)

    cast_eng = ["act", "dve", "pool"]

    def stage_load(k):
        s = k % 2
        src_ap = chunk_src(k)
        stg_v = u_f32(16 * s, 16).rearrange("p (c n) -> p c n", c=src_ap.shape[1])
        dma(stg_v, src_ap, f"stg{s}", writes=UK(16 * s, 16 * s + 16))

    stage_load(0)
    stage_load(1)
    for k in range(NCHUNK):
        s = k % 2
        slot = k % 3
        stg = u_f32(16 * s, 16)
        ce = cast_eng[k % 3]
        if ce == "act":
            P.op("act", lambda e, o=wbuf[slot][:, :], i=stg: e.activation(out=o, in_=i, func=AF.Copy),
                 reads=UK(16 * s, 16 * s + 16), writes=[("wbuf", slot)])
        else:
            P.op(ce, lambda e, o=wbuf[slot][:, :], i=stg: e.tensor_copy(out=o, in_=i),
                 reads=UK(16 * s, 16 * s + 16), writes=[("wbuf", slot)])
        if k + 2 < NCHUNK:
            stage_load(k + 2)
        dma(wbf_d[k], wbuf[slot][:, :], f"wst{slot}", reads=[("wbuf", slot)], writes=[("wbf", k)])

    for cc in range(8):
        P.op("pe", lambda e, cc=cc: e.transpose(out=PS[0][:, cc * 4:(cc + 1) * 4], in_=c4[:, cc * 128:(cc + 1) * 128],
                                               identity=identf[0:4, 0:4]),
             reads=["c4", "identf"], writes=PK(0))
    P.op("act", lambda e: e.activation(out=ctmp[:, :, :], in_=PS[0][:, 0:32].rearrange("p (c b) -> p c b", b=4),
                                       func=AF.Exp, scale=-1.0), reads=PK(0), writes=["ctmp"])
    P.op("dve", lambda e: e.tensor_scalar(out=ctmp[:, :, :], in0=ctmp[:, :, :], scalar1=1.0, scalar2=None, op0=ALU.add),
         reads=["ctmp"], writes=["ctmp"])
    P.op("dve", lambda e: e.reciprocal(out=ctmp[:, :, :], in_=ctmp[:, :, :]), reads=["ctmp"], writes=["ctmp"])
    P.op("dve", lambda e: e.tensor_tensor(out=cactT[:, :, :], in0=ctmp[:, :, :],
                                          in1=PS[0][:, 0:32].rearrange("p (c b) -> p c b", b=4), op=ALU.mult),
         reads=["ctmp"] + PK(0), writes=["cactT"])

    modT_dst = {0: (sh_m, False), 1: (opm_m, True), 3: (sh_f, False), 4: (opm_f, True)}
    for kb in range(12):
        s = kb % 2
        v = kb // 2
        half = kb % 2
        stg = u_f32(16 * s, 16).rearrange("p (c n) -> p c n", c=8)
        dma(stg, wada_d[:, kb * 512:(kb + 1) * 512].rearrange("(c p) n -> p c n", p=128), f"stg{s}",
            writes=UK(16 * s, 16 * s + 16))
        rk = UK(16 * s, 16 * s + 16)
        bd = badar[kb % 2]
        bdk = ("TB", kb % 2)
        dma(bd, bada_d[0:1, kb * 512:(kb + 1) * 512], f"bd{kb % 2}", writes=[bdk])
        if v in modT_dst:
            dst, plus1 = modT_dst[v]
            bank = 1 + (kb % 2)
            first = True
            for ec in range(4):
                col = kb * 512 + ec * 128
                for dc in range(8):
                    P.op("pe", lambda e, bank=bank, ec=ec, dc=dc, stg=stg, first=first: e.matmul(
                        PS[bank][:, ec * 4:(ec + 1) * 4], lhsT=stg[:, dc, ec * 128:(ec + 1) * 128],
                        rhs=cactT[:, dc, :], start=first, stop=False, skip_group_check=True),
                        reads=rk + ["cactT"], writes=PK(bank))
                    first = False
                P.op("pe", lambda e, bank=bank, ec=ec, bd=bd: e.matmul(
                    PS[bank][:, ec * 4:(ec + 1) * 4], lhsT=bd[0:1, ec * 128:(ec + 1) * 128], rhs=ones14[0:1, :],
                    start=False, stop=True, skip_group_check=True),
                    reads=[bdk, "ones14"], writes=PK(bank))
            src_v = PS[bank][:, 0:16].rearrange("p (c b) -> p c b", b=4)
            dst_v = dst[:, half * 4:(half + 1) * 4, :]
            if plus1:
                P.op("dve", lambda e, o=dst_v, i=src_v: e.tensor_scalar(out=o, in0=i, scalar1=1.0, scalar2=None,
                                                                        op0=ALU.add),
                     reads=PK(bank), writes=[("modT", v, half)])
            else:
                P.op("dve", lambda e, o=dst_v, i=src_v: e.tensor_copy(out=o, in_=i),
                     reads=PK(bank), writes=[("modT", v, half)])
        else:
            bank = 3 + (kb % 2)
            gi = 0 if v == 2 else 1
            for dc in range(8):
                P.op("pe", lambda e, bank=bank, dc=dc, stg=stg: e.matmul(
                    PS[bank][0:4, :], lhsT=cactT[:, dc, :], rhs=stg[:, dc, :], start=(dc == 0), stop=False),
                    reads=rk + ["cactT"], writes=PK(bank))
            P.op("pe", lambda e, bank=bank, bd=bd: e.matmul(
                PS[bank][0:4, :], lhsT=ones14[0:1, :], rhs=bd[0:1, :],
                start=False, stop=True), reads=[bdk, "ones14"], writes=PK(bank))
            P.op("dve", lambda e, bank=bank, gi=gi, half=half: e.tensor_copy(
                out=grow[:, gi * 1024 + half * 512: gi * 1024 + (half + 1) * 512], in_=PS[bank][0:4, :]),
                reads=PK(bank), writes=["grow"])
    dma(gates_d, grow, "gsc", reads=["grow"], writes=["gates_d"])

    stream = [k for _ in range(NSEQ * NG) for k in range(NCHUNK)]
    pf = {"next": 0, "slots": {}, "ctr": 0, "cons": 0}

    def prefetch_upto(n):
        while pf["next"] < min(n, len(stream)):
            i = pf["next"]
            slot = i % 3
            dma(wbuf[slot][:, :], wbf_d[stream[i]], f"wld{slot}", reads=[("wbf", stream[i])], writes=[("wbuf", slot)])
            pf["slots"][i] = slot
            pf["next"] += 1

    def next_chunk():
        i = pf["cons"]
        prefetch_upto(i + 2)
        pf["cons"] += 1
        return pf["slots"][i], i

    def after_chunk(i):
        prefetch_upto(i + 3)

    rr = {"proj": 0, "tp": 0, "tq": 0, "qa": 0, "ka": 0, "mi": 0, "tmp": 0, "rq": 0, "rk": 0}
    store_toks = []

    def bc3(ap2, n):
        return ap2.unsqueeze(2).to_broadcast([128, ap2.shape[1], n])

    def bcmid(ap2, m):
        return ap2.unsqueeze(1).to_broadcast([128, m, ap2.shape[1]])

    def rms_to_hT(b, xn_gran0, opm, shf, vs, vh):
        for t in range(G):
            P.op("act", lambda e, t=t: e.activation(out=junk, in_=xs[:, t, :], func=AF.Square,
                                                    accum_out=st[:, t:t + 1]),
                 reads=[("xs", t)], writes=[("hTB", 0), ("hTB", 1), ("st", 0)])
        P.op("dve", lambda e: e.tensor_scalar(out=st[:, 4:8], in0=st[:, 0:4], scalar1=1.0 / D, scalar2=EPS,
                                              op0=ALU.mult, op1=ALU.add),
             reads=[("st", 0)], writes=[("st", 4)])
        P.op("pool", lambda e: e.tensor_tensor(out=st[:, 8:12], in0=st[:, 4:8], in1=nhalf[:, 0:4], op=ALU.pow),
             reads=[("st", 4), "nhalf"], writes=[("st", 8)])
        xn = u_f32(xn_gran0, 16).rearrange("p (t d) -> p t d", t=G)
        for t in range(G):
            P.op("act", lambda e, t=t: e.activation(out=xn[:, t, :], in_=xs[:, t, :], func=AF.Identity,
                                                    scale=st[:, 8 + t:9 + t]),
                 reads=[("xs", t), ("st", 8)], writes=UK(xn_gran0 + 4 * t, xn_gran0 + 4 * t + 4))
        for c in range(8):
            bank = rr["tp"] % 2
            rr["tp"] += 1
            for t in range(G):
                P.op("pe", lambda e, c=c, t=t, bank=bank: e.transpose(
                    out=PS[bank][:, t * 128:(t + 1) * 128], in_=xn[:, t, c * 128:(c + 1) * 128],
                    identity=identf[:, :]),
                    reads=UK(xn_gran0 + 4 * t, xn_gran0 + 4 * t + 4) + ["identf"], writes=PK(bank))
            P.op("act", lambda e, c=c, bank=bank: e.activation(
                out=hTA[:, c, :], in_=PS[bank][:, :], func=AF.Identity,
                scale=opm[:, c, b:b + 1], bias=shf[:, c, b:b + 1]),
                reads=PK(bank) + [("modT", vs, c // 4), ("modT", vh, c // 4)], writes=[("hTA", c)])

    def proj_mm(src, t, slot, bank, key):
        for c in range(8):
            P.op("pe", lambda e, c=c: e.matmul(
                PS[bank][:, :], lhsT=src[:, c, t * 128:(t + 1) * 128],
                rhs=wbuf[slot][:, c * 512:(c + 1) * 512], start=(c == 0), stop=(c == 7)),
                reads=[(key, c), ("wbuf", slot)], writes=PK(bank))

    def next_proj_bank():
        bk = 2 + rr["proj"] % 3
        rr["proj"] += 1
        return bk

    def small_rstd(n, inv, c0):
        P.op("dve", lambda e: e.tensor_scalar(out=st[:, c0 + 8:c0 + 8 + n], in0=st[:, c0:c0 + n], scalar1=inv,
                                              scalar2=EPS, op0=ALU.mult, op1=ALU.add),
             reads=[("st", c0)], writes=[("st", c0 + 8)])
        P.op("pool", lambda e: e.tensor_tensor(out=st[:, c0 + 16:c0 + 16 + n], in0=st[:, c0 + 8:c0 + 8 + n],
                                               in1=nhalf[:, 0:n], op=ALU.pow),
             reads=[("st", c0 + 8), "nhalf"], writes=[("st", c0 + 16)])
        return st[:, c0 + 16:c0 + 16 + n], ("st", c0 + 16)

    def sigmoid_to(tmp, tkey, bank):
        P.op("act", lambda e: e.activation(out=tmp[:, :], in_=PS[bank][:, :], func=AF.Exp, scale=-1.0),
             reads=PK(bank), writes=[tkey])
        P.op("dve", lambda e: e.tensor_scalar(out=tmp[:, :], in0=tmp[:, :], scalar1=1.0, scalar2=None, op0=ALU.add),
             reads=[tkey], writes=[tkey])
        P.op("dve", lambda e: e.reciprocal(out=tmp[:, :], in_=tmp[:, :]), reads=[tkey], writes=[tkey])

    for b in range(NSEQ):
        dma(gm_bc[:, :], gates_d[b:b + 1, 0:1024].partition_broadcast(128), "gbm", reads=["gates_d"], writes=["gm_bc"])
        dma(gf_bc[:, :], gates_d[b:b + 1, 1024:2048].partition_broadcast(128), "gbf", reads=["gates_d"], writes=["gf_bc"])
        P.op("pool", lambda e: e.memset(rs_run[:, :], 0.0), writes=["rs_run"])
        P.op("pool", lambda e: e.memset(state[:, :, :], 0.0), writes=["state"])
        P.op("pool", lambda e: e.memset(state_bf[:, :, :], 0.0), writes=["state_bf"])

        for g in range(NG):
            row0 = b * S + g * 512
            dma(xs[:, :, :], x_d[row0:row0 + 512, :].rearrange("(t p) d -> p t d", p=128), "xld",
                writes=[("xs", t) for t in range(G)])
            dma(ropeg[:, :, :, :], rope_d[:, :, g * G:(g + 1) * G, :].rearrange("r p t i -> p r t i"), "rope",
                writes=["ropeg"])

            rms_to_hT(b, 0, opm_m, sh_m, 1, 0)

            for t in range(G):
                for c in range(8):
                    P.op("pe", lambda e, c=c, t=t: e.matmul(PS[7][:, 0:8], lhsT=hTA[:, c, t * 128:(t + 1) * 128],
                                                            rhs=wfg[:, c, :], start=(c == 0), stop=(c == 7)),
                         reads=[("hTA", c), "wfg"], writes=PK(7))
                P.op("dve", lambda e: e.tensor_tensor(out=fz[:, 0, :], in0=PS[7][:, 0:8], in1=bfg_bc[:, :], op=ALU.add),
                     reads=PK(7) + ["bfg"], writes=[("fz", 0)])
                P.op("act", lambda e: e.activation(out=fz[:, 1, :], in_=fz[:, 0, :], func=AF.Exp, scale=-1.0),
                     reads=[("fz", 0)], writes=[("fz", 1)])
                P.op("act", lambda e: e.activation(out=fz[:, 2, :], in_=fz[:, 1, :], func=AF.Ln, bias=1.0),
                     reads=[("fz", 1)], writes=[("fz", 2)])
                P.op("pe", lambda e: e.matmul(PS[7][:, 8:16], lhsT=trif[:, :], rhs=fz[:, 2, :], start=True, stop=False),
                     reads=[("fz", 2), "trif"], writes=PK(7))
                P.op("pe", lambda e: e.matmul(PS[7][:, 8:16], lhsT=onesf[:, :], rhs=rs_run[:, :], start=False, stop=True),
                     reads=["rs_run", "onesf"], writes=PK(7))
                P.op("dve", lambda e: e.tensor_tensor(out=rs_run[:, :], in0=rs_run[:, :], in1=fz[:, 2, :], op=ALU.add),
                     reads=["rs_run", ("fz", 2)], writes=["rs_run"])
                ncum = PS[7][:, 8:16]
                ck = [("cumsp", t)]
                P.op("dve", lambda e, t=t: e.tensor_copy(out=cumsp[:, t, :, 0], in_=ncum), reads=PK(7), writes=ck)
                P.op("dve", lambda e, t=t: e.tensor_tensor(out=cr[:, 0, :], in0=ncum, in1=cumsp[:, t, :, 0], op=ALU.subtract),
                     reads=PK(7) + ck, writes=[("cr", 0)])
                P.op("dve", lambda e, t=t: e.tensor_copy(out=cumsp[:, t, :, 1], in_=cr[:, 0, :]), reads=[("cr", 0)], writes=ck)
                P.op("dve", lambda e, t=t: e.tensor_tensor(out=cr[:, 1, :], in0=cr[:, 0, :], in1=cumsp[:, t, :, 1],
                                                           op=ALU.subtract),
                     reads=[("cr", 0)] + ck, writes=[("cr", 1)])
                P.op("dve", lambda e, t=t: e.tensor_copy(out=cumsp[:, t, :, 2], in_=cr[:, 1, :]), reads=[("cr", 1)], writes=ck)

            def qk_evac(t, bank, is_q):
                if is_q:
                    par = rr["qa"] % 2
                    rr["qa"] += 1
                    aug, akey, gain, gkey = qaug[par], ("qaug", par), qg_bc, "qg"
                else:
                    par = rr["ka"] % 2
                    rr["ka"] += 1
                    aug, akey, gain, gkey = kaug[par], ("kaug", par), kg_bc, "kg"
                ta, tb = TA[t % 2], TB[t % 2]
                tak, tbk = ("TA", t % 2), ("TB", t % 2)
                P.op("act", lambda e: e.activation(out=ta[:, :], in_=PS[bank][:, :], func=AF.Square),
                     reads=PK(bank), writes=[tak])
                P.op("dve", lambda e: e.tensor_reduce(out=st[:, 16:24], in_=ta[:, :].rearrange("p (h i) -> p h i", i=64),
                                                      axis=AX.X, op=ALU.add), reads=[tak], writes=[("st", 16)])
                rs, rsk = small_rstd(8, 1.0 / 64, 16)
                P.op("dve", lambda e: e.tensor_tensor(out=tb[:, :].rearrange("p (h i) -> p h i", i=64),
                                                      in0=PS[bank][:, :].rearrange("p (h i) -> p h i", i=64),
                                                      in1=bc3(rs, 64), op=ALU.mult),
                     reads=PK(bank) + [rsk], writes=[tbk])
                P.op("pool", lambda e: e.tensor_tensor(out=aug[:, :, 0:64], in0=tb[:, :].rearrange("p (h i) -> p h i", i=64),
                                                       in1=bcmid(gain[:, :], 8), op=ALU.mult),
                     reads=[tbk, gkey], writes=[akey])
                if is_q:
                    P.op("pool", lambda e: e.tensor_scalar(out=aug[:, :, 64:67], in0=cumsp[:, t, :, :], scalar1=-1.0,
                                                           scalar2=None, op0=ALU.mult),
                         reads=[("cumsp", t)], writes=[akey])
                else:
                    P.op("pool", lambda e: e.tensor_copy(out=aug[:, :, 67:70], in_=cumsp[:, t, :, :]),
                         reads=[("cumsp", t)], writes=[akey])
                bq = 5 + rr["tq"] % 2
                rr["tq"] += 1
                for h in range(8):
                    P.op("pe", lambda e, h=h: e.transpose(out=Pb(bq)[0:70, h * 128:(h + 1) * 128], in_=aug[:, h, :],
                                                          identity=identb[:, :]),
                         reads=[akey, "identb"], writes=PK(bq))
                srcv = Pb(bq)[0:70, :].rearrange("p (h n) -> p h n", n=128)
                if is_q:
                    act_copy(QT_v[:, :, t * 128:(t + 1) * 128], srcv, PK(bq), UK(0, 8))
                else:
                    blk_i = g * G + t
                    act_copy(KT[:, :, blk_i * 128:(blk_i + 1) * 128], srcv, PK(bq), [("KT", blk_i)])

            def act_copy(out, in_, reads, writes):
                P.op("act", lambda e: e.activation(out=out, in_=in_, func=AF.Copy), reads=reads, writes=writes)

            hkey = "hTA"
            for is_q in (True, False):
                slot, ci = next_chunk()
                for t in range(G):
                    bank = next_proj_bank()
                    proj_mm(hTA, t, slot, bank, hkey)
                    qk_evac(t, bank, is_q)
                after_chunk(ci)

            slot, ci = next_chunk()
            for t in range(G):
                bank = next_proj_bank()
                proj_mm(hTA, t, slot, bank, hkey)
                blk_i = g * G + t
                act_copy(Vaug[:, blk_i, :, 0:64], PS[bank][:, :].rearrange("p (h i) -> p h i", i=64), PK(bank),
                         [("Vaug", blk_i)])
            after_chunk(ci)

            slot, ci = next_chunk()
            for t in range(G):
                bank = next_proj_bank()
                proj_mm(hTA, t, slot, bank, hkey)
                ta, tak = TA[t % 2], ("TA", t % 2)
                sigmoid_to(ta, tak, bank)
                P.op("pool", lambda e, t=t, ta=ta: e.tensor_tensor(out=gates_f[:, t, :], in0=ta[:, :], in1=fxg_bc[:, :],
                                                                   op=ALU.mult),
                     reads=[tak, "fxg"], writes=[("gates_f", t)])
            after_chunk(ci)

            def rope_evac(t, bank, is_q):
                r0 = 0 if is_q else 3
                cosv, sinv, nsinv = ropeg[:, r0, t, :], ropeg[:, r0 + 1, t, :], ropeg[:, r0 + 2, t, :]
                ta, tb = TA[t % 2], TB[t % 2]
                tak, tbk = ("TA", t % 2), ("TB", t % 2)
                pv = PS[bank][:, :].rearrange("p (h w i) -> p h w i", h=4, w=2)
                ta4 = ta[:, :].rearrange("p (h w i) -> p h w i", h=4, w=2)
                tb4 = tb[:, :].rearrange("p (h w i) -> p h w i", h=4, w=2)
                cos4 = cosv.unsqueeze(1).unsqueeze(1).to_broadcast([128, 4, 2, 64])
                P.op("dve", lambda e: e.tensor_tensor(out=ta4, in0=pv, in1=cos4, op=ALU.mult),
                     reads=PK(bank) + ["ropeg"], writes=[tak])
                P.op("dve", lambda e: e.tensor_tensor(out=tb4[:, :, 0, :], in0=pv[:, :, 1, :], in1=bcmid(nsinv, 4),
                                                      op=ALU.mult), reads=PK(bank) + ["ropeg"], writes=[tbk])
                P.op("dve", lambda e: e.tensor_tensor(out=tb4[:, :, 1, :], in0=pv[:, :, 0, :], in1=bcmid(sinv, 4),
                                                      op=ALU.mult), reads=PK(bank) + ["ropeg"], writes=[tbk])
                if is_q:
                    i = rr["rq"] % 2
                    rr["rq"] += 1
                    rt, rtk = rqt_v[i], ("U", 28 + i)
                else:
                    i = rr["rk"] % 2
                    rr["rk"] += 1
                    rt, rtk = rkt_v[i], ("U", 30 + i)
                P.op("pool", lambda e: e.tensor_tensor(out=rt, in0=ta[:, :], in1=tb[:, :], op=ALU.add),
                     reads=[tak, tbk], writes=[rtk])
                bq = 5 + rr["tq"] % 2
                rr["tq"] += 1
                for h in range(4):
                    P.op("pe", lambda e, h=h: e.transpose(out=Pb(bq)[:, h * 128:(h + 1) * 128],
                                                          in_=rt[:, h * 128:(h + 1) * 128], identity=identb[:, :]),
                         reads=[rtk, "identb"], writes=PK(bq))
                srcv = Pb(bq)[:, 0:512].rearrange("p (h n) -> p h n", n=128)
                tc_ = slice(t * 128, (t + 1) * 128)
                if is_q:
                    act_copy(QTr_v[:, :, tc_], srcv, PK(bq), UK(8, 12))
                    P.op("pool", lambda e: e.tensor_tensor(out=QxT_v[:, :, tc_], in0=QTr_v[:, :, tc_], in1=xibc[:, :, :],
                                                           op=ALU.mult),
                         reads=UK(8, 12) + ["xibc"], writes=UK(12, 16))
                else:
                    act_copy(KTr_v[:, :, tc_], srcv, PK(bq), UK(16, 20))
                    P.op("pool", lambda e: e.tensor_tensor(out=Kz_v[:, t, :].rearrange("p (h i) -> p h i", i=128),
                                                           in0=rt.rearrange("p (h i) -> p h i", i=128),
                                                           in1=bc3(zeta[:, :], 128), op=ALU.mult),
                         reads=[rtk, "zeta"], writes=[("U", 20 + t)])

            for is_q in (True, False):
                slot, ci = next_chunk()
                for t in range(G):
                    bank = next_proj_bank()
                    proj_mm(hTA, t, slot, bank, hkey)
                    rope_evac(t, bank, is_q)
                after_chunk(ci)

            slot, ci = next_chunk()
            for t in range(G):
                bank = next_proj_bank()
                proj_mm(hTA, t, slot, bank, hkey)
                act_copy(Vr_v[:, t, :], PS[bank][:, :], PK(bank), [("U", 24 + t)])
            after_chunk(ci)

            slot, ci = next_chunk()
            for t in range(G):
                bank = next_proj_bank()
                proj_mm(hTA, t, slot, bank, hkey)
                ta, tak = TA[t % 2], ("TA", t % 2)
                tb, tbk = TB[t % 2], ("TB", t % 2)
                sigmoid_to(ta, tak, bank)
                P.op("dve", lambda e, ta=ta, tb=tb, bank=bank: e.tensor_tensor(out=tb[:, :], in0=ta[:, :], in1=PS[bank][:, :],
                                                                              op=ALU.mult),
                     reads=[tak] + PK(bank), writes=[tbk])
                P.op("pool", lambda e, t=t, tb=tb: e.tensor_tensor(out=gates_r[:, t, :], in0=tb[:, :], in1=rtg_bc[:, :],
                                                                   op=ALU.mult),
                     reads=[tbk, "rtg"], writes=[("gates_r", t)])
            after_chunk(ci)

            nkb = 4 * g + 4
            tasks = [(h, kb) for h in range(8) for kb in range(nkb)]
            mixed_all = [("mixed", t) for t in range(G)]

            def fox_qk(i):
                h, kb = tasks[i]
                jlo = max(0, kb - 4 * g)
                n = (4 - jlo) * 128
                bank = i % 3
                pt = PT[i % 3]
                P.op("pe", lambda e: e.matmul(PS[bank][:, 0:n], lhsT=KT[:, h, kb * 128:(kb + 1) * 128],
                                              rhs=QT_v[:, h, jlo * 128:512], start=True, stop=True),
                     reads=[("KT", kb), ("U", h)], writes=PK(bank))
                P.op("act", lambda e: e.activation(out=pt[:, 0:n], in_=PS[bank][:, 0:n], func=AF.Exp),
                     reads=PK(bank), writes=[("PT", i % 3)])
                if kb >= 4 * g:
                    P.op("pool", lambda e: e.tensor_tensor(out=pt[:, 0:128], in0=pt[:, 0:128], in1=trib[:, :], op=ALU.mult),
                         reads=[("PT", i % 3), "trib"], writes=[("PT", i % 3)])

            def fox_pv(i):
                h, kb = tasks[i]
                jlo = max(0, kb - 4 * g)
                ob = 3 + (h % 2)
                pt = PT[i % 3]
                for j in range(jlo, 4):
                    P.op("pe", lambda e, j=j: e.matmul(PS[ob][:, j * 65:(j + 1) * 65],
                                                       lhsT=pt[:, (j - jlo) * 128:(j - jlo + 1) * 128],
                                                       rhs=Vaug[:, kb, h, :], start=(kb == 0 and j == 0),
                                                       stop=(kb == 4 * g + j), skip_group_check=True),
                         reads=[("PT", i % 3), ("Vaug", kb), "Vaug_ones"], writes=PK(ob))
                if kb == nkb - 1:
                    fox_epilogue(h, ob)

            def fox_epilogue(h, ob):
                O = PS[ob][:, 0:260].rearrange("p (j e) -> p j e", e=65)
                P.op("dve", lambda e: e.reciprocal(out=st[:, 40:44], in_=O[:, :, 64]), reads=PK(ob), writes=[("st", 40)])
                P.op("dve", lambda e: e.tensor_tensor(out=TE[0][:, :, :], in0=O[:, :, 0:64], in1=bc3(st[:, 40:44], 64),
                                                      op=ALU.mult), reads=PK(ob) + [("st", 40)], writes=[("TE", 0)])
                P.op("act", lambda e: e.activation(out=TE[1][:, :, :], in_=TE[0][:, :, :], func=AF.Square),
                     reads=[("TE", 0)], writes=[("TE", 1)])
                P.op("dve", lambda e: e.tensor_reduce(out=st[:, 44:48], in_=TE[1][:, :, :], axis=AX.X, op=ALU.add),
                     reads=[("TE", 1)], writes=[("st", 44)])
                P.op("dve", lambda e: e.tensor_scalar(out=st[:, 48:52], in0=st[:, 44:48], scalar1=1.0 / 64, scalar2=EPS,
                                                      op0=ALU.mult, op1=ALU.add), reads=[("st", 44)], writes=[("st", 48)])
                P.op("pool", lambda e: e.tensor_tensor(out=st[:, 52:56], in0=st[:, 48:52], in1=nhalf[:, 0:4], op=ALU.pow),
                     reads=[("st", 48), "nhalf"], writes=[("st", 52)])
                P.op("dve", lambda e: e.tensor_tensor(out=TE[2][:, :, :], in0=TE[0][:, :, :], in1=bc3(st[:, 52:56], 64),
                                                      op=ALU.mult), reads=[("TE", 0), ("st", 52)], writes=[("TE", 2)])
                P.op("pool", lambda e: e.tensor_tensor(out=mixed[:, :, h * 64:(h + 1) * 64], in0=TE[2][:, :, :],
                                                       in1=gates_f[:, :, h * 64:(h + 1) * 64], op=ALU.mult),
                     reads=[("TE", 2)] + [("gates_f", t) for t in range(G)], writes=mixed_all)

            def ret_stage1(j):
                jc = slice(j * 128, (j + 1) * 128)
                for h in range(4):
                    P.op("pe", lambda e, h=h: e.matmul(PS[5][:, h * 128:(h + 1) * 128], lhsT=KTr_v[:, h, jc],
                                                       rhs=QTr_v[:, h, jc], start=(h == 0), stop=True,
                                                       skip_group_check=True),
                         reads=UK(16, 20) + UK(8, 12), writes=PK(5))
                for h in range(4):
                    hc = slice(h * 128, (h + 1) * 128)
                    P.op("pe", lambda e, hc=hc, h=h: e.matmul(PS[7][:, hc], lhsT=Kz_v[:, j, hc], rhs=Vr_v[:, j, hc],
                                                              start=(h == 0), stop=True, skip_group_check=True),
                         reads=[("U", 20 + j), ("U", 24 + j)], writes=PK(7))
                sj = j % 2
                P.op("dve", lambda e: e.tensor_tensor(out=STr[sj][:, :, :],
                                                      in0=PS[5][:, :].rearrange("p (h n) -> p h n", n=128),
                                                      in1=retm[:, :, :], op=ALU.mult),
                     reads=PK(5) + ["retm"], writes=[("STr", sj)])

            def ret_stage2(j):
                jc = slice(j * 128, (j + 1) * 128)
                sj = j % 2
                for h in range(4):
                    hc = slice(h * 128, (h + 1) * 128)
                    P.op("pe", lambda e, hc=hc, h=h: e.matmul(PS[6][:, hc], lhsT=STr[sj][:, h, :], rhs=Vr_v[:, j, hc],
                                                              start=(h == 0), stop=False, skip_group_check=True),
                         reads=[("STr", sj), ("U", 24 + j)], writes=PK(6))
                    P.op("pe", lambda e, hc=hc, h=h: e.matmul(PS[6][:, hc], lhsT=QxT_v[:, h, jc], rhs=state_bf[:, h, :],
                                                              start=False, stop=True, skip_group_check=True),
                         reads=UK(12, 16) + ["state_bf"], writes=PK(6))
                for h in range(4):
                    hc = slice(h * 128, (h + 1) * 128)
                    P.op("dve", lambda e, hc=hc, h=h: e.scalar_tensor_tensor(
                        out=state[:, h, :], in0=state[:, h, :], scalar=g_chunk[h], in1=PS[7][:, hc],
                        op0=ALU.mult, op1=ALU.add), reads=["state"] + PK(7), writes=["state"])
                P.op("act", lambda e: e.activation(out=state_bf[:, :, :], in_=state[:, :, :], func=AF.Copy),
                     reads=["state"], writes=["state_bf"])
                P.op("act", lambda e: e.activation(out=TA[0][:, :], in_=PS[6][:, :], func=AF.Square),
                     reads=PK(6), writes=[("TA", 0)])
                P.op("dve", lambda e: e.tensor_reduce(out=st[:, 56:60], in_=TA[0][:, :].rearrange("p (h i) -> p h i", i=128),
                                                      axis=AX.X, op=ALU.add), reads=[("TA", 0)], writes=[("st", 56)])
                P.op("dve", lambda e: e.tensor_scalar(out=st[:, 60:64], in0=st[:, 56:60], scalar1=1.0 / 128, scalar2=EPS,
                                                      op0=ALU.mult, op1=ALU.add), reads=[("st", 56)], writes=[("st", 60)])
                P.op("pool", lambda e: e.tensor_tensor(out=st[:, 12:16], in0=st[:, 60:64], in1=nhalf[:, 0:4], op=ALU.pow),
                     reads=[("st", 60), "nhalf"], writes=[("st", 12)])
                P.op("dve", lambda e: e.tensor_tensor(out=TB[0][:, :].rearrange("p (h i) -> p h i", i=128),
                                                      in0=PS[6][:, :].rearrange("p (h i) -> p h i", i=128),
                                                      in1=bc3(st[:, 12:16], 128), op=ALU.mult),
                     reads=PK(6) + [("st", 12)], writes=[("TB", 0)])
                P.op("pool", lambda e: e.tensor_tensor(out=mixed[:, j, 512:1024], in0=TB[0][:, :], in1=gates_r[:, j, :],
                                                       op=ALU.mult),
                     reads=[("TB", 0), ("gates_r", j)], writes=[("mixed", j)])

            ntask = len(tasks)
            sched = {}
            for j in range(4):
                sched.setdefault((2 * j) * ntask // 8, []).append(lambda j=j: ret_stage1(j))
                sched.setdefault((2 * j + 1) * ntask // 8, []).append(lambda j=j: ret_stage2(j))
            for i in range(ntask + 2):
                if i < ntask:
                    for f in sched.get(i, []):
                        f()
                    fox_qk(i)
                if i >= 2:
                    fox_pv(i - 2)

            for c in range(8):
                bank = rr["tp"] % 2
                rr["tp"] += 1
                for t in range(G):
                    P.op("pe", lambda e, c=c, t=t, bank=bank: e.transpose(
                        out=Pb(bank)[:, t * 128:(t + 1) * 128], in_=mixed[:, t, c * 128:(c + 1) * 128],
                        identity=identb[:, :]), reads=[("mixed", t), "identb"], writes=PK(bank))
                if c % 2 == 0:
                    act_copy(hTB[:, c, :], Pb(bank)[:, 0:512], PK(bank), [("hTB", c)])
                else:
                    P.op("dve", lambda e, c=c, bank=bank: e.tensor_copy(out=hTB[:, c, :], in_=Pb(bank)[:, 0:512]),
                         reads=PK(bank), writes=[("hTB", c)])

            for q in range(2):
                slot, ci = next_chunk()
                for t in range(G):
                    bank = next_proj_bank()
                    proj_mm(hTB, t, slot, bank, "hTB")
                    ta, tak = TA[t % 2], ("TA", t % 2)
                    qc = slice(q * 512, (q + 1) * 512)
                    P.op("dve", lambda e, ta=ta, bank=bank, qc=qc: e.tensor_tensor(out=ta[:, :], in0=PS[bank][:, :],
                                                                                  in1=gm_bc[:, qc], op=ALU.mult),
                         reads=PK(bank) + ["gm_bc"], writes=[tak])
                    P.op("pool", lambda e, ta=ta, t=t, qc=qc: e.tensor_tensor(out=xs[:, t, qc], in0=xs[:, t, qc],
                                                                             in1=ta[:, :], op=ALU.add),
                         reads=[tak, ("xs", t)], writes=[("xs", t)])
                after_chunk(ci)

            rms_to_hT(b, 16, opm_f, sh_f, 4, 3)

            tmps = [(TA[0], ("TA", 0)), (TB[0], ("TB", 0)), (TA[1], ("TA", 1)), (TB[1], ("TB", 1))]
            for j in range(8):
                slot, ci = next_chunk()
                for fc in range(4):
                    bank = rr["mi"] % 4
                    tmp, tkey = tmps[rr["mi"] % 4]
                    rr["mi"] += 1
                    for c in range(8):
                        P.op("pe", lambda e, c=c, fc=fc, bank=bank, slot=slot: e.matmul(
                            PS[bank][:, :], lhsT=wbuf[slot][:, c * 512 + fc * 128: c * 512 + (fc + 1) * 128],
                            rhs=hTA[:, c, :], start=(c == 0), stop=(c == 7)),
                            reads=[("hTA", c), ("wbuf", slot)], writes=PK(bank))
                    P.op("act", lambda e, tmp=tmp, bank=bank: e.activation(out=tmp[:, :], in_=PS[bank][:, :], func=AF.Relu),
                         reads=PK(bank), writes=[tkey])
                    uc = u_chunk(4 * j + fc)
                    P.op("pool", lambda e, tmp=tmp, uc=uc: e.tensor_tensor(out=uc, in0=tmp[:, :], in1=tmp[:, :], op=ALU.mult),
                         reads=[tkey], writes=[("U", 4 * j + fc)])
                after_chunk(ci)

            for j in range(8):
                slot, ci = next_chunk()
                for t in range(G):
                    for hf in range(2):
                        bank = t * 2 + hf
                        for fc in range(4):
                            uc = u_chunk(4 * j + fc)
                            P.op("pe", lambda e, uc=uc, t=t, hf=hf, fc=fc, bank=bank, slot=slot, j=j: e.matmul(
                                PS[bank][:, :], lhsT=uc[:, t * 128:(t + 1) * 128],
                                rhs=wbuf[slot][:, fc * 1024 + hf * 512: fc * 1024 + (hf + 1) * 512],
                                start=(j == 0 and fc == 0), stop=(j == 7 and fc == 3)),
                                reads=[("U", 4 * j + fc), ("wbuf", slot)], writes=PK(bank))
                after_chunk(ci)
            for t in range(G):
                for hf in range(2):
                    bank = t * 2 + hf
                    tmp, tkey = tmps[(t * 2 + hf) % 4]
                    qc = slice(hf * 512, (hf + 1) * 512)
                    P.op("dve", lambda e, tmp=tmp, bank=bank, qc=qc: e.tensor_tensor(out=tmp[:, :], in0=PS[bank][:, :],
                                                                                    in1=gf_bc[:, qc], op=ALU.mult),
                         reads=PK(bank) + ["gf_bc"], writes=[tkey])
                    P.op("pool", lambda e, tmp=tmp, t=t, qc=qc: e.tensor_tensor(out=xs[:, t, qc], in0=xs[:, t, qc],
                                                                               in1=tmp[:, :], op=ALU.add),
                         reads=[tkey, ("xs", t)], writes=[("xs", t)])
                tok = dma(y_d[row0 + t * 128: row0 + (t + 1) * 128, :], xs[:, t, :], "yst", reads=[("xs", t)],
                          writes=[("y", row0 + t * 128)])
                store_toks.append(tok)

    P.wait_all("sp", [max(store_toks, key=lambda tk: tk[1])])
    return nc, P, es, consts


_BUILT = None


def _get_built():
    global _BUILT
    if _BUILT is None:
        nc, P, es, consts = build_program()
        P.emit(nc, es)
        es.close()
        _BUILT = (nc, consts)
    return _BUILT


def kernel(x, c, w_ada, b_ada, w_in, b_forget, q_norm_gain, k_norm_gain, fox_out_gain, ret_out_gain,
           w_out, w_mlp_in, w_mlp_out):
    nc, consts = _get_built()
    f = np.float32
    x = np.asarray(x, f)
    c = np.asarray(c, f)
    shared = {
        "w_ada": np.ascontiguousarray(np.asarray(w_ada, f)[0]),
        "b_ada": np.ascontiguousarray(np.asarray(b_ada, f)[0].reshape(1, -1)),
        "w_in": np.ascontiguousarray(np.asarray(w_in, f)[0]),
        "b_forget": np.ascontiguousarray(np.asarray(b_forget, f)[0].reshape(1, 8)),
        "q_gain": np.ascontiguousarray(np.asarray(q_norm_gain, f)[0].reshape(1, 64)),
        "k_gain": np.ascontiguousarray(np.asarray(k_norm_gain, f)[0].reshape(1, 64)),
        "fox_gain": np.ascontiguousarray(np.asarray(fox_out_gain, f)[0].reshape(1, 512)),
        "ret_gain": np.ascontiguousarray(np.asarray(ret_out_gain, f)[0].reshape(1, 512)),
        "w_out": np.ascontiguousarray(np.asarray(w_out, f)[0]),
        "w1": np.ascontiguousarray(np.asarray(w_mlp_in, f)[0]),
        "w2": np.ascontiguousarray(np.asarray(w_mlp_out, f)[0]),
    }
    for k in ("identf", "identb", "trib", "trif", "onesf", "retmaskT", "xi_bc", "zeta_t", "rope"):
        shared[k] = consts[k]
    in_maps = []
    for i in range(NCORES):
        m = dict(shared)
        m["x"] = np.ascontiguousarray(x[i * NSEQ:(i + 1) * NSEQ].reshape(NSEQ * S, D))
        m["c"] = np.ascontiguousarray(c[i * NSEQ:(i + 1) * NSEQ])
        in_maps.append(m)
    res = run_bass_kernel_spmd(nc, in_maps, core_ids=list(range(NCORES)))
    out = np.concatenate([np.asarray(r["y"], f).reshape(NSEQ, S, D) for r in res.results], axis=0)
    return out
```

```python
import numpy as np
import ml_dtypes
from contextlib import ExitStack
import concourse.bass as bass
import concourse.mybir as mybir
from concourse.bass_utils import run_bass_kernel_spmd

F32 = mybir.dt.float32
BF16 = mybir.dt.bfloat16
AF = mybir.ActivationFunctionType
ALU = mybir.AluOpType
AX = mybir.AxisListType

NCORES = 8
D = 1024
S = 2048
NSEQ = 4
NT = 16
G = 4
NG = NT // G
DFF = 4096
EPS = 1e-6
IN_COLS = 4104
NCHUNK = 26

ENGS = ("pe", "act", "dve", "pool", "sp")
import os
STRICT = bool(int(os.environ.get("KSTRICT", "0")))


class _Op:
    __slots__ = ("fn", "waits", "sig", "dma", "tok")


class Prog:
    def __init__(self):
        self.ops = {e: [] for e in ENGS}
        self.res = {}
        self.known = {e: {} for e in ENGS}
        self.clock = {}
        self.dma_count = {}
        self.needed = set()

    def _deps(self, eng, reads, writes):
        deps = set()
        for k in reads:
            r = self.res.get(k)
            if r is not None and r[0] is not None:
                deps.add(r[0])
        for k in writes:
            r = self.res.get(k)
            if r is not None:
                if r[0] is not None and (STRICT or r[0][0] != eng):
                    deps.add(r[0])
                for src, v in r[1].items():
                    if STRICT or src != eng:
                        deps.add((src, v))
        return deps

    def _commit(self, tok, reads, writes):
        for k in reads:
            r = self.res.get(k)
            if r is None:
                r = [None, {}]
                self.res[k] = r
            if r[1].get(tok[0], 0) < tok[1]:
                r[1][tok[0]] = tok[1]
        for k in writes:
            self.res[k] = [tok, {}]

    def op(self, eng, fn, reads=(), writes=(), dma=None):
        deps = self._deps(eng if dma is None else "dma:" + dma, reads, writes)
        kn = self.known[eng]
        waits = []
        for (src, v) in sorted(deps, key=lambda t: (str(t[0]), t[1])):
            if kn.get(src, 0) >= v:
                continue
            waits.append((src, v))
            self.needed.add((src, v))
            for s2, v2 in self.clock[(src, v)].items():
                if kn.get(s2, 0) < v2:
                    kn[s2] = v2
        o = _Op()
        o.fn = fn
        o.waits = waits
        o.dma = dma
        self.ops[eng].append(o)
        if dma is None:
            tok = (eng, len(self.ops[eng]))
        else:
            src = "dma:" + dma
            self.dma_count[src] = self.dma_count.get(src, 0) + 16
            tok = (src, self.dma_count[src])
        o.tok = tok
        ck = dict(kn)
        ck[tok[0]] = tok[1]
        self.clock[tok] = ck
        self._commit(tok, reads, writes)
        return tok

    def wait_all(self, eng, toks):
        kn = self.known[eng]
        waits = []
        for (src, v) in toks:
            if kn.get(src, 0) >= v:
                continue
            waits.append((src, v))
            self.needed.add((src, v))
            kn[src] = v
        o = _Op()
        o.fn = None
        o.waits = waits
        o.dma = None
        o.tok = None
        self.ops[eng].append(o)

    def emit(self, nc, es):
        sems = {}
        for e in ("pe", "act", "dve", "pool"):
            sems[e] = es.enter_context(nc.semaphore("sem_" + e))
        for src in self.dma_count:
            sems[src] = es.enter_context(nc.semaphore("sem_" + src.replace(":", "_")))
        sigval = {}
        for e in ("pe", "act", "dve", "pool"):
            cnt = 0
            for i, o in enumerate(self.ops[e]):
                o.sig = False
                if o.fn is not None and o.dma is None and (e, i + 1) in self.needed:
                    cnt += 1
                    o.sig = True
                    sigval[(e, i + 1)] = cnt
        blk = es.enter_context(nc.Block())

        def run(e, name):
            for o in self.ops[name]:
                for (src, v) in o.waits:
                    val = v if src.startswith("dma:") else sigval[(src, v)]
                    e.wait_ge(sems[src], val)
                if o.fn is None:
                    continue
                ins = o.fn(e)
                if o.dma is not None:
                    ins.then_inc(sems["dma:" + o.dma], 16)
                elif o.sig:
                    ins.then_inc(sems[name], 1)

        @blk.tensor
        def _(e):
            run(e, "pe")

        @blk.scalar
        def _(e):
            run(e, "act")

        @blk.vector
        def _(e):
            run(e, "dve")

        @blk.gpsimd
        def _(e):
            run(e, "pool")

        @blk.sync
        def _(e):
            run(e, "sp")


def _constants():
    f = np.float32
    n = np.arange(128, dtype=f)
    ident = np.eye(128, dtype=f)
    tri = (n[:, None] <= n[None, :]).astype(f)
    ones = np.ones((128, 128), f)
    h = np.arange(4, dtype=f)
    log_g = np.log(f(1.0) - f(2.0) ** (f(-5.0) - h)).astype(f)
    diff = n[None, :] - n[:, None]
    maskT = np.where(diff[None] >= 0, np.exp(np.maximum(diff, 0.0)[None] * log_g[:, None, None]), 0.0).astype(f)
    xi = np.exp((n[None, :] + 1.0) * log_g[:, None]).astype(f)
    xi_bc = np.ascontiguousarray(np.broadcast_to(xi[None], (128, 4, 128))).astype(f)
    ixi = np.exp(-(n[None, :] + 1.0) * log_g[:, None]).astype(f)
    ixi_bc = np.ascontiguousarray(np.broadcast_to(ixi[None], (128, 4, 128))).astype(f)
    zeta = np.exp((128 - 1.0 - n[None, :]) * log_g[:, None]).astype(f)
    zeta_t = np.ascontiguousarray(zeta.T)
    g_chunk = np.exp(f(128.0) * log_g).astype(f)
    pos = np.arange(S, dtype=f)
    inv_freq = (f(10000.0) ** (-np.arange(0, 128, 2, dtype=f) / f(128))).astype(f)
    ang = (pos[:, None] * inv_freq[None, :]).astype(f)
    cos = np.cos(ang).astype(f)
    sin = np.sin(ang).astype(f)
    ks = f(128.0 ** -0.5)

    def lay(a):
        return np.ascontiguousarray(a.reshape(16, 128, 64).transpose(1, 0, 2))

    rope = np.stack([lay(cos), lay(sin), lay(-sin), lay(cos * ks), lay(sin * ks), lay(-sin * ks)], 0)
    sel = np.zeros((4, 4, 128), f)
    for b in range(4):
        sel[b, b, :] = 1.0
    return dict(
        identf=ident, identb=ident.astype(ml_dtypes.bfloat16), trib=tri.astype(ml_dtypes.bfloat16),
        trif=tri, onesf=ones, ixi_bc=ixi_bc, xi_bc=xi_bc, zeta_t=zeta_t,
        rope=np.ascontiguousarray(rope), g_chunk=g_chunk,
    )


_CONST = None


def build_program():
    consts = _constants()
    g_chunk = [float(v) for v in consts["g_chunk"]]
    nc = bass.Bass("TRN2", target_bir_lowering=False)
    P = Prog()
    es = ExitStack()

    def din(name, shape, dt=F32):
        return nc.dram_tensor(name, list(shape), dt, kind="ExternalInput").ap()

    x_d = din("x", [NSEQ * S, D])
    c_d = din("c", [NSEQ, D])
    wada_d = din("w_ada", [D, 6 * D])
    bada_d = din("b_ada", [1, 6 * D])
    win_d = din("w_in", [D, IN_COLS])
    bfg_d = din("b_forget", [1, 8])
    qg_d = din("q_gain", [1, 64])
    kg_d = din("k_gain", [1, 64])
    fxg_d = din("fox_gain", [1, 512])
    rtg_d = din("ret_gain", [1, 512])
    wout_d = din("w_out", [D, D])
    w1_d = din("w1", [D, DFF])
    w2_d = din("w2", [DFF, D])
    identf_d = din("identf", [128, 128])
    identb_d = din("identb", [128, 128], BF16)
    trib_d = din("trib", [128, 128], BF16)
    trif_d = din("trif", [128, 128])
    onesf_d = din("onesf", [128, 128])
    ixibc_d = din("ixi_bc", [128, 4, 128])
    xibc_d = din("xi_bc", [128, 4, 128])
    zeta_d = din("zeta_t", [128, 4])
    rope_d = din("rope", [6, 128, 16, 64])
    y_d = nc.dram_tensor("y", [NSEQ * S, D], F32, kind="ExternalOutput").ap()
    wbf_d = nc.dram_tensor("wbf", [NCHUNK, 128, 4096], BF16, kind="Internal").ap()
    gates_d = nc.dram_tensor("gates_scr", [4, 2048], F32, kind="Internal").ap()

    def sb(name, shape, dt=F32):
        return es.enter_context(nc.sbuf_tensor(name, list(shape), dt))

    wbuf = [sb(f"wbuf{i}", [128, 4096], BF16) for i in range(3)]
    xs = sb("xs", [128, G, D])
    hTA = sb("hTA", [128, 8, 512], BF16)
    hTB = sb("hTB", [128, 8, 512], BF16)
    U = sb("U", [128, 16384], BF16)
    KT = sb("KT", [70, 8, S], BF16)
    Vaug = sb("Vaug", [128, NT, 8, 65], BF16)
    qaug = [sb(f"qaug{i}", [128, 8, 70], BF16) for i in range(3)]
    kaug = [sb(f"kaug{i}", [128, 8, 70], BF16) for i in range(3)]
    state = sb("state", [128, 4, 128])
    state_bf = sb("state_bf", [128, 4, 128], BF16)
    MG = sb("MG", [128, 8192], BF16)
    mixed = MG[:, 0:4096].rearrange("p (t d) -> p t d", d=1024)
    gates_f = MG[:, 4096:6144].rearrange("p (t d) -> p t d", d=512)
    gates_r = MG[:, 6144:8192].rearrange("p (t d) -> p t d", d=512)
    xnext = MG[:, :].bitcast(F32).rearrange("p (t d) -> p t d", d=1024)
    XNK = [[("mixed", 0), ("mixed", 1)], [("mixed", 2), ("mixed", 3)],
           [("gates_f", t) for t in range(G)], [("gates_r", t) for t in range(G)]]
    PT = [sb(f"PT{i}", [128, 512], BF16) for i in range(3)]
    STr = [sb(f"STr{i}", [128, 4, 128], BF16) for i in range(2)]
    TA = [sb(f"TA{i}", [128, 512]) for i in range(2)]
    TB = [sb(f"TB{i}", [128, 512]) for i in range(2)]
    TE = [sb(f"TE{i}", [128, 4, 64]) for i in range(3)]
    ropeg = sb("ropeg", [128, 6, G, 64])
    gm_bc = sb("gm_bc", [128, D])
    gf_bc = sb("gf_bc", [128, D])
    opm_m = sb("opm_m", [128, 8, 4])
    sh_m = sb("sh_m", [128, 8, 4])
    opm_f = sb("opm_f", [128, 8, 4])
    sh_f = sb("sh_f", [128, 8, 4])
    identf = sb("identf_s", [128, 128])
    identb = sb("identb_s", [128, 128], BF16)
    trib = sb("trib_s", [128, 128], BF16)
    trif = sb("trif_s", [128, 128])
    onesf = sb("onesf_s", [128, 128])
    ixibc = sb("ixibc_s", [128, 4, 128])
    xibc = sb("xibc_s", [128, 4, 128])
    zeta = sb("zeta_s", [128, 4])
    qg_col = sb("qg_col", [70, 1])
    kg_col = sb("kg_col", [70, 1])
    fxg_bc = sb("fxg_bc", [128, 512])
    rtg_bc = sb("rtg_bc", [128, 512])
    bfg_bc = sb("bfg_bc", [128, 8])
    wfg = sb("wfg", [128, 8, 8], BF16)
    wfg32 = sb("wfg32", [128, 8, 8])
    nhalf = sb("nhalf", [128, 8])
    epsc = sb("epsc", [128, 1])
    st = sb("st", [128, 64])
    rs_run = sb("rs_run", [128, 8])
    fz = sb("fz", [128, 3, 8])
    cr = sb("cr", [128, 2, 8])
    cumsp = sb("cumsp", [128, G, 8, 3], BF16)
    cactT = sb("cactT", [128, 8, 4])
    ctmp = sb("ctmp", [128, 8, 4])
    ones14 = sb("ones14", [1, 4])

    c4 = xs[0:4, 0, :]
    badar = [TB[0][0:1, :], TB[1][0:1, :]]
    grow = xs[0:4, 1:3, :].rearrange("p t d -> p (t d)")
    PS = [es.enter_context(nc.psum_tensor(f"P{i}", [128, 512], F32)) for i in range(8)]

    def UK(lo, hi):
        return [("U", i) for i in range(lo, hi)]

    def u_chunk(fc):
        return U[:, fc * 512:(fc + 1) * 512]

    def u_f32(lo_gran, n_gran):
        return U[:, lo_gran * 512:(lo_gran + n_gran) * 512].bitcast(F32)

    QT_v = U[0:70, 0:4096].rearrange("p (h n) -> p h n", n=512)
    QTr_v = U[:, 4096:6144].rearrange("p (h n) -> p h n", n=512)
    QxT_v = U[:, 6144:8192].rearrange("p (h n) -> p h n", n=512)
    KTr_v = U[:, 8192:10240].rearrange("p (h n) -> p h n", n=512)
    Kz_v = U[:, 10240:12288].rearrange("p (t n) -> p t n", n=512)
    Vr_v = U[:, 12288:14336].rearrange("p (t n) -> p t n", n=512)
    rqt_v = [U[:, 14336:14848], U[:, 14848:15360], U[:, 6144:6656]]
    rkt_v = [U[:, 15360:15872], U[:, 15872:16384], U[:, 6656:7168]]
    rqt_g = [28, 29, 12]
    rkt_g = [30, 31, 13]

    def PK(i):
        return [("P", i)]

    def Pb(i):
        return PS[i][:, :].bitcast(BF16)

    def dma(out, in_, sem, reads=(), writes=(), eng="sp"):
        return P.op(eng, lambda e, o=out, i=in_: e.dma_start(out=o, in_=i), reads=reads, writes=writes, dma=sem)

    dma(identf[:, :], identf_d, "c0", writes=["identf"])
    dma(identb[:, :], identb_d, "c0", writes=["identb"])
    dma(trib[:, :], trib_d, "c0", writes=["trib"])
    dma(trif[:, :], trif_d, "c0", writes=["trif"])
    dma(onesf[:, :], onesf_d, "c0", writes=["onesf"])
    dma(ixibc[:, :, :], ixibc_d, "c0", writes=["ixibc"])
    dma(xibc[:, :, :], xibc_d, "c0", writes=["xibc"])
    dma(zeta[:, :], zeta_d, "c0", writes=["zeta"])
    dma(qg_col[0:64, :], qg_d.rearrange("o d -> d o"), "c0", writes=["qg"])
    dma(kg_col[0:64, :], kg_d.rearrange("o d -> d o"), "c0", writes=["kg"])
    dma(fxg_bc[:, :], fxg_d.partition_broadcast(128), "c0", writes=["fxg"])
    dma(rtg_bc[:, :], rtg_d.partition_broadcast(128), "c0", writes=["rtg"])
    dma(bfg_bc[:, :], bfg_d.partition_broadcast(128), "c0", writes=["bfg"])
    dma(c4, c_d, "c0", writes=["c4"])
    dma(wfg32[:, :, :], win_d[:, 2048:2056].rearrange("(c p) n -> p c n", p=128), "c0", writes=["wfg32"])
    c0_total = P.dma_count["dma:c0"]
    for k in ["identf", "identb", "trib", "trif", "onesf", "ixibc", "xibc", "zeta", "qg", "kg", "fxg",
              "rtg", "bfg", "c4", "wfg32"]:
        P.res[k][0] = ("dma:c0", c0_total)
    P.clock[("dma:c0", c0_total)] = {"dma:c0": c0_total}

    P.op("pool", lambda e: e.memset(nhalf[:, :], -0.5), writes=["nhalf"])
    P.op("pool", lambda e: e.memset(epsc[:, :], EPS), writes=["epsc"])
    P.op("pool", lambda e: e.memset(ones14[:, :], 1.0), writes=["ones14"])
    P.op("pool", lambda e: e.memset(Vaug[:, :, :, 64:65], 1.0), writes=["Vaug_ones"])
    for i in range(3):
        P.op("pool", lambda e, i=i: e.memset(qaug[i][:, :, 67:70], 1.0), writes=[("qaug", i)])
        P.op("pool", lambda e, i=i: e.memset(kaug[i][:, :, 64:67], 1.0), writes=[("kaug", i)])
    P.op("dve", lambda e: e.tensor_copy(out=wfg[:, :, :], in_=wfg32[:, :, :]), reads=["wfg32"], writes=["wfg"])
    P.op("pool", lambda e: e.memset(qg_col[64:70, :], 1.0), writes=["qg1"])
    P.op("pool", lambda e: e.memset(kg_col[64:70, :], 1.0), writes=["kg1"])
    P.op("dve", lambda e: e.tensor_scalar(out=qg_col[0:64, :], in0=qg_col[0:64, :], scalar1=0.125, scalar2=None,
                                          op0=ALU.mult), reads=["qg"], writes=["qg"])

    def chunk_src(k):
        if k < 8:
            col0 = [0, 512, 1024, 1536, 2056, 2568, 3080, 3592][k]
            return win_d[:, col0:col0 + 512].rearrange("(c p) n -> p c n", p=128)
        if k < 10:
            q = k - 8
            return wout_d[:, q * 512:(q + 1) * 512].rearrange("(c p) n -> p c n", p=128)
        if k < 18:
            j = k - 10
            return w1_d[:, j * 512:(j + 1) * 512].rearrange("(c p) n -> p c n", p=128)
        j = k - 18
        return w2_d[j * 512:(j + 1) * 512, :].rearrange("(c p) n -> p c n", p=128)

    cast_eng = ["act", "dve", "pool"]

    def stage_load(k):
        s = k % 2
        src_ap = chunk_src(k)
        stg_v = u_f32(16 * s, 16).rearrange("p (c n) -> p c n", c=src_ap.shape[1])
        dma(stg_v, src_ap, f"stg{s}", writes=UK(16 * s, 16 * s + 16))

    stage_load(0)
    stage_load(1)
    for k in range(NCHUNK):
        s = k % 2
        slot = k % 3
        stg = u_f32(16 * s, 16)
        ce = cast_eng[k % 3]
        if ce == "act":
            P.op("act", lambda e, o=wbuf[slot][:, :], i=stg: e.activation(out=o, in_=i, func=AF.Copy),
                 reads=UK(16 * s, 16 * s + 16), writes=[("wbuf", slot)])
        else:
            P.op(ce, lambda e, o=wbuf[slot][:, :], i=stg: e.tensor_copy(out=o, in_=i),
                 reads=UK(16 * s, 16 * s + 16), writes=[("wbuf", slot)])
        if k + 2 < NCHUNK:
            stage_load(k + 2)
        dma(wbf_d[k], wbuf[slot][:, :], f"wst{slot}", reads=[("wbuf", slot)], writes=[("wbf", k)])

    for cc in range(8):
        P.op("pe", lambda e, cc=cc: e.transpose(out=PS[0][:, cc * 4:(cc + 1) * 4], in_=c4[:, cc * 128:(cc + 1) * 128],
                                               identity=identf[0:4, 0:4]),
             reads=["c4", "identf"], writes=PK(0))
    P.op("act", lambda e: e.activation(out=ctmp[:, :, :], in_=PS[0][:, 0:32].rearrange("p (c b) -> p c b", b=4),
                                       func=AF.Exp, scale=-1.0), reads=PK(0), writes=["ctmp"])
    P.op("dve", lambda e: e.tensor_scalar(out=ctmp[:, :, :], in0=ctmp[:, :, :], scalar1=1.0, scalar2=None, op0=ALU.add),
         reads=["ctmp"], writes=["ctmp"])
    P.op("dve", lambda e: e.reciprocal(out=ctmp[:, :, :], in_=ctmp[:, :, :]), reads=["ctmp"], writes=["ctmp"])
    P.op("dve", lambda e: e.tensor_tensor(out=cactT[:, :, :], in0=ctmp[:, :, :],
                                          in1=PS[0][:, 0:32].rearrange("p (c b) -> p c b", b=4), op=ALU.mult),
         reads=["ctmp"] + PK(0), writes=["cactT"])

    modT_dst = {0: (sh_m, False), 1: (opm_m, True), 3: (sh_f, False), 4: (opm_f, True)}
    for kb in range(12):
        s = kb % 2
        v = kb // 2
        half = kb % 2
        stg = u_f32(16 * s, 16).rearrange("p (c n) -> p c n", c=8)
        dma(stg, wada_d[:, kb * 512:(kb + 1) * 512].rearrange("(c p) n -> p c n", p=128), f"stg{s}",
            writes=UK(16 * s, 16 * s + 16))
        rk = UK(16 * s, 16 * s + 16)
        bd = badar[kb % 2]
        bdk = ("TB", kb % 2)
        dma(bd, bada_d[0:1, kb * 512:(kb + 1) * 512], f"bd{kb % 2}", writes=[bdk])
        if v in modT_dst:
            dst, plus1 = modT_dst[v]
            bank = 1 + (kb % 2)
            first = True
            for ec in range(4):
                col = kb * 512 + ec * 128
                for dc in range(8):
                    P.op("pe", lambda e, bank=bank, ec=ec, dc=dc, stg=stg, first=first: e.matmul(
                        PS[bank][:, ec * 4:(ec + 1) * 4], lhsT=stg[:, dc, ec * 128:(ec + 1) * 128],
                        rhs=cactT[:, dc, :], start=first, stop=False, skip_group_check=True),
                        reads=rk + ["cactT"], writes=PK(bank))
                    first = False
                P.op("pe", lambda e, bank=bank, ec=ec, bd=bd: e.matmul(
                    PS[bank][:, ec * 4:(ec + 1) * 4], lhsT=bd[0:1, ec * 128:(ec + 1) * 128], rhs=ones14[0:1, :],
                    start=False, stop=True, skip_group_check=True),
                    reads=[bdk, "ones14"], writes=PK(bank))
            src_v = PS[bank][:, 0:16].rearrange("p (c b) -> p c b", b=4)
            dst_v = dst[:, half * 4:(half + 1) * 4, :]
            if plus1:
                P.op("dve", lambda e, o=dst_v, i=src_v: e.tensor_scalar(out=o, in0=i, scalar1=1.0, scalar2=None,
                                                                        op0=ALU.add),
                     reads=PK(bank), writes=[("modT", v, half)])
            else:
                P.op("dve", lambda e, o=dst_v, i=src_v: e.tensor_copy(out=o, in_=i),
                     reads=PK(bank), writes=[("modT", v, half)])
        else:
            bank = 3 + (kb % 2)
            gi = 0 if v == 2 else 1
            for dc in range(8):
                P.op("pe", lambda e, bank=bank, dc=dc, stg=stg: e.matmul(
                    PS[bank][0:4, :], lhsT=cactT[:, dc, :], rhs=stg[:, dc, :], start=(dc == 0), stop=False),
                    reads=rk + ["cactT"], writes=PK(bank))
            P.op("pe", lambda e, bank=bank, bd=bd: e.matmul(
                PS[bank][0:4, :], lhsT=ones14[0:1, :], rhs=bd[0:1, :],
                start=False, stop=True), reads=[bdk, "ones14"], writes=PK(bank))
            P.op("dve", lambda e, bank=bank, gi=gi, half=half: e.tensor_copy(
                out=grow[:, gi * 1024 + half * 512: gi * 1024 + (half + 1) * 512], in_=PS[bank][0:4, :]),
                reads=PK(bank), writes=["grow"])
    dma(gates_d, grow, "gsc", reads=["grow"], writes=["gates_d"])

    from collections import deque
    GROUP_ORDER = [3, 7, 0, 1, 2, 4, 5, 6] + list(range(8, NCHUNK))
    stream = [k for _ in range(NSEQ * NG) for k in GROUP_ORDER]
    pf = {"next": 0, "slots": {}, "cons": 0}

    def prefetch_upto(n):
        while pf["next"] < min(n, len(stream)):
            i = pf["next"]
            slot = i % 3
            dma(wbuf[slot][:, :], wbf_d[stream[i]], f"wld{slot}", reads=[("wbf", stream[i])], writes=[("wbuf", slot)])
            pf["slots"][i] = slot
            pf["next"] += 1

    def next_chunk(expect):
        i = pf["cons"]
        assert stream[i] == expect, (stream[i], expect)
        prefetch_upto(i + 2)
        pf["cons"] += 1
        return pf["slots"][i], i

    def after_chunk(i):
        prefetch_upto(i + 3)

    rr = {"proj": 0, "tp": 0, "tq": 0, "qa": 0, "ka": 0, "mi": 0, "tmp": 0, "rq": 0, "rk": 0}
    store_toks = []
    tmps = [(TA[0], ("TA", 0)), (TB[0], ("TB", 0)), (TA[1], ("TA", 1)), (TB[1], ("TB", 1))]

    def next_tmp():
        r = tmps[rr["tmp"] % 4]
        rr["tmp"] += 1
        return r

    def bc3(ap2, n):
        return ap2.unsqueeze(2).to_broadcast([128, ap2.shape[1], n])

    def bcmid(ap2, m):
        return ap2.unsqueeze(1).to_broadcast([128, m, ap2.shape[1]])

    def act_rstd(c_in, c_out, n, inv):
        P.op("act", lambda e: e.activation(out=st[:, c_out:c_out + n], in_=st[:, c_in:c_in + n], func=AF.Ln,
                                           scale=inv, bias=epsc[:, 0:1]),
             reads=[("st", c_in), "epsc"], writes=[("st", c_out)])
        P.op("act", lambda e: e.activation(out=st[:, c_out:c_out + n], in_=st[:, c_out:c_out + n], func=AF.Exp,
                                           scale=-0.5),
             reads=[("st", c_out)], writes=[("st", c_out)])

    def act_copy(out, in_, reads, writes):
        P.op("act", lambda e: e.activation(out=out, in_=in_, func=AF.Copy), reads=reads, writes=writes)

    def dve_copy(out, in_, reads, writes):
        P.op("dve", lambda e: e.tensor_copy(out=out, in_=in_), reads=reads, writes=writes)

    HB = [hTA, hTB]
    HK = ["hTA", "hTB"]

    def rms_chain(b_, src_t, src_keys, xn, xn_keys, hT, hkey, opm, shf, vs, vh, banks):
        junk = hT[:, 0:2, :].rearrange("p a n -> p (a n)")
        jk = [(hkey, 0), (hkey, 1)]

        def stage0():
            for t in range(G):
                P.op("act", lambda e, t=t: e.activation(out=junk, in_=src_t[:, t, :], func=AF.Square,
                                                        accum_out=st[:, t:t + 1]),
                     reads=src_keys[t], writes=jk + [("st", 0)])
            act_rstd(0, 8, 4, 1.0 / D)
            for t in range(G):
                P.op("act", lambda e, t=t: e.activation(out=xn[:, t, :], in_=src_t[:, t, :], func=AF.Identity,
                                                        scale=st[:, 8 + t:9 + t]),
                     reads=src_keys[t] + [("st", 8)], writes=xn_keys[t])

        def mk(c0):
            def stage():
                for c in (c0, c0 + 1):
                    bank = banks[c % len(banks)]
                    for t in range(G):
                        P.op("pe", lambda e, c=c, t=t, bank=bank: e.transpose(
                            out=PS[bank][:, t * 128:(t + 1) * 128], in_=xn[:, t, c * 128:(c + 1) * 128],
                            identity=identf[:, :]),
                            reads=xn_keys[t] + ["identf"], writes=PK(bank))
                    P.op("act", lambda e, c=c, bank=bank: e.activation(
                        out=hT[:, c, :], in_=PS[bank][:, :], func=AF.Identity,
                        scale=opm[:, c, b_:b_ + 1], bias=shf[:, c, b_:b_ + 1]),
                        reads=PK(bank) + [("modT", vs, c // 4), ("modT", vh, c // 4)], writes=[(hkey, c)])
            return stage
        return [stage0] + [mk(c0) for c0 in (0, 2, 4, 6)]

    def prefetch_B(gi_n):
        b_n, g_n = gi_n // NG, gi_n % NG
        r0 = b_n * S + g_n * 512
        dma(xnext[:, :, :], x_d[r0:r0 + 512, :].rearrange("(t p) d -> p t d", p=128), "xnl",
            writes=[k for ks in XNK for k in ks])
        return rms_chain(b_n, xnext, XNK, xnext, XNK, HB[gi_n % 2], HK[gi_n % 2], opm_m, sh_m, 1, 0, [4, 5, 6, 7])

    def proj_mm(src, t, slot, bank, key):
        for c in range(8):
            P.op("pe", lambda e, c=c: e.matmul(
                PS[bank][:, :], lhsT=src[:, c, t * 128:(t + 1) * 128],
                rhs=wbuf[slot][:, c * 512:(c + 1) * 512], start=(c == 0), stop=(c == 7)),
                reads=[(key, c), ("wbuf", slot)], writes=PK(bank))

    def next_proj_bank():
        bk = 2 + rr["proj"] % 3
        rr["proj"] += 1
        return bk

    for b in range(NSEQ):
        dma(gm_bc[:, :], gates_d[b:b + 1, 0:1024].partition_broadcast(128), "gbm", reads=["gates_d"], writes=["gm_bc"])
        dma(gf_bc[:, :], gates_d[b:b + 1, 1024:2048].partition_broadcast(128), "gbf", reads=["gates_d"], writes=["gf_bc"])
        P.op("pool", lambda e: e.memset(rs_run[:, :], 0.0), writes=["rs_run"])
        P.op("pool", lambda e: e.memset(state[:, :, :], 0.0), writes=["state"])
        P.op("pool", lambda e: e.memset(state_bf[:, :, :], 0.0), writes=["state_bf"])

        for g in range(NG):
            row0 = b * S + g * 512
            dma(xs[:, :, :], x_d[row0:row0 + 512, :].rearrange("(t p) d -> p t d", p=128), "xld",
                writes=[("xs", t) for t in range(G)])
            dma(ropeg[:, :, :, :], rope_d[:, :, g * G:(g + 1) * G, :].rearrange("r p t i -> p r t i"), "rope",
                writes=["ropeg"])

            gi = b * NG + g
            if gi == 0:
                for stg_ in prefetch_B(0):
                    stg_()
            hT_cur, hkey = HB[gi % 2], HK[gi % 2]
            hT_oth, okey = HB[(gi + 1) % 2], HK[(gi + 1) % 2]

            for t in range(G):
                for c in range(8):
                    P.op("pe", lambda e, c=c, t=t, hT_cur=hT_cur: e.matmul(PS[7][:, 0:8], lhsT=hT_cur[:, c, t * 128:(t + 1) * 128],
                                                            rhs=wfg[:, c, :], start=(c == 0), stop=(c == 7)),
                         reads=[(hkey, c), "wfg"], writes=PK(7))
                P.op("dve", lambda e: e.tensor_tensor(out=fz[:, 0, :], in0=PS[7][:, 0:8], in1=bfg_bc[:, :], op=ALU.add),
                     reads=PK(7) + ["bfg"], writes=[("fz", 0)])
                P.op("act", lambda e: e.activation(out=fz[:, 1, :], in_=fz[:, 0, :], func=AF.Exp, scale=-1.0),
                     reads=[("fz", 0)], writes=[("fz", 1)])
                P.op("act", lambda e: e.activation(out=fz[:, 2, :], in_=fz[:, 1, :], func=AF.Ln, bias=1.0),
                     reads=[("fz", 1)], writes=[("fz", 2)])
                P.op("pe", lambda e: e.matmul(PS[7][:, 8:16], lhsT=trif[:, :], rhs=fz[:, 2, :], start=True, stop=False),
                     reads=[("fz", 2), "trif"], writes=PK(7))
                P.op("pe", lambda e: e.matmul(PS[7][:, 8:16], lhsT=onesf[:, :], rhs=rs_run[:, :], start=False, stop=True),
                     reads=["rs_run", "onesf"], writes=PK(7))
                P.op("dve", lambda e: e.tensor_tensor(out=rs_run[:, :], in0=rs_run[:, :], in1=fz[:, 2, :], op=ALU.add),
                     reads=["rs_run", ("fz", 2)], writes=["rs_run"])
                ncum = PS[7][:, 8:16]
                ck = [("cumsp", t)]
                P.op("dve", lambda e, t=t: e.tensor_copy(out=cumsp[:, t, :, 0], in_=ncum), reads=PK(7), writes=ck)
                P.op("dve", lambda e, t=t: e.tensor_tensor(out=cr[:, 0, :], in0=ncum, in1=cumsp[:, t, :, 0], op=ALU.subtract),
                     reads=PK(7) + ck, writes=[("cr", 0)])
                P.op("dve", lambda e, t=t: e.tensor_copy(out=cumsp[:, t, :, 1], in_=cr[:, 0, :]), reads=[("cr", 0)], writes=ck)
                P.op("dve", lambda e, t=t: e.tensor_tensor(out=cr[:, 1, :], in0=cr[:, 0, :], in1=cumsp[:, t, :, 1],
                                                           op=ALU.subtract),
                     reads=[("cr", 0)] + ck, writes=[("cr", 1)])
                P.op("dve", lambda e, t=t: e.tensor_copy(out=cumsp[:, t, :, 2], in_=cr[:, 1, :]), reads=[("cr", 1)], writes=ck)

            pending = deque()
            LAG = 2

            def push_deferred(fn):
                pending.append(fn)
                while len(pending) > LAG:
                    pending.popleft()()

            def flush_deferred():
                while pending:
                    pending.popleft()()

            slot, ci = next_chunk(3)
            for t in range(G):
                bank = next_proj_bank()
                proj_mm(hT_cur, t, slot, bank, hkey)
                tmp, tkey = next_tmp()
                P.op("act", lambda e, tmp=tmp, bank=bank: e.activation(out=tmp[:, :], in_=PS[bank][:, :], func=AF.Sigmoid),
                     reads=PK(bank), writes=[tkey])
                P.op("pool", lambda e, t=t, tmp=tmp: e.tensor_tensor(out=gates_f[:, t, :], in0=tmp[:, :], in1=fxg_bc[:, :],
                                                                     op=ALU.mult),
                     reads=[tkey, "fxg"], writes=[("gates_f", t)])
            after_chunk(ci)
            slot, ci = next_chunk(7)
            for t in range(G):
                bank = next_proj_bank()
                proj_mm(hT_cur, t, slot, bank, hkey)
                tmp, tkey = next_tmp()
                tmp2, tkey2 = next_tmp()
                P.op("act", lambda e, tmp=tmp, bank=bank: e.activation(out=tmp[:, :], in_=PS[bank][:, :], func=AF.Sigmoid),
                     reads=PK(bank), writes=[tkey])
                P.op("dve", lambda e, tmp=tmp, tmp2=tmp2, bank=bank: e.tensor_tensor(out=tmp2[:, :], in0=PS[bank][:, :],
                                                                                    in1=tmp[:, :], op=ALU.mult),
                     reads=[tkey] + PK(bank), writes=[tkey2])
                P.op("pool", lambda e, t=t, tmp2=tmp2: e.tensor_tensor(out=gates_r[:, t, :], in0=tmp2[:, :], in1=rtg_bc[:, :],
                                                                       op=ALU.mult),
                     reads=[tkey2, "rtg"], writes=[("gates_r", t)])
            after_chunk(ci)

            def qk_step(t, slot, is_q):
                bank = next_proj_bank()
                proj_mm(hT_cur, t, slot, bank, hkey)
                if is_q:
                    par = rr["qa"] % 3
                    rr["qa"] += 1
                    aug, akey, gcol, gkeys = qaug[par], ("qaug", par), qg_col, ["qg", "qg1"]
                else:
                    par = rr["ka"] % 3
                    rr["ka"] += 1
                    aug, akey, gcol, gkeys = kaug[par], ("kaug", par), kg_col, ["kg", "kg1"]
                tmp, tkey = next_tmp()
                P.op("act", lambda e: e.activation(out=tmp[:, :], in_=PS[bank][:, :], func=AF.Square),
                     reads=PK(bank), writes=[tkey])
                P.op("dve", lambda e: e.tensor_reduce(out=st[:, 16:24], in_=tmp[:, :].rearrange("p (h i) -> p h i", i=64),
                                                      axis=AX.X, op=ALU.add), reads=[tkey], writes=[("st", 16)])
                act_rstd(16, 24, 8, 1.0 / 64)
                P.op("dve", lambda e: e.tensor_tensor(out=aug[:, :, 0:64],
                                                      in0=PS[bank][:, :].rearrange("p (h i) -> p h i", i=64),
                                                      in1=bc3(st[:, 24:32], 64), op=ALU.mult),
                     reads=PK(bank) + [("st", 24)], writes=[akey])
                if is_q:
                    P.op("pool", lambda e: e.tensor_scalar(out=aug[:, :, 64:67], in0=cumsp[:, t, :, :], scalar1=-1.0,
                                                           scalar2=None, op0=ALU.mult),
                         reads=[("cumsp", t)], writes=[akey])
                else:
                    P.op("pool", lambda e: e.tensor_copy(out=aug[:, :, 67:70], in_=cumsp[:, t, :, :]),
                         reads=[("cumsp", t)], writes=[akey])

                def deferred():
                    bq = 5 + rr["tq"] % 2
                    rr["tq"] += 1
                    for h in range(8):
                        P.op("pe", lambda e, h=h: e.transpose(out=Pb(bq)[0:70, h * 128:(h + 1) * 128], in_=aug[:, h, :],
                                                              identity=identb[:, :]),
                             reads=[akey, "identb"], writes=PK(bq))
                    srcv = Pb(bq)[0:70, :].rearrange("p (h n) -> p h n", n=128)
                    if is_q:
                        dst, wk = QT_v[:, :, t * 128:(t + 1) * 128], UK(0, 8)
                    else:
                        blk_i = g * G + t
                        dst, wk = KT[:, :, blk_i * 128:(blk_i + 1) * 128], [("KT", blk_i)]
                    P.op("dve", lambda e: e.tensor_scalar(out=dst, in0=srcv, scalar1=gcol[:, 0:1], scalar2=None,
                                                          op0=ALU.mult),
                         reads=PK(bq) + gkeys, writes=wk)
                push_deferred(deferred)

            for is_q, ck_ in ((True, 0), (False, 1)):
                slot, ci = next_chunk(ck_)
                for t in range(G):
                    qk_step(t, slot, is_q)
                after_chunk(ci)

            slot, ci = next_chunk(2)
            for t in range(G):
                bank = next_proj_bank()
                proj_mm(hT_cur, t, slot, bank, hkey)
                blk_i = g * G + t
                act_copy(Vaug[:, blk_i, :, 0:64], PS[bank][:, :].rearrange("p (h i) -> p h i", i=64), PK(bank),
                         [("Vaug", blk_i)])
                push_deferred(lambda: None)
            after_chunk(ci)
            flush_deferred()

            def side_bank():
                bk = (5, 7)[rr["proj"] % 2]
                rr["proj"] += 1
                return bk

            def rope_step(t, slot, is_q):
                bank = side_bank()
                proj_mm(hT_cur, t, slot, bank, hkey)
                r0 = 0 if is_q else 3
                cosv, sinv, nsinv = ropeg[:, r0, t, :], ropeg[:, r0 + 1, t, :], ropeg[:, r0 + 2, t, :]
                ta, tak = next_tmp()
                tb, tbk = next_tmp()
                pv = PS[bank][:, :].rearrange("p (h w i) -> p h w i", h=4, w=2)
                ta4 = ta[:, :].rearrange("p (h w i) -> p h w i", h=4, w=2)
                tb4 = tb[:, :].rearrange("p (h w i) -> p h w i", h=4, w=2)
                cos4 = cosv.unsqueeze(1).unsqueeze(1).to_broadcast([128, 4, 2, 64])
                P.op("dve", lambda e: e.tensor_tensor(out=ta4, in0=pv, in1=cos4, op=ALU.mult),
                     reads=PK(bank) + ["ropeg"], writes=[tak])
                P.op("dve", lambda e: e.tensor_tensor(out=tb4[:, :, 0, :], in0=pv[:, :, 1, :], in1=bcmid(nsinv, 4),
                                                      op=ALU.mult), reads=PK(bank) + ["ropeg"], writes=[tbk])
                P.op("dve", lambda e: e.tensor_tensor(out=tb4[:, :, 1, :], in0=pv[:, :, 0, :], in1=bcmid(sinv, 4),
                                                      op=ALU.mult), reads=PK(bank) + ["ropeg"], writes=[tbk])
                if is_q:
                    i = rr["rq"] % 3
                    rr["rq"] += 1
                    rt, rtk = rqt_v[i], ("U", rqt_g[i])
                else:
                    i = rr["rk"] % 3
                    rr["rk"] += 1
                    rt, rtk = rkt_v[i], ("U", rkt_g[i])
                P.op("pool", lambda e: e.tensor_tensor(out=rt, in0=ta[:, :], in1=tb[:, :], op=ALU.add),
                     reads=[tak, tbk], writes=[rtk])
                if not is_q:
                    P.op("pool", lambda e: e.tensor_tensor(out=Kz_v[:, t, :].rearrange("p (h i) -> p h i", i=128),
                                                           in0=rt.rearrange("p (h i) -> p h i", i=128),
                                                           in1=bc3(zeta[:, :], 128), op=ALU.mult),
                         reads=[rtk, "zeta"], writes=[("U", 20 + t)])

                def deferred():
                    bq = 6
                    for h in range(4):
                        P.op("pe", lambda e, h=h: e.transpose(out=Pb(bq)[:, h * 128:(h + 1) * 128],
                                                              in_=rt[:, h * 128:(h + 1) * 128], identity=identb[:, :]),
                             reads=[rtk, "identb"], writes=PK(bq))
                    srcv = Pb(bq)[:, 0:512].rearrange("p (h n) -> p h n", n=128)
                    tc_ = slice(t * 128, (t + 1) * 128)
                    if is_q:
                        P.op("dve", lambda e: e.tensor_tensor(out=QTr_v[:, :, tc_], in0=srcv, in1=xibc[:, :, :], op=ALU.mult),
                             reads=PK(bq) + ["xibc"], writes=UK(8, 12))
                    else:
                        P.op("dve", lambda e: e.tensor_tensor(out=KTr_v[:, :, tc_], in0=srcv, in1=ixibc[:, :, :], op=ALU.mult),
                             reads=PK(bq) + ["ixibc"], writes=UK(16, 20))
                return deferred

            def rv_step(t, slot):
                bank = side_bank()
                proj_mm(hT_cur, t, slot, bank, hkey)
                dve_copy(Vr_v[:, t, :], PS[bank][:, :], PK(bank), [("U", 24 + t)])
                return lambda: None

            chunk_state = {}

            def side_steps():
                units = []
                for kind, ck_ in (("rq", 4), ("rk", 5), ("rv", 6)):
                    for t in range(G):
                        units.append((kind, ck_, t))
                return units

            units = side_steps()

            def run_unit(u):
                kind, ck_, t = u
                if t == 0:
                    chunk_state["cur"] = next_chunk(ck_)
                slot, ci = chunk_state["cur"]
                if kind == "rq":
                    push_deferred(rope_step(t, slot, True))
                elif kind == "rk":
                    push_deferred(rope_step(t, slot, False))
                else:
                    push_deferred(rv_step(t, slot))
                if t == G - 1:
                    after_chunk(ci)

            nkb = 4 * g + 4
            tasks = [(h, kb) for h in range(8) for kb in range(nkb)]
            mixed_all = [("mixed", t) for t in range(G)]

            def fox_qk(i):
                h, kb = tasks[i]
                jlo = max(0, kb - 4 * g)
                n = (4 - jlo) * 128
                bank = i % 3
                pt = PT[i % 3]
                P.op("pe", lambda e: e.matmul(PS[bank][:, 0:n], lhsT=KT[:, h, kb * 128:(kb + 1) * 128],
                                              rhs=QT_v[:, h, jlo * 128:512], start=True, stop=True),
                     reads=[("KT", kb), ("U", h)], writes=PK(bank))
                P.op("act", lambda e: e.activation(out=pt[:, 0:n], in_=PS[bank][:, 0:n], func=AF.Exp),
                     reads=PK(bank), writes=[("PT", i % 3)])
                if kb >= 4 * g:
                    P.op("pool", lambda e: e.tensor_tensor(out=pt[:, 0:128], in0=pt[:, 0:128], in1=trib[:, :], op=ALU.mult),
                         reads=[("PT", i % 3), "trib"], writes=[("PT", i % 3)])

            def fox_pv(i):
                h, kb = tasks[i]
                jlo = max(0, kb - 4 * g)
                ob = 3 + (h % 2)
                pt = PT[i % 3]
                for j in range(jlo, 4):
                    P.op("pe", lambda e, j=j, last=(kb == 4 * g + j): e.matmul(PS[ob][:, j * 65:(j + 1) * 65],
                                                       lhsT=pt[:, (j - jlo) * 128:(j - jlo + 1) * 128],
                                                       rhs=Vaug[:, kb, h, :], start=(kb == 0 and j == 0),
                                                       stop=last, skip_group_check=True),
                         reads=[("PT", i % 3), ("Vaug", kb), "Vaug_ones"], writes=PK(ob))
                if kb == nkb - 1:
                    fox_epilogue(h, ob)

            def fox_epilogue(h, ob):
                O = PS[ob][:, 0:260].rearrange("p (j e) -> p j e", e=65)
                P.op("dve", lambda e: e.reciprocal(out=st[:, 40:44], in_=O[:, :, 64]), reads=PK(ob), writes=[("st", 40)])
                P.op("dve", lambda e: e.tensor_tensor(out=TE[0][:, :, :], in0=O[:, :, 0:64], in1=bc3(st[:, 40:44], 64),
                                                      op=ALU.mult), reads=PK(ob) + [("st", 40)], writes=[("TE", 0)])
                P.op("pool", lambda e: e.tensor_tensor(out=TE[1][:, :, :], in0=TE[0][:, :, :], in1=TE[0][:, :, :], op=ALU.mult),
                     reads=[("TE", 0)], writes=[("TE", 1)])
                P.op("dve", lambda e: e.tensor_reduce(out=st[:, 44:48], in_=TE[1][:, :, :], axis=AX.X, op=ALU.add),
                     reads=[("TE", 1)], writes=[("st", 44)])
                act_rstd(44, 52, 4, 1.0 / 64)
                P.op("dve", lambda e: e.tensor_tensor(out=TE[2][:, :, :], in0=TE[0][:, :, :], in1=bc3(st[:, 52:56], 64),
                                                      op=ALU.mult), reads=[("TE", 0), ("st", 52)], writes=[("TE", 2)])
                P.op("pool", lambda e: e.tensor_tensor(out=mixed[:, :, h * 64:(h + 1) * 64], in0=TE[2][:, :, :],
                                                       in1=gates_f[:, :, h * 64:(h + 1) * 64], op=ALU.mult),
                     reads=[("TE", 2)] + [("gates_f", t) for t in range(G)], writes=mixed_all)

            def ret_stage1(j):
                jc = slice(j * 128, (j + 1) * 128)
                for h in range(4):
                    P.op("pe", lambda e, h=h: e.matmul(PS[5][:, h * 128:(h + 1) * 128], lhsT=KTr_v[:, h, jc],
                                                       rhs=QTr_v[:, h, jc], start=(h == 0), stop=True,
                                                       skip_group_check=True),
                         reads=UK(16, 20) + UK(8, 12), writes=PK(5))
                for h in range(4):
                    hc = slice(h * 128, (h + 1) * 128)
                    P.op("pe", lambda e, hc=hc, h=h: e.matmul(PS[7][:, hc], lhsT=Kz_v[:, j, hc], rhs=Vr_v[:, j, hc],
                                                              start=(h == 0), stop=True, skip_group_check=True),
                         reads=[("U", 20 + j), ("U", 24 + j)], writes=PK(7))
                sj = j % 2
                P.op("dve", lambda e: e.tensor_tensor(out=STr[sj][:, :, :],
                                                      in0=PS[5][:, :].rearrange("p (h n) -> p h n", n=128),
                                                      in1=bcmid(trib[:, :], 4), op=ALU.mult),
                     reads=PK(5) + ["trib"], writes=[("STr", sj)])

            def ret_stage2(j):
                jc = slice(j * 128, (j + 1) * 128)
                sj = j % 2
                for h in range(4):
                    hc = slice(h * 128, (h + 1) * 128)
                    P.op("pe", lambda e, hc=hc, h=h: e.matmul(PS[6][:, hc], lhsT=STr[sj][:, h, :], rhs=Vr_v[:, j, hc],
                                                              start=(h == 0), stop=False, skip_group_check=True),
                         reads=[("STr", sj), ("U", 24 + j)], writes=PK(6))
                    P.op("pe", lambda e, hc=hc, h=h: e.matmul(PS[6][:, hc], lhsT=QTr_v[:, h, jc], rhs=state_bf[:, h, :],
                                                              start=False, stop=True, skip_group_check=True),
                         reads=UK(8, 12) + ["state_bf"], writes=PK(6))
                for h in range(4):
                    hc = slice(h * 128, (h + 1) * 128)
                    P.op("dve", lambda e, hc=hc, h=h: e.scalar_tensor_tensor(
                        out=state[:, h, :], in0=state[:, h, :], scalar=g_chunk[h], in1=PS[7][:, hc],
                        op0=ALU.mult, op1=ALU.add), reads=["state"] + PK(7), writes=["state"])
                P.op("pool", lambda e: e.tensor_copy(out=state_bf[:, :, :], in_=state[:, :, :]),
                     reads=["state"], writes=["state_bf"])
                tmp, tkey = next_tmp()
                tmp2, tkey2 = next_tmp()
                P.op("act", lambda e: e.activation(out=tmp[:, :], in_=PS[6][:, :], func=AF.Square),
                     reads=PK(6), writes=[tkey])
                P.op("dve", lambda e: e.tensor_reduce(out=st[:, 56:60], in_=tmp[:, :].rearrange("p (h i) -> p h i", i=128),
                                                      axis=AX.X, op=ALU.add), reads=[tkey], writes=[("st", 56)])
                act_rstd(56, 60, 4, 1.0 / 128)
                P.op("dve", lambda e: e.tensor_tensor(out=tmp2[:, :].rearrange("p (h i) -> p h i", i=128),
                                                      in0=PS[6][:, :].rearrange("p (h i) -> p h i", i=128),
                                                      in1=bc3(st[:, 60:64], 128), op=ALU.mult),
                     reads=PK(6) + [("st", 60)], writes=[tkey2])
                P.op("pool", lambda e: e.tensor_tensor(out=mixed[:, j, 512:1024], in0=tmp2[:, :], in1=gates_r[:, j, :],
                                                       op=ALU.mult),
                     reads=[tkey2, ("gates_r", j)], writes=[("mixed", j)])

            ntask = len(tasks)
            sched = {}
            nun = len(units)
            span = max(nun, int(ntask * 0.55))
            for ui, u in enumerate(units):
                sched.setdefault(min(ntask - 1, ui * span // nun), []).append(lambda u=u: run_unit(u))
            sched.setdefault(min(ntask - 1, span), []).append(flush_deferred)
            rem0 = min(ntask - 1, span + 1)
            for j in range(4):
                p1 = rem0 + (2 * j) * (ntask - rem0) // 8
                p2 = rem0 + (2 * j + 1) * (ntask - rem0) // 8
                sched.setdefault(min(ntask - 1, p1), []).append(lambda j=j: ret_stage1(j))
                sched.setdefault(min(ntask - 1, p2), []).append(lambda j=j: ret_stage2(j))
            for i in range(ntask + 2):
                if i < ntask:
                    for f in sched.get(i, []):
                        f()
                    fox_qk(i)
                if i >= 2:
                    fox_pv(i - 2)

            for c in range(8):
                bank = rr["tp"] % 2
                rr["tp"] += 1
                for t in range(G):
                    P.op("pe", lambda e, c=c, t=t, bank=bank: e.transpose(
                        out=Pb(bank)[:, t * 128:(t + 1) * 128], in_=mixed[:, t, c * 128:(c + 1) * 128],
                        identity=identb[:, :]), reads=[("mixed", t), "identb"], writes=PK(bank))
                if c % 2 == 0:
                    act_copy(hT_oth[:, c, :], Pb(bank)[:, 0:512], PK(bank), [(okey, c)])
                else:
                    dve_copy(hT_oth[:, c, :], Pb(bank)[:, 0:512], PK(bank), [(okey, c)])

            for q in range(2):
                slot, ci = next_chunk(8 + q)
                for t in range(G):
                    bank = next_proj_bank()
                    proj_mm(hT_oth, t, slot, bank, okey)
                    tmp, tkey = next_tmp()
                    qc = slice(q * 512, (q + 1) * 512)
                    P.op("dve", lambda e, tmp=tmp, bank=bank, qc=qc: e.tensor_tensor(out=tmp[:, :], in0=PS[bank][:, :],
                                                                                    in1=gm_bc[:, qc], op=ALU.mult),
                         reads=PK(bank) + ["gm_bc"], writes=[tkey])
                    P.op("pool", lambda e, tmp=tmp, t=t, qc=qc: e.tensor_tensor(out=xs[:, t, qc], in0=xs[:, t, qc],
                                                                               in1=tmp[:, :], op=ALU.add),
                         reads=[tkey, ("xs", t)], writes=[("xs", t)])
                after_chunk(ci)

            xn2 = u_f32(16, 16).rearrange("p (t d) -> p t d", t=G)
            for stg_ in rms_chain(b, xs, [[("xs", t)] for t in range(G)], xn2, [UK(16 + 4 * t, 20 + 4 * t) for t in range(G)],
                                  hT_cur, hkey, opm_f, sh_f, 4, 3, [0, 1]):
                stg_()

            pstages = prefetch_B(gi + 1) if gi + 1 < NSEQ * NG else []
            for j in range(8):
                slot, ci = next_chunk(10 + j)
                if pstages and 1 <= j <= 5:
                    pstages[j - 1]()
                for fc in range(4):
                    bank = rr["mi"] % 4
                    rr["mi"] += 1
                    tmp, tkey = next_tmp()
                    for c in range(8):
                        P.op("pe", lambda e, c=c, fc=fc, bank=bank, slot=slot, hT_cur=hT_cur: e.matmul(
                            PS[bank][:, :], lhsT=wbuf[slot][:, c * 512 + fc * 128: c * 512 + (fc + 1) * 128],
                            rhs=hT_cur[:, c, :], start=(c == 0), stop=(c == 7)),
                            reads=[(hkey, c), ("wbuf", slot)], writes=PK(bank))
                    P.op("act", lambda e, tmp=tmp, bank=bank: e.activation(out=tmp[:, :], in_=PS[bank][:, :], func=AF.Relu),
                         reads=PK(bank), writes=[tkey])
                    uc = u_chunk(4 * j + fc)
                    P.op("pool", lambda e, tmp=tmp, uc=uc: e.tensor_tensor(out=uc, in0=tmp[:, :], in1=tmp[:, :], op=ALU.mult),
                         reads=[tkey], writes=[("U", 4 * j + fc)])
                after_chunk(ci)

            for j in range(8):
                slot, ci = next_chunk(18 + j)
                for t in range(G):
                    for hf in range(2):
                        bank = t * 2 + hf
                        for fc in range(4):
                            uc = u_chunk(4 * j + fc)
                            P.op("pe", lambda e, uc=uc, t=t, hf=hf, fc=fc, bank=bank, slot=slot, j=j: e.matmul(
                                PS[bank][:, :], lhsT=uc[:, t * 128:(t + 1) * 128],
                                rhs=wbuf[slot][:, fc * 1024 + hf * 512: fc * 1024 + (hf + 1) * 512],
                                start=(j == 0 and fc == 0), stop=(j == 7 and fc == 3)),
                                reads=[("U", 4 * j + fc), ("wbuf", slot)], writes=PK(bank))
                after_chunk(ci)
            done_half = {}
            for (t, hf) in ((3, 1), (1, 0), (1, 1), (2, 0), (0, 0), (0, 1), (2, 1), (3, 0)):
                if True:
                    bank = t * 2 + hf
                    tmp, tkey = next_tmp()
                    qc = slice(hf * 512, (hf + 1) * 512)
                    P.op("dve", lambda e, tmp=tmp, bank=bank, qc=qc: e.tensor_tensor(out=tmp[:, :], in0=PS[bank][:, :],
                                                                                    in1=gf_bc[:, qc], op=ALU.mult),
                         reads=PK(bank) + ["gf_bc"], writes=[tkey])
                    P.op("pool", lambda e, tmp=tmp, t=t, qc=qc: e.tensor_tensor(out=xs[:, t, qc], in0=xs[:, t, qc],
                                                                               in1=tmp[:, :], op=ALU.add),
                         reads=[tkey, ("xs", t)], writes=[("xs", t)])
                done_half[t] = done_half.get(t, 0) + 1
                if done_half[t] == 2:
                    tok = dma(y_d[row0 + t * 128: row0 + (t + 1) * 128, :], xs[:, t, :], "yst", reads=[("xs", t)],
                              writes=[("y", row0 + t * 128)])
                    store_toks.append(tok)

    P.wait_all("sp", [max(store_toks, key=lambda tk: tk[1])])
    return nc, P, es, consts


_BUILT = None


def _get_built():
    global _BUILT
    if _BUILT is None:
        nc, P, es, consts = build_program()
        P.emit(nc, es)
        es.close()
        _BUILT = (nc, consts)
    return _BUILT


def kernel(x, c, w_ada, b_ada, w_in, b_forget, q_norm_gain, k_norm_gain, fox_out_gain, ret_out_gain,
           w_out, w_mlp_in, w_mlp_out):
    nc, consts = _get_built()
    f = np.float32
    x = np.asarray(x, f)
    c = np.asarray(c, f)
    shared = {
        "w_ada": np.ascontiguousarray(np.asarray(w_ada, f)[0]),
        "b_ada": np.ascontiguousarray(np.asarray(b_ada, f)[0].reshape(1, -1)),
        "w_in": np.ascontiguousarray(np.asarray(w_in, f)[0]),
        "b_forget": np.ascontiguousarray(np.asarray(b_forget, f)[0].reshape(1, 8)),
        "q_gain": np.ascontiguousarray(np.asarray(q_norm_gain, f)[0].reshape(1, 64)),
        "k_gain": np.ascontiguousarray(np.asarray(k_norm_gain, f)[0].reshape(1, 64)),
        "fox_gain": np.ascontiguousarray(np.asarray(fox_out_gain, f)[0].reshape(1, 512)),
        "ret_gain": np.ascontiguousarray(np.asarray(ret_out_gain, f)[0].reshape(1, 512)),
        "w_out": np.ascontiguousarray(np.asarray(w_out, f)[0]),
        "w1": np.ascontiguousarray(np.asarray(w_mlp_in, f)[0]),
        "w2": np.ascontiguousarray(np.asarray(w_mlp_out, f)[0]),
    }
    for k in ("identf", "identb", "trib", "trif", "onesf", "ixi_bc", "xi_bc", "zeta_t", "rope"):
        shared[k] = consts[k]
    in_maps = []
    for i in range(NCORES):
        m = dict(shared)
        m["x"] = np.ascontiguousarray(x[i * NSEQ:(i + 1) * NSEQ].reshape(NSEQ * S, D))
        m["c"] = np.ascontiguousarray(c[i * NSEQ:(i + 1) * NSEQ])
        in_maps.append(m)
    res = run_bass_kernel_spmd(nc, in_maps, core_ids=list(range(NCORES)))
    out = np.concatenate([np.asarray(r["y"], f).reshape(NSEQ, S, D) for r in res.results], axis=0)
    return out
```

```python
import numpy as np
import ml_dtypes
from contextlib import ExitStack
import concourse.bass as bass
import concourse.mybir as mybir
from concourse.bass_utils import run_bass_kernel_spmd

F32 = mybir.dt.float32
BF16 = mybir.dt.bfloat16
AF = mybir.ActivationFunctionType
ALU = mybir.AluOpType
AX = mybir.AxisListType

NCORES = 8
D = 1024
S = 2048
NSEQ = 4
NT = 16
G = 4
NG = NT // G
DFF = 4096
EPS = 1e-6
IN_COLS = 4104
NCHUNK = 26

ENGS = ("pe", "act", "dve", "pool", "sp")
import os
STRICT = bool(int(os.environ.get("KSTRICT", "0")))


class _Op:
    __slots__ = ("fn", "waits", "sig", "dma", "tok")


class Prog:
    def __init__(self):
        self.ops = {e: [] for e in ENGS}
        self.res = {}
        self.known = {e: {} for e in ENGS}
        self.clock = {}
        self.dma_count = {}
        self.needed = set()

    def _deps(self, eng, reads, writes):
        deps = set()
        for k in reads:
            r = self.res.get(k)
            if r is not None and r[0] is not None:
                deps.add(r[0])
        for k in writes:
            r = self.res.get(k)
            if r is not None:
                if r[0] is not None and (STRICT or r[0][0] != eng):
                    deps.add(r[0])
                for src, v in r[1].items():
                    if STRICT or src != eng:
                        deps.add((src, v))
        return deps

    def _commit(self, tok, reads, writes):
        for k in reads:
            r = self.res.get(k)
            if r is None:
                r = [None, {}]
                self.res[k] = r
            if r[1].get(tok[0], 0) < tok[1]:
                r[1][tok[0]] = tok[1]
        for k in writes:
            self.res[k] = [tok, {}]

    def op(self, eng, fn, reads=(), writes=(), dma=None):
        deps = self._deps(eng if dma is None else "dma:" + dma, reads, writes)
        kn = self.known[eng]
        waits = []
        best = {}
        for (src, v) in deps:
            if best.get(src, 0) < v:
                best[src] = v
        deps = set(best.items())
        for (src, v) in sorted(deps, key=lambda t: (str(t[0]), t[1])):
            if kn.get(src, 0) >= v:
                continue
            waits.append((src, v))
            self.needed.add((src, v))
            for s2, v2 in self.clock[(src, v)].items():
                if kn.get(s2, 0) < v2:
                    kn[s2] = v2
        o = _Op()
        o.fn = fn
        o.waits = waits
        o.dma = dma
        self.ops[eng].append(o)
        if dma is None:
            tok = (eng, len(self.ops[eng]))
        else:
            src = "dma:" + dma
            self.dma_count[src] = self.dma_count.get(src, 0) + 16
            tok = (src, self.dma_count[src])
        o.tok = tok
        ck = dict(kn)
        ck[tok[0]] = tok[1]
        self.clock[tok] = ck
        self._commit(tok, reads, writes)
        return tok

    def wait_all(self, eng, toks):
        kn = self.known[eng]
        waits = []
        for (src, v) in toks:
            if kn.get(src, 0) >= v:
                continue
            waits.append((src, v))
            self.needed.add((src, v))
            kn[src] = v
        o = _Op()
        o.fn = None
        o.waits = waits
        o.dma = None
        o.tok = None
        self.ops[eng].append(o)

    def emit(self, nc, es):
        sems = {}
        for e in ("pe", "act", "dve", "pool"):
            sems[e] = es.enter_context(nc.semaphore("sem_" + e))
        for src in self.dma_count:
            sems[src] = es.enter_context(nc.semaphore("sem_" + src.replace(":", "_")))
        sigval = {}
        for e in ("pe", "act", "dve", "pool"):
            cnt = 0
            for i, o in enumerate(self.ops[e]):
                o.sig = False
                if o.fn is not None and o.dma is None and (e, i + 1) in self.needed:
                    cnt += 1
                    o.sig = True
                    sigval[(e, i + 1)] = cnt
        blk = es.enter_context(nc.Block())

        def run(e, name):
            for o in self.ops[name]:
                for (src, v) in o.waits:
                    val = v if src.startswith("dma:") else sigval[(src, v)]
                    e.wait_ge(sems[src], val)
                if o.fn is None:
                    continue
                ins = o.fn(e)
                if o.dma is not None:
                    ins.then_inc(sems["dma:" + o.dma], 16)
                elif o.sig:
                    ins.then_inc(sems[name], 1)

        @blk.tensor
        def _(e):
            run(e, "pe")

        @blk.scalar
        def _(e):
            run(e, "act")

        @blk.vector
        def _(e):
            run(e, "dve")

        @blk.gpsimd
        def _(e):
            run(e, "pool")

        @blk.sync
        def _(e):
            run(e, "sp")


def _constants():
    f = np.float32
    n = np.arange(128, dtype=f)
    ident = np.eye(128, dtype=f)
    tri = (n[:, None] <= n[None, :]).astype(f)
    ones = np.ones((128, 128), f)
    h = np.arange(4, dtype=f)
    log_g = np.log(f(1.0) - f(2.0) ** (f(-5.0) - h)).astype(f)
    diff = n[None, :] - n[:, None]
    maskT = np.where(diff[None] >= 0, np.exp(np.maximum(diff, 0.0)[None] * log_g[:, None, None]), 0.0).astype(f)
    xi = np.exp((n[None, :] + 1.0) * log_g[:, None]).astype(f)
    xi_bc = np.ascontiguousarray(np.broadcast_to(xi[None], (128, 4, 128))).astype(f)
    ixi = np.exp(-(n[None, :] + 1.0) * log_g[:, None]).astype(f)
    ixi_bc = np.ascontiguousarray(np.broadcast_to(ixi[None], (128, 4, 128))).astype(f)
    zeta = np.exp((128 - 1.0 - n[None, :]) * log_g[:, None]).astype(f)
    zeta_t = np.ascontiguousarray(zeta.T)
    g_chunk = np.exp(f(128.0) * log_g).astype(f)
    pos = np.arange(S, dtype=f)
    inv_freq = (f(10000.0) ** (-np.arange(0, 128, 2, dtype=f) / f(128))).astype(f)
    ang = (pos[:, None] * inv_freq[None, :]).astype(f)
    cos = np.cos(ang).astype(f)
    sin = np.sin(ang).astype(f)
    ks = f(128.0 ** -0.5)

    def lay(a):
        return np.ascontiguousarray(a.reshape(16, 128, 64).transpose(1, 0, 2))

    rope = np.stack([lay(cos), lay(sin), lay(-sin), lay(cos * ks), lay(sin * ks), lay(-sin * ks)], 0)
    sel = np.zeros((4, 4, 128), f)
    for b in range(4):
        sel[b, b, :] = 1.0
    return dict(
        identf=ident, identb=ident.astype(ml_dtypes.bfloat16), trib=tri.astype(ml_dtypes.bfloat16),
        trif=tri, onesf=ones, ixi_bc=ixi_bc, xi_bc=xi_bc, zeta_t=zeta_t,
        rope=np.ascontiguousarray(rope), g_chunk=g_chunk,
    )


_CONST = None


def build_program():
    consts = _constants()
    g_chunk = [float(v) for v in consts["g_chunk"]]
    nc = bass.Bass("TRN2", target_bir_lowering=False)
    P = Prog()
    es = ExitStack()

    def din(name, shape, dt=F32):
        return nc.dram_tensor(name, list(shape), dt, kind="ExternalInput").ap()

    x_d = din("x", [NSEQ * S, D])
    c_d = din("c", [NSEQ, D])
    wada_d = din("w_ada", [D, 6 * D])
    bada_d = din("b_ada", [1, 6 * D])
    win_d = din("w_in", [D, IN_COLS])
    bfg_d = din("b_forget", [1, 8])
    qg_d = din("q_gain", [1, 64])
    kg_d = din("k_gain", [1, 64])
    fxg_d = din("fox_gain", [1, 512])
    rtg_d = din("ret_gain", [1, 512])
    wout_d = din("w_out", [D, D])
    w1_d = din("w1", [D, DFF])
    w2_d = din("w2", [DFF, D])
    identf_d = din("identf", [128, 128])
    identb_d = din("identb", [128, 128], BF16)
    trib_d = din("trib", [128, 128], BF16)
    trif_d = din("trif", [128, 128])
    onesf_d = din("onesf", [128, 128])
    ixibc_d = din("ixi_bc", [128, 4, 128])
    xibc_d = din("xi_bc", [128, 4, 128])
    zeta_d = din("zeta_t", [128, 4])
    rope_d = din("rope", [6, 128, 16, 64])
    y_d = nc.dram_tensor("y", [NSEQ * S, D], F32, kind="ExternalOutput").ap()
    wbf_d = nc.dram_tensor("wbf", [NCHUNK, 128, 4096], BF16, kind="Internal").ap()
    gates_d = nc.dram_tensor("gates_scr", [4, 2048], F32, kind="Internal").ap()

    def sb(name, shape, dt=F32):
        return es.enter_context(nc.sbuf_tensor(name, list(shape), dt))

    wbuf = [sb(f"wbuf{i}", [128, 4096], BF16) for i in range(3)]
    xs = sb("xs", [128, G, D])
    hTA = sb("hTA", [128, 8, 512], BF16)
    hTB = sb("hTB", [128, 8, 512], BF16)
    U = sb("U", [128, 16384], BF16)
    KT = sb("KT", [70, 8, S], BF16)
    Vaug = sb("Vaug", [128, NT, 8, 65], BF16)
    qaug = [sb(f"qaug{i}", [128, 8, 70], BF16) for i in range(3)]
    kaug = [sb(f"kaug{i}", [128, 8, 70], BF16) for i in range(3)]
    state = sb("state", [128, 4, 128])
    state_bf = sb("state_bf", [128, 4, 128], BF16)
    MG = sb("MG", [128, 8192], BF16)
    mixed = MG[:, 0:4096].rearrange("p (t d) -> p t d", d=1024)
    gates_f = MG[:, 4096:6144].rearrange("p (t d) -> p t d", d=512)
    gates_r = MG[:, 6144:8192].rearrange("p (t d) -> p t d", d=512)
    xnext = MG[:, :].bitcast(F32).rearrange("p (t d) -> p t d", d=1024)
    XNK = [[("mixed", 0), ("mixed", 1)], [("mixed", 2), ("mixed", 3)],
           [("gates_f", t) for t in range(G)], [("gates_r", t) for t in range(G)]]
    PT = [sb(f"PT{i}", [128, 512], BF16) for i in range(3)]
    STr = [sb(f"STr{i}", [128, 4, 128], BF16) for i in range(2)]
    TA = [sb(f"TA{i}", [128, 512]) for i in range(2)]
    TB = [sb(f"TB{i}", [128, 512]) for i in range(2)]
    TE = [sb(f"TE{i}", [128, 4, 64]) for i in range(4)]
    TR = sb("TR", [128, 512])
    ropeg = sb("ropeg", [128, 6, G, 64])
    gm_bc = sb("gm_bc", [128, D])
    gf_bc = sb("gf_bc", [128, D])
    opm_m = sb("opm_m", [128, 8, 4])
    sh_m = sb("sh_m", [128, 8, 4])
    opm_f = sb("opm_f", [128, 8, 4])
    sh_f = sb("sh_f", [128, 8, 4])
    identf = sb("identf_s", [128, 128])
    identb = sb("identb_s", [128, 128], BF16)
    trib = sb("trib_s", [128, 128], BF16)
    trif = sb("trif_s", [128, 128])
    onesf = sb("onesf_s", [128, 128])
    ixibc = sb("ixibc_s", [128, 4, 128])
    xibc = sb("xibc_s", [128, 4, 128])
    zeta = sb("zeta_s", [128, 4])
    qg_col = sb("qg_col", [70, 1])
    kg_col = sb("kg_col", [70, 1])
    fxg_bc = sb("fxg_bc", [128, 512])
    rtg_bc = sb("rtg_bc", [128, 512])
    bfg_bc = sb("bfg_bc", [128, 8])
    wfg = sb("wfg", [128, 8, 8], BF16)
    wfg32 = sb("wfg32", [128, 8, 8])
    nhalf = sb("nhalf", [128, 8])
    epsc = sb("epsc", [128, 1])
    st = sb("st", [128, 128])
    rs_run = sb("rs_run", [128, 8])
    fz = sb("fz", [128, 3, 32])
    cr = sb("cr", [128, 2, 32])
    pre = sb("pre", [128, G, 8])
    cumsp = sb("cumsp", [128, G, 8, 3], BF16)
    cactT = sb("cactT", [128, 8, 4])
    ctmp = sb("ctmp", [128, 8, 4])
    ones14 = sb("ones14", [1, 4])

    c4 = xs[0:4, 0, :]
    badar = [TB[0][0:1, :], TB[1][0:1, :]]
    grow = xs[0:4, 1:3, :].rearrange("p t d -> p (t d)")
    PS = [es.enter_context(nc.psum_tensor(f"P{i}", [128, 512], F32)) for i in range(8)]

    def UK(lo, hi):
        return [("U", i) for i in range(lo, hi)]

    def u_chunk(fc):
        return U[:, fc * 512:(fc + 1) * 512]

    def u_f32(lo_gran, n_gran):
        return U[:, lo_gran * 512:(lo_gran + n_gran) * 512].bitcast(F32)

    QT_v = U[0:70, 0:4096].rearrange("p (h n) -> p h n", n=512)
    QTr_v = U[:, 4096:6144].rearrange("p (h n) -> p h n", n=512)
    QxT_v = U[:, 6144:8192].rearrange("p (h n) -> p h n", n=512)
    KTr_v = U[:, 8192:10240].rearrange("p (h n) -> p h n", n=512)
    Kz_v = U[:, 10240:12288].rearrange("p (t n) -> p t n", n=512)
    Vr_v = U[:, 12288:14336].rearrange("p (t n) -> p t n", n=512)
    rqt_v = [U[:, 14336:14848], U[:, 14848:15360], U[:, 6144:6656]]
    rkt_v = [U[:, 15360:15872], U[:, 15872:16384], U[:, 6656:7168]]
    rqt_g = [28, 29, 12]
    rkt_g = [30, 31, 13]

    def PK(i):
        return [("P", i)]

    def Pb(i):
        return PS[i][:, :].bitcast(BF16)

    def dma(out, in_, sem, reads=(), writes=(), eng="sp"):
        return P.op(eng, lambda e, o=out, i=in_: e.dma_start(out=o, in_=i), reads=reads, writes=writes, dma=sem)

    dma(identf[:, :], identf_d, "c0", writes=["identf"])
    dma(identb[:, :], identb_d, "c0", writes=["identb"])
    dma(trib[:, :], trib_d, "c0", writes=["trib"])
    dma(trif[:, :], trif_d, "c0", writes=["trif"])
    dma(onesf[:, :], onesf_d, "c0", writes=["onesf"])
    dma(ixibc[:, :, :], ixibc_d, "c0", writes=["ixibc"])
    dma(xibc[:, :, :], xibc_d, "c0", writes=["xibc"])
    dma(zeta[:, :], zeta_d, "c0", writes=["zeta"])
    dma(qg_col[0:64, :], qg_d.rearrange("o d -> d o"), "c0", writes=["qg"])
    dma(kg_col[0:64, :], kg_d.rearrange("o d -> d o"), "c0", writes=["kg"])
    dma(fxg_bc[:, :], fxg_d.partition_broadcast(128), "c0", writes=["fxg"])
    dma(rtg_bc[:, :], rtg_d.partition_broadcast(128), "c0", writes=["rtg"])
    dma(bfg_bc[:, :], bfg_d.partition_broadcast(128), "c0", writes=["bfg"])
    dma(c4, c_d, "c0", writes=["c4"])
    dma(wfg32[:, :, :], win_d[:, 2048:2056].rearrange("(c p) n -> p c n", p=128), "c0", writes=["wfg32"])
    c0_total = P.dma_count["dma:c0"]
    for k in ["identf", "identb", "trib", "trif", "onesf", "ixibc", "xibc", "zeta", "qg", "kg", "fxg",
              "rtg", "bfg", "c4", "wfg32"]:
        P.res[k][0] = ("dma:c0", c0_total)
    P.clock[("dma:c0", c0_total)] = {"dma:c0": c0_total}

    P.op("pool", lambda e: e.memset(nhalf[:, :], -0.5), writes=["nhalf"])
    P.op("pool", lambda e: e.memset(epsc[:, :], EPS), writes=["epsc"])
    P.op("pool", lambda e: e.memset(ones14[:, :], 1.0), writes=["ones14"])
    P.op("pool", lambda e: e.memset(Vaug[:, :, :, 64:65], 1.0), writes=["Vaug_ones"])
    for i in range(3):
        P.op("pool", lambda e, i=i: e.memset(qaug[i][:, :, 67:70], 1.0), writes=[("qaug", i)])
        P.op("pool", lambda e, i=i: e.memset(kaug[i][:, :, 64:67], 1.0), writes=[("kaug", i)])
    P.op("dve", lambda e: e.tensor_copy(out=wfg[:, :, :], in_=wfg32[:, :, :]), reads=["wfg32"], writes=["wfg"])
    P.op("pool", lambda e: e.memset(qg_col[64:70, :], 1.0), writes=["qg1"])
    P.op("pool", lambda e: e.memset(kg_col[64:70, :], 1.0), writes=["kg1"])
    P.op("dve", lambda e: e.tensor_scalar(out=qg_col[0:64, :], in0=qg_col[0:64, :], scalar1=0.125, scalar2=None,
                                          op0=ALU.mult), reads=["qg"], writes=["qg"])

    def chunk_src(k):
        if k < 8:
            col0 = [0, 512, 1024, 1536, 2056, 2568, 3080, 3592][k]
            return win_d[:, col0:col0 + 512].rearrange("(c p) n -> p c n", p=128)
        if k < 10:
            q = k - 8
            return wout_d[:, q * 512:(q + 1) * 512].rearrange("(c p) n -> p c n", p=128)
        if k < 18:
            j = k - 10
            return w1_d[:, j * 512:(j + 1) * 512].rearrange("(c p) n -> p c n", p=128)
        j = k - 18
        return w2_d[j * 512:(j + 1) * 512, :].rearrange("(c p) n -> p c n", p=128)

    cast_eng = ["act", "dve", "pool"]

    def stage_load(k):
        s = k % 2
        src_ap = chunk_src(k)
        stg_v = u_f32(16 * s, 16).rearrange("p (c n) -> p c n", c=src_ap.shape[1])
        dma(stg_v, src_ap, f"stg{s}", writes=UK(16 * s, 16 * s + 16))

    stage_load(0)
    stage_load(1)
    for k in range(NCHUNK):
        s = k % 2
        slot = k % 3
        stg = u_f32(16 * s, 16)
        ce = cast_eng[k % 3]
        if ce == "act":
            P.op("act", lambda e, o=wbuf[slot][:, :], i=stg: e.activation(out=o, in_=i, func=AF.Copy),
                 reads=UK(16 * s, 16 * s + 16), writes=[("wbuf", slot)])
        else:
            P.op(ce, lambda e, o=wbuf[slot][:, :], i=stg: e.tensor_copy(out=o, in_=i),
                 reads=UK(16 * s, 16 * s + 16), writes=[("wbuf", slot)])
        if k + 2 < NCHUNK:
            stage_load(k + 2)
        dma(wbf_d[k], wbuf[slot][:, :], f"wst{slot}", reads=[("wbuf", slot)], writes=[("wbf", k)])

    for cc in range(8):
        P.op("pe", lambda e, cc=cc: e.transpose(out=PS[0][:, cc * 4:(cc + 1) * 4], in_=c4[:, cc * 128:(cc + 1) * 128],
                                               identity=identf[0:4, 0:4]),
             reads=["c4", "identf"], writes=PK(0))
    P.op("act", lambda e: e.activation(out=ctmp[:, :, :], in_=PS[0][:, 0:32].rearrange("p (c b) -> p c b", b=4),
                                       func=AF.Exp, scale=-1.0), reads=PK(0), writes=["ctmp"])
    P.op("dve", lambda e: e.tensor_scalar(out=ctmp[:, :, :], in0=ctmp[:, :, :], scalar1=1.0, scalar2=None, op0=ALU.add),
         reads=["ctmp"], writes=["ctmp"])
    P.op("dve", lambda e: e.reciprocal(out=ctmp[:, :, :], in_=ctmp[:, :, :]), reads=["ctmp"], writes=["ctmp"])
    P.op("dve", lambda e: e.tensor_tensor(out=cactT[:, :, :], in0=ctmp[:, :, :],
                                          in1=PS[0][:, 0:32].rearrange("p (c b) -> p c b", b=4), op=ALU.mult),
         reads=["ctmp"] + PK(0), writes=["cactT"])

    modT_dst = {0: (sh_m, False), 1: (opm_m, True), 3: (sh_f, False), 4: (opm_f, True)}
    for kb in range(12):
        s = kb % 2
        v = kb // 2
        half = kb % 2
        stg = u_f32(16 * s, 16).rearrange("p (c n) -> p c n", c=8)
        dma(stg, wada_d[:, kb * 512:(kb + 1) * 512].rearrange("(c p) n -> p c n", p=128), f"stg{s}",
            writes=UK(16 * s, 16 * s + 16))
        rk = UK(16 * s, 16 * s + 16)
        bd = badar[kb % 2]
        bdk = ("TB", kb % 2)
        dma(bd, bada_d[0:1, kb * 512:(kb + 1) * 512], f"bd{kb % 2}", writes=[bdk])
        if v in modT_dst:
            dst, plus1 = modT_dst[v]
            bank = 1 + (kb % 2)
            first = True
            for ec in range(4):
                col = kb * 512 + ec * 128
                for dc in range(8):
                    P.op("pe", lambda e, bank=bank, ec=ec, dc=dc, stg=stg, first=first: e.matmul(
                        PS[bank][:, ec * 4:(ec + 1) * 4], lhsT=stg[:, dc, ec * 128:(ec + 1) * 128],
                        rhs=cactT[:, dc, :], start=first, stop=False, skip_group_check=True),
                        reads=rk + ["cactT"], writes=PK(bank))
                    first = False
                P.op("pe", lambda e, bank=bank, ec=ec, bd=bd: e.matmul(
                    PS[bank][:, ec * 4:(ec + 1) * 4], lhsT=bd[0:1, ec * 128:(ec + 1) * 128], rhs=ones14[0:1, :],
                    start=False, stop=True, skip_group_check=True),
                    reads=[bdk, "ones14"], writes=PK(bank))
            src_v = PS[bank][:, 0:16].rearrange("p (c b) -> p c b", b=4)
            dst_v = dst[:, half * 4:(half + 1) * 4, :]
            if plus1:
                P.op("dve", lambda e, o=dst_v, i=src_v: e.tensor_scalar(out=o, in0=i, scalar1=1.0, scalar2=None,
                                                                        op0=ALU.add),
                     reads=PK(bank), writes=[("modT", v, half)])
            else:
                P.op("dve", lambda e, o=dst_v, i=src_v: e.tensor_copy(out=o, in_=i),
                     reads=PK(bank), writes=[("modT", v, half)])
        else:
            bank = 3 + (kb % 2)
            gi = 0 if v == 2 else 1
            for dc in range(8):
                P.op("pe", lambda e, bank=bank, dc=dc, stg=stg: e.matmul(
                    PS[bank][0:4, :], lhsT=cactT[:, dc, :], rhs=stg[:, dc, :], start=(dc == 0), stop=False),
                    reads=rk + ["cactT"], writes=PK(bank))
            P.op("pe", lambda e, bank=bank, bd=bd: e.matmul(
                PS[bank][0:4, :], lhsT=ones14[0:1, :], rhs=bd[0:1, :],
                start=False, stop=True), reads=[bdk, "ones14"], writes=PK(bank))
            P.op("dve", lambda e, bank=bank, gi=gi, half=half: e.tensor_copy(
                out=grow[:, gi * 1024 + half * 512: gi * 1024 + (half + 1) * 512], in_=PS[bank][0:4, :]),
                reads=PK(bank), writes=["grow"])
    dma(gates_d, grow, "gsc", reads=["grow"], writes=["gates_d"])

    from collections import deque
    GROUP_ORDER = [3, 7, 0, 1, 2, 4, 5, 6] + list(range(8, NCHUNK))
    stream = [k for _ in range(NSEQ * NG) for k in GROUP_ORDER]
    pf = {"next": 0, "slots": {}, "cons": 0}

    def prefetch_upto(n):
        while pf["next"] < min(n, len(stream)):
            i = pf["next"]
            slot = i % 3
            dma(wbuf[slot][:, :], wbf_d[stream[i]], f"wld{slot}", reads=[("wbf", stream[i])], writes=[("wbuf", slot)])
            pf["slots"][i] = slot
            pf["next"] += 1

    def next_chunk(expect):
        i = pf["cons"]
        assert stream[i] == expect, (stream[i], expect)
        prefetch_upto(i + 2)
        pf["cons"] += 1
        return pf["slots"][i], i

    def after_chunk(i):
        prefetch_upto(i + 3)

    rr = {"proj": 0, "tp": 0, "tq": 0, "qa": 0, "ka": 0, "mi": 0, "tmp": 0, "rq": 0, "rk": 0, "qkp": 0}
    store_toks = []
    tmps = [(TA[0], ("TA", 0)), (TB[0], ("TB", 0)), (TA[1], ("TA", 1)), (TB[1], ("TB", 1))]

    def next_tmp():
        r = tmps[rr["tmp"] % 4]
        rr["tmp"] += 1
        return r

    def bc3(ap2, n):
        return ap2.unsqueeze(2).to_broadcast([128, ap2.shape[1], n])

    def bcmid(ap2, m):
        return ap2.unsqueeze(1).to_broadcast([128, m, ap2.shape[1]])

    def act_rstd(c_in, c_out, n, inv):
        P.op("act", lambda e: e.activation(out=st[:, c_out:c_out + n], in_=st[:, c_in:c_in + n], func=AF.Ln,
                                           scale=inv, bias=epsc[:, 0:1]),
             reads=[("st", c_in), "epsc"], writes=[("st", c_out)])
        P.op("act", lambda e: e.activation(out=st[:, c_out:c_out + n], in_=st[:, c_out:c_out + n], func=AF.Exp,
                                           scale=-0.5),
             reads=[("st", c_out)], writes=[("st", c_out)])

    def act_copy(out, in_, reads, writes):
        P.op("act", lambda e: e.activation(out=out, in_=in_, func=AF.Copy), reads=reads, writes=writes)

    def dve_copy(out, in_, reads, writes):
        P.op("dve", lambda e: e.tensor_copy(out=out, in_=in_), reads=reads, writes=writes)

    HB = [hTA, hTB]
    HK = ["hTA", "hTB"]

    def rms_chain(b_, src_t, src_keys, xn, xn_keys, hT, hkey, opm, shf, vs, vh, banks):
        junk = hT[:, 0:2, :].rearrange("p a n -> p (a n)")
        jk = [(hkey, 0), (hkey, 1)]

        def stage0():
            for t in range(G):
                P.op("act", lambda e, t=t: e.activation(out=junk, in_=src_t[:, t, :], func=AF.Square,
                                                        accum_out=st[:, t:t + 1]),
                     reads=src_keys[t], writes=jk + [("st", 0)])
            act_rstd(0, 8, 4, 1.0 / D)
            for t in range(G):
                P.op("act", lambda e, t=t: e.activation(out=xn[:, t, :], in_=src_t[:, t, :], func=AF.Identity,
                                                        scale=st[:, 8 + t:9 + t]),
                     reads=src_keys[t] + [("st", 8)], writes=xn_keys[t])

        def mk(c0):
            def stage():
                for c in (c0, c0 + 1):
                    bank = banks[c % len(banks)]
                    for t in range(G):
                        P.op("pe", lambda e, c=c, t=t, bank=bank: e.transpose(
                            out=PS[bank][:, t * 128:(t + 1) * 128], in_=xn[:, t, c * 128:(c + 1) * 128],
                            identity=identf[:, :]),
                            reads=xn_keys[t] + ["identf"], writes=PK(bank))
                    P.op("act", lambda e, c=c, bank=bank: e.activation(
                        out=hT[:, c, :], in_=PS[bank][:, :], func=AF.Identity,
                        scale=opm[:, c, b_:b_ + 1], bias=shf[:, c, b_:b_ + 1]),
                        reads=PK(bank) + [("modT", vs, c // 4), ("modT", vh, c // 4)], writes=[(hkey, c)])
            return stage
        return [stage0] + [mk(c0) for c0 in (0, 2, 4, 6)]

    def prefetch_B(gi_n):
        b_n, g_n = gi_n // NG, gi_n % NG
        r0 = b_n * S + g_n * 512
        dma(xnext[:, :, :], x_d[r0:r0 + 512, :].rearrange("(t p) d -> p t d", p=128), "xnl",
            writes=[k for ks in XNK for k in ks])
        return rms_chain(b_n, xnext, XNK, xnext, XNK, HB[gi_n % 2], HK[gi_n % 2], opm_m, sh_m, 1, 0, [4, 5, 6, 7])

    def proj_mm(src, t, slot, bank, key):
        for c in range(8):
            P.op("pe", lambda e, c=c: e.matmul(
                PS[bank][:, :], lhsT=src[:, c, t * 128:(t + 1) * 128],
                rhs=wbuf[slot][:, c * 512:(c + 1) * 512], start=(c == 0), stop=(c == 7)),
                reads=[(key, c), ("wbuf", slot)], writes=PK(bank))

    def next_proj_bank():
        bk = 2 + rr["proj"] % 3
        rr["proj"] += 1
        return bk

    for b in range(NSEQ):
        dma(gm_bc[:, :], gates_d[b:b + 1, 0:1024].partition_broadcast(128), "gbm", reads=["gates_d"], writes=["gm_bc"])
        dma(gf_bc[:, :], gates_d[b:b + 1, 1024:2048].partition_broadcast(128), "gbf", reads=["gates_d"], writes=["gf_bc"])
        P.op("pool", lambda e: e.memset(rs_run[:, :], 0.0), writes=["rs_run"])
        P.op("pool", lambda e: e.memset(state[:, :, :], 0.0), writes=["state"])
        P.op("pool", lambda e: e.memset(state_bf[:, :, :], 0.0), writes=["state_bf"])

        for g in range(NG):
            row0 = b * S + g * 512
            dma(xs[:, :, :], x_d[row0:row0 + 512, :].rearrange("(t p) d -> p t d", p=128), "xld",
                writes=[("xs", t) for t in range(G)])
            dma(ropeg[:, :, :, :], rope_d[:, :, g * G:(g + 1) * G, :].rearrange("r p t i -> p r t i"), "rope",
                writes=["ropeg"])

            gi = b * NG + g
            if gi == 0:
                for stg_ in prefetch_B(0):
                    stg_()
            hT_cur, hkey = HB[gi % 2], HK[gi % 2]
            hT_oth, okey = HB[(gi + 1) % 2], HK[(gi + 1) % 2]

            first = True
            for t in range(G):
                for c in range(8):
                    P.op("pe", lambda e, c=c, t=t, hT_cur=hT_cur, first=first: e.matmul(
                        PS[7][:, t * 8:(t + 1) * 8], lhsT=hT_cur[:, c, t * 128:(t + 1) * 128],
                        rhs=wfg[:, c, :], start=first, stop=(c == 7), skip_group_check=True),
                        reads=[(hkey, c), "wfg"], writes=PK(7))
                    first = False
            P.op("dve", lambda e: e.tensor_tensor(out=fz[:, 0, :].rearrange("p (t h) -> p t h", h=8),
                                                  in0=PS[7][:, 0:32].rearrange("p (t h) -> p t h", h=8),
                                                  in1=bcmid(bfg_bc[:, :], G), op=ALU.add),
                 reads=PK(7) + ["bfg"], writes=[("fz", 0)])
            P.op("act", lambda e: e.activation(out=fz[:, 1, :], in_=fz[:, 0, :], func=AF.Exp, scale=-1.0),
                 reads=[("fz", 0)], writes=[("fz", 1)])
            P.op("act", lambda e: e.activation(out=fz[:, 2, :], in_=fz[:, 1, :], func=AF.Ln, bias=1.0),
                 reads=[("fz", 1)], writes=[("fz", 2)])
            lall = fz[:, 2, :].rearrange("p (t h) -> p t h", h=8)
            P.op("dve", lambda e: e.tensor_copy(out=pre[:, 0, :], in_=rs_run[:, :]), reads=["rs_run"], writes=["pre"])
            for t in range(1, G):
                P.op("dve", lambda e, t=t: e.tensor_tensor(out=pre[:, t, :], in0=pre[:, t - 1, :], in1=lall[:, t - 1, :],
                                                           op=ALU.add), reads=["pre", ("fz", 2)], writes=["pre"])
            P.op("dve", lambda e: e.tensor_tensor(out=rs_run[:, :], in0=pre[:, G - 1, :], in1=lall[:, G - 1, :], op=ALU.add),
                 reads=["pre", ("fz", 2)], writes=["rs_run"])

            def cum_finish():
                P.op("pe", lambda e: e.matmul(PS[7][:, 32:64], lhsT=trif[:, :], rhs=fz[:, 2, :], start=True, stop=False),
                     reads=[("fz", 2), "trif"], writes=PK(7))
                P.op("pe", lambda e: e.matmul(PS[7][:, 32:64], lhsT=onesf[:, :], rhs=pre[:, :, :].rearrange("p t h -> p (t h)"),
                                              start=False, stop=True), reads=["pre", "onesf"], writes=PK(7))
                ncum = PS[7][:, 32:64].rearrange("p (t h) -> p t h", h=8)
                ck = [("cumsp", t) for t in range(G)]
                cr0 = cr[:, 0, :].rearrange("p (t h) -> p t h", h=8)
                cr1 = cr[:, 1, :].rearrange("p (t h) -> p t h", h=8)
                P.op("dve", lambda e: e.tensor_copy(out=cumsp[:, :, :, 0], in_=ncum), reads=PK(7), writes=ck)
                P.op("dve", lambda e: e.tensor_tensor(out=cr0, in0=ncum, in1=cumsp[:, :, :, 0], op=ALU.subtract),
                     reads=PK(7) + ck, writes=[("cr", 0)])
                P.op("dve", lambda e: e.tensor_copy(out=cumsp[:, :, :, 1], in_=cr0), reads=[("cr", 0)], writes=ck)
                P.op("dve", lambda e: e.tensor_tensor(out=cr1, in0=cr0, in1=cumsp[:, :, :, 1], op=ALU.subtract),
                     reads=[("cr", 0)] + ck, writes=[("cr", 1)])
                P.op("dve", lambda e: e.tensor_copy(out=cumsp[:, :, :, 2], in_=cr1), reads=[("cr", 1)], writes=ck)

            pipe = []

            def pipe_tick():
                keep = []
                for it in list(pipe):
                    it[0] += 1
                    a = it[0]
                    if a - 1 < len(it[1]) and it[1][a - 1] is not None:
                        it[1][a - 1]()
                    if a < len(it[1]):
                        keep.append(it)
                pipe[:] = keep

            def pipe_push(stages):
                pipe_tick()
                pipe.append([0, stages])

            def push_deferred(fn):
                pipe_push([None, fn])

            def flush_deferred():
                while pipe:
                    pipe_tick()

            slot, ci = next_chunk(3)
            for t in range(G):
                bank = next_proj_bank()
                proj_mm(hT_cur, t, slot, bank, hkey)
                tmp, tkey = next_tmp()
                P.op("act", lambda e, tmp=tmp, bank=bank: e.activation(out=tmp[:, :], in_=PS[bank][:, :], func=AF.Sigmoid),
                     reads=PK(bank), writes=[tkey])
                P.op("pool", lambda e, t=t, tmp=tmp: e.tensor_tensor(out=gates_f[:, t, :], in0=tmp[:, :], in1=fxg_bc[:, :],
                                                                     op=ALU.mult),
                     reads=[tkey, "fxg"], writes=[("gates_f", t)])
            after_chunk(ci)
            slot, ci = next_chunk(7)
            for t in range(G):
                bank = next_proj_bank()
                proj_mm(hT_cur, t, slot, bank, hkey)
                tmp, tkey = next_tmp()
                tmp2, tkey2 = next_tmp()
                P.op("act", lambda e, tmp=tmp, bank=bank: e.activation(out=tmp[:, :], in_=PS[bank][:, :], func=AF.Sigmoid),
                     reads=PK(bank), writes=[tkey])
                P.op("dve", lambda e, tmp=tmp, tmp2=tmp2, bank=bank: e.tensor_tensor(out=tmp2[:, :], in0=PS[bank][:, :],
                                                                                    in1=tmp[:, :], op=ALU.mult),
                     reads=[tkey] + PK(bank), writes=[tkey2])
                P.op("pool", lambda e, t=t, tmp2=tmp2: e.tensor_tensor(out=gates_r[:, t, :], in0=tmp2[:, :], in1=rtg_bc[:, :],
                                                                       op=ALU.mult),
                     reads=[tkey2, "rtg"], writes=[("gates_r", t)])
            after_chunk(ci)

            cum_finish()

            def qk_step(t, slot, is_q):
                bank = next_proj_bank()
                proj_mm(hT_cur, t, slot, bank, hkey)
                if is_q:
                    par = rr["qa"] % 3
                    rr["qa"] += 1
                    aug, akey, gcol, gkeys = qaug[par], ("qaug", par), qg_col, ["qg", "qg1"]
                else:
                    par = rr["ka"] % 3
                    rr["ka"] += 1
                    aug, akey, gcol, gkeys = kaug[par], ("kaug", par), kg_col, ["kg", "kg1"]
                tmp, tkey = next_tmp()
                par2 = rr["qkp"] % 2
                rr["qkp"] += 1
                cs, cr_ = 64 + 16 * par2, 72 + 16 * par2
                P.op("act", lambda e: e.activation(out=tmp[:, :], in_=PS[bank][:, :], func=AF.Square),
                     reads=PK(bank), writes=[tkey])
                P.op("dve", lambda e: e.tensor_reduce(out=st[:, cs:cs + 8], in_=tmp[:, :].rearrange("p (h i) -> p h i", i=64),
                                                      axis=AX.X, op=ALU.add), reads=[tkey], writes=[("st", cs)])

                def stage_b():
                    act_rstd(cs, cr_, 8, 1.0 / 64)
                    P.op("dve", lambda e: e.tensor_tensor(out=aug[:, :, 0:64],
                                                          in0=PS[bank][:, :].rearrange("p (h i) -> p h i", i=64),
                                                          in1=bc3(st[:, cr_:cr_ + 8], 64), op=ALU.mult),
                         reads=PK(bank) + [("st", cr_)], writes=[akey])
                    if is_q:
                        P.op("pool", lambda e: e.tensor_scalar(out=aug[:, :, 64:67], in0=cumsp[:, t, :, :], scalar1=-1.0,
                                                               scalar2=None, op0=ALU.mult),
                             reads=[("cumsp", t)], writes=[akey])
                    else:
                        P.op("pool", lambda e: e.tensor_copy(out=aug[:, :, 67:70], in_=cumsp[:, t, :, :]),
                             reads=[("cumsp", t)], writes=[akey])

                def deferred():
                    bq = 5 + rr["tq"] % 2
                    rr["tq"] += 1
                    for h in range(8):
                        P.op("pe", lambda e, h=h: e.transpose(out=Pb(bq)[0:70, h * 128:(h + 1) * 128], in_=aug[:, h, :],
                                                              identity=identb[:, :]),
                             reads=[akey, "identb"], writes=PK(bq))
                    srcv = Pb(bq)[0:70, :].rearrange("p (h n) -> p h n", n=128)
                    if is_q:
                        dst, wk = QT_v[:, :, t * 128:(t + 1) * 128], UK(0, 8)
                    else:
                        blk_i = g * G + t
                        dst, wk = KT[:, :, blk_i * 128:(blk_i + 1) * 128], [("KT", blk_i)]
                    P.op("dve", lambda e: e.tensor_scalar(out=dst, in0=srcv, scalar1=gcol[:, 0:1], scalar2=None,
                                                          op0=ALU.mult),
                         reads=PK(bq) + gkeys, writes=wk)
                pipe_push([stage_b, deferred])

            for is_q, ck_ in ((True, 0), (False, 1)):
                slot, ci = next_chunk(ck_)
                for t in range(G):
                    qk_step(t, slot, is_q)
                after_chunk(ci)

            slot, ci = next_chunk(2)
            for t in range(G):
                bank = next_proj_bank()
                proj_mm(hT_cur, t, slot, bank, hkey)
                blk_i = g * G + t
                act_copy(Vaug[:, blk_i, :, 0:64], PS[bank][:, :].rearrange("p (h i) -> p h i", i=64), PK(bank),
                         [("Vaug", blk_i)])
                pipe_push([])
            after_chunk(ci)
            flush_deferred()

            def side_bank():
                bk = (5, 7)[rr["proj"] % 2]
                rr["proj"] += 1
                return bk

            def rope_step(t, slot, is_q):
                bank = side_bank()
                proj_mm(hT_cur, t, slot, bank, hkey)
                r0 = 0 if is_q else 3
                cosv, sinv, nsinv = ropeg[:, r0, t, :], ropeg[:, r0 + 1, t, :], ropeg[:, r0 + 2, t, :]
                ta, tak = next_tmp()
                tb, tbk = next_tmp()
                pv = PS[bank][:, :].rearrange("p (h w i) -> p h w i", h=4, w=2)
                ta4 = ta[:, :].rearrange("p (h w i) -> p h w i", h=4, w=2)
                tb4 = tb[:, :].rearrange("p (h w i) -> p h w i", h=4, w=2)
                cos4 = cosv.unsqueeze(1).unsqueeze(1).to_broadcast([128, 4, 2, 64])
                P.op("dve", lambda e: e.tensor_tensor(out=ta4, in0=pv, in1=cos4, op=ALU.mult),
                     reads=PK(bank) + ["ropeg"], writes=[tak])
                P.op("dve", lambda e: e.tensor_tensor(out=tb4[:, :, 0, :], in0=pv[:, :, 1, :], in1=bcmid(nsinv, 4),
                                                      op=ALU.mult), reads=PK(bank) + ["ropeg"], writes=[tbk])
                P.op("dve", lambda e: e.tensor_tensor(out=tb4[:, :, 1, :], in0=pv[:, :, 0, :], in1=bcmid(sinv, 4),
                                                      op=ALU.mult), reads=PK(bank) + ["ropeg"], writes=[tbk])
                if is_q:
                    i = rr["rq"] % 3
                    rr["rq"] += 1
                    rt, rtk = rqt_v[i], ("U", rqt_g[i])
                else:
                    i = rr["rk"] % 3
                    rr["rk"] += 1
                    rt, rtk = rkt_v[i], ("U", rkt_g[i])
                P.op("pool", lambda e: e.tensor_tensor(out=rt, in0=ta[:, :], in1=tb[:, :], op=ALU.add),
                     reads=[tak, tbk], writes=[rtk])
                if not is_q:
                    P.op("pool", lambda e: e.tensor_tensor(out=Kz_v[:, t, :].rearrange("p (h i) -> p h i", i=128),
                                                           in0=rt.rearrange("p (h i) -> p h i", i=128),
                                                           in1=bc3(zeta[:, :], 128), op=ALU.mult),
                         reads=[rtk, "zeta"], writes=[("U", 20 + t)])

                def deferred():
                    bq = 6
                    for h in range(4):
                        P.op("pe", lambda e, h=h: e.transpose(out=Pb(bq)[:, h * 128:(h + 1) * 128],
                                                              in_=rt[:, h * 128:(h + 1) * 128], identity=identb[:, :]),
                             reads=[rtk, "identb"], writes=PK(bq))
                    srcv = Pb(bq)[:, 0:512].rearrange("p (h n) -> p h n", n=128)
                    tc_ = slice(t * 128, (t + 1) * 128)
                    if is_q:
                        P.op("dve", lambda e: e.tensor_tensor(out=QTr_v[:, :, tc_], in0=srcv, in1=xibc[:, :, :], op=ALU.mult),
                             reads=PK(bq) + ["xibc"], writes=UK(8, 12))
                    else:
                        P.op("dve", lambda e: e.tensor_tensor(out=KTr_v[:, :, tc_], in0=srcv, in1=ixibc[:, :, :], op=ALU.mult),
                             reads=PK(bq) + ["ixibc"], writes=UK(16, 20))
                return deferred

            def rv_step(t, slot):
                bank = side_bank()
                proj_mm(hT_cur, t, slot, bank, hkey)
                dve_copy(Vr_v[:, t, :], PS[bank][:, :], PK(bank), [("U", 24 + t)])
                return lambda: None

            chunk_state = {}

            def side_steps():
                units = []
                for kind, ck_ in (("rq", 4), ("rk", 5), ("rv", 6)):
                    for t in range(G):
                        units.append((kind, ck_, t))
                return units

            units = side_steps()

            def run_unit(u):
                kind, ck_, t = u
                if t == 0:
                    chunk_state["cur"] = next_chunk(ck_)
                slot, ci = chunk_state["cur"]
                if kind == "rq":
                    push_deferred(rope_step(t, slot, True))
                elif kind == "rk":
                    push_deferred(rope_step(t, slot, False))
                else:
                    push_deferred(rv_step(t, slot))
                if t == G - 1:
                    after_chunk(ci)

            nkb = 4 * g + 4
            tasks = [(h, kb) for h in range(8) for kb in range(nkb)]
            mixed_all = [("mixed", t) for t in range(G)]

            def fox_qk(i):
                h, kb = tasks[i]
                jlo = max(0, kb - 4 * g)
                n = (4 - jlo) * 128
                bank = i % 3
                pt = PT[i % 3]
                P.op("pe", lambda e: e.matmul(PS[bank][:, 0:n], lhsT=KT[:, h, kb * 128:(kb + 1) * 128],
                                              rhs=QT_v[:, h, jlo * 128:512], start=True, stop=True),
                     reads=[("KT", kb), ("U", h)], writes=PK(bank))
                P.op("act", lambda e: e.activation(out=pt[:, 0:n], in_=PS[bank][:, 0:n], func=AF.Exp),
                     reads=PK(bank), writes=[("PT", i % 3)])
                if kb >= 4 * g:
                    P.op("dve", lambda e: e.tensor_tensor(out=pt[:, 0:128], in0=pt[:, 0:128], in1=trib[:, :], op=ALU.mult),
                         reads=[("PT", i % 3), "trib"], writes=[("PT", i % 3)])

            def fox_pv(i):
                h, kb = tasks[i]
                jlo = max(0, kb - 4 * g)
                ob = 3 + (h % 2)
                pt = PT[i % 3]
                for j in range(jlo, 4):
                    P.op("pe", lambda e, j=j, last=(kb == 4 * g + j): e.matmul(PS[ob][:, j * 65:(j + 1) * 65],
                                                       lhsT=pt[:, (j - jlo) * 128:(j - jlo + 1) * 128],
                                                       rhs=Vaug[:, kb, h, :], start=(kb == 0 and j == 0),
                                                       stop=last, skip_group_check=True),
                         reads=[("PT", i % 3), ("Vaug", kb), "Vaug_ones"], writes=PK(ob))
                if kb == nkb - 1:
                    fox_epilogue(h, ob, i + 2)

            def at(idx, fn):
                sched.setdefault(idx, []).append(fn)

            def fox_epilogue(h, ob, i_now):
                O = PS[ob][:, 0:260].rearrange("p (j e) -> p j e", e=65)
                p_ = h % 2
                oz, ozk = (TE[0], ("TE", 0)) if p_ == 0 else (TE[3], ("TE", 3))
                c_ss, c_rs = 96 + 16 * p_, 104 + 16 * p_
                P.op("dve", lambda e: e.reciprocal(out=st[:, 40:44], in_=O[:, :, 64]), reads=PK(ob), writes=[("st", 40)])
                P.op("dve", lambda e: e.tensor_tensor(out=oz[:, :, :], in0=O[:, :, 0:64], in1=bc3(st[:, 40:44], 64),
                                                      op=ALU.mult), reads=PK(ob) + [("st", 40)], writes=[ozk])
                P.op("pool", lambda e: e.tensor_tensor(out=TE[1][:, :, :], in0=oz[:, :, :], in1=oz[:, :, :], op=ALU.mult),
                     reads=[ozk], writes=[("TE", 1)])
                P.op("dve", lambda e: e.tensor_reduce(out=st[:, c_ss:c_ss + 4], in_=TE[1][:, :, :], axis=AX.X, op=ALU.add),
                     reads=[("TE", 1)], writes=[("st", c_ss)])

                def e1():
                    act_rstd(c_ss, c_rs, 4, 1.0 / 64)

                def e2():
                    P.op("dve", lambda e: e.tensor_tensor(out=TE[2][:, :, :], in0=oz[:, :, :], in1=bc3(st[:, c_rs:c_rs + 4], 64),
                                                          op=ALU.mult), reads=[ozk, ("st", c_rs)], writes=[("TE", 2)])
                    P.op("pool", lambda e: e.tensor_tensor(out=mixed[:, :, h * 64:(h + 1) * 64], in0=TE[2][:, :, :],
                                                           in1=gates_f[:, :, h * 64:(h + 1) * 64], op=ALU.mult),
                         reads=[("TE", 2)] + [("gates_f", t) for t in range(G)], writes=mixed_all)
                at(i_now + 2, e1)
                at(i_now + 3, e2)

            def ret_stage1(j):
                jc = slice(j * 128, (j + 1) * 128)
                for h in range(4):
                    P.op("pe", lambda e, h=h: e.matmul(PS[5][:, h * 128:(h + 1) * 128], lhsT=KTr_v[:, h, jc],
                                                       rhs=QTr_v[:, h, jc], start=(h == 0), stop=True,
                                                       skip_group_check=True),
                         reads=UK(16, 20) + UK(8, 12), writes=PK(5))
                for h in range(4):
                    hc = slice(h * 128, (h + 1) * 128)
                    P.op("pe", lambda e, hc=hc, h=h: e.matmul(PS[7][:, hc], lhsT=Kz_v[:, j, hc], rhs=Vr_v[:, j, hc],
                                                              start=(h == 0), stop=True, skip_group_check=True),
                         reads=[("U", 20 + j), ("U", 24 + j)], writes=PK(7))
                sj = j % 2
                P.op("dve", lambda e: e.tensor_tensor(out=STr[sj][:, :, :],
                                                      in0=PS[5][:, :].rearrange("p (h n) -> p h n", n=128),
                                                      in1=bcmid(trib[:, :], 4), op=ALU.mult),
                     reads=PK(5) + ["trib"], writes=[("STr", sj)])

            def ret_stage2(j):
                jc = slice(j * 128, (j + 1) * 128)
                sj = j % 2
                for h in range(4):
                    hc = slice(h * 128, (h + 1) * 128)
                    P.op("pe", lambda e, hc=hc, h=h: e.matmul(PS[6][:, hc], lhsT=STr[sj][:, h, :], rhs=Vr_v[:, j, hc],
                                                              start=(h == 0), stop=False, skip_group_check=True),
                         reads=[("STr", sj), ("U", 24 + j)], writes=PK(6))
                    P.op("pe", lambda e, hc=hc, h=h: e.matmul(PS[6][:, hc], lhsT=QTr_v[:, h, jc], rhs=state_bf[:, h, :],
                                                              start=False, stop=True, skip_group_check=True),
                         reads=UK(8, 12) + ["state_bf"], writes=PK(6))
                for h in range(4):
                    hc = slice(h * 128, (h + 1) * 128)
                    P.op("dve", lambda e, hc=hc, h=h: e.scalar_tensor_tensor(
                        out=state[:, h, :], in0=state[:, h, :], scalar=g_chunk[h], in1=PS[7][:, hc],
                        op0=ALU.mult, op1=ALU.add), reads=["state"] + PK(7), writes=["state"])
                P.op("pool", lambda e: e.tensor_copy(out=state_bf[:, :, :], in_=state[:, :, :]),
                     reads=["state"], writes=["state_bf"])
                pj = j % 2
                c_ss, c_rs = 16 + 8 * pj, 20 + 8 * pj
                dve_copy(TR[:, :], PS[6][:, :], PK(6), ["TR"])
                tmp, tkey = next_tmp()
                P.op("pool", lambda e: e.tensor_tensor(out=tmp[:, :], in0=TR[:, :], in1=TR[:, :], op=ALU.mult),
                     reads=["TR"], writes=[tkey])
                P.op("dve", lambda e: e.tensor_reduce(out=st[:, c_ss:c_ss + 4], in_=tmp[:, :].rearrange("p (h i) -> p h i", i=128),
                                                      axis=AX.X, op=ALU.add), reads=[tkey], writes=[("st", c_ss)])

                def r2c():
                    act_rstd(c_ss, c_rs, 4, 1.0 / 128)
                    tmp2, tkey2 = next_tmp()
                    P.op("dve", lambda e: e.tensor_tensor(out=tmp2[:, :].rearrange("p (h i) -> p h i", i=128),
                                                          in0=TR[:, :].rearrange("p (h i) -> p h i", i=128),
                                                          in1=bc3(st[:, c_rs:c_rs + 4], 128), op=ALU.mult),
                         reads=["TR", ("st", c_rs)], writes=[tkey2])
                    P.op("pool", lambda e: e.tensor_tensor(out=mixed[:, j, 512:1024], in0=tmp2[:, :], in1=gates_r[:, j, :],
                                                           op=ALU.mult),
                         reads=[tkey2, ("gates_r", j)], writes=[("mixed", j)])
                at(cur_i[0] + 2, r2c)

            ntask = len(tasks)
            sched = {}
            nun = len(units)
            span = max(nun, int(ntask * 0.55))
            for ui, u in enumerate(units):
                sched.setdefault(min(ntask - 1, ui * span // nun), []).append(lambda u=u: run_unit(u))
            sched.setdefault(min(ntask - 1, span), []).append(flush_deferred)
            rem0 = min(ntask - 1, span + 1)
            for j in range(4):
                p1 = rem0 + (2 * j) * (ntask - rem0) // 8
                p2 = rem0 + (2 * j + 1) * (ntask - rem0) // 8
                sched.setdefault(min(ntask - 1, p1), []).append(lambda j=j: ret_stage1(j))
                sched.setdefault(min(ntask - 1, p2), []).append(lambda j=j: ret_stage2(j))
            cur_i = [0]
            for i in range(ntask + 2):
                cur_i[0] = i
                for f in sched.pop(i, []):
                    f()
                if i < ntask:
                    fox_qk(i)
                if i >= 2:
                    fox_pv(i - 2)
            while sched:
                k_ = min(sched)
                cur_i[0] = k_
                for f in sched.pop(k_):
                    f()

            for c in range(8):
                bank = rr["tp"] % 2
                rr["tp"] += 1
                for t in range(G):
                    P.op("pe", lambda e, c=c, t=t, bank=bank: e.transpose(
                        out=Pb(bank)[:, t * 128:(t + 1) * 128], in_=mixed[:, t, c * 128:(c + 1) * 128],
                        identity=identb[:, :]), reads=[("mixed", t), "identb"], writes=PK(bank))
                if c % 2 == 0:
                    act_copy(hT_oth[:, c, :], Pb(bank)[:, 0:512], PK(bank), [(okey, c)])
                else:
                    dve_copy(hT_oth[:, c, :], Pb(bank)[:, 0:512], PK(bank), [(okey, c)])

            for q in range(2):
                slot, ci = next_chunk(8 + q)
                for t in range(G):
                    bank = next_proj_bank()
                    proj_mm(hT_oth, t, slot, bank, okey)
                    tmp, tkey = next_tmp()
                    qc = slice(q * 512, (q + 1) * 512)
                    P.op("dve", lambda e, tmp=tmp, bank=bank, qc=qc: e.tensor_tensor(out=tmp[:, :], in0=PS[bank][:, :],
                                                                                    in1=gm_bc[:, qc], op=ALU.mult),
                         reads=PK(bank) + ["gm_bc"], writes=[tkey])
                    P.op("pool", lambda e, tmp=tmp, t=t, qc=qc: e.tensor_tensor(out=xs[:, t, qc], in0=xs[:, t, qc],
                                                                               in1=tmp[:, :], op=ALU.add),
                         reads=[tkey, ("xs", t)], writes=[("xs", t)])
                after_chunk(ci)

            xn2 = u_f32(16, 16).rearrange("p (t d) -> p t d", t=G)
            for stg_ in rms_chain(b, xs, [[("xs", t)] for t in range(G)], xn2, [UK(16 + 4 * t, 20 + 4 * t) for t in range(G)],
                                  hT_cur, hkey, opm_f, sh_f, 4, 3, [0, 1]):
                stg_()

            pstages = prefetch_B(gi + 1) if gi + 1 < NSEQ * NG else []
            for j in range(8):
                slot, ci = next_chunk(10 + j)
                if pstages and 1 <= j <= 5:
                    pstages[j - 1]()
                for fc in range(4):
                    bank = rr["mi"] % 4
                    rr["mi"] += 1
                    tmp, tkey = next_tmp()
                    for c in range(8):
                        P.op("pe", lambda e, c=c, fc=fc, bank=bank, slot=slot, hT_cur=hT_cur: e.matmul(
                            PS[bank][:, :], lhsT=wbuf[slot][:, c * 512 + fc * 128: c * 512 + (fc + 1) * 128],
                            rhs=hT_cur[:, c, :], start=(c == 0), stop=(c == 7)),
                            reads=[(hkey, c), ("wbuf", slot)], writes=PK(bank))
                    P.op("act", lambda e, tmp=tmp, bank=bank: e.activation(out=tmp[:, :], in_=PS[bank][:, :], func=AF.Relu),
                         reads=PK(bank), writes=[tkey])
                    uc = u_chunk(4 * j + fc)
                    P.op("pool", lambda e, tmp=tmp, uc=uc: e.tensor_tensor(out=uc, in0=tmp[:, :], in1=tmp[:, :], op=ALU.mult),
                         reads=[tkey], writes=[("U", 4 * j + fc)])
                after_chunk(ci)

            for j in range(8):
                slot, ci = next_chunk(18 + j)
                for t in range(G):
                    for hf in range(2):
                        bank = t * 2 + hf
                        for fc in range(4):
                            uc = u_chunk(4 * j + fc)
                            P.op("pe", lambda e, uc=uc, t=t, hf=hf, fc=fc, bank=bank, slot=slot, j=j: e.matmul(
                                PS[bank][:, :], lhsT=uc[:, t * 128:(t + 1) * 128],
                                rhs=wbuf[slot][:, fc * 1024 + hf * 512: fc * 1024 + (hf + 1) * 512],
                                start=(j == 0 and fc == 0), stop=(j == 7 and fc == 3)),
                                reads=[("U", 4 * j + fc), ("wbuf", slot)], writes=PK(bank))
                after_chunk(ci)
            done_half = {}
            for (t, hf) in ((3, 1), (1, 0), (1, 1), (2, 0), (0, 0), (0, 1), (2, 1), (3, 0)):
                if True:
                    bank = t * 2 + hf
                    tmp, tkey = next_tmp()
                    qc = slice(hf * 512, (hf + 1) * 512)
                    P.op("dve", lambda e, tmp=tmp, bank=bank, qc=qc: e.tensor_tensor(out=tmp[:, :], in0=PS[bank][:, :],
                                                                                    in1=gf_bc[:, qc], op=ALU.mult),
                         reads=PK(bank) + ["gf_bc"], writes=[tkey])
                    P.op("pool", lambda e, tmp=tmp, t=t, qc=qc: e.tensor_tensor(out=xs[:, t, qc], in0=xs[:, t, qc],
                                                                               in1=tmp[:, :], op=ALU.add),
                         reads=[tkey, ("xs", t)], writes=[("xs", t)])
                done_half[t] = done_half.get(t, 0) + 1
                if done_half[t] == 2:
                    tok = dma(y_d[row0 + t * 128: row0 + (t + 1) * 128, :], xs[:, t, :], "yst", reads=[("xs", t)],
                              writes=[("y", row0 + t * 128)])
                    store_toks.append(tok)

    P.wait_all("sp", [max(store_toks, key=lambda tk: tk[1])])
    return nc, P, es, consts


_BUILT = None


def _get_built():
    global _BUILT
    if _BUILT is None:
        nc, P, es, consts = build_program()
        P.emit(nc, es)
        es.close()
        _BUILT = (nc, consts)
    return _BUILT


def kernel(x, c, w_ada, b_ada, w_in, b_forget, q_norm_gain, k_norm_gain, fox_out_gain, ret_out_gain,
           w_out, w_mlp_in, w_mlp_out):
    nc, consts = _get_built()
    f = np.float32
    x = np.asarray(x, f)
    c = np.asarray(c, f)
    shared = {
        "w_ada": np.ascontiguousarray(np.asarray(w_ada, f)[0]),
        "b_ada": np.ascontiguousarray(np.asarray(b_ada, f)[0].reshape(1, -1)),
        "w_in": np.ascontiguousarray(np.asarray(w_in, f)[0]),
        "b_forget": np.ascontiguousarray(np.asarray(b_forget, f)[0].reshape(1, 8)),
        "q_gain": np.ascontiguousarray(np.asarray(q_norm_gain, f)[0].reshape(1, 64)),
        "k_gain": np.ascontiguousarray(np.asarray(k_norm_gain, f)[0].reshape(1, 64)),
        "fox_gain": np.ascontiguousarray(np.asarray(fox_out_gain, f)[0].reshape(1, 512)),
        "ret_gain": np.ascontiguousarray(np.asarray(ret_out_gain, f)[0].reshape(1, 512)),
        "w_out": np.ascontiguousarray(np.asarray(w_out, f)[0]),
        "w1": np.ascontiguousarray(np.asarray(w_mlp_in, f)[0]),
        "w2": np.ascontiguousarray(np.asarray(w_mlp_out, f)[0]),
    }
    for k in ("identf", "identb", "trib", "trif", "onesf", "ixi_bc", "xi_bc", "zeta_t", "rope"):
        shared[k] = consts[k]
    in_maps = []
    for i in range(NCORES):
        m = dict(shared)
        m["x"] = np.ascontiguousarray(x[i * NSEQ:(i + 1) * NSEQ].reshape(NSEQ * S, D))
        m["c"] = np.ascontiguousarray(c[i * NSEQ:(i + 1) * NSEQ])
        in_maps.append(m)
    res = run_bass_kernel_spmd(nc, in_maps, core_ids=list(range(NCORES)))
    out = np.concatenate([np.asarray(r["y"], f).reshape(NSEQ, S, D) for r in res.results], axis=0)
    return out
```

```python
import numpy as np
import ml_dtypes
from contextlib import ExitStack
import concourse.bass as bass
import concourse.mybir as mybir
from concourse.bass_utils import run_bass_kernel_spmd

F32 = mybir.dt.float32
BF16 = mybir.dt.bfloat16
AF = mybir.ActivationFunctionType
ALU = mybir.AluOpType
AX = mybir.AxisListType

NCORES = 8
D = 1024
S = 2048
NSEQ = 4
NT = 16
G = 4
NG = NT // G
DFF = 4096
EPS = 1e-6
IN_COLS = 4104
NCHUNK = 26

ENGS = ("pe", "act", "dve", "pool", "sp")
import os
STRICT = bool(int(os.environ.get("KSTRICT", "0")))


class _Op:
    __slots__ = ("fn", "waits", "sig", "dma", "tok")


class Prog:
    def __init__(self):
        self.ops = {e: [] for e in ENGS}
        self.res = {}
        self.known = {e: {} for e in ENGS}
        self.clock = {}
        self.dma_count = {}
        self.needed = set()

    def _deps(self, eng, reads, writes):
        deps = set()
        for k in reads:
            r = self.res.get(k)
            if r is not None and r[0] is not None:
                deps.add(r[0])
        for k in writes:
            r = self.res.get(k)
            if r is not None:
                if r[0] is not None and (STRICT or r[0][0] != eng):
                    deps.add(r[0])
                for src, v in r[1].items():
                    if STRICT or src != eng:
                        deps.add((src, v))
        return deps

    def _commit(self, tok, reads, writes):
        for k in reads:
            r = self.res.get(k)
            if r is None:
                r = [None, {}]
                self.res[k] = r
            if r[1].get(tok[0], 0) < tok[1]:
                r[1][tok[0]] = tok[1]
        for k in writes:
            self.res[k] = [tok, {}]

    def op(self, eng, fn, reads=(), writes=(), dma=None):
        deps = self._deps(eng if dma is None else "dma:" + dma, reads, writes)
        kn = self.known[eng]
        waits = []
        best = {}
        for (src, v) in deps:
            if best.get(src, 0) < v:
                best[src] = v
        deps = set(best.items())
        for (src, v) in sorted(deps, key=lambda t: (str(t[0]), t[1])):
            if kn.get(src, 0) >= v:
                continue
            waits.append((src, v))
            self.needed.add((src, v))
            for s2, v2 in self.clock[(src, v)].items():
                if kn.get(s2, 0) < v2:
                    kn[s2] = v2
        o = _Op()
        o.fn = fn
        o.waits = waits
        o.dma = dma
        self.ops[eng].append(o)
        if dma is None:
            tok = (eng, len(self.ops[eng]))
        else:
            src = "dma:" + dma
            self.dma_count[src] = self.dma_count.get(src, 0) + 16
            tok = (src, self.dma_count[src])
        o.tok = tok
        ck = dict(kn)
        ck[tok[0]] = tok[1]
        self.clock[tok] = ck
        self._commit(tok, reads, writes)
        return tok

    def wait_all(self, eng, toks):
        kn = self.known[eng]
        waits = []
        for (src, v) in toks:
            if kn.get(src, 0) >= v:
                continue
            waits.append((src, v))
            self.needed.add((src, v))
            kn[src] = v
        o = _Op()
        o.fn = None
        o.waits = waits
        o.dma = None
        o.tok = None
        self.ops[eng].append(o)

    def emit(self, nc, es):
        sems = {}
        for e in ("pe", "act", "dve", "pool"):
            sems[e] = es.enter_context(nc.semaphore("sem_" + e))
        for src in self.dma_count:
            sems[src] = es.enter_context(nc.semaphore("sem_" + src.replace(":", "_")))
        sigval = {}
        for e in ("pe", "act", "dve", "pool"):
            cnt = 0
            for i, o in enumerate(self.ops[e]):
                o.sig = False
                if o.fn is not None and o.dma is None and (e, i + 1) in self.needed:
                    cnt += 1
                    o.sig = True
                    sigval[(e, i + 1)] = cnt
        blk = es.enter_context(nc.Block())

        def run(e, name):
            for o in self.ops[name]:
                for (src, v) in o.waits:
                    val = v if src.startswith("dma:") else sigval[(src, v)]
                    e.wait_ge(sems[src], val)
                if o.fn is None:
                    continue
                ins = o.fn(e)
                if o.dma is not None:
                    ins.then_inc(sems["dma:" + o.dma], 16)
                elif o.sig:
                    ins.then_inc(sems[name], 1)

        @blk.tensor
        def _(e):
            run(e, "pe")

        @blk.scalar
        def _(e):
            run(e, "act")

        @blk.vector
        def _(e):
            run(e, "dve")

        @blk.gpsimd
        def _(e):
            run(e, "pool")

        @blk.sync
        def _(e):
            run(e, "sp")


def _constants():
    f = np.float32
    n = np.arange(128, dtype=f)
    ident = np.eye(128, dtype=f)
    tri = (n[:, None] <= n[None, :]).astype(f)
    ones = np.ones((128, 128), f)
    h = np.arange(4, dtype=f)
    log_g = np.log(f(1.0) - f(2.0) ** (f(-5.0) - h)).astype(f)
    diff = n[None, :] - n[:, None]
    maskT = np.where(diff[None] >= 0, np.exp(np.maximum(diff, 0.0)[None] * log_g[:, None, None]), 0.0).astype(f)
    xi = np.exp((n[None, :] + 1.0) * log_g[:, None]).astype(f)
    xi_bc = np.ascontiguousarray(np.broadcast_to(xi[None], (128, 4, 128))).astype(f)
    ixi = np.exp(-(n[None, :] + 1.0) * log_g[:, None]).astype(f)
    ixi_bc = np.ascontiguousarray(np.broadcast_to(ixi[None], (128, 4, 128))).astype(f)
    zeta = np.exp((128 - 1.0 - n[None, :]) * log_g[:, None]).astype(f)
    zeta_t = np.ascontiguousarray(zeta.T)
    g_chunk = np.exp(f(128.0) * log_g).astype(f)
    pos = np.arange(S, dtype=f)
    inv_freq = (f(10000.0) ** (-np.arange(0, 128, 2, dtype=f) / f(128))).astype(f)
    ang = (pos[:, None] * inv_freq[None, :]).astype(f)
    cos = np.cos(ang).astype(f)
    sin = np.sin(ang).astype(f)
    ks = f(128.0 ** -0.5)

    def lay(a):
        return np.ascontiguousarray(a.reshape(16, 128, 64).transpose(1, 0, 2))

    rope = np.stack([lay(cos), lay(sin), lay(-sin), lay(cos * ks), lay(sin * ks), lay(-sin * ks)], 0)
    sel = np.zeros((4, 4, 128), f)
    for b in range(4):
        sel[b, b, :] = 1.0
    return dict(
        identf=ident, identb=ident.astype(ml_dtypes.bfloat16), trib=tri.astype(ml_dtypes.bfloat16),
        trif=tri, onesf=ones, ixi_bc=ixi_bc, xi_bc=xi_bc, zeta_t=zeta_t,
        rope=np.ascontiguousarray(rope), g_chunk=g_chunk,
    )


_CONST = None


def build_program():
    consts = _constants()
    g_chunk = [float(v) for v in consts["g_chunk"]]
    nc = bass.Bass("TRN2", target_bir_lowering=False)
    P = Prog()
    es = ExitStack()

    def din(name, shape, dt=F32):
        return nc.dram_tensor(name, list(shape), dt, kind="ExternalInput").ap()

    x_d = din("x", [NSEQ * S, D])
    c_d = din("c", [NSEQ, D])
    wada_d = din("w_ada", [D, 6 * D])
    bada_d = din("b_ada", [1, 6 * D])
    win_d = din("w_in", [D, IN_COLS])
    bfg_d = din("b_forget", [1, 8])
    qg_d = din("q_gain", [1, 64])
    kg_d = din("k_gain", [1, 64])
    fxg_d = din("fox_gain", [1, 512])
    rtg_d = din("ret_gain", [1, 512])
    wout_d = din("w_out", [D, D])
    w1_d = din("w1", [D, DFF])
    w2_d = din("w2", [DFF, D])
    identf_d = din("identf", [128, 128])
    identb_d = din("identb", [128, 128], BF16)
    trib_d = din("trib", [128, 128], BF16)
    trif_d = din("trif", [128, 128])
    onesf_d = din("onesf", [128, 128])
    ixibc_d = din("ixi_bc", [128, 4, 128])
    xibc_d = din("xi_bc", [128, 4, 128])
    zeta_d = din("zeta_t", [128, 4])
    rope_d = din("rope", [6, 128, 16, 64])
    y_d = nc.dram_tensor("y", [NSEQ * S, D], F32, kind="ExternalOutput").ap()
    wbf_d = nc.dram_tensor("wbf", [NCHUNK, 128, 4096], BF16, kind="Internal").ap()
    gates_d = nc.dram_tensor("gates_scr", [4, 2048], F32, kind="Internal").ap()

    def sb(name, shape, dt=F32):
        return es.enter_context(nc.sbuf_tensor(name, list(shape), dt))

    wbuf = [sb(f"wbuf{i}", [128, 4096], BF16) for i in range(3)]
    xs = sb("xs", [128, G, D])
    hTA = sb("hTA", [128, 8, 512], BF16)
    hTB = sb("hTB", [128, 8, 512], BF16)
    U = sb("U", [128, 16384], BF16)
    KT = sb("KT", [70, 8, S], BF16)
    Vaug = sb("Vaug", [128, NT, 8, 65], BF16)
    qaug = [sb(f"qaug{i}", [128, 8, 70], BF16) for i in range(3)]
    kaug = [sb(f"kaug{i}", [128, 8, 70], BF16) for i in range(3)]
    state = sb("state", [128, 4, 128])
    state_bf = sb("state_bf", [128, 4, 128], BF16)
    MG = sb("MG", [128, 8192], BF16)
    mixed = MG[:, 0:4096].rearrange("p (t d) -> p t d", d=1024)
    gates_f = MG[:, 4096:6144].rearrange("p (t d) -> p t d", d=512)
    gates_r = MG[:, 6144:8192].rearrange("p (t d) -> p t d", d=512)
    xnext = MG[:, :].bitcast(F32).rearrange("p (t d) -> p t d", d=1024)
    XNK = [[("mixed", 0), ("mixed", 1)], [("mixed", 2), ("mixed", 3)],
           [("gates_f", t) for t in range(G)], [("gates_r", t) for t in range(G)]]
    PT = [sb(f"PT{i}", [128, 512], BF16) for i in range(3)]
    STr = [sb(f"STr{i}", [128, 4, 128], BF16) for i in range(2)]
    TA = [sb(f"TA{i}", [128, 512]) for i in range(2)]
    TB = [sb(f"TB{i}", [128, 512]) for i in range(2)]
    TE = [sb(f"TE{i}", [128, 4, 64]) for i in range(4)]
    TR = sb("TR", [128, 512])
    ropeg = sb("ropeg", [128, 6, G, 64])
    gm_bc = sb("gm_bc", [128, D])
    gf_bc = sb("gf_bc", [128, D])
    opm_m = sb("opm_m", [128, 8, 4])
    sh_m = sb("sh_m", [128, 8, 4])
    opm_f = sb("opm_f", [128, 8, 4])
    sh_f = sb("sh_f", [128, 8, 4])
    identf = sb("identf_s", [128, 128])
    identb = sb("identb_s", [128, 128], BF16)
    trib = sb("trib_s", [128, 128], BF16)
    trif = sb("trif_s", [128, 128])
    onesf = sb("onesf_s", [128, 128])
    ixibc = sb("ixibc_s", [128, 4, 128])
    xibc = sb("xibc_s", [128, 4, 128])
    zeta = sb("zeta_s", [128, 4])
    qg_col = sb("qg_col", [70, 1])
    kg_col = sb("kg_col", [70, 1])
    fxg_bc = sb("fxg_bc", [128, 512])
    rtg_bc = sb("rtg_bc", [128, 512])
    bfg_bc = sb("bfg_bc", [128, 8])
    wfg = sb("wfg", [128, 8, 8], BF16)
    wfg32 = sb("wfg32", [128, 8, 8])
    nhalf = sb("nhalf", [128, 8])
    epsc = sb("epsc", [128, 1])
    st = sb("st", [128, 128])
    rs_run = sb("rs_run", [128, 8])
    fz = sb("fz", [128, 3, 32])
    cr = sb("cr", [128, 2, 32])
    pre = sb("pre", [128, G, 8])
    cumsp = sb("cumsp", [128, G, 8, 3], BF16)
    cactT = sb("cactT", [128, 8, 4])
    ctmp = sb("ctmp", [128, 8, 4])
    ones14 = sb("ones14", [1, 4])

    c4 = xs[0:4, 0, :]
    badar = [TB[0][0:1, :], TB[1][0:1, :]]
    grow = xs[0:4, 1:3, :].rearrange("p t d -> p (t d)")
    PS = [es.enter_context(nc.psum_tensor(f"P{i}", [128, 512], F32)) for i in range(8)]

    def UK(lo, hi):
        return [("U", i) for i in range(lo, hi)]

    def u_chunk(fc):
        return U[:, fc * 512:(fc + 1) * 512]

    def u_f32(lo_gran, n_gran):
        return U[:, lo_gran * 512:(lo_gran + n_gran) * 512].bitcast(F32)

    QT_v = U[0:70, 0:4096].rearrange("p (h n) -> p h n", n=512)
    QTr_v = U[:, 4096:6144].rearrange("p (h n) -> p h n", n=512)
    QxT_v = U[:, 6144:8192].rearrange("p (h n) -> p h n", n=512)
    KTr_v = U[:, 8192:10240].rearrange("p (h n) -> p h n", n=512)
    Kz_v = U[:, 10240:12288].rearrange("p (t n) -> p t n", n=512)
    Vr_v = U[:, 12288:14336].rearrange("p (t n) -> p t n", n=512)
    rqt_v = [U[:, 14336:14848], U[:, 14848:15360], U[:, 6144:6656]]
    rkt_v = [U[:, 15360:15872], U[:, 15872:16384], U[:, 6656:7168]]
    rqt_g = [28, 29, 12]
    rkt_g = [30, 31, 13]

    def PK(i):
        return [("P", i)]

    def Pb(i):
        return PS[i][:, :].bitcast(BF16)

    def dma(out, in_, sem, reads=(), writes=(), eng="sp"):
        return P.op(eng, lambda e, o=out, i=in_: e.dma_start(out=o, in_=i), reads=reads, writes=writes, dma=sem)

    dma(identf[:, :], identf_d, "c0", writes=["identf"])
    dma(identb[:, :], identb_d, "c0", writes=["identb"])
    dma(trib[:, :], trib_d, "c0", writes=["trib"])
    dma(trif[:, :], trif_d, "c0", writes=["trif"])
    dma(onesf[:, :], onesf_d, "c0", writes=["onesf"])
    dma(ixibc[:, :, :], ixibc_d, "c0", writes=["ixibc"])
    dma(xibc[:, :, :], xibc_d, "c0", writes=["xibc"])
    dma(zeta[:, :], zeta_d, "c0", writes=["zeta"])
    dma(qg_col[0:64, :], qg_d.rearrange("o d -> d o"), "c0", writes=["qg"])
    dma(kg_col[0:64, :], kg_d.rearrange("o d -> d o"), "c0", writes=["kg"])
    dma(fxg_bc[:, :], fxg_d.partition_broadcast(128), "c0", writes=["fxg"])
    dma(rtg_bc[:, :], rtg_d.partition_broadcast(128), "c0", writes=["rtg"])
    dma(bfg_bc[:, :], bfg_d.partition_broadcast(128), "c0", writes=["bfg"])
    dma(c4, c_d, "c0", writes=["c4", ("xs", 0)])
    dma(wfg32[:, :, :], win_d[:, 2048:2056].rearrange("(c p) n -> p c n", p=128), "c0", writes=["wfg32"])
    c0_total = P.dma_count["dma:c0"]
    for k in ["identf", "identb", "trib", "trif", "onesf", "ixibc", "xibc", "zeta", "qg", "kg", "fxg",
              "rtg", "bfg", "c4", "wfg32"]:
        P.res[k][0] = ("dma:c0", c0_total)
    P.clock[("dma:c0", c0_total)] = {"dma:c0": c0_total}

    P.op("pool", lambda e: e.memset(nhalf[:, :], -0.5), writes=["nhalf"])
    P.op("pool", lambda e: e.memset(epsc[:, :], EPS), writes=["epsc"])
    P.op("pool", lambda e: e.memset(ones14[:, :], 1.0), writes=["ones14"])
    P.op("pool", lambda e: e.memset(Vaug[:, :, :, 64:65], 1.0), writes=["Vaug_ones"])
    for i in range(3):
        P.op("pool", lambda e, i=i: e.memset(qaug[i][:, :, 67:70], 1.0), writes=[("qaug", i)])
        P.op("pool", lambda e, i=i: e.memset(kaug[i][:, :, 64:67], 1.0), writes=[("kaug", i)])
    P.op("dve", lambda e: e.tensor_copy(out=wfg[:, :, :], in_=wfg32[:, :, :]), reads=["wfg32"], writes=["wfg"])
    P.op("pool", lambda e: e.memset(qg_col[64:70, :], 1.0), writes=["qg1"])
    P.op("pool", lambda e: e.memset(kg_col[64:70, :], 1.0), writes=["kg1"])
    P.op("dve", lambda e: e.tensor_scalar(out=qg_col[0:64, :], in0=qg_col[0:64, :], scalar1=0.125, scalar2=None,
                                          op0=ALU.mult), reads=["qg"], writes=["qg"])

    def chunk_src(k):
        if k < 8:
            col0 = [0, 512, 1024, 1536, 2056, 2568, 3080, 3592][k]
            return win_d[:, col0:col0 + 512].rearrange("(c p) n -> p c n", p=128)
        if k < 10:
            q = k - 8
            return wout_d[:, q * 512:(q + 1) * 512].rearrange("(c p) n -> p c n", p=128)
        if k < 18:
            j = k - 10
            return w1_d[:, j * 512:(j + 1) * 512].rearrange("(c p) n -> p c n", p=128)
        j = k - 18
        return w2_d[j * 512:(j + 1) * 512, :].rearrange("(c p) n -> p c n", p=128)

    CAST_ORDER = [3, 7, 0, 1, 2, 4, 5, 6] + list(range(8, NCHUNK))

    def cast_chunk(k, extra_reads=()):
        src_ap = chunk_src(k)
        dst = wbf_d[k].rearrange("p (c n) -> p c n", c=src_ap.shape[1])
        dma(dst, src_ap, f"cst{k}", reads=list(extra_reads), writes=[("wbf", k)], eng="pool")

    for k in CAST_ORDER[:5]:
        cast_chunk(k)

    c4k = [("xs", 0)]
    growk = [("xs", 1), ("xs", 2)]
    mrow = xs[0:4, 3, 0:512]
    mrowk = [("xs", 3)]
    for cc in range(8):
        P.op("pe", lambda e, cc=cc: e.transpose(out=PS[0][:, cc * 4:(cc + 1) * 4], in_=c4[:, cc * 128:(cc + 1) * 128],
                                               identity=identf[0:4, 0:4]),
             reads=["c4", "identf"] + c4k, writes=PK(0))
    P.op("act", lambda e: e.activation(out=ctmp[:, :, :], in_=PS[0][:, 0:32].rearrange("p (c b) -> p c b", b=4),
                                       func=AF.Exp, scale=-1.0), reads=PK(0), writes=["ctmp"])
    P.op("dve", lambda e: e.tensor_scalar(out=ctmp[:, :, :], in0=ctmp[:, :, :], scalar1=1.0, scalar2=None, op0=ALU.add),
         reads=["ctmp"], writes=["ctmp"])
    P.op("dve", lambda e: e.reciprocal(out=ctmp[:, :, :], in_=ctmp[:, :, :]), reads=["ctmp"], writes=["ctmp"])
    P.op("dve", lambda e: e.tensor_tensor(out=cactT[:, :, :], in0=ctmp[:, :, :],
                                          in1=PS[0][:, 0:32].rearrange("p (c b) -> p c b", b=4), op=ALU.mult),
         reads=["ctmp"] + PK(0), writes=["cactT"])

    modT_dst = {0: (sh_m, False), 1: (opm_m, True), 3: (sh_f, False), 4: (opm_f, True)}
    for kb in range(12):
        s = kb % 2
        v = kb // 2
        half = kb % 2
        stg = u_f32(16 * s, 16).rearrange("p (c n) -> p c n", c=8)
        dma(stg, wada_d[:, kb * 512:(kb + 1) * 512].rearrange("(c p) n -> p c n", p=128), f"stg{s}",
            writes=UK(16 * s, 16 * s + 16) + [("wada_blk", kb)])
        rk = UK(16 * s, 16 * s + 16)
        bd = badar[kb % 2]
        bdk = ("TB", kb % 2)
        dma(bd, bada_d[0:1, kb * 512:(kb + 1) * 512], f"bd{kb % 2}", writes=[bdk])
        bank = 3 + (kb % 2)
        for dc in range(8):
            P.op("pe", lambda e, bank=bank, dc=dc, stg=stg: e.matmul(
                PS[bank][0:4, :], lhsT=cactT[:, dc, :], rhs=stg[:, dc, :], start=(dc == 0), stop=False),
                reads=rk + ["cactT"], writes=PK(bank))
        P.op("pe", lambda e, bank=bank, bd=bd: e.matmul(
            PS[bank][0:4, :], lhsT=ones14[0:1, :], rhs=bd[0:1, :],
            start=False, stop=True), reads=[bdk, "ones14"], writes=PK(bank))
        if v in modT_dst:
            dst, plus1 = modT_dst[v]
            P.op("dve", lambda e, bank=bank: e.tensor_copy(out=mrow, in_=PS[bank][0:4, :]), reads=PK(bank), writes=mrowk)
            tb_ = 1 + (kb % 2)
            for ec in range(4):
                P.op("pe", lambda e, tb_=tb_, ec=ec: e.transpose(out=PS[tb_][:, ec * 4:(ec + 1) * 4],
                                                                 in_=mrow[:, ec * 128:(ec + 1) * 128],
                                                                 identity=identf[0:4, 0:4]),
                     reads=mrowk + ["identf"], writes=PK(tb_))
            src_v = PS[tb_][:, 0:16].rearrange("p (c b) -> p c b", b=4)
            dst_v = dst[:, half * 4:(half + 1) * 4, :]
            if plus1:
                P.op("dve", lambda e, o=dst_v, i=src_v: e.tensor_scalar(out=o, in0=i, scalar1=1.0, scalar2=None,
                                                                        op0=ALU.add),
                     reads=PK(tb_), writes=[("modT", v, half)])
            else:
                P.op("dve", lambda e, o=dst_v, i=src_v: e.tensor_copy(out=o, in_=i),
                     reads=PK(tb_), writes=[("modT", v, half)])
        else:
            gi = 0 if v == 2 else 1
            P.op("dve", lambda e, bank=bank, gi=gi, half=half: e.tensor_copy(
                out=grow[:, gi * 1024 + half * 512: gi * 1024 + (half + 1) * 512], in_=PS[bank][0:4, :]),
                reads=PK(bank), writes=growk)
    dma(gates_d, grow, "gsc", reads=growk, writes=["gates_d"])
    for k in CAST_ORDER[5:]:
        cast_chunk(k, extra_reads=[("wada_blk", 10), ("wada_blk", 11)])

    from collections import deque
    GROUP_ORDER = [3, 7, 0, 1, 2, 4, 5, 6] + list(range(8, NCHUNK))
    stream = [k for _ in range(NSEQ * NG) for k in GROUP_ORDER]
    pf = {"next": 0, "slots": {}, "cons": 0}

    def prefetch_upto(n):
        while pf["next"] < min(n, len(stream)):
            i = pf["next"]
            slot = i % 3
            dma(wbuf[slot][:, :], wbf_d[stream[i]], f"wld{slot}", reads=[("wbf", stream[i])], writes=[("wbuf", slot)])
            pf["slots"][i] = slot
            pf["next"] += 1

    def next_chunk(expect):
        i = pf["cons"]
        assert stream[i] == expect, (stream[i], expect)
        prefetch_upto(i + 2)
        pf["cons"] += 1
        return pf["slots"][i], i

    def after_chunk(i):
        prefetch_upto(i + 3)

    rr = {"proj": 0, "tp": 0, "tq": 0, "qa": 0, "ka": 0, "mi": 0, "tmp": 0, "rq": 0, "rk": 0, "qkp": 0}
    store_toks = []
    tmps = [(TA[0], ("TA", 0)), (TB[0], ("TB", 0)), (TA[1], ("TA", 1)), (TB[1], ("TB", 1))]

    def next_tmp():
        r = tmps[rr["tmp"] % 4]
        rr["tmp"] += 1
        return r

    def bc3(ap2, n):
        return ap2.unsqueeze(2).to_broadcast([128, ap2.shape[1], n])

    def bcmid(ap2, m):
        return ap2.unsqueeze(1).to_broadcast([128, m, ap2.shape[1]])

    def act_rstd(c_in, c_out, n, inv):
        P.op("act", lambda e: e.activation(out=st[:, c_out:c_out + n], in_=st[:, c_in:c_in + n], func=AF.Ln,
                                           scale=inv, bias=epsc[:, 0:1]),
             reads=[("st", c_in), "epsc"], writes=[("st", c_out)])
        P.op("act", lambda e: e.activation(out=st[:, c_out:c_out + n], in_=st[:, c_out:c_out + n], func=AF.Exp,
                                           scale=-0.5),
             reads=[("st", c_out)], writes=[("st", c_out)])

    def act_copy(out, in_, reads, writes):
        P.op("act", lambda e: e.activation(out=out, in_=in_, func=AF.Copy), reads=reads, writes=writes)

    def dve_copy(out, in_, reads, writes):
        P.op("dve", lambda e: e.tensor_copy(out=out, in_=in_), reads=reads, writes=writes)

    HB = [hTA, hTB]
    HK = ["hTA", "hTB"]

    def rms_chain(b_, src_t, src_keys, xn, xn_keys, hT, hkey, opm, shf, vs, vh, banks):
        junk = hT[:, 0:2, :].rearrange("p a n -> p (a n)")
        jk = [(hkey, 0), (hkey, 1)]

        def stage0():
            for t in range(G):
                P.op("act", lambda e, t=t: e.activation(out=junk, in_=src_t[:, t, :], func=AF.Square,
                                                        accum_out=st[:, t:t + 1]),
                     reads=src_keys[t], writes=jk + [("st", 0)])
            act_rstd(0, 8, 4, 1.0 / D)
            for t in range(G):
                P.op("act", lambda e, t=t: e.activation(out=xn[:, t, :], in_=src_t[:, t, :], func=AF.Identity,
                                                        scale=st[:, 8 + t:9 + t]),
                     reads=src_keys[t] + [("st", 8)], writes=xn_keys[t])

        def mk(c0):
            def stage():
                for c in (c0, c0 + 1):
                    bank = banks[c % len(banks)]
                    for t in range(G):
                        P.op("pe", lambda e, c=c, t=t, bank=bank: e.transpose(
                            out=PS[bank][:, t * 128:(t + 1) * 128], in_=xn[:, t, c * 128:(c + 1) * 128],
                            identity=identf[:, :]),
                            reads=xn_keys[t] + ["identf"], writes=PK(bank))
                    P.op("act", lambda e, c=c, bank=bank: e.activation(
                        out=hT[:, c, :], in_=PS[bank][:, :], func=AF.Identity,
                        scale=opm[:, c, b_:b_ + 1], bias=shf[:, c, b_:b_ + 1]),
                        reads=PK(bank) + [("modT", vs, c // 4), ("modT", vh, c // 4)], writes=[(hkey, c)])
            return stage
        return [stage0] + [mk(c0) for c0 in (0, 2, 4, 6)]

    def prefetch_B(gi_n):
        b_n, g_n = gi_n // NG, gi_n % NG
        r0 = b_n * S + g_n * 512
        dma(xnext[:, :, :], x_d[r0:r0 + 512, :].rearrange("(t p) d -> p t d", p=128), "xnl",
            writes=[k for ks in XNK for k in ks])
        return rms_chain(b_n, xnext, XNK, xnext, XNK, HB[gi_n % 2], HK[gi_n % 2], opm_m, sh_m, 1, 0, [4, 5, 6, 7])

    def proj_mm(src, t, slot, bank, key):
        for c in range(8):
            P.op("pe", lambda e, c=c: e.matmul(
                PS[bank][:, :], lhsT=src[:, c, t * 128:(t + 1) * 128],
                rhs=wbuf[slot][:, c * 512:(c + 1) * 512], start=(c == 0), stop=(c == 7)),
                reads=[(key, c), ("wbuf", slot)], writes=PK(bank))

    def next_proj_bank():
        bk = 2 + rr["proj"] % 3
        rr["proj"] += 1
        return bk

    for b in range(NSEQ):
        dma(gm_bc[:, :], gates_d[b:b + 1, 0:1024].partition_broadcast(128), "gbm", reads=["gates_d"], writes=["gm_bc"])
        dma(gf_bc[:, :], gates_d[b:b + 1, 1024:2048].partition_broadcast(128), "gbf", reads=["gates_d"], writes=["gf_bc"])
        P.op("pool", lambda e: e.memset(rs_run[:, :], 0.0), writes=["rs_run"])
        P.op("pool", lambda e: e.memset(state[:, :, :], 0.0), writes=["state"])
        P.op("pool", lambda e: e.memset(state_bf[:, :, :], 0.0), writes=["state_bf"])

        for g in range(NG):
            row0 = b * S + g * 512
            dma(xs[:, :, :], x_d[row0:row0 + 512, :].rearrange("(t p) d -> p t d", p=128), "xld",
                writes=[("xs", t) for t in range(G)])
            dma(ropeg[:, :, :, :], rope_d[:, :, g * G:(g + 1) * G, :].rearrange("r p t i -> p r t i"), "rope",
                writes=["ropeg"])

            gi = b * NG + g
            if gi == 0:
                for stg_ in prefetch_B(0):
                    stg_()
            hT_cur, hkey = HB[gi % 2], HK[gi % 2]
            hT_oth, okey = HB[(gi + 1) % 2], HK[(gi + 1) % 2]

            first = True
            for t in range(G):
                for c in range(8):
                    P.op("pe", lambda e, c=c, t=t, hT_cur=hT_cur, first=first: e.matmul(
                        PS[7][:, t * 8:(t + 1) * 8], lhsT=hT_cur[:, c, t * 128:(t + 1) * 128],
                        rhs=wfg[:, c, :], start=first, stop=(c == 7), skip_group_check=True),
                        reads=[(hkey, c), "wfg"], writes=PK(7))
                    first = False
            P.op("dve", lambda e: e.tensor_tensor(out=fz[:, 0, :].rearrange("p (t h) -> p t h", h=8),
                                                  in0=PS[7][:, 0:32].rearrange("p (t h) -> p t h", h=8),
                                                  in1=bcmid(bfg_bc[:, :], G), op=ALU.add),
                 reads=PK(7) + ["bfg"], writes=[("fz", 0)])
            P.op("act", lambda e: e.activation(out=fz[:, 1, :], in_=fz[:, 0, :], func=AF.Exp, scale=-1.0),
                 reads=[("fz", 0)], writes=[("fz", 1)])
            P.op("act", lambda e: e.activation(out=fz[:, 2, :], in_=fz[:, 1, :], func=AF.Ln, bias=1.0),
                 reads=[("fz", 1)], writes=[("fz", 2)])
            lall = fz[:, 2, :].rearrange("p (t h) -> p t h", h=8)
            P.op("dve", lambda e: e.tensor_copy(out=pre[:, 0, :], in_=rs_run[:, :]), reads=["rs_run"], writes=["pre"])
            for t in range(1, G):
                P.op("dve", lambda e, t=t: e.tensor_tensor(out=pre[:, t, :], in0=pre[:, t - 1, :], in1=lall[:, t - 1, :],
                                                           op=ALU.add), reads=["pre", ("fz", 2)], writes=["pre"])
            P.op("dve", lambda e: e.tensor_tensor(out=rs_run[:, :], in0=pre[:, G - 1, :], in1=lall[:, G - 1, :], op=ALU.add),
                 reads=["pre", ("fz", 2)], writes=["rs_run"])

            def cum_finish():
                P.op("pe", lambda e: e.matmul(PS[7][:, 32:64], lhsT=trif[:, :], rhs=fz[:, 2, :], start=True, stop=False),
                     reads=[("fz", 2), "trif"], writes=PK(7))
                P.op("pe", lambda e: e.matmul(PS[7][:, 32:64], lhsT=onesf[:, :], rhs=pre[:, :, :].rearrange("p t h -> p (t h)"),
                                              start=False, stop=True), reads=["pre", "onesf"], writes=PK(7))
                ncum = PS[7][:, 32:64].rearrange("p (t h) -> p t h", h=8)
                ck = [("cumsp", t) for t in range(G)]
                cr0 = cr[:, 0, :].rearrange("p (t h) -> p t h", h=8)
                cr1 = cr[:, 1, :].rearrange("p (t h) -> p t h", h=8)
                P.op("dve", lambda e: e.tensor_copy(out=cumsp[:, :, :, 0], in_=ncum), reads=PK(7), writes=ck)
                P.op("dve", lambda e: e.tensor_tensor(out=cr0, in0=ncum, in1=cumsp[:, :, :, 0], op=ALU.subtract),
                     reads=PK(7) + ck, writes=[("cr", 0)])
                P.op("dve", lambda e: e.tensor_copy(out=cumsp[:, :, :, 1], in_=cr0), reads=[("cr", 0)], writes=ck)
                P.op("dve", lambda e: e.tensor_tensor(out=cr1, in0=cr0, in1=cumsp[:, :, :, 1], op=ALU.subtract),
                     reads=[("cr", 0)] + ck, writes=[("cr", 1)])
                P.op("dve", lambda e: e.tensor_copy(out=cumsp[:, :, :, 2], in_=cr1), reads=[("cr", 1)], writes=ck)

            pipe = []

            def pipe_tick():
                keep = []
                for it in list(pipe):
                    it[0] += 1
                    a = it[0]
                    if a - 1 < len(it[1]) and it[1][a - 1] is not None:
                        it[1][a - 1]()
                    if a < len(it[1]):
                        keep.append(it)
                pipe[:] = keep

            def pipe_push(stages):
                pipe_tick()
                pipe.append([0, stages])

            def push_deferred(fn):
                pipe_push([None, fn])

            def flush_deferred():
                while pipe:
                    pipe_tick()

            slot, ci = next_chunk(3)
            for t in range(G):
                bank = next_proj_bank()
                proj_mm(hT_cur, t, slot, bank, hkey)
                tmp, tkey = next_tmp()
                P.op("act", lambda e, tmp=tmp, bank=bank: e.activation(out=tmp[:, :], in_=PS[bank][:, :], func=AF.Sigmoid),
                     reads=PK(bank), writes=[tkey])
                P.op("pool", lambda e, t=t, tmp=tmp: e.tensor_tensor(out=gates_f[:, t, :], in0=tmp[:, :], in1=fxg_bc[:, :],
                                                                     op=ALU.mult),
                     reads=[tkey, "fxg"], writes=[("gates_f", t)])
            after_chunk(ci)
            slot, ci = next_chunk(7)
            for t in range(G):
                bank = next_proj_bank()
                proj_mm(hT_cur, t, slot, bank, hkey)
                tmp, tkey = next_tmp()
                tmp2, tkey2 = next_tmp()
                P.op("act", lambda e, tmp=tmp, bank=bank: e.activation(out=tmp[:, :], in_=PS[bank][:, :], func=AF.Sigmoid),
                     reads=PK(bank), writes=[tkey])
                P.op("dve", lambda e, tmp=tmp, tmp2=tmp2, bank=bank: e.tensor_tensor(out=tmp2[:, :], in0=PS[bank][:, :],
                                                                                    in1=tmp[:, :], op=ALU.mult),
                     reads=[tkey] + PK(bank), writes=[tkey2])
                P.op("pool", lambda e, t=t, tmp2=tmp2: e.tensor_tensor(out=gates_r[:, t, :], in0=tmp2[:, :], in1=rtg_bc[:, :],
                                                                       op=ALU.mult),
                     reads=[tkey2, "rtg"], writes=[("gates_r", t)])
            after_chunk(ci)

            cum_finish()

            def qk_step(t, slot, is_q):
                bank = next_proj_bank()
                proj_mm(hT_cur, t, slot, bank, hkey)
                if is_q:
                    par = rr["qa"] % 3
                    rr["qa"] += 1
                    aug, akey, gcol, gkeys = qaug[par], ("qaug", par), qg_col, ["qg", "qg1"]
                else:
                    par = rr["ka"] % 3
                    rr["ka"] += 1
                    aug, akey, gcol, gkeys = kaug[par], ("kaug", par), kg_col, ["kg", "kg1"]
                tmp, tkey = next_tmp()
                par2 = rr["qkp"] % 2
                rr["qkp"] += 1
                cs, cr_ = 64 + 16 * par2, 72 + 16 * par2
                P.op("act", lambda e: e.activation(out=tmp[:, :], in_=PS[bank][:, :], func=AF.Square),
                     reads=PK(bank), writes=[tkey])
                P.op("dve", lambda e: e.tensor_reduce(out=st[:, cs:cs + 8], in_=tmp[:, :].rearrange("p (h i) -> p h i", i=64),
                                                      axis=AX.X, op=ALU.add), reads=[tkey], writes=[("st", cs)])

                def stage_b():
                    act_rstd(cs, cr_, 8, 1.0 / 64)
                    P.op("dve", lambda e: e.tensor_tensor(out=aug[:, :, 0:64],
                                                          in0=PS[bank][:, :].rearrange("p (h i) -> p h i", i=64),
                                                          in1=bc3(st[:, cr_:cr_ + 8], 64), op=ALU.mult),
                         reads=PK(bank) + [("st", cr_)], writes=[akey])
                    if is_q:
                        P.op("pool", lambda e: e.tensor_scalar(out=aug[:, :, 64:67], in0=cumsp[:, t, :, :], scalar1=-1.0,
                                                               scalar2=None, op0=ALU.mult),
                             reads=[("cumsp", t)], writes=[akey])
                    else:
                        P.op("pool", lambda e: e.tensor_copy(out=aug[:, :, 67:70], in_=cumsp[:, t, :, :]),
                             reads=[("cumsp", t)], writes=[akey])

                def deferred():
                    bq = 5 + rr["tq"] % 2
                    rr["tq"] += 1
                    for h in range(8):
                        P.op("pe", lambda e, h=h: e.transpose(out=Pb(bq)[0:70, h * 128:(h + 1) * 128], in_=aug[:, h, :],
                                                              identity=identb[:, :]),
                             reads=[akey, "identb"], writes=PK(bq))
                    srcv = Pb(bq)[0:70, :].rearrange("p (h n) -> p h n", n=128)
                    if is_q:
                        dst, wk = QT_v[:, :, t * 128:(t + 1) * 128], UK(0, 8)
                    else:
                        blk_i = g * G + t
                        dst, wk = KT[:, :, blk_i * 128:(blk_i + 1) * 128], [("KT", blk_i)]
                    P.op("dve", lambda e: e.tensor_scalar(out=dst, in0=srcv, scalar1=gcol[:, 0:1], scalar2=None,
                                                          op0=ALU.mult),
                         reads=PK(bq) + gkeys, writes=wk)
                pipe_push([stage_b, deferred])

            for is_q, ck_ in ((True, 0), (False, 1)):
                slot, ci = next_chunk(ck_)
                for t in range(G):
                    qk_step(t, slot, is_q)
                after_chunk(ci)

            slot, ci = next_chunk(2)
            for t in range(G):
                bank = next_proj_bank()
                proj_mm(hT_cur, t, slot, bank, hkey)
                blk_i = g * G + t
                act_copy(Vaug[:, blk_i, :, 0:64], PS[bank][:, :].rearrange("p (h i) -> p h i", i=64), PK(bank),
                         [("Vaug", blk_i)])
                pipe_push([])
            after_chunk(ci)
            flush_deferred()

            def side_bank():
                bk = (5, 7)[rr["proj"] % 2]
                rr["proj"] += 1
                return bk

            def rope_step(t, slot, is_q):
                bank = side_bank()
                proj_mm(hT_cur, t, slot, bank, hkey)
                r0 = 0 if is_q else 3
                cosv, sinv, nsinv = ropeg[:, r0, t, :], ropeg[:, r0 + 1, t, :], ropeg[:, r0 + 2, t, :]
                ta, tak = next_tmp()
                tb, tbk = next_tmp()
                pv = PS[bank][:, :].rearrange("p (h w i) -> p h w i", h=4, w=2)
                ta4 = ta[:, :].rearrange("p (h w i) -> p h w i", h=4, w=2)
                tb4 = tb[:, :].rearrange("p (h w i) -> p h w i", h=4, w=2)
                cos4 = cosv.unsqueeze(1).unsqueeze(1).to_broadcast([128, 4, 2, 64])
                P.op("dve", lambda e: e.tensor_tensor(out=ta4, in0=pv, in1=cos4, op=ALU.mult),
                     reads=PK(bank) + ["ropeg"], writes=[tak])
                P.op("dve", lambda e: e.tensor_tensor(out=tb4[:, :, 0, :], in0=pv[:, :, 1, :], in1=bcmid(nsinv, 4),
                                                      op=ALU.mult), reads=PK(bank) + ["ropeg"], writes=[tbk])
                P.op("dve", lambda e: e.tensor_tensor(out=tb4[:, :, 1, :], in0=pv[:, :, 0, :], in1=bcmid(sinv, 4),
                                                      op=ALU.mult), reads=PK(bank) + ["ropeg"], writes=[tbk])
                if is_q:
                    i = rr["rq"] % 3
                    rr["rq"] += 1
                    rt, rtk = rqt_v[i], ("U", rqt_g[i])
                else:
                    i = rr["rk"] % 3
                    rr["rk"] += 1
                    rt, rtk = rkt_v[i], ("U", rkt_g[i])
                P.op("pool", lambda e: e.tensor_tensor(out=rt, in0=ta[:, :], in1=tb[:, :], op=ALU.add),
                     reads=[tak, tbk], writes=[rtk])
                if not is_q:
                    P.op("pool", lambda e: e.tensor_tensor(out=Kz_v[:, t, :].rearrange("p (h i) -> p h i", i=128),
                                                           in0=rt.rearrange("p (h i) -> p h i", i=128),
                                                           in1=bc3(zeta[:, :], 128), op=ALU.mult),
                         reads=[rtk, "zeta"], writes=[("U", 20 + t)])

                def deferred():
                    bq = 6
                    for h in range(4):
                        P.op("pe", lambda e, h=h: e.transpose(out=Pb(bq)[:, h * 128:(h + 1) * 128],
                                                              in_=rt[:, h * 128:(h + 1) * 128], identity=identb[:, :]),
                             reads=[rtk, "identb"], writes=PK(bq))
                    srcv = Pb(bq)[:, 0:512].rearrange("p (h n) -> p h n", n=128)
                    tc_ = slice(t * 128, (t + 1) * 128)
                    if is_q:
                        P.op("dve", lambda e: e.tensor_tensor(out=QTr_v[:, :, tc_], in0=srcv, in1=xibc[:, :, :], op=ALU.mult),
                             reads=PK(bq) + ["xibc"], writes=UK(8, 12))
                    else:
                        P.op("dve", lambda e: e.tensor_tensor(out=KTr_v[:, :, tc_], in0=srcv, in1=ixibc[:, :, :], op=ALU.mult),
                             reads=PK(bq) + ["ixibc"], writes=UK(16, 20))
                return deferred

            def rv_step(t, slot):
                bank = side_bank()
                proj_mm(hT_cur, t, slot, bank, hkey)
                dve_copy(Vr_v[:, t, :], PS[bank][:, :], PK(bank), [("U", 24 + t)])
                return lambda: None

            chunk_state = {}

            def side_steps():
                units = []
                for kind, ck_ in (("rq", 4), ("rk", 5), ("rv", 6)):
                    for t in range(G):
                        units.append((kind, ck_, t))
                return units

            units = side_steps()

            def run_unit(u):
                kind, ck_, t = u
                if t == 0:
                    chunk_state["cur"] = next_chunk(ck_)
                slot, ci = chunk_state["cur"]
                if kind == "rq":
                    push_deferred(rope_step(t, slot, True))
                elif kind == "rk":
                    push_deferred(rope_step(t, slot, False))
                else:
                    push_deferred(rv_step(t, slot))
                if t == G - 1:
                    after_chunk(ci)

            nkb = 4 * g + 4
            tasks = [(h, kb) for h in range(8) for kb in range(nkb)]
            mixed_all = [("mixed", t) for t in range(G)]

            def fox_qk(i):
                h, kb = tasks[i]
                jlo = max(0, kb - 4 * g)
                n = (4 - jlo) * 128
                bank = i % 3
                pt = PT[i % 3]
                P.op("pe", lambda e: e.matmul(PS[bank][:, 0:n], lhsT=KT[:, h, kb * 128:(kb + 1) * 128],
                                              rhs=QT_v[:, h, jlo * 128:512], start=True, stop=True),
                     reads=[("KT", kb), ("U", h)], writes=PK(bank))
                P.op("act", lambda e: e.activation(out=pt[:, 0:n], in_=PS[bank][:, 0:n], func=AF.Exp),
                     reads=PK(bank), writes=[("PT", i % 3)])
                if kb >= 4 * g:
                    P.op("dve", lambda e: e.tensor_tensor(out=pt[:, 0:128], in0=pt[:, 0:128], in1=trib[:, :], op=ALU.mult),
                         reads=[("PT", i % 3), "trib"], writes=[("PT", i % 3)])

            def fox_pv(i):
                h, kb = tasks[i]
                jlo = max(0, kb - 4 * g)
                ob = 3 + (h % 2)
                pt = PT[i % 3]
                for j in range(jlo, 4):
                    P.op("pe", lambda e, j=j, last=(kb == 4 * g + j): e.matmul(PS[ob][:, j * 65:(j + 1) * 65],
                                                       lhsT=pt[:, (j - jlo) * 128:(j - jlo + 1) * 128],
                                                       rhs=Vaug[:, kb, h, :], start=(kb == 0 and j == 0),
                                                       stop=last, skip_group_check=True),
                         reads=[("PT", i % 3), ("Vaug", kb), "Vaug_ones"], writes=PK(ob))
                if kb == nkb - 1:
                    fox_epilogue(h, ob, i + 2)

            def at(idx, fn):
                sched.setdefault(idx, []).append(fn)

            def fox_epilogue(h, ob, i_now):
                O = PS[ob][:, 0:260].rearrange("p (j e) -> p j e", e=65)
                p_ = h % 2
                oz, ozk = (TE[0], ("TE", 0)) if p_ == 0 else (TE[3], ("TE", 3))
                c_ss, c_rs = 96 + 16 * p_, 104 + 16 * p_
                P.op("dve", lambda e: e.reciprocal(out=st[:, 40:44], in_=O[:, :, 64]), reads=PK(ob), writes=[("st", 40)])
                P.op("dve", lambda e: e.tensor_tensor(out=oz[:, :, :], in0=O[:, :, 0:64], in1=bc3(st[:, 40:44], 64),
                                                      op=ALU.mult), reads=PK(ob) + [("st", 40)], writes=[ozk])
                P.op("pool", lambda e: e.tensor_tensor(out=TE[1][:, :, :], in0=oz[:, :, :], in1=oz[:, :, :], op=ALU.mult),
                     reads=[ozk], writes=[("TE", 1)])
                P.op("dve", lambda e: e.tensor_reduce(out=st[:, c_ss:c_ss + 4], in_=TE[1][:, :, :], axis=AX.X, op=ALU.add),
                     reads=[("TE", 1)], writes=[("st", c_ss)])

                def e1():
                    act_rstd(c_ss, c_rs, 4, 1.0 / 64)

                def e2():
                    P.op("dve", lambda e: e.tensor_tensor(out=TE[2][:, :, :], in0=oz[:, :, :], in1=bc3(st[:, c_rs:c_rs + 4], 64),
                                                          op=ALU.mult), reads=[ozk, ("st", c_rs)], writes=[("TE", 2)])
                    P.op("pool", lambda e: e.tensor_tensor(out=mixed[:, :, h * 64:(h + 1) * 64], in0=TE[2][:, :, :],
                                                           in1=gates_f[:, :, h * 64:(h + 1) * 64], op=ALU.mult),
                         reads=[("TE", 2)] + [("gates_f", t) for t in range(G)], writes=mixed_all)
                at(i_now + 2, e1)
                at(i_now + 3, e2)

            def ret_stage1(j):
                jc = slice(j * 128, (j + 1) * 128)
                for h in range(4):
                    P.op("pe", lambda e, h=h: e.matmul(PS[5][:, h * 128:(h + 1) * 128], lhsT=KTr_v[:, h, jc],
                                                       rhs=QTr_v[:, h, jc], start=(h == 0), stop=True,
                                                       skip_group_check=True),
                         reads=UK(16, 20) + UK(8, 12), writes=PK(5))
                for h in range(4):
                    hc = slice(h * 128, (h + 1) * 128)
                    P.op("pe", lambda e, hc=hc, h=h: e.matmul(PS[7][:, hc], lhsT=Kz_v[:, j, hc], rhs=Vr_v[:, j, hc],
                                                              start=(h == 0), stop=True, skip_group_check=True),
                         reads=[("U", 20 + j), ("U", 24 + j)], writes=PK(7))
                sj = j % 2
                P.op("dve", lambda e: e.tensor_tensor(out=STr[sj][:, :, :],
                                                      in0=PS[5][:, :].rearrange("p (h n) -> p h n", n=128),
                                                      in1=bcmid(trib[:, :], 4), op=ALU.mult),
                     reads=PK(5) + ["trib"], writes=[("STr", sj)])

            def ret_stage2(j):
                jc = slice(j * 128, (j + 1) * 128)
                sj = j % 2
                for h in range(4):
                    hc = slice(h * 128, (h + 1) * 128)
                    P.op("pe", lambda e, hc=hc, h=h: e.matmul(PS[6][:, hc], lhsT=STr[sj][:, h, :], rhs=Vr_v[:, j, hc],
                                                              start=(h == 0), stop=False, skip_group_check=True),
                         reads=[("STr", sj), ("U", 24 + j)], writes=PK(6))
                    P.op("pe", lambda e, hc=hc, h=h: e.matmul(PS[6][:, hc], lhsT=QTr_v[:, h, jc], rhs=state_bf[:, h, :],
                                                              start=False, stop=True, skip_group_check=True),
                         reads=UK(8, 12) + ["state_bf"], writes=PK(6))
                for h in range(4):
                    hc = slice(h * 128, (h + 1) * 128)
                    P.op("dve", lambda e, hc=hc, h=h: e.scalar_tensor_tensor(
                        out=state[:, h, :], in0=state[:, h, :], scalar=g_chunk[h], in1=PS[7][:, hc],
                        op0=ALU.mult, op1=ALU.add), reads=["state"] + PK(7), writes=["state"])
                P.op("pool", lambda e: e.tensor_copy(out=state_bf[:, :, :], in_=state[:, :, :]),
                     reads=["state"], writes=["state_bf"])
                pj = j % 2
                c_ss, c_rs = 16 + 8 * pj, 20 + 8 * pj
                dve_copy(TR[:, :], PS[6][:, :], PK(6), ["TR"])
                tmp, tkey = next_tmp()
                P.op("pool", lambda e: e.tensor_tensor(out=tmp[:, :], in0=TR[:, :], in1=TR[:, :], op=ALU.mult),
                     reads=["TR"], writes=[tkey])
                P.op("dve", lambda e: e.tensor_reduce(out=st[:, c_ss:c_ss + 4], in_=tmp[:, :].rearrange("p (h i) -> p h i", i=128),
                                                      axis=AX.X, op=ALU.add), reads=[tkey], writes=[("st", c_ss)])

                def r2c():
                    act_rstd(c_ss, c_rs, 4, 1.0 / 128)
                    tmp2, tkey2 = next_tmp()
                    P.op("dve", lambda e: e.tensor_tensor(out=tmp2[:, :].rearrange("p (h i) -> p h i", i=128),
                                                          in0=TR[:, :].rearrange("p (h i) -> p h i", i=128),
                                                          in1=bc3(st[:, c_rs:c_rs + 4], 128), op=ALU.mult),
                         reads=["TR", ("st", c_rs)], writes=[tkey2])
                    P.op("pool", lambda e: e.tensor_tensor(out=mixed[:, j, 512:1024], in0=tmp2[:, :], in1=gates_r[:, j, :],
                                                           op=ALU.mult),
                         reads=[tkey2, ("gates_r", j)], writes=[("mixed", j)])
                at(cur_i[0] + 2, r2c)

            ntask = len(tasks)
            sched = {}
            nun = len(units)
            span = max(nun, int(ntask * 0.55))
            for ui, u in enumerate(units):
                sched.setdefault(min(ntask - 1, ui * span // nun), []).append(lambda u=u: run_unit(u))
            sched.setdefault(min(ntask - 1, span), []).append(flush_deferred)
            rem0 = min(ntask - 1, span + 1)
            for j in range(4):
                p1 = rem0 + (2 * j) * (ntask - rem0) // 8
                p2 = rem0 + (2 * j + 1) * (ntask - rem0) // 8
                sched.setdefault(min(ntask - 1, p1), []).append(lambda j=j: ret_stage1(j))
                sched.setdefault(min(ntask - 1, p2), []).append(lambda j=j: ret_stage2(j))
            cur_i = [0]
            for i in range(ntask + 2):
                cur_i[0] = i
                for f in sched.pop(i, []):
                    f()
                if i < ntask:
                    fox_qk(i)
                if i >= 2:
                    fox_pv(i - 2)
            while sched:
                k_ = min(sched)
                cur_i[0] = k_
                for f in sched.pop(k_):
                    f()

            for c in range(8):
                bank = rr["tp"] % 2
                rr["tp"] += 1
                for t in range(G):
                    P.op("pe", lambda e, c=c, t=t, bank=bank: e.transpose(
                        out=Pb(bank)[:, t * 128:(t + 1) * 128], in_=mixed[:, t, c * 128:(c + 1) * 128],
                        identity=identb[:, :]), reads=[("mixed", t), "identb"], writes=PK(bank))
                if c % 2 == 0:
                    act_copy(hT_oth[:, c, :], Pb(bank)[:, 0:512], PK(bank), [(okey, c)])
                else:
                    dve_copy(hT_oth[:, c, :], Pb(bank)[:, 0:512], PK(bank), [(okey, c)])

            for q in range(2):
                slot, ci = next_chunk(8 + q)
                for t in range(G):
                    bank = next_proj_bank()
                    proj_mm(hT_oth, t, slot, bank, okey)
                    tmp, tkey = next_tmp()
                    qc = slice(q * 512, (q + 1) * 512)
                    P.op("dve", lambda e, tmp=tmp, bank=bank, qc=qc: e.tensor_tensor(out=tmp[:, :], in0=PS[bank][:, :],
                                                                                    in1=gm_bc[:, qc], op=ALU.mult),
                         reads=PK(bank) + ["gm_bc"], writes=[tkey])
                    P.op("pool", lambda e, tmp=tmp, t=t, qc=qc: e.tensor_tensor(out=xs[:, t, qc], in0=xs[:, t, qc],
                                                                               in1=tmp[:, :], op=ALU.add),
                         reads=[tkey, ("xs", t)], writes=[("xs", t)])
                after_chunk(ci)

            xn2 = u_f32(16, 16).rearrange("p (t d) -> p t d", t=G)
            for stg_ in rms_chain(b, xs, [[("xs", t)] for t in range(G)], xn2, [UK(16 + 4 * t, 20 + 4 * t) for t in range(G)],
                                  hT_cur, hkey, opm_f, sh_f, 4, 3, [0, 1]):
                stg_()

            pstages = prefetch_B(gi + 1) if gi + 1 < NSEQ * NG else []
            for j in range(8):
                slot, ci = next_chunk(10 + j)
                if pstages and 1 <= j <= 5:
                    pstages[j - 1]()
                for fc in range(4):
                    bank = rr["mi"] % 4
                    rr["mi"] += 1
                    tmp, tkey = next_tmp()
                    for c in range(8):
                        P.op("pe", lambda e, c=c, fc=fc, bank=bank, slot=slot, hT_cur=hT_cur: e.matmul(
                            PS[bank][:, :], lhsT=wbuf[slot][:, c * 512 + fc * 128: c * 512 + (fc + 1) * 128],
                            rhs=hT_cur[:, c, :], start=(c == 0), stop=(c == 7)),
                            reads=[(hkey, c), ("wbuf", slot)], writes=PK(bank))
                    P.op("act", lambda e, tmp=tmp, bank=bank: e.activation(out=tmp[:, :], in_=PS[bank][:, :], func=AF.Relu),
                         reads=PK(bank), writes=[tkey])
                    uc = u_chunk(4 * j + fc)
                    P.op("pool", lambda e, tmp=tmp, uc=uc: e.tensor_tensor(out=uc, in0=tmp[:, :], in1=tmp[:, :], op=ALU.mult),
                         reads=[tkey], writes=[("U", 4 * j + fc)])
                after_chunk(ci)

            for j in range(8):
                slot, ci = next_chunk(18 + j)
                for t in range(G):
                    for hf in range(2):
                        bank = t * 2 + hf
                        for fc in range(4):
                            uc = u_chunk(4 * j + fc)
                            P.op("pe", lambda e, uc=uc, t=t, hf=hf, fc=fc, bank=bank, slot=slot, j=j: e.matmul(
                                PS[bank][:, :], lhsT=uc[:, t * 128:(t + 1) * 128],
                                rhs=wbuf[slot][:, fc * 1024 + hf * 512: fc * 1024 + (hf + 1) * 512],
                                start=(j == 0 and fc == 0), stop=(j == 7 and fc == 3)),
                                reads=[("U", 4 * j + fc), ("wbuf", slot)], writes=PK(bank))
                after_chunk(ci)
            done_half = {}
            for (t, hf) in ((3, 1), (1, 0), (1, 1), (2, 0), (0, 0), (0, 1), (2, 1), (3, 0)):
                if True:
                    bank = t * 2 + hf
                    tmp, tkey = next_tmp()
                    qc = slice(hf * 512, (hf + 1) * 512)
                    P.op("dve", lambda e, tmp=tmp, bank=bank, qc=qc: e.tensor_tensor(out=tmp[:, :], in0=PS[bank][:, :],
                                                                                    in1=gf_bc[:, qc], op=ALU.mult),
                         reads=PK(bank) + ["gf_bc"], writes=[tkey])
                    P.op("pool", lambda e, tmp=tmp, t=t, qc=qc: e.tensor_tensor(out=xs[:, t, qc], in0=xs[:, t, qc],
                                                                               in1=tmp[:, :], op=ALU.add),
                         reads=[tkey, ("xs", t)], writes=[("xs", t)])
                done_half[t] = done_half.get(t, 0) + 1
                if done_half[t] == 2:
                    tok = dma(y_d[row0 + t * 128: row0 + (t + 1) * 128, :], xs[:, t, :], "yst", reads=[("xs", t)],
                              writes=[("y", row0 + t * 128)])
                    store_toks.append(tok)

    P.wait_all("sp", [max(store_toks, key=lambda tk: tk[1])])
    return nc, P, es, consts


_BUILT = None


def _get_built():
    global _BUILT
    if _BUILT is None:
        nc, P, es, consts = build_program()
        P.emit(nc, es)
        es.close()
        _BUILT = (nc, consts)
    return _BUILT


def kernel(x, c, w_ada, b_ada, w_in, b_forget, q_norm_gain, k_norm_gain, fox_out_gain, ret_out_gain,
           w_out, w_mlp_in, w_mlp_out):
    nc, consts = _get_built()
    f = np.float32
    x = np.asarray(x, f)
    c = np.asarray(c, f)
    shared = {
        "w_ada": np.ascontiguousarray(np.asarray(w_ada, f)[0]),
        "b_ada": np.ascontiguousarray(np.asarray(b_ada, f)[0].reshape(1, -1)),
        "w_in": np.ascontiguousarray(np.asarray(w_in, f)[0]),
        "b_forget": np.ascontiguousarray(np.asarray(b_forget, f)[0].reshape(1, 8)),
        "q_gain": np.ascontiguousarray(np.asarray(q_norm_gain, f)[0].reshape(1, 64)),
        "k_gain": np.ascontiguousarray(np.asarray(k_norm_gain, f)[0].reshape(1, 64)),
        "fox_gain": np.ascontiguousarray(np.asarray(fox_out_gain, f)[0].reshape(1, 512)),
        "ret_gain": np.ascontiguousarray(np.asarray(ret_out_gain, f)[0].reshape(1, 512)),
        "w_out": np.ascontiguousarray(np.asarray(w_out, f)[0]),
        "w1": np.ascontiguousarray(np.asarray(w_mlp_in, f)[0]),
        "w2": np.ascontiguousarray(np.asarray(w_mlp_out, f)[0]),
    }
    for k in ("identf", "identb", "trib", "trif", "onesf", "ixi_bc", "xi_bc", "zeta_t", "rope"):
        shared[k] = consts[k]
    in_maps = []
    for i in range(NCORES):
        m = dict(shared)
        m["x"] = np.ascontiguousarray(x[i * NSEQ:(i + 1) * NSEQ].reshape(NSEQ * S, D))
        m["c"] = np.ascontiguousarray(c[i * NSEQ:(i + 1) * NSEQ])
        in_maps.append(m)
    res = run_bass_kernel_spmd(nc, in_maps, core_ids=list(range(NCORES)))
    out = np.concatenate([np.asarray(r["y"], f).reshape(NSEQ, S, D) for r in res.results], axis=0)
    return out
```

```python
import numpy as np
import ml_dtypes
from contextlib import ExitStack
import concourse.bass as bass
import concourse.mybir as mybir
from concourse.bass_utils import run_bass_kernel_spmd

F32 = mybir.dt.float32
BF16 = mybir.dt.bfloat16
AF = mybir.ActivationFunctionType
ALU = mybir.AluOpType
AX = mybir.AxisListType

NCORES = 8
D = 1024
S = 2048
NSEQ = 4
NT = 16
G = 4
NG = NT // G
DFF = 4096
EPS = 1e-6
IN_COLS = 4104
NCHUNK = 26

ENGS = ("pe", "act", "dve", "pool", "sp")
import os
STRICT = bool(int(os.environ.get("KSTRICT", "0")))


class _Op:
    __slots__ = ("fn", "waits", "sig", "dma", "tok")


class Prog:
    def __init__(self):
        self.ops = {e: [] for e in ENGS}
        self.res = {}
        self.known = {e: {} for e in ENGS}
        self.clock = {}
        self.dma_count = {}
        self.needed = set()

    def _deps(self, eng, reads, writes):
        deps = set()
        for k in reads:
            r = self.res.get(k)
            if r is not None and r[0] is not None:
                deps.add(r[0])
        for k in writes:
            r = self.res.get(k)
            if r is not None:
                if r[0] is not None and (STRICT or r[0][0] != eng):
                    deps.add(r[0])
                for src, v in r[1].items():
                    if STRICT or src != eng:
                        deps.add((src, v))
        return deps

    def _commit(self, tok, reads, writes):
        for k in reads:
            r = self.res.get(k)
            if r is None:
                r = [None, {}]
                self.res[k] = r
            if r[1].get(tok[0], 0) < tok[1]:
                r[1][tok[0]] = tok[1]
        for k in writes:
            self.res[k] = [tok, {}]

    def op(self, eng, fn, reads=(), writes=(), dma=None):
        deps = self._deps(eng if dma is None else "dma:" + dma, reads, writes)
        kn = self.known[eng]
        waits = []
        best = {}
        for (src, v) in deps:
            if best.get(src, 0) < v:
                best[src] = v
        deps = set(best.items())
        for (src, v) in sorted(deps, key=lambda t: (str(t[0]), t[1])):
            if kn.get(src, 0) >= v:
                continue
            waits.append((src, v))
            self.needed.add((src, v))
            for s2, v2 in self.clock[(src, v)].items():
                if kn.get(s2, 0) < v2:
                    kn[s2] = v2
        o = _Op()
        o.fn = fn
        o.waits = waits
        o.dma = dma
        self.ops[eng].append(o)
        if dma is None:
            tok = (eng, len(self.ops[eng]))
        else:
            src = "dma:" + dma
            self.dma_count[src] = self.dma_count.get(src, 0) + 16
            tok = (src, self.dma_count[src])
        o.tok = tok
        ck = dict(kn)
        ck[tok[0]] = tok[1]
        self.clock[tok] = ck
        self._commit(tok, reads, writes)
        return tok

    def wait_all(self, eng, toks):
        kn = self.known[eng]
        waits = []
        for (src, v) in toks:
            if kn.get(src, 0) >= v:
                continue
            waits.append((src, v))
            self.needed.add((src, v))
            kn[src] = v
        o = _Op()
        o.fn = None
        o.waits = waits
        o.dma = None
        o.tok = None
        self.ops[eng].append(o)

    def emit(self, nc, es):
        sems = {}
        for e in ("pe", "act", "dve", "pool"):
            sems[e] = es.enter_context(nc.semaphore("sem_" + e))
        for src in self.dma_count:
            sems[src] = es.enter_context(nc.semaphore("sem_" + src.replace(":", "_")))
        sigval = {}
        for e in ("pe", "act", "dve", "pool"):
            cnt = 0
            for i, o in enumerate(self.ops[e]):
                o.sig = False
                if o.fn is not None and o.dma is None and (e, i + 1) in self.needed:
                    cnt += 1
                    o.sig = True
                    sigval[(e, i + 1)] = cnt
        blk = es.enter_context(nc.Block())

        def run(e, name):
            for o in self.ops[name]:
                for (src, v) in o.waits:
                    val = v if src.startswith("dma:") else sigval[(src, v)]
                    e.wait_ge(sems[src], val)
                if o.fn is None:
                    continue
                ins = o.fn(e)
                if o.dma is not None:
                    ins.then_inc(sems["dma:" + o.dma], 16)
                elif o.sig:
                    ins.then_inc(sems[name], 1)

        @blk.tensor
        def _(e):
            run(e, "pe")

        @blk.scalar
        def _(e):
            run(e, "act")

        @blk.vector
        def _(e):
            run(e, "dve")

        @blk.gpsimd
        def _(e):
            run(e, "pool")

        @blk.sync
        def _(e):
            run(e, "sp")


def _constants():
    f = np.float32
    n = np.arange(128, dtype=f)
    ident = np.eye(128, dtype=f)
    tri = (n[:, None] <= n[None, :]).astype(f)
    ones = np.ones((128, 128), f)
    h = np.arange(4, dtype=f)
    log_g = np.log(f(1.0) - f(2.0) ** (f(-5.0) - h)).astype(f)
    diff = n[None, :] - n[:, None]
    maskT = np.where(diff[None] >= 0, np.exp(np.maximum(diff, 0.0)[None] * log_g[:, None, None]), 0.0).astype(f)
    xi = np.exp((n[None, :] + 1.0) * log_g[:, None]).astype(f)
    xi_bc = np.ascontiguousarray(np.broadcast_to(xi[None], (128, 4, 128))).astype(f)
    ixi = np.exp(-(n[None, :] + 1.0) * log_g[:, None]).astype(f)
    ixi_bc = np.ascontiguousarray(np.broadcast_to(ixi[None], (128, 4, 128))).astype(f)
    zeta = np.exp((128 - 1.0 - n[None, :]) * log_g[:, None]).astype(f)
    zeta_t = np.ascontiguousarray(zeta.T)
    g_chunk = np.exp(f(128.0) * log_g).astype(f)
    pos = np.arange(S, dtype=f)
    inv_freq = (f(10000.0) ** (-np.arange(0, 128, 2, dtype=f) / f(128))).astype(f)
    ang = (pos[:, None] * inv_freq[None, :]).astype(f)
    cos = np.cos(ang).astype(f)
    sin = np.sin(ang).astype(f)
    ks = f(128.0 ** -0.5)

    def lay(a):
        return np.ascontiguousarray(a.reshape(16, 128, 64).transpose(1, 0, 2))

    rope = np.stack([lay(cos), lay(sin), lay(-sin), lay(cos * ks), lay(sin * ks), lay(-sin * ks)], 0)
    sel = np.zeros((4, 4, 128), f)
    for b in range(4):
        sel[b, b, :] = 1.0
    return dict(
        identf=ident, identb=ident.astype(ml_dtypes.bfloat16), trib=tri.astype(ml_dtypes.bfloat16),
        negm=((1.0 - tri) * -30000.0).astype(ml_dtypes.bfloat16),
        trif=tri, onesf=ones, ixi_bc=ixi_bc, xi_bc=xi_bc, zeta_t=zeta_t,
        rope=np.ascontiguousarray(rope), g_chunk=g_chunk,
    )


_CONST = None


def build_program():
    consts = _constants()
    g_chunk = [float(v) for v in consts["g_chunk"]]
    nc = bass.Bass("TRN2", target_bir_lowering=False)
    P = Prog()
    es = ExitStack()

    def din(name, shape, dt=F32):
        return nc.dram_tensor(name, list(shape), dt, kind="ExternalInput").ap()

    x_d = din("x", [NSEQ * S, D])
    c_d = din("c", [NSEQ, D])
    wada_d = din("w_ada", [D, 6 * D])
    bada_d = din("b_ada", [1, 6 * D])
    win_d = din("w_in", [D, IN_COLS])
    bfg_d = din("b_forget", [1, 8])
    qg_d = din("q_gain", [1, 64])
    kg_d = din("k_gain", [1, 64])
    fxg_d = din("fox_gain", [1, 512])
    rtg_d = din("ret_gain", [1, 512])
    wout_d = din("w_out", [D, D])
    w1_d = din("w1", [D, DFF])
    w2_d = din("w2", [DFF, D])
    identf_d = din("identf", [128, 128])
    identb_d = din("identb", [128, 128], BF16)
    trib_d = din("trib", [128, 128], BF16)
    negm_d = din("negm", [128, 128], BF16)
    trif_d = din("trif", [128, 128])
    onesf_d = din("onesf", [128, 128])
    ixibc_d = din("ixi_bc", [128, 4, 128])
    xibc_d = din("xi_bc", [128, 4, 128])
    zeta_d = din("zeta_t", [128, 4])
    rope_d = din("rope", [6, 128, 16, 64])
    y_d = nc.dram_tensor("y", [NSEQ * S, D], F32, kind="ExternalOutput").ap()
    wbf_d = nc.dram_tensor("wbf", [NCHUNK, 128, 4096], BF16, kind="Internal").ap()
    gates_d = nc.dram_tensor("gates_scr", [4, 2048], F32, kind="Internal").ap()

    def sb(name, shape, dt=F32):
        return es.enter_context(nc.sbuf_tensor(name, list(shape), dt))

    wbuf = [sb(f"wbuf{i}", [128, 4096], BF16) for i in range(3)]
    xs = sb("xs", [128, G, D])
    hTA = sb("hTA", [128, 8, 512], BF16)
    hTB = sb("hTB", [128, 8, 512], BF16)
    U = sb("U", [128, 16384], BF16)
    KT = sb("KT", [70, 8, S], BF16)
    Vaug = sb("Vaug", [128, NT, 8, 65], BF16)
    qaug = [sb(f"qaug{i}", [128, 8, 70], BF16) for i in range(3)]
    kaug = [sb(f"kaug{i}", [128, 8, 70], BF16) for i in range(3)]
    state = sb("state", [128, 4, 128])
    state_bf = sb("state_bf", [128, 4, 128], BF16)
    MG = sb("MG", [128, 8192], BF16)
    mixed = MG[:, 0:4096].rearrange("p (t d) -> p t d", d=1024)
    gates_f = MG[:, 4096:6144].rearrange("p (t d) -> p t d", d=512)
    gates_r = MG[:, 6144:8192].rearrange("p (t d) -> p t d", d=512)
    xnext = MG[:, :].bitcast(F32).rearrange("p (t d) -> p t d", d=1024)
    XNK = [[("mixed", 0), ("mixed", 1)], [("mixed", 2), ("mixed", 3)],
           [("gates_f", t) for t in range(G)], [("gates_r", t) for t in range(G)]]
    PT = [sb(f"PT{i}", [128, 512], BF16) for i in range(3)]
    STr = [sb(f"STr{i}", [128, 4, 128], BF16) for i in range(2)]
    TA = [sb(f"TA{i}", [128, 512]) for i in range(2)]
    TB = [sb(f"TB{i}", [128, 512]) for i in range(2)]
    TE = [sb(f"TE{i}", [128, 4, 64]) for i in range(4)]
    TR = sb("TR", [128, 512])
    ropeg = sb("ropeg", [128, 6, G, 64])
    gm_bc = sb("gm_bc", [128, D])
    gf_bc = sb("gf_bc", [128, D])
    opm_m = sb("opm_m", [128, 8, 4])
    sh_m = sb("sh_m", [128, 8, 4])
    opm_f = sb("opm_f", [128, 8, 4])
    sh_f = sb("sh_f", [128, 8, 4])
    identf = sb("identf_s", [128, 128])
    identb = sb("identb_s", [128, 128], BF16)
    trib = sb("trib_s", [128, 128], BF16)
    negm = sb("negm_s", [128, 128], BF16)
    trif = sb("trif_s", [128, 128])
    onesf = sb("onesf_s", [128, 128])
    ixibc = sb("ixibc_s", [128, 4, 128])
    xibc = sb("xibc_s", [128, 4, 128])
    zeta = sb("zeta_s", [128, 4])
    qg_col = sb("qg_col", [70, 1])
    kg_col = sb("kg_col", [70, 1])
    fxg_bc = sb("fxg_bc", [128, 512])
    rtg_bc = sb("rtg_bc", [128, 512])
    bfg_bc = sb("bfg_bc", [128, 8])
    wfg = sb("wfg", [128, 8, 8], BF16)
    wfg32 = sb("wfg32", [128, 8, 8])
    epsc = sb("epsc", [128, 1])
    st = sb("st", [128, 128])
    rs_run = sb("rs_run", [128, 8])
    fz = sb("fz", [128, 3, 32])
    cr = sb("cr", [128, 2, 32])
    pre = sb("pre", [128, G, 8])
    cumsp = sb("cumsp", [128, G, 8, 3], BF16)
    cactT = sb("cactT", [128, 8, 4])
    ctmp = sb("ctmp", [128, 8, 4])
    ones14 = sb("ones14", [1, 4])

    c4 = xs[0:4, 0, :]
    badar = [TB[0][0:1, :], TB[1][0:1, :]]
    grow = xs[0:4, 1:3, :].rearrange("p t d -> p (t d)")
    PS = [es.enter_context(nc.psum_tensor(f"P{i}", [128, 512], F32)) for i in range(8)]

    def UK(lo, hi):
        return [("U", i) for i in range(lo, hi)]

    def u_chunk(fc):
        return U[:, fc * 512:(fc + 1) * 512]

    def u_f32(lo_gran, n_gran):
        return U[:, lo_gran * 512:(lo_gran + n_gran) * 512].bitcast(F32)

    QT_v = U[0:70, 0:4096].rearrange("p (h n) -> p h n", n=512)
    QTr_v = U[:, 4096:6144].rearrange("p (h n) -> p h n", n=512)
    QxT_v = U[:, 6144:8192].rearrange("p (h n) -> p h n", n=512)
    KTr_v = U[:, 8192:10240].rearrange("p (h n) -> p h n", n=512)
    Kz_v = U[:, 10240:12288].rearrange("p (t n) -> p t n", n=512)
    Vr_v = U[:, 12288:14336].rearrange("p (t n) -> p t n", n=512)
    rqt_v = [U[:, 14336:14848], U[:, 14848:15360], U[:, 6144:6656]]
    rkt_v = [U[:, 15360:15872], U[:, 15872:16384], U[:, 6656:7168]]
    rqt_g = [28, 29, 12]
    rkt_g = [30, 31, 13]

    def PK(i):
        return [("P", i)]

    def Pb(i):
        return PS[i][:, :].bitcast(BF16)

    def dma(out, in_, sem, reads=(), writes=(), eng="sp"):
        return P.op(eng, lambda e, o=out, i=in_: e.dma_start(out=o, in_=i), reads=reads, writes=writes, dma=sem)

    dma(identf[:, :], identf_d, "c0", writes=["identf"])
    dma(identb[:, :], identb_d, "c0", writes=["identb"])
    dma(trib[:, :], trib_d, "c0", writes=["trib"])
    dma(negm[:, :], negm_d, "c0", writes=["negm"])
    dma(trif[:, :], trif_d, "c0", writes=["trif"])
    dma(onesf[:, :], onesf_d, "c0", writes=["onesf"])
    dma(ixibc[:, :, :], ixibc_d, "c0", writes=["ixibc"])
    dma(xibc[:, :, :], xibc_d, "c0", writes=["xibc"])
    dma(zeta[:, :], zeta_d, "c0", writes=["zeta"])
    dma(qg_col[0:64, :], qg_d.rearrange("o d -> d o"), "c0", writes=["qg"])
    dma(kg_col[0:64, :], kg_d.rearrange("o d -> d o"), "c0", writes=["kg"])
    dma(fxg_bc[:, :], fxg_d.partition_broadcast(128), "c0", writes=["fxg"])
    dma(rtg_bc[:, :], rtg_d.partition_broadcast(128), "c0", writes=["rtg"])
    dma(bfg_bc[:, :], bfg_d.partition_broadcast(128), "c0", writes=["bfg"])
    dma(c4, c_d, "c0", writes=["c4", ("xs", 0)])
    dma(wfg32[:, :, :], win_d[:, 2048:2056].rearrange("(c p) n -> p c n", p=128), "c0", writes=["wfg32"])
    c0_total = P.dma_count["dma:c0"]
    for k in ["identf", "identb", "trib", "negm", "trif", "onesf", "ixibc", "xibc", "zeta", "qg", "kg", "fxg",
              "rtg", "bfg", "c4", "wfg32"]:
        P.res[k][0] = ("dma:c0", c0_total)
    P.clock[("dma:c0", c0_total)] = {"dma:c0": c0_total}

    P.op("pool", lambda e: e.memset(epsc[:, :], EPS), writes=["epsc"])
    P.op("pool", lambda e: e.memset(ones14[:, :], 1.0), writes=["ones14"])
    P.op("pool", lambda e: e.memset(Vaug[:, :, :, 64:65], 1.0), writes=["Vaug_ones"])
    for i in range(3):
        P.op("pool", lambda e, i=i: e.memset(qaug[i][:, :, 67:70], 1.0), writes=[("qaug", i)])
        P.op("pool", lambda e, i=i: e.memset(kaug[i][:, :, 64:67], 1.0), writes=[("kaug", i)])
    P.op("dve", lambda e: e.tensor_copy(out=wfg[:, :, :], in_=wfg32[:, :, :]), reads=["wfg32"], writes=["wfg"])
    P.op("pool", lambda e: e.memset(qg_col[64:70, :], 1.0), writes=["qg1"])
    P.op("pool", lambda e: e.memset(kg_col[64:70, :], 1.0), writes=["kg1"])
    P.op("dve", lambda e: e.tensor_scalar(out=qg_col[0:64, :], in0=qg_col[0:64, :], scalar1=0.125, scalar2=None,
                                          op0=ALU.mult), reads=["qg"], writes=["qg"])

    def chunk_src(k):
        if k < 8:
            col0 = [0, 512, 1024, 1536, 2056, 2568, 3080, 3592][k]
            return win_d[:, col0:col0 + 512].rearrange("(c p) n -> p c n", p=128)
        if k < 10:
            q = k - 8
            return wout_d[:, q * 512:(q + 1) * 512].rearrange("(c p) n -> p c n", p=128)
        if k < 18:
            j = k - 10
            return w1_d[:, j * 512:(j + 1) * 512].rearrange("(c p) n -> p c n", p=128)
        j = k - 18
        return w2_d[j * 512:(j + 1) * 512, :].rearrange("(c p) n -> p c n", p=128)

    CAST_ORDER = [3, 7, 0, 1, 2, 4, 5, 6] + list(range(8, NCHUNK))

    def cast_chunk(k, extra_reads=()):
        src_ap = chunk_src(k)
        dst = wbf_d[k].rearrange("p (c n) -> p c n", c=src_ap.shape[1])
        dma(dst, src_ap, f"cst{k}", reads=list(extra_reads), writes=[("wbf", k)], eng="pool")

    for k in CAST_ORDER[:5]:
        cast_chunk(k)

    c4k = [("xs", 0)]
    growk = [("xs", 1), ("xs", 2)]
    mrow = xs[0:4, 3, 0:512]
    mrowk = [("xs", 3)]
    for cc in range(8):
        P.op("pe", lambda e, cc=cc: e.transpose(out=PS[0][:, cc * 4:(cc + 1) * 4], in_=c4[:, cc * 128:(cc + 1) * 128],
                                               identity=identf[0:4, 0:4]),
             reads=["c4", "identf"] + c4k, writes=PK(0))
    P.op("act", lambda e: e.activation(out=ctmp[:, :, :], in_=PS[0][:, 0:32].rearrange("p (c b) -> p c b", b=4),
                                       func=AF.Exp, scale=-1.0), reads=PK(0), writes=["ctmp"])
    P.op("dve", lambda e: e.tensor_scalar(out=ctmp[:, :, :], in0=ctmp[:, :, :], scalar1=1.0, scalar2=None, op0=ALU.add),
         reads=["ctmp"], writes=["ctmp"])
    P.op("dve", lambda e: e.reciprocal(out=ctmp[:, :, :], in_=ctmp[:, :, :]), reads=["ctmp"], writes=["ctmp"])
    P.op("dve", lambda e: e.tensor_tensor(out=cactT[:, :, :], in0=ctmp[:, :, :],
                                          in1=PS[0][:, 0:32].rearrange("p (c b) -> p c b", b=4), op=ALU.mult),
         reads=["ctmp"] + PK(0), writes=["cactT"])

    modT_dst = {0: (sh_m, False), 1: (opm_m, True), 3: (sh_f, False), 4: (opm_f, True)}
    for kb in range(12):
        s = kb % 2
        v = kb // 2
        half = kb % 2
        stg = u_f32(16 * s, 16).rearrange("p (c n) -> p c n", c=8)
        dma(stg, wada_d[:, kb * 512:(kb + 1) * 512].rearrange("(c p) n -> p c n", p=128), f"stg{s}",
            writes=UK(16 * s, 16 * s + 16) + [("wada_blk", kb)])
        rk = UK(16 * s, 16 * s + 16)
        bd = badar[kb % 2]
        bdk = ("TB", kb % 2)
        dma(bd, bada_d[0:1, kb * 512:(kb + 1) * 512], f"bd{kb % 2}", writes=[bdk])
        bank = 3 + (kb % 2)
        for dc in range(8):
            P.op("pe", lambda e, bank=bank, dc=dc, stg=stg: e.matmul(
                PS[bank][0:4, :], lhsT=cactT[:, dc, :], rhs=stg[:, dc, :], start=(dc == 0), stop=False),
                reads=rk + ["cactT"], writes=PK(bank))
        P.op("pe", lambda e, bank=bank, bd=bd: e.matmul(
            PS[bank][0:4, :], lhsT=ones14[0:1, :], rhs=bd[0:1, :],
            start=False, stop=True), reads=[bdk, "ones14"], writes=PK(bank))
        if v in modT_dst:
            dst, plus1 = modT_dst[v]
            P.op("dve", lambda e, bank=bank: e.tensor_copy(out=mrow, in_=PS[bank][0:4, :]), reads=PK(bank), writes=mrowk)
            tb_ = 1 + (kb % 2)
            for ec in range(4):
                P.op("pe", lambda e, tb_=tb_, ec=ec: e.transpose(out=PS[tb_][:, ec * 4:(ec + 1) * 4],
                                                                 in_=mrow[:, ec * 128:(ec + 1) * 128],
                                                                 identity=identf[0:4, 0:4]),
                     reads=mrowk + ["identf"], writes=PK(tb_))
            src_v = PS[tb_][:, 0:16].rearrange("p (c b) -> p c b", b=4)
            dst_v = dst[:, half * 4:(half + 1) * 4, :]
            if plus1:
                P.op("dve", lambda e, o=dst_v, i=src_v: e.tensor_scalar(out=o, in0=i, scalar1=1.0, scalar2=None,
                                                                        op0=ALU.add),
                     reads=PK(tb_), writes=[("modT", v, half)])
            else:
                P.op("dve", lambda e, o=dst_v, i=src_v: e.tensor_copy(out=o, in_=i),
                     reads=PK(tb_), writes=[("modT", v, half)])
        else:
            gi = 0 if v == 2 else 1
            P.op("dve", lambda e, bank=bank, gi=gi, half=half: e.tensor_copy(
                out=grow[:, gi * 1024 + half * 512: gi * 1024 + (half + 1) * 512], in_=PS[bank][0:4, :]),
                reads=PK(bank), writes=growk)
    dma(gates_d, grow, "gsc", reads=growk, writes=["gates_d"])
    for k in CAST_ORDER[5:]:
        cast_chunk(k, extra_reads=[("wada_blk", 10), ("wada_blk", 11)])

    from collections import deque
    GROUP_ORDER = [3, 7, 0, 1, 2, 4, 5, 6] + list(range(8, NCHUNK))
    stream = [k for _ in range(NSEQ * NG) for k in GROUP_ORDER]
    pf = {"next": 0, "slots": {}, "cons": 0}

    def prefetch_upto(n):
        while pf["next"] < min(n, len(stream)):
            i = pf["next"]
            slot = i % 3
            dma(wbuf[slot][:, :], wbf_d[stream[i]], f"wld{slot}", reads=[("wbf", stream[i])], writes=[("wbuf", slot)])
            pf["slots"][i] = slot
            pf["next"] += 1

    def next_chunk(expect):
        i = pf["cons"]
        assert stream[i] == expect, (stream[i], expect)
        prefetch_upto(i + 2)
        pf["cons"] += 1
        return pf["slots"][i], i

    def after_chunk(i):
        prefetch_upto(i + 3)

    rr = {"proj": 0, "tp": 0, "tq": 0, "qa": 0, "ka": 0, "mi": 0, "tmp": 0, "rq": 0, "rk": 0, "qkp": 0}
    store_toks = []
    tmps = [(TA[0], ("TA", 0)), (TB[0], ("TB", 0)), (TA[1], ("TA", 1)), (TB[1], ("TB", 1))]

    def next_tmp():
        r = tmps[rr["tmp"] % 4]
        rr["tmp"] += 1
        return r

    def bc3(ap2, n):
        return ap2.unsqueeze(2).to_broadcast([128, ap2.shape[1], n])

    def bcmid(ap2, m):
        return ap2.unsqueeze(1).to_broadcast([128, m, ap2.shape[1]])

    def act_rstd(c_in, c_out, n, inv):
        P.op("act", lambda e: e.activation(out=st[:, c_out:c_out + n], in_=st[:, c_in:c_in + n], func=AF.Ln,
                                           scale=inv, bias=epsc[:, 0:1]),
             reads=[("st", c_in), "epsc"], writes=[("st", c_out)])
        P.op("act", lambda e: e.activation(out=st[:, c_out:c_out + n], in_=st[:, c_out:c_out + n], func=AF.Exp,
                                           scale=-0.5),
             reads=[("st", c_out)], writes=[("st", c_out)])

    def act_copy(out, in_, reads, writes):
        P.op("act", lambda e: e.activation(out=out, in_=in_, func=AF.Copy), reads=reads, writes=writes)

    def dve_copy(out, in_, reads, writes):
        P.op("dve", lambda e: e.tensor_copy(out=out, in_=in_), reads=reads, writes=writes)

    HB = [hTA, hTB]
    HK = ["hTA", "hTB"]

    def rms_chain(b_, src_t, src_keys, xn, xn_keys, hT, hkey, opm, shf, vs, vh, banks):
        junk = hT[:, 0:2, :].rearrange("p a n -> p (a n)")
        jk = [(hkey, 0), (hkey, 1)]

        def stage0():
            for t in range(G):
                P.op("act", lambda e, t=t: e.activation(out=junk, in_=src_t[:, t, :], func=AF.Square,
                                                        accum_out=st[:, t:t + 1]),
                     reads=src_keys[t], writes=jk + [("st", 0)])
            act_rstd(0, 8, 4, 1.0 / D)
            for t in range(G):
                P.op("dve", lambda e, t=t: e.tensor_scalar(out=xn[:, t, :], in0=src_t[:, t, :], scalar1=st[:, 8 + t:9 + t],
                                                           scalar2=None, op0=ALU.mult),
                     reads=src_keys[t] + [("st", 8)], writes=xn_keys[t])

        def mk(c0):
            def stage():
                for c in (c0, c0 + 1):
                    bank = banks[c % len(banks)]
                    for t in range(G):
                        P.op("pe", lambda e, c=c, t=t, bank=bank: e.transpose(
                            out=PS[bank][:, t * 128:(t + 1) * 128], in_=xn[:, t, c * 128:(c + 1) * 128],
                            identity=identf[:, :]),
                            reads=xn_keys[t] + ["identf"], writes=PK(bank))
                    if c % 2 == 0:
                        P.op("act", lambda e, c=c, bank=bank: e.activation(
                            out=hT[:, c, :], in_=PS[bank][:, :], func=AF.Identity,
                            scale=opm[:, c, b_:b_ + 1], bias=shf[:, c, b_:b_ + 1]),
                            reads=PK(bank) + [("modT", vs, c // 4), ("modT", vh, c // 4)], writes=[(hkey, c)])
                    else:
                        P.op("dve", lambda e, c=c, bank=bank: e.tensor_scalar(
                            out=hT[:, c, :], in0=PS[bank][:, :], scalar1=opm[:, c, b_:b_ + 1],
                            scalar2=shf[:, c, b_:b_ + 1], op0=ALU.mult, op1=ALU.add),
                            reads=PK(bank) + [("modT", vs, c // 4), ("modT", vh, c // 4)], writes=[(hkey, c)])
            return stage
        return [stage0] + [mk(c0) for c0 in (0, 2, 4, 6)]

    def prefetch_B(gi_n):
        b_n, g_n = gi_n // NG, gi_n % NG
        r0 = b_n * S + g_n * 512
        dma(xnext[:, :, :], x_d[r0:r0 + 512, :].rearrange("(t p) d -> p t d", p=128), "xnl",
            writes=[k for ks in XNK for k in ks])
        return rms_chain(b_n, xnext, XNK, xnext, XNK, HB[gi_n % 2], HK[gi_n % 2], opm_m, sh_m, 1, 0, [4, 5, 6, 7])

    def proj_mm(src, t, slot, bank, key):
        for c in range(8):
            P.op("pe", lambda e, c=c: e.matmul(
                PS[bank][:, :], lhsT=src[:, c, t * 128:(t + 1) * 128],
                rhs=wbuf[slot][:, c * 512:(c + 1) * 512], start=(c == 0), stop=(c == 7)),
                reads=[(key, c), ("wbuf", slot)], writes=PK(bank))

    def next_proj_bank():
        bk = 2 + rr["proj"] % 3
        rr["proj"] += 1
        return bk

    for b in range(NSEQ):
        dma(gm_bc[:, :], gates_d[b:b + 1, 0:1024].partition_broadcast(128), "gbm", reads=["gates_d"], writes=["gm_bc"])
        dma(gf_bc[:, :], gates_d[b:b + 1, 1024:2048].partition_broadcast(128), "gbf", reads=["gates_d"], writes=["gf_bc"])
        P.op("pool", lambda e: e.memset(rs_run[:, :], 0.0), writes=["rs_run"])
        P.op("pool", lambda e: e.memset(state[:, :, :], 0.0), writes=["state"])
        P.op("pool", lambda e: e.memset(state_bf[:, :, :], 0.0), writes=["state_bf"])

        for g in range(NG):
            row0 = b * S + g * 512
            dma(xs[:, :, :], x_d[row0:row0 + 512, :].rearrange("(t p) d -> p t d", p=128), "xld",
                writes=[("xs", t) for t in range(G)])
            dma(ropeg[:, :, :, :], rope_d[:, :, g * G:(g + 1) * G, :].rearrange("r p t i -> p r t i"), "rope",
                writes=["ropeg"])

            gi = b * NG + g
            if gi == 0:
                for stg_ in prefetch_B(0):
                    stg_()
            hT_cur, hkey = HB[gi % 2], HK[gi % 2]
            hT_oth, okey = HB[(gi + 1) % 2], HK[(gi + 1) % 2]

            first = True
            for t in range(G):
                for c in range(8):
                    P.op("pe", lambda e, c=c, t=t, hT_cur=hT_cur, first=first: e.matmul(
                        PS[7][:, t * 8:(t + 1) * 8], lhsT=hT_cur[:, c, t * 128:(t + 1) * 128],
                        rhs=wfg[:, c, :], start=first, stop=(c == 7), skip_group_check=True),
                        reads=[(hkey, c), "wfg"], writes=PK(7))
                    first = False
            P.op("dve", lambda e: e.tensor_tensor(out=fz[:, 0, :].rearrange("p (t h) -> p t h", h=8),
                                                  in0=PS[7][:, 0:32].rearrange("p (t h) -> p t h", h=8),
                                                  in1=bcmid(bfg_bc[:, :], G), op=ALU.add),
                 reads=PK(7) + ["bfg"], writes=[("fz", 0)])
            P.op("act", lambda e: e.activation(out=fz[:, 1, :], in_=fz[:, 0, :], func=AF.Exp, scale=-1.0),
                 reads=[("fz", 0)], writes=[("fz", 1)])
            P.op("act", lambda e: e.activation(out=fz[:, 2, :], in_=fz[:, 1, :], func=AF.Ln, bias=1.0),
                 reads=[("fz", 1)], writes=[("fz", 2)])
            lall = fz[:, 2, :].rearrange("p (t h) -> p t h", h=8)
            P.op("dve", lambda e: e.tensor_copy(out=pre[:, 0, :], in_=rs_run[:, :]), reads=["rs_run"], writes=["pre"])
            for t in range(1, G):
                P.op("dve", lambda e, t=t: e.tensor_tensor(out=pre[:, t, :], in0=pre[:, t - 1, :], in1=lall[:, t - 1, :],
                                                           op=ALU.add), reads=["pre", ("fz", 2)], writes=["pre"])
            P.op("dve", lambda e: e.tensor_tensor(out=rs_run[:, :], in0=pre[:, G - 1, :], in1=lall[:, G - 1, :], op=ALU.add),
                 reads=["pre", ("fz", 2)], writes=["rs_run"])

            def cum_finish():
                P.op("pe", lambda e: e.matmul(PS[7][:, 32:64], lhsT=trif[:, :], rhs=fz[:, 2, :], start=True, stop=False),
                     reads=[("fz", 2), "trif"], writes=PK(7))
                P.op("pe", lambda e: e.matmul(PS[7][:, 32:64], lhsT=onesf[:, :], rhs=pre[:, :, :].rearrange("p t h -> p (t h)"),
                                              start=False, stop=True), reads=["pre", "onesf"], writes=PK(7))
                ncum = PS[7][:, 32:64].rearrange("p (t h) -> p t h", h=8)
                ck = [("cumsp", t) for t in range(G)]
                cr0 = cr[:, 0, :].rearrange("p (t h) -> p t h", h=8)
                cr1 = cr[:, 1, :].rearrange("p (t h) -> p t h", h=8)
                P.op("dve", lambda e: e.tensor_copy(out=cumsp[:, :, :, 0], in_=ncum), reads=PK(7), writes=ck)
                P.op("dve", lambda e: e.tensor_tensor(out=cr0, in0=ncum, in1=cumsp[:, :, :, 0], op=ALU.subtract),
                     reads=PK(7) + ck, writes=[("cr", 0)])
                P.op("dve", lambda e: e.tensor_copy(out=cumsp[:, :, :, 1], in_=cr0), reads=[("cr", 0)], writes=ck)
                P.op("dve", lambda e: e.tensor_tensor(out=cr1, in0=cr0, in1=cumsp[:, :, :, 1], op=ALU.subtract),
                     reads=[("cr", 0)] + ck, writes=[("cr", 1)])
                P.op("dve", lambda e: e.tensor_copy(out=cumsp[:, :, :, 2], in_=cr1), reads=[("cr", 1)], writes=ck)

            pipe = []

            def pipe_tick():
                keep = []
                for it in list(pipe):
                    it[0] += 1
                    a = it[0]
                    if a - 1 < len(it[1]) and it[1][a - 1] is not None:
                        it[1][a - 1]()
                    if a < len(it[1]):
                        keep.append(it)
                pipe[:] = keep

            def pipe_push(stages):
                pipe_tick()
                pipe.append([0, stages])

            def push_deferred(fn):
                pipe_push([None, fn])

            def flush_deferred():
                while pipe:
                    pipe_tick()

            slot, ci = next_chunk(3)
            for t in range(G):
                bank = next_proj_bank()
                proj_mm(hT_cur, t, slot, bank, hkey)
                tmp, tkey = next_tmp()
                P.op("act", lambda e, tmp=tmp, bank=bank: e.activation(out=tmp[:, :], in_=PS[bank][:, :], func=AF.Sigmoid),
                     reads=PK(bank), writes=[tkey])
                P.op("pool", lambda e, t=t, tmp=tmp: e.tensor_tensor(out=gates_f[:, t, :], in0=tmp[:, :], in1=fxg_bc[:, :],
                                                                     op=ALU.mult),
                     reads=[tkey, "fxg"], writes=[("gates_f", t)])
            after_chunk(ci)
            slot, ci = next_chunk(7)
            for t in range(G):
                bank = next_proj_bank()
                proj_mm(hT_cur, t, slot, bank, hkey)
                tmp, tkey = next_tmp()
                tmp2, tkey2 = next_tmp()
                P.op("act", lambda e, tmp=tmp, bank=bank: e.activation(out=tmp[:, :], in_=PS[bank][:, :], func=AF.Sigmoid),
                     reads=PK(bank), writes=[tkey])
                P.op("dve", lambda e, tmp=tmp, tmp2=tmp2, bank=bank: e.tensor_tensor(out=tmp2[:, :], in0=PS[bank][:, :],
                                                                                    in1=tmp[:, :], op=ALU.mult),
                     reads=[tkey] + PK(bank), writes=[tkey2])
                P.op("pool", lambda e, t=t, tmp2=tmp2: e.tensor_tensor(out=gates_r[:, t, :], in0=tmp2[:, :], in1=rtg_bc[:, :],
                                                                       op=ALU.mult),
                     reads=[tkey2, "rtg"], writes=[("gates_r", t)])
            after_chunk(ci)

            cum_finish()

            def qk_step(t, slot, is_q):
                bank = next_proj_bank()
                proj_mm(hT_cur, t, slot, bank, hkey)
                if is_q:
                    par = rr["qa"] % 3
                    rr["qa"] += 1
                    aug, akey, gcol, gkeys = qaug[par], ("qaug", par), qg_col, ["qg", "qg1"]
                else:
                    par = rr["ka"] % 3
                    rr["ka"] += 1
                    aug, akey, gcol, gkeys = kaug[par], ("kaug", par), kg_col, ["kg", "kg1"]
                tmp, tkey = next_tmp()
                par2 = rr["qkp"] % 2
                rr["qkp"] += 1
                cs, cr_ = 64 + 16 * par2, 72 + 16 * par2
                P.op("act", lambda e: e.activation(out=tmp[:, :], in_=PS[bank][:, :], func=AF.Square),
                     reads=PK(bank), writes=[tkey])
                P.op("dve", lambda e: e.tensor_reduce(out=st[:, cs:cs + 8], in_=tmp[:, :].rearrange("p (h i) -> p h i", i=64),
                                                      axis=AX.X, op=ALU.add), reads=[tkey], writes=[("st", cs)])

                def stage_b():
                    act_rstd(cs, cr_, 8, 1.0 / 64)
                    P.op("dve", lambda e: e.tensor_tensor(out=aug[:, :, 0:64],
                                                          in0=PS[bank][:, :].rearrange("p (h i) -> p h i", i=64),
                                                          in1=bc3(st[:, cr_:cr_ + 8], 64), op=ALU.mult),
                         reads=PK(bank) + [("st", cr_)], writes=[akey])
                    if is_q:
                        P.op("pool", lambda e: e.tensor_scalar(out=aug[:, :, 64:67], in0=cumsp[:, t, :, :], scalar1=-1.0,
                                                               scalar2=None, op0=ALU.mult),
                             reads=[("cumsp", t)], writes=[akey])
                    else:
                        P.op("pool", lambda e: e.tensor_copy(out=aug[:, :, 67:70], in_=cumsp[:, t, :, :]),
                             reads=[("cumsp", t)], writes=[akey])

                def deferred():
                    bq = 5 + rr["tq"] % 2
                    rr["tq"] += 1
                    for h in range(8):
                        P.op("pe", lambda e, h=h: e.transpose(out=Pb(bq)[0:70, h * 128:(h + 1) * 128], in_=aug[:, h, :],
                                                              identity=identb[:, :]),
                             reads=[akey, "identb"], writes=PK(bq))
                    srcv = Pb(bq)[0:70, :].rearrange("p (h n) -> p h n", n=128)
                    if is_q:
                        dst, wk = QT_v[:, :, t * 128:(t + 1) * 128], UK(0, 8)
                    else:
                        blk_i = g * G + t
                        dst, wk = KT[:, :, blk_i * 128:(blk_i + 1) * 128], [("KT", blk_i)]
                    P.op("dve", lambda e: e.tensor_scalar(out=dst, in0=srcv, scalar1=gcol[:, 0:1], scalar2=None,
                                                          op0=ALU.mult),
                         reads=PK(bq) + gkeys, writes=wk)
                pipe_push([stage_b, deferred])

            for is_q, ck_ in ((True, 0), (False, 1)):
                slot, ci = next_chunk(ck_)
                for t in range(G):
                    qk_step(t, slot, is_q)
                after_chunk(ci)

            slot, ci = next_chunk(2)
            for t in range(G):
                bank = next_proj_bank()
                proj_mm(hT_cur, t, slot, bank, hkey)
                blk_i = g * G + t
                act_copy(Vaug[:, blk_i, :, 0:64], PS[bank][:, :].rearrange("p (h i) -> p h i", i=64), PK(bank),
                         [("Vaug", blk_i)])
                pipe_push([])
            after_chunk(ci)
            flush_deferred()

            def side_bank():
                bk = (5, 7)[rr["proj"] % 2]
                rr["proj"] += 1
                return bk

            def rope_step(t, slot, is_q):
                bank = side_bank()
                proj_mm(hT_cur, t, slot, bank, hkey)
                r0 = 0 if is_q else 3
                cosv, sinv, nsinv = ropeg[:, r0, t, :], ropeg[:, r0 + 1, t, :], ropeg[:, r0 + 2, t, :]
                ta, tak = next_tmp()
                tb, tbk = next_tmp()
                pv = PS[bank][:, :].rearrange("p (h w i) -> p h w i", h=4, w=2)
                ta4 = ta[:, :].rearrange("p (h w i) -> p h w i", h=4, w=2)
                tb4 = tb[:, :].rearrange("p (h w i) -> p h w i", h=4, w=2)
                cos4 = cosv.unsqueeze(1).unsqueeze(1).to_broadcast([128, 4, 2, 64])
                P.op("dve", lambda e: e.tensor_tensor(out=ta4, in0=pv, in1=cos4, op=ALU.mult),
                     reads=PK(bank) + ["ropeg"], writes=[tak])
                P.op("dve", lambda e: e.tensor_tensor(out=tb4[:, :, 0, :], in0=pv[:, :, 1, :], in1=bcmid(nsinv, 4),
                                                      op=ALU.mult), reads=PK(bank) + ["ropeg"], writes=[tbk])
                P.op("dve", lambda e: e.tensor_tensor(out=tb4[:, :, 1, :], in0=pv[:, :, 0, :], in1=bcmid(sinv, 4),
                                                      op=ALU.mult), reads=PK(bank) + ["ropeg"], writes=[tbk])
                if is_q:
                    i = rr["rq"] % 3
                    rr["rq"] += 1
                    rt, rtk = rqt_v[i], ("U", rqt_g[i])
                else:
                    i = rr["rk"] % 3
                    rr["rk"] += 1
                    rt, rtk = rkt_v[i], ("U", rkt_g[i])
                P.op("pool", lambda e: e.tensor_tensor(out=rt, in0=ta[:, :], in1=tb[:, :], op=ALU.add),
                     reads=[tak, tbk], writes=[rtk])
                if not is_q:
                    P.op("pool", lambda e: e.tensor_tensor(out=Kz_v[:, t, :].rearrange("p (h i) -> p h i", i=128),
                                                           in0=rt.rearrange("p (h i) -> p h i", i=128),
                                                           in1=bc3(zeta[:, :], 128), op=ALU.mult),
                         reads=[rtk, "zeta"], writes=[("U", 20 + t)])

                def deferred():
                    bq = 6
                    for h in range(4):
                        P.op("pe", lambda e, h=h: e.transpose(out=Pb(bq)[:, h * 128:(h + 1) * 128],
                                                              in_=rt[:, h * 128:(h + 1) * 128], identity=identb[:, :]),
                             reads=[rtk, "identb"], writes=PK(bq))
                    srcv = Pb(bq)[:, 0:512].rearrange("p (h n) -> p h n", n=128)
                    tc_ = slice(t * 128, (t + 1) * 128)
                    if is_q:
                        P.op("dve", lambda e: e.tensor_tensor(out=QTr_v[:, :, tc_], in0=srcv, in1=xibc[:, :, :], op=ALU.mult),
                             reads=PK(bq) + ["xibc"], writes=UK(8, 12))
                    else:
                        P.op("dve", lambda e: e.tensor_tensor(out=KTr_v[:, :, tc_], in0=srcv, in1=ixibc[:, :, :], op=ALU.mult),
                             reads=PK(bq) + ["ixibc"], writes=UK(16, 20))
                return deferred

            def rv_step(t, slot):
                bank = side_bank()
                proj_mm(hT_cur, t, slot, bank, hkey)
                dve_copy(Vr_v[:, t, :], PS[bank][:, :], PK(bank), [("U", 24 + t)])
                return lambda: None

            chunk_state = {}

            def side_steps():
                units = []
                for kind, ck_ in (("rq", 4), ("rk", 5), ("rv", 6)):
                    for t in range(G):
                        units.append((kind, ck_, t))
                return units

            units = side_steps()

            def run_unit(u):
                kind, ck_, t = u
                if t == 0:
                    chunk_state["cur"] = next_chunk(ck_)
                slot, ci = chunk_state["cur"]
                if kind == "rq":
                    push_deferred(rope_step(t, slot, True))
                elif kind == "rk":
                    push_deferred(rope_step(t, slot, False))
                else:
                    push_deferred(rv_step(t, slot))
                if t == G - 1:
                    after_chunk(ci)

            nkb = 4 * g + 4
            tasks = [(h, kb) for h in range(8) for kb in range(nkb)]
            mixed_all = [("mixed", t) for t in range(G)]

            def fox_qk(i):
                h, kb = tasks[i]
                jlo = max(0, kb - 4 * g)
                n = (4 - jlo) * 128
                bank = i % 3
                pt = PT[i % 3]
                diag = kb >= 4 * g
                P.op("pe", lambda e: e.matmul(PS[bank][:, 0:n], lhsT=KT[:, h, kb * 128:(kb + 1) * 128],
                                              rhs=QT_v[:, h, jlo * 128:512], start=True, stop=not diag),
                     reads=[("KT", kb), ("U", h)], writes=PK(bank))
                if diag:
                    P.op("pe", lambda e: e.matmul(PS[bank][:, 0:128], lhsT=identb[:, :], rhs=negm[:, :],
                                                  start=False, stop=True, skip_group_check=True),
                         reads=["identb", "negm"], writes=PK(bank))
                P.op("act", lambda e: e.activation(out=pt[:, 0:n], in_=PS[bank][:, 0:n], func=AF.Exp),
                     reads=PK(bank), writes=[("PT", i % 3)])

            def fox_pv(i):
                h, kb = tasks[i]
                jlo = max(0, kb - 4 * g)
                ob = 3 + (h % 2)
                pt = PT[i % 3]
                for j in range(jlo, 4):
                    P.op("pe", lambda e, j=j, last=(kb == 4 * g + j): e.matmul(PS[ob][:, j * 65:(j + 1) * 65],
                                                       lhsT=pt[:, (j - jlo) * 128:(j - jlo + 1) * 128],
                                                       rhs=Vaug[:, kb, h, :], start=(kb == 0 and j == 0),
                                                       stop=last, skip_group_check=True),
                         reads=[("PT", i % 3), ("Vaug", kb), "Vaug_ones"], writes=PK(ob))
                if kb == nkb - 1:
                    fox_epilogue(h, ob, i + 2)

            def at(idx, fn):
                sched.setdefault(idx, []).append(fn)

            def fox_epilogue(h, ob, i_now):
                O = PS[ob][:, 0:260].rearrange("p (j e) -> p j e", e=65)
                p_ = h % 2
                oz, ozk = (TE[0], ("TE", 0)) if p_ == 0 else (TE[3], ("TE", 3))
                c_ss, c_rs = 96 + 16 * p_, 104 + 16 * p_
                P.op("dve", lambda e: e.reciprocal(out=st[:, 40:44], in_=O[:, :, 64]), reads=PK(ob), writes=[("st", 40)])
                P.op("dve", lambda e: e.tensor_tensor(out=oz[:, :, :], in0=O[:, :, 0:64], in1=bc3(st[:, 40:44], 64),
                                                      op=ALU.mult), reads=PK(ob) + [("st", 40)], writes=[ozk])
                P.op("pool", lambda e: e.tensor_tensor(out=TE[1][:, :, :], in0=oz[:, :, :], in1=oz[:, :, :], op=ALU.mult),
                     reads=[ozk], writes=[("TE", 1)])
                P.op("dve", lambda e: e.tensor_reduce(out=st[:, c_ss:c_ss + 4], in_=TE[1][:, :, :], axis=AX.X, op=ALU.add),
                     reads=[("TE", 1)], writes=[("st", c_ss)])

                def e1():
                    act_rstd(c_ss, c_rs, 4, 1.0 / 64)

                def e2():
                    P.op("dve", lambda e: e.tensor_tensor(out=TE[2][:, :, :], in0=oz[:, :, :], in1=bc3(st[:, c_rs:c_rs + 4], 64),
                                                          op=ALU.mult), reads=[ozk, ("st", c_rs)], writes=[("TE", 2)])
                    P.op("pool", lambda e: e.tensor_tensor(out=mixed[:, :, h * 64:(h + 1) * 64], in0=TE[2][:, :, :],
                                                           in1=gates_f[:, :, h * 64:(h + 1) * 64], op=ALU.mult),
                         reads=[("TE", 2)] + [("gates_f", t) for t in range(G)], writes=mixed_all)
                at(i_now + 2, e1)
                at(i_now + 3, e2)

            def ret_stage1(j):
                jc = slice(j * 128, (j + 1) * 128)
                for h in range(4):
                    P.op("pe", lambda e, h=h: e.matmul(PS[5][:, h * 128:(h + 1) * 128], lhsT=KTr_v[:, h, jc],
                                                       rhs=QTr_v[:, h, jc], start=(h == 0), stop=True,
                                                       skip_group_check=True),
                         reads=UK(16, 20) + UK(8, 12), writes=PK(5))
                for h in range(4):
                    hc = slice(h * 128, (h + 1) * 128)
                    P.op("pe", lambda e, hc=hc, h=h: e.matmul(PS[7][:, hc], lhsT=Kz_v[:, j, hc], rhs=Vr_v[:, j, hc],
                                                              start=(h == 0), stop=True, skip_group_check=True),
                         reads=[("U", 20 + j), ("U", 24 + j)], writes=PK(7))
                sj = j % 2
                P.op("dve", lambda e: e.tensor_tensor(out=STr[sj][:, :, :],
                                                      in0=PS[5][:, :].rearrange("p (h n) -> p h n", n=128),
                                                      in1=bcmid(trib[:, :], 4), op=ALU.mult),
                     reads=PK(5) + ["trib"], writes=[("STr", sj)])

            def ret_stage2(j):
                jc = slice(j * 128, (j + 1) * 128)
                sj = j % 2
                for h in range(4):
                    hc = slice(h * 128, (h + 1) * 128)
                    P.op("pe", lambda e, hc=hc, h=h: e.matmul(PS[6][:, hc], lhsT=STr[sj][:, h, :], rhs=Vr_v[:, j, hc],
                                                              start=(h == 0), stop=False, skip_group_check=True),
                         reads=[("STr", sj), ("U", 24 + j)], writes=PK(6))
                    P.op("pe", lambda e, hc=hc, h=h: e.matmul(PS[6][:, hc], lhsT=QTr_v[:, h, jc], rhs=state_bf[:, h, :],
                                                              start=False, stop=True, skip_group_check=True),
                         reads=UK(8, 12) + ["state_bf"], writes=PK(6))
                for h in range(4):
                    hc = slice(h * 128, (h + 1) * 128)
                    P.op("dve", lambda e, hc=hc, h=h: e.scalar_tensor_tensor(
                        out=state[:, h, :], in0=state[:, h, :], scalar=g_chunk[h], in1=PS[7][:, hc],
                        op0=ALU.mult, op1=ALU.add), reads=["state"] + PK(7), writes=["state"])
                P.op("pool", lambda e: e.tensor_copy(out=state_bf[:, :, :], in_=state[:, :, :]),
                     reads=["state"], writes=["state_bf"])
                pj = j % 2
                c_ss, c_rs = 16 + 8 * pj, 20 + 8 * pj
                dve_copy(TR[:, :], PS[6][:, :], PK(6), ["TR"])
                tmp, tkey = next_tmp()
                P.op("pool", lambda e: e.tensor_tensor(out=tmp[:, :], in0=TR[:, :], in1=TR[:, :], op=ALU.mult),
                     reads=["TR"], writes=[tkey])
                P.op("dve", lambda e: e.tensor_reduce(out=st[:, c_ss:c_ss + 4], in_=tmp[:, :].rearrange("p (h i) -> p h i", i=128),
                                                      axis=AX.X, op=ALU.add), reads=[tkey], writes=[("st", c_ss)])

                def r2c():
                    act_rstd(c_ss, c_rs, 4, 1.0 / 128)
                    tmp2, tkey2 = next_tmp()
                    P.op("dve", lambda e: e.tensor_tensor(out=tmp2[:, :].rearrange("p (h i) -> p h i", i=128),
                                                          in0=TR[:, :].rearrange("p (h i) -> p h i", i=128),
                                                          in1=bc3(st[:, c_rs:c_rs + 4], 128), op=ALU.mult),
                         reads=["TR", ("st", c_rs)], writes=[tkey2])
                    P.op("pool", lambda e: e.tensor_tensor(out=mixed[:, j, 512:1024], in0=tmp2[:, :], in1=gates_r[:, j, :],
                                                           op=ALU.mult),
                         reads=[tkey2, ("gates_r", j)], writes=[("mixed", j)])
                at(cur_i[0] + 2, r2c)

            ntask = len(tasks)
            sched = {}
            nun = len(units)
            span = max(nun, int(ntask * 0.55))
            for ui, u in enumerate(units):
                sched.setdefault(min(ntask - 1, ui * span // nun), []).append(lambda u=u: run_unit(u))
            sched.setdefault(min(ntask - 1, span), []).append(flush_deferred)
            rem0 = min(ntask - 1, span + 1)
            for j in range(4):
                p1 = rem0 + (2 * j) * (ntask - rem0) // 8
                p2 = rem0 + (2 * j + 1) * (ntask - rem0) // 8
                sched.setdefault(min(ntask - 1, p1), []).append(lambda j=j: ret_stage1(j))
                sched.setdefault(min(ntask - 1, p2), []).append(lambda j=j: ret_stage2(j))
            cur_i = [0]
            for i in range(ntask + 2):
                cur_i[0] = i
                for f in sched.pop(i, []):
                    f()
                if i < ntask:
                    fox_qk(i)
                if i >= 2:
                    fox_pv(i - 2)
            while sched:
                k_ = min(sched)
                cur_i[0] = k_
                for f in sched.pop(k_):
                    f()

            for c in range(8):
                bank = rr["tp"] % 2
                rr["tp"] += 1
                for t in range(G):
                    P.op("pe", lambda e, c=c, t=t, bank=bank: e.transpose(
                        out=Pb(bank)[:, t * 128:(t + 1) * 128], in_=mixed[:, t, c * 128:(c + 1) * 128],
                        identity=identb[:, :]), reads=[("mixed", t), "identb"], writes=PK(bank))
                if c % 2 == 0:
                    act_copy(hT_oth[:, c, :], Pb(bank)[:, 0:512], PK(bank), [(okey, c)])
                else:
                    dve_copy(hT_oth[:, c, :], Pb(bank)[:, 0:512], PK(bank), [(okey, c)])

            for q in range(2):
                slot, ci = next_chunk(8 + q)
                for t in range(G):
                    bank = next_proj_bank()
                    proj_mm(hT_oth, t, slot, bank, okey)
                    tmp, tkey = next_tmp()
                    qc = slice(q * 512, (q + 1) * 512)
                    P.op("dve", lambda e, tmp=tmp, bank=bank, qc=qc: e.tensor_tensor(out=tmp[:, :], in0=PS[bank][:, :],
                                                                                    in1=gm_bc[:, qc], op=ALU.mult),
                         reads=PK(bank) + ["gm_bc"], writes=[tkey])
                    P.op("pool" if t % 2 else "dve", lambda e, tmp=tmp, t=t, qc=qc: e.tensor_tensor(
                        out=xs[:, t, qc], in0=xs[:, t, qc], in1=tmp[:, :], op=ALU.add),
                         reads=[tkey, ("xs", t)], writes=[("xs", t)])
                after_chunk(ci)

            xn2 = u_f32(16, 16).rearrange("p (t d) -> p t d", t=G)
            for stg_ in rms_chain(b, xs, [[("xs", t)] for t in range(G)], xn2, [UK(16 + 4 * t, 20 + 4 * t) for t in range(G)],
                                  hT_cur, hkey, opm_f, sh_f, 4, 3, [0, 1]):
                stg_()

            pstages = prefetch_B(gi + 1) if gi + 1 < NSEQ * NG else []
            for j in range(8):
                slot, ci = next_chunk(10 + j)
                if pstages and 1 <= j <= 5:
                    pstages[j - 1]()
                for fc in range(4):
                    bank = rr["mi"] % 4
                    rr["mi"] += 1
                    tmp, tkey = next_tmp()
                    for c in range(8):
                        P.op("pe", lambda e, c=c, fc=fc, bank=bank, slot=slot, hT_cur=hT_cur: e.matmul(
                            PS[bank][:, :], lhsT=wbuf[slot][:, c * 512 + fc * 128: c * 512 + (fc + 1) * 128],
                            rhs=hT_cur[:, c, :], start=(c == 0), stop=(c == 7)),
                            reads=[(hkey, c), ("wbuf", slot)], writes=PK(bank))
                    P.op("act", lambda e, tmp=tmp, bank=bank: e.activation(out=tmp[:, :], in_=PS[bank][:, :], func=AF.Relu),
                         reads=PK(bank), writes=[tkey])
                    uc = u_chunk(4 * j + fc)
                    P.op("pool", lambda e, tmp=tmp, uc=uc: e.tensor_tensor(out=uc, in0=tmp[:, :], in1=tmp[:, :], op=ALU.mult),
                         reads=[tkey], writes=[("U", 4 * j + fc)])
                after_chunk(ci)

            for j in range(8):
                slot, ci = next_chunk(18 + j)
                for t in range(G):
                    for hf in range(2):
                        bank = t * 2 + hf
                        for fc in range(4):
                            uc = u_chunk(4 * j + fc)
                            P.op("pe", lambda e, uc=uc, t=t, hf=hf, fc=fc, bank=bank, slot=slot, j=j: e.matmul(
                                PS[bank][:, :], lhsT=uc[:, t * 128:(t + 1) * 128],
                                rhs=wbuf[slot][:, fc * 1024 + hf * 512: fc * 1024 + (hf + 1) * 512],
                                start=(j == 0 and fc == 0), stop=(j == 7 and fc == 3)),
                                reads=[("U", 4 * j + fc), ("wbuf", slot)], writes=PK(bank))
                after_chunk(ci)
            done_half = {}
            for (t, hf) in ((3, 1), (1, 0), (1, 1), (2, 0), (0, 0), (0, 1), (2, 1), (3, 0)):
                if True:
                    bank = t * 2 + hf
                    tmp, tkey = next_tmp()
                    qc = slice(hf * 512, (hf + 1) * 512)
                    P.op("dve", lambda e, tmp=tmp, bank=bank, qc=qc: e.tensor_tensor(out=tmp[:, :], in0=PS[bank][:, :],
                                                                                    in1=gf_bc[:, qc], op=ALU.mult),
                         reads=PK(bank) + ["gf_bc"], writes=[tkey])
                    P.op("pool" if hf else "dve", lambda e, tmp=tmp, t=t, qc=qc: e.tensor_tensor(
                        out=xs[:, t, qc], in0=xs[:, t, qc], in1=tmp[:, :], op=ALU.add),
                         reads=[tkey, ("xs", t)], writes=[("xs", t)])
                done_half[t] = done_half.get(t, 0) + 1
                if done_half[t] == 2:
                    tok = dma(y_d[row0 + t * 128: row0 + (t + 1) * 128, :], xs[:, t, :], "yst", reads=[("xs", t)],
                              writes=[("y", row0 + t * 128)])
                    store_toks.append(tok)

    P.wait_all("sp", [max(store_toks, key=lambda tk: tk[1])])
    return nc, P, es, consts


_BUILT = None


def _get_built():
    global _BUILT
    if _BUILT is None:
        nc, P, es, consts = build_program()
        P.emit(nc, es)
        es.close()
        _BUILT = (nc, consts)
    return _BUILT


def kernel(x, c, w_ada, b_ada, w_in, b_forget, q_norm_gain, k_norm_gain, fox_out_gain, ret_out_gain,
           w_out, w_mlp_in, w_mlp_out):
    nc, consts = _get_built()
    f = np.float32
    x = np.asarray(x, f)
    c = np.asarray(c, f)
    shared = {
        "w_ada": np.ascontiguousarray(np.asarray(w_ada, f)[0]),
        "b_ada": np.ascontiguousarray(np.asarray(b_ada, f)[0].reshape(1, -1)),
        "w_in": np.ascontiguousarray(np.asarray(w_in, f)[0]),
        "b_forget": np.ascontiguousarray(np.asarray(b_forget, f)[0].reshape(1, 8)),
        "q_gain": np.ascontiguousarray(np.asarray(q_norm_gain, f)[0].reshape(1, 64)),
        "k_gain": np.ascontiguousarray(np.asarray(k_norm_gain, f)[0].reshape(1, 64)),
        "fox_gain": np.ascontiguousarray(np.asarray(fox_out_gain, f)[0].reshape(1, 512)),
        "ret_gain": np.ascontiguousarray(np.asarray(ret_out_gain, f)[0].reshape(1, 512)),
        "w_out": np.ascontiguousarray(np.asarray(w_out, f)[0]),
        "w1": np.ascontiguousarray(np.asarray(w_mlp_in, f)[0]),
        "w2": np.ascontiguousarray(np.asarray(w_mlp_out, f)[0]),
    }
    for k in ("identf", "identb", "trib", "negm", "trif", "onesf", "ixi_bc", "xi_bc", "zeta_t", "rope"):
        shared[k] = consts[k]
    in_maps = []
    for i in range(NCORES):
        m = dict(shared)
        m["x"] = np.ascontiguousarray(x[i * NSEQ:(i + 1) * NSEQ].reshape(NSEQ * S, D))
        m["c"] = np.ascontiguousarray(c[i * NSEQ:(i + 1) * NSEQ])
        in_maps.append(m)
    res = run_bass_kernel_spmd(nc, in_maps, core_ids=list(range(NCORES)))
    out = np.concatenate([np.asarray(r["y"], f).reshape(NSEQ, S, D) for r in res.results], axis=0)
    return out
```

```python
import numpy as np
import ml_dtypes
from contextlib import ExitStack
import concourse.bass as bass
import concourse.mybir as mybir
from concourse.bass_utils import run_bass_kernel_spmd

F32 = mybir.dt.float32
BF16 = mybir.dt.bfloat16
AF = mybir.ActivationFunctionType
ALU = mybir.AluOpType
AX = mybir.AxisListType

NCORES = 8
D = 1024
S = 2048
NSEQ = 4
NT = 16
G = 4
NG = NT // G
DFF = 4096
EPS = 1e-6
IN_COLS = 4104
NCHUNK = 26

ENGS = ("pe", "act", "dve", "pool", "sp")
import os
STRICT = bool(int(os.environ.get("KSTRICT", "0")))


class _Op:
    __slots__ = ("fn", "waits", "sig", "dma", "tok")


class Prog:
    def __init__(self):
        self.ops = {e: [] for e in ENGS}
        self.res = {}
        self.known = {e: {} for e in ENGS}
        self.clock = {}
        self.dma_count = {}
        self.needed = set()

    def _deps(self, eng, reads, writes):
        deps = set()
        for k in reads:
            r = self.res.get(k)
            if r is not None and r[0] is not None:
                deps.add(r[0])
        for k in writes:
            r = self.res.get(k)
            if r is not None:
                if r[0] is not None and (STRICT or r[0][0] != eng):
                    deps.add(r[0])
                for src, v in r[1].items():
                    if STRICT or src != eng:
                        deps.add((src, v))
        return deps

    def _commit(self, tok, reads, writes):
        for k in reads:
            r = self.res.get(k)
            if r is None:
                r = [None, {}]
                self.res[k] = r
            if r[1].get(tok[0], 0) < tok[1]:
                r[1][tok[0]] = tok[1]
        for k in writes:
            self.res[k] = [tok, {}]

    def op(self, eng, fn, reads=(), writes=(), dma=None):
        deps = self._deps(eng if dma is None else "dma:" + dma, reads, writes)
        kn = self.known[eng]
        waits = []
        best = {}
        for (src, v) in deps:
            if best.get(src, 0) < v:
                best[src] = v
        deps = set(best.items())
        for (src, v) in sorted(deps, key=lambda t: (str(t[0]), t[1])):
            if kn.get(src, 0) >= v:
                continue
            waits.append((src, v))
            self.needed.add((src, v))
            for s2, v2 in self.clock[(src, v)].items():
                if kn.get(s2, 0) < v2:
                    kn[s2] = v2
        o = _Op()
        o.fn = fn
        o.waits = waits
        o.dma = dma
        self.ops[eng].append(o)
        if dma is None:
            tok = (eng, len(self.ops[eng]))
        else:
            src = "dma:" + dma
            self.dma_count[src] = self.dma_count.get(src, 0) + 16
            tok = (src, self.dma_count[src])
        o.tok = tok
        ck = dict(kn)
        ck[tok[0]] = tok[1]
        self.clock[tok] = ck
        self._commit(tok, reads, writes)
        return tok

    def wait_all(self, eng, toks):
        kn = self.known[eng]
        waits = []
        for (src, v) in toks:
            if kn.get(src, 0) >= v:
                continue
            waits.append((src, v))
            self.needed.add((src, v))
            kn[src] = v
        o = _Op()
        o.fn = None
        o.waits = waits
        o.dma = None
        o.tok = None
        self.ops[eng].append(o)

    def emit(self, nc, es):
        sems = {}
        for e in ("pe", "act", "dve", "pool"):
            sems[e] = es.enter_context(nc.semaphore("sem_" + e))
        for src in self.dma_count:
            sems[src] = es.enter_context(nc.semaphore("sem_" + src.replace(":", "_")))
        sigval = {}
        for e in ("pe", "act", "dve", "pool"):
            cnt = 0
            for i, o in enumerate(self.ops[e]):
                o.sig = False
                if o.fn is not None and o.dma is None and (e, i + 1) in self.needed:
                    cnt += 1
                    o.sig = True
                    sigval[(e, i + 1)] = cnt
        blk = es.enter_context(nc.Block())

        def run(e, name):
            for o in self.ops[name]:
                for (src, v) in o.waits:
                    val = v if src.startswith("dma:") else sigval[(src, v)]
                    e.wait_ge(sems[src], val)
                if o.fn is None:
                    continue
                ins = o.fn(e)
                if o.dma is not None:
                    ins.then_inc(sems["dma:" + o.dma], 16)
                elif o.sig:
                    ins.then_inc(sems[name], 1)

        @blk.tensor
        def _(e):
            run(e, "pe")

        @blk.scalar
        def _(e):
            run(e, "act")

        @blk.vector
        def _(e):
            run(e, "dve")

        @blk.gpsimd
        def _(e):
            run(e, "pool")

        @blk.sync
        def _(e):
            run(e, "sp")


def _constants():
    f = np.float32
    n = np.arange(128, dtype=f)
    ident = np.eye(128, dtype=f)
    tri = (n[:, None] <= n[None, :]).astype(f)
    ones = np.ones((128, 128), f)
    h = np.arange(4, dtype=f)
    log_g = np.log(f(1.0) - f(2.0) ** (f(-5.0) - h)).astype(f)
    diff = n[None, :] - n[:, None]
    maskT = np.where(diff[None] >= 0, np.exp(np.maximum(diff, 0.0)[None] * log_g[:, None, None]), 0.0).astype(f)
    xi = np.exp((n[None, :] + 1.0) * log_g[:, None]).astype(f)
    xi_bc = np.ascontiguousarray(np.broadcast_to(xi[None], (128, 4, 128))).astype(f)
    ixi = np.exp(-(n[None, :] + 1.0) * log_g[:, None]).astype(f)
    ixi_bc = np.ascontiguousarray(np.broadcast_to(ixi[None], (128, 4, 128))).astype(f)
    zeta = np.exp((128 - 1.0 - n[None, :]) * log_g[:, None]).astype(f)
    zeta_t = np.ascontiguousarray(zeta.T)
    g_chunk = np.exp(f(128.0) * log_g).astype(f)
    pos = np.arange(S, dtype=f)
    inv_freq = (f(10000.0) ** (-np.arange(0, 128, 2, dtype=f) / f(128))).astype(f)
    ang = (pos[:, None] * inv_freq[None, :]).astype(f)
    cos = np.cos(ang).astype(f)
    sin = np.sin(ang).astype(f)
    ks = f(128.0 ** -0.5)

    def lay(a):
        return np.ascontiguousarray(a.reshape(16, 128, 64).transpose(1, 0, 2))

    rope = np.stack([lay(cos), lay(sin), lay(-sin), lay(cos * ks), lay(sin * ks), lay(-sin * ks)], 0)
    sel = np.zeros((4, 4, 128), f)
    for b in range(4):
        sel[b, b, :] = 1.0
    return dict(
        identf=ident, identb=ident.astype(ml_dtypes.bfloat16), trib=tri.astype(ml_dtypes.bfloat16),
        negm=((1.0 - tri) * -30000.0).astype(ml_dtypes.bfloat16),
        trif=tri, onesf=ones, ixi_bc=ixi_bc, xi_bc=xi_bc, zeta_t=zeta_t,
        rope=np.ascontiguousarray(rope), g_chunk=g_chunk,
    )


_CONST = None


def build_program():
    consts = _constants()
    g_chunk = [float(v) for v in consts["g_chunk"]]
    nc = bass.Bass("TRN2", target_bir_lowering=False)
    P = Prog()
    es = ExitStack()

    def din(name, shape, dt=F32):
        return nc.dram_tensor(name, list(shape), dt, kind="ExternalInput").ap()

    x_d = din("x", [NSEQ * S, D])
    c_d = din("c", [NSEQ, D])
    wada_d = din("w_ada", [D, 6 * D])
    bada_d = din("b_ada", [1, 6 * D])
    win_d = din("w_in", [D, IN_COLS])
    bfg_d = din("b_forget", [1, 8])
    qg_d = din("q_gain", [1, 64])
    kg_d = din("k_gain", [1, 64])
    fxg_d = din("fox_gain", [1, 512])
    rtg_d = din("ret_gain", [1, 512])
    wout_d = din("w_out", [D, D])
    w1_d = din("w1", [D, DFF])
    w2_d = din("w2", [DFF, D])
    identf_d = din("identf", [128, 128])
    identb_d = din("identb", [128, 128], BF16)
    trib_d = din("trib", [128, 128], BF16)
    negm_d = din("negm", [128, 128], BF16)
    trif_d = din("trif", [128, 128])
    onesf_d = din("onesf", [128, 128])
    ixibc_d = din("ixi_bc", [128, 4, 128])
    xibc_d = din("xi_bc", [128, 4, 128])
    zeta_d = din("zeta_t", [128, 4])
    rope_d = din("rope", [6, 128, 16, 64])
    y_d = nc.dram_tensor("y", [NSEQ * S, D], F32, kind="ExternalOutput").ap()
    wbf_d = nc.dram_tensor("wbf", [NCHUNK, 128, 4096], BF16, kind="Internal").ap()
    gates_d = nc.dram_tensor("gates_scr", [4, 2048], F32, kind="Internal").ap()

    def sb(name, shape, dt=F32):
        return es.enter_context(nc.sbuf_tensor(name, list(shape), dt))

    wbuf = [sb(f"wbuf{i}", [128, 4096], BF16) for i in range(3)]
    xs = sb("xs", [128, G, D])
    hTA = sb("hTA", [128, 8, 512], BF16)
    hTB = sb("hTB", [128, 8, 512], BF16)
    U = sb("U", [128, 16384], BF16)
    KT = sb("KT", [70, 8, S], BF16)
    Vaug = sb("Vaug", [128, NT, 8, 65], BF16)
    qaug = [sb(f"qaug{i}", [128, 8, 70], BF16) for i in range(3)]
    kaug = [sb(f"kaug{i}", [128, 8, 70], BF16) for i in range(3)]
    state = sb("state", [128, 4, 128])
    state_bf = sb("state_bf", [128, 4, 128], BF16)
    MG = sb("MG", [128, 8192], BF16)
    mixed = MG[:, 0:4096].rearrange("p (t d) -> p t d", d=1024)
    gates_f = MG[:, 4096:6144].rearrange("p (t d) -> p t d", d=512)
    gates_r = MG[:, 6144:8192].rearrange("p (t d) -> p t d", d=512)
    xnext = MG[:, :].bitcast(F32).rearrange("p (t d) -> p t d", d=1024)
    XNK = [[("mixed", 0), ("mixed", 1)], [("mixed", 2), ("mixed", 3)],
           [("gates_f", t) for t in range(G)], [("gates_r", t) for t in range(G)]]
    PT = [sb(f"PT{i}", [128, 512], BF16) for i in range(3)]
    STr = [sb(f"STr{i}", [128, 4, 128], BF16) for i in range(2)]
    TA = [sb(f"TA{i}", [128, 512]) for i in range(2)]
    TB = [sb(f"TB{i}", [128, 512]) for i in range(2)]
    TE = [sb(f"TE{i}", [128, 4, 64]) for i in range(4)]
    TR = sb("TR", [128, 512])
    ropeg = sb("ropeg", [128, 6, G, 64])
    gm_bc = sb("gm_bc", [128, D])
    gf_bc = sb("gf_bc", [128, D])
    opm_m = sb("opm_m", [128, 8, 4])
    sh_m = sb("sh_m", [128, 8, 4])
    opm_f = sb("opm_f", [128, 8, 4])
    sh_f = sb("sh_f", [128, 8, 4])
    identf = sb("identf_s", [128, 128])
    identb = sb("identb_s", [128, 128], BF16)
    trib = sb("trib_s", [128, 128], BF16)
    negm = sb("negm_s", [128, 128], BF16)
    trif = sb("trif_s", [128, 128])
    onesf = sb("onesf_s", [128, 128])
    ixibc = sb("ixibc_s", [128, 4, 128])
    xibc = sb("xibc_s", [128, 4, 128])
    zeta = sb("zeta_s", [128, 4])
    qg_col = sb("qg_col", [70, 1])
    kg_col = sb("kg_col", [70, 1])
    fxg_bc = sb("fxg_bc", [128, 512])
    rtg_bc = sb("rtg_bc", [128, 512])
    bfg_bc = sb("bfg_bc", [128, 8])
    wfg = sb("wfg", [128, 8, 8], BF16)
    wfg32 = sb("wfg32", [128, 8, 8])
    epsc = sb("epsc", [128, 1])
    st = sb("st", [128, 128])
    rs_run = sb("rs_run", [128, 8])
    fz = sb("fz", [128, 3, 32])
    cr = sb("cr", [128, 2, 32])
    pre = sb("pre", [128, G, 8])
    cumsp = sb("cumsp", [128, G, 8, 3], BF16)
    cactT = sb("cactT", [128, 8, 4])
    ctmp = sb("ctmp", [128, 8, 4])
    ones14 = sb("ones14", [1, 4])

    c4 = xs[0:4, 0, :]
    badar = [TB[0][0:1, :], TB[1][0:1, :]]
    grow = xs[0:4, 1:3, :].rearrange("p t d -> p (t d)")
    PS = [es.enter_context(nc.psum_tensor(f"P{i}", [128, 512], F32)) for i in range(8)]

    def UK(lo, hi):
        return [("U", i) for i in range(lo, hi)]

    def u_chunk(fc):
        return U[:, fc * 512:(fc + 1) * 512]

    def u_f32(lo_gran, n_gran):
        return U[:, lo_gran * 512:(lo_gran + n_gran) * 512].bitcast(F32)

    QT_v = U[0:70, 0:4096].rearrange("p (h n) -> p h n", n=512)
    QTr_v = U[:, 4096:6144].rearrange("p (h n) -> p h n", n=512)
    QxT_v = U[:, 6144:8192].rearrange("p (h n) -> p h n", n=512)
    KTr_v = U[:, 8192:10240].rearrange("p (h n) -> p h n", n=512)
    Kz_v = U[:, 10240:12288].rearrange("p (t n) -> p t n", n=512)
    Vr_v = U[:, 12288:14336].rearrange("p (t n) -> p t n", n=512)
    rqt_v = [U[:, 14336:14848], U[:, 14848:15360], U[:, 6144:6656]]
    rkt_v = [U[:, 15360:15872], U[:, 15872:16384], U[:, 6656:7168]]
    rqt_g = [28, 29, 12]
    rkt_g = [30, 31, 13]

    def PK(i):
        return [("P", i)]

    def Pb(i):
        return PS[i][:, :].bitcast(BF16)

    def dma(out, in_, sem, reads=(), writes=(), eng="sp"):
        return P.op(eng, lambda e, o=out, i=in_: e.dma_start(out=o, in_=i), reads=reads, writes=writes, dma=sem)

    dma(identf[:, :], identf_d, "c0", writes=["identf"])
    dma(identb[:, :], identb_d, "c0", writes=["identb"])
    dma(trib[:, :], trib_d, "c0", writes=["trib"])
    dma(negm[:, :], negm_d, "c0", writes=["negm"])
    dma(trif[:, :], trif_d, "c0", writes=["trif"])
    dma(onesf[:, :], onesf_d, "c0", writes=["onesf"])
    dma(ixibc[:, :, :], ixibc_d, "c0", writes=["ixibc"])
    dma(xibc[:, :, :], xibc_d, "c0", writes=["xibc"])
    dma(zeta[:, :], zeta_d, "c0", writes=["zeta"])
    dma(qg_col[0:64, :], qg_d.rearrange("o d -> d o"), "c0", writes=["qg"])
    dma(kg_col[0:64, :], kg_d.rearrange("o d -> d o"), "c0", writes=["kg"])
    dma(fxg_bc[:, :], fxg_d.partition_broadcast(128), "c0", writes=["fxg"])
    dma(rtg_bc[:, :], rtg_d.partition_broadcast(128), "c0", writes=["rtg"])
    dma(bfg_bc[:, :], bfg_d.partition_broadcast(128), "c0", writes=["bfg"])
    dma(c4, c_d, "c0", writes=["c4", ("xs", 0)])
    dma(wfg32[:, :, :], win_d[:, 2048:2056].rearrange("(c p) n -> p c n", p=128), "c0", writes=["wfg32"])
    c0_total = P.dma_count["dma:c0"]
    for k in ["identf", "identb", "trib", "negm", "trif", "onesf", "ixibc", "xibc", "zeta", "qg", "kg", "fxg",
              "rtg", "bfg", "c4", "wfg32"]:
        P.res[k][0] = ("dma:c0", c0_total)
    P.clock[("dma:c0", c0_total)] = {"dma:c0": c0_total}

    P.op("pool", lambda e: e.memset(epsc[:, :], EPS), writes=["epsc"])
    P.op("pool", lambda e: e.memset(ones14[:, :], 1.0), writes=["ones14"])
    P.op("pool", lambda e: e.memset(Vaug[:, :, :, 64:65], 1.0), writes=["Vaug_ones"])
    for i in range(3):
        P.op("pool", lambda e, i=i: e.memset(qaug[i][:, :, 67:70], 1.0), writes=[("qaug", i)])
        P.op("pool", lambda e, i=i: e.memset(kaug[i][:, :, 64:67], 1.0), writes=[("kaug", i)])
    P.op("dve", lambda e: e.tensor_copy(out=wfg[:, :, :], in_=wfg32[:, :, :]), reads=["wfg32"], writes=["wfg"])
    P.op("pool", lambda e: e.memset(qg_col[64:70, :], 1.0), writes=["qg1"])
    P.op("pool", lambda e: e.memset(kg_col[64:70, :], 1.0), writes=["kg1"])
    P.op("dve", lambda e: e.tensor_scalar(out=qg_col[0:64, :], in0=qg_col[0:64, :], scalar1=0.125, scalar2=None,
                                          op0=ALU.mult), reads=["qg"], writes=["qg"])

    def chunk_src(k):
        if k < 8:
            col0 = [0, 512, 1024, 1536, 2056, 2568, 3080, 3592][k]
            return win_d[:, col0:col0 + 512].rearrange("(c p) n -> p c n", p=128)
        if k < 10:
            q = k - 8
            return wout_d[:, q * 512:(q + 1) * 512].rearrange("(c p) n -> p c n", p=128)
        if k < 18:
            j = k - 10
            return w1_d[:, j * 512:(j + 1) * 512].rearrange("(c p) n -> p c n", p=128)
        j = k - 18
        return w2_d[j * 512:(j + 1) * 512, :].rearrange("(c p) n -> p c n", p=128)

    CAST_ORDER = [3, 7, 0, 1, 2, 4, 5, 6] + list(range(8, NCHUNK))

    def cast_chunk(k, extra_reads=()):
        src_ap = chunk_src(k)
        dst = wbf_d[k].rearrange("p (c n) -> p c n", c=src_ap.shape[1])
        dma(dst, src_ap, f"cst{k}", reads=list(extra_reads), writes=[("wbf", k)], eng="pool")

    for k in CAST_ORDER[:5]:
        cast_chunk(k)

    c4k = [("xs", 0)]
    growk = [("xs", 1), ("xs", 2)]
    mrow = xs[0:4, 3, 0:512]
    mrowk = [("xs", 3)]
    for cc in range(8):
        P.op("pe", lambda e, cc=cc: e.transpose(out=PS[0][:, cc * 4:(cc + 1) * 4], in_=c4[:, cc * 128:(cc + 1) * 128],
                                               identity=identf[0:4, 0:4]),
             reads=["c4", "identf"] + c4k, writes=PK(0))
    P.op("act", lambda e: e.activation(out=ctmp[:, :, :], in_=PS[0][:, 0:32].rearrange("p (c b) -> p c b", b=4),
                                       func=AF.Exp, scale=-1.0), reads=PK(0), writes=["ctmp"])
    P.op("dve", lambda e: e.tensor_scalar(out=ctmp[:, :, :], in0=ctmp[:, :, :], scalar1=1.0, scalar2=None, op0=ALU.add),
         reads=["ctmp"], writes=["ctmp"])
    P.op("dve", lambda e: e.reciprocal(out=ctmp[:, :, :], in_=ctmp[:, :, :]), reads=["ctmp"], writes=["ctmp"])
    P.op("dve", lambda e: e.tensor_tensor(out=cactT[:, :, :], in0=ctmp[:, :, :],
                                          in1=PS[0][:, 0:32].rearrange("p (c b) -> p c b", b=4), op=ALU.mult),
         reads=["ctmp"] + PK(0), writes=["cactT"])

    modT_dst = {0: (sh_m, False), 1: (opm_m, True), 3: (sh_f, False), 4: (opm_f, True)}
    for kb in range(12):
        s = kb % 2
        v = kb // 2
        half = kb % 2
        stg = u_f32(16 * s, 16).rearrange("p (c n) -> p c n", c=8)
        dma(stg, wada_d[:, kb * 512:(kb + 1) * 512].rearrange("(c p) n -> p c n", p=128), f"stg{s}",
            writes=UK(16 * s, 16 * s + 16) + [("wada_blk", kb)])
        rk = UK(16 * s, 16 * s + 16)
        bd = badar[kb % 2]
        bdk = ("TB", kb % 2)
        dma(bd, bada_d[0:1, kb * 512:(kb + 1) * 512], f"bd{kb % 2}", writes=[bdk])
        bank = 3 + (kb % 2)
        for dc in range(8):
            P.op("pe", lambda e, bank=bank, dc=dc, stg=stg: e.matmul(
                PS[bank][0:4, :], lhsT=cactT[:, dc, :], rhs=stg[:, dc, :], start=(dc == 0), stop=False),
                reads=rk + ["cactT"], writes=PK(bank))
        P.op("pe", lambda e, bank=bank, bd=bd: e.matmul(
            PS[bank][0:4, :], lhsT=ones14[0:1, :], rhs=bd[0:1, :],
            start=False, stop=True), reads=[bdk, "ones14"], writes=PK(bank))
        if v in modT_dst:
            dst, plus1 = modT_dst[v]
            P.op("dve", lambda e, bank=bank: e.tensor_copy(out=mrow, in_=PS[bank][0:4, :]), reads=PK(bank), writes=mrowk)
            tb_ = 1 + (kb % 2)
            for ec in range(4):
                P.op("pe", lambda e, tb_=tb_, ec=ec: e.transpose(out=PS[tb_][:, ec * 4:(ec + 1) * 4],
                                                                 in_=mrow[:, ec * 128:(ec + 1) * 128],
                                                                 identity=identf[0:4, 0:4]),
                     reads=mrowk + ["identf"], writes=PK(tb_))
            src_v = PS[tb_][:, 0:16].rearrange("p (c b) -> p c b", b=4)
            dst_v = dst[:, half * 4:(half + 1) * 4, :]
            if plus1:
                P.op("dve", lambda e, o=dst_v, i=src_v: e.tensor_scalar(out=o, in0=i, scalar1=1.0, scalar2=None,
                                                                        op0=ALU.add),
                     reads=PK(tb_), writes=[("modT", v, half)])
            else:
                P.op("dve", lambda e, o=dst_v, i=src_v: e.tensor_copy(out=o, in_=i),
                     reads=PK(tb_), writes=[("modT", v, half)])
        else:
            gi = 0 if v == 2 else 1
            P.op("dve", lambda e, bank=bank, gi=gi, half=half: e.tensor_copy(
                out=grow[:, gi * 1024 + half * 512: gi * 1024 + (half + 1) * 512], in_=PS[bank][0:4, :]),
                reads=PK(bank), writes=growk)
    dma(gates_d, grow, "gsc", reads=growk, writes=["gates_d"])
    for k in CAST_ORDER[5:]:
        cast_chunk(k, extra_reads=[("wada_blk", 10), ("wada_blk", 11)])

    from collections import deque
    GROUP_ORDER = [3, 7, 0, 1, 2, 4, 5, 6] + list(range(8, NCHUNK))
    stream = [k for _ in range(NSEQ * NG) for k in GROUP_ORDER]
    pf = {"next": 0, "slots": {}, "cons": 0}

    def prefetch_upto(n):
        while pf["next"] < min(n, len(stream)):
            i = pf["next"]
            slot = i % 3
            dma(wbuf[slot][:, :], wbf_d[stream[i]], f"wld{slot}", reads=[("wbf", stream[i])], writes=[("wbuf", slot)])
            pf["slots"][i] = slot
            pf["next"] += 1

    def next_chunk(expect):
        i = pf["cons"]
        assert stream[i] == expect, (stream[i], expect)
        prefetch_upto(i + 2)
        pf["cons"] += 1
        return pf["slots"][i], i

    def after_chunk(i):
        prefetch_upto(i + 3)

    rr = {"proj": 0, "tp": 0, "tq": 0, "qa": 0, "ka": 0, "mi": 0, "tmp": 0, "rq": 0, "rk": 0, "qkp": 0}
    store_toks = []
    tmps = [(TA[0], ("TA", 0)), (TB[0], ("TB", 0)), (TA[1], ("TA", 1)), (TB[1], ("TB", 1))]

    def next_tmp():
        r = tmps[rr["tmp"] % 4]
        rr["tmp"] += 1
        return r

    def bc3(ap2, n):
        return ap2.unsqueeze(2).to_broadcast([128, ap2.shape[1], n])

    def bcmid(ap2, m):
        return ap2.unsqueeze(1).to_broadcast([128, m, ap2.shape[1]])

    def act_rstd(c_in, c_out, n, inv):
        P.op("act", lambda e: e.activation(out=st[:, c_out:c_out + n], in_=st[:, c_in:c_in + n], func=AF.Ln,
                                           scale=inv, bias=epsc[:, 0:1]),
             reads=[("st", c_in), "epsc"], writes=[("st", c_out)])
        P.op("act", lambda e: e.activation(out=st[:, c_out:c_out + n], in_=st[:, c_out:c_out + n], func=AF.Exp,
                                           scale=-0.5),
             reads=[("st", c_out)], writes=[("st", c_out)])

    def act_copy(out, in_, reads, writes):
        P.op("act", lambda e: e.activation(out=out, in_=in_, func=AF.Copy), reads=reads, writes=writes)

    def dve_copy(out, in_, reads, writes):
        P.op("dve", lambda e: e.tensor_copy(out=out, in_=in_), reads=reads, writes=writes)

    HB = [hTA, hTB]
    HK = ["hTA", "hTB"]

    def rms_chain(b_, src_t, src_keys, xn, xn_keys, hT, hkey, opm, shf, vs, vh, banks):
        junk = hT[:, 0:2, :].rearrange("p a n -> p (a n)")
        jk = [(hkey, 0), (hkey, 1)]

        def mk_sq(t):
            def f():
                P.op("act", lambda e: e.activation(out=junk, in_=src_t[:, t, :], func=AF.Square,
                                                   accum_out=st[:, t:t + 1]),
                     reads=src_keys[t], writes=jk + [("st", 0)])
            return f

        def norm():
            act_rstd(0, 8, 4, 1.0 / D)
            for t in range(G):
                P.op("dve", lambda e, t=t: e.tensor_scalar(out=xn[:, t, :], in0=src_t[:, t, :], scalar1=st[:, 8 + t:9 + t],
                                                           scalar2=None, op0=ALU.mult),
                     reads=src_keys[t] + [("st", 8)], writes=xn_keys[t])

        def mk(c):
            def stage():
                bank = banks[c % len(banks)]
                for t in range(G):
                    P.op("pe", lambda e, t=t: e.transpose(
                        out=PS[bank][:, t * 128:(t + 1) * 128], in_=xn[:, t, c * 128:(c + 1) * 128],
                        identity=identf[:, :]),
                        reads=xn_keys[t] + ["identf"], writes=PK(bank))
                if c % 2 == 0:
                    P.op("act", lambda e: e.activation(
                        out=hT[:, c, :], in_=PS[bank][:, :], func=AF.Identity,
                        scale=opm[:, c, b_:b_ + 1], bias=shf[:, c, b_:b_ + 1]),
                        reads=PK(bank) + [("modT", vs, c // 4), ("modT", vh, c // 4)], writes=[(hkey, c)])
                else:
                    P.op("dve", lambda e: e.tensor_scalar(
                        out=hT[:, c, :], in0=PS[bank][:, :], scalar1=opm[:, c, b_:b_ + 1],
                        scalar2=shf[:, c, b_:b_ + 1], op0=ALU.mult, op1=ALU.add),
                        reads=PK(bank) + [("modT", vs, c // 4), ("modT", vh, c // 4)], writes=[(hkey, c)])
            return stage
        return [mk_sq(t) for t in range(G)] + [norm] + [mk(c) for c in range(8)]

    def prefetch_B(gi_n):
        b_n, g_n = gi_n // NG, gi_n % NG
        r0 = b_n * S + g_n * 512
        dma(xnext[:, :, :], x_d[r0:r0 + 512, :].rearrange("(t p) d -> p t d", p=128), "xnl",
            writes=[k for ks in XNK for k in ks])
        return rms_chain(b_n, xnext, XNK, xnext, XNK, HB[gi_n % 2], HK[gi_n % 2], opm_m, sh_m, 1, 0, [4, 5, 6, 7])

    def proj_mm(src, t, slot, bank, key):
        for c in range(8):
            P.op("pe", lambda e, c=c: e.matmul(
                PS[bank][:, :], lhsT=src[:, c, t * 128:(t + 1) * 128],
                rhs=wbuf[slot][:, c * 512:(c + 1) * 512], start=(c == 0), stop=(c == 7)),
                reads=[(key, c), ("wbuf", slot)], writes=PK(bank))

    def next_proj_bank():
        bk = 2 + rr["proj"] % 3
        rr["proj"] += 1
        return bk

    for b in range(NSEQ):
        dma(gm_bc[:, :], gates_d[b:b + 1, 0:1024].partition_broadcast(128), "gbm", reads=["gates_d"], writes=["gm_bc"])
        dma(gf_bc[:, :], gates_d[b:b + 1, 1024:2048].partition_broadcast(128), "gbf", reads=["gates_d"], writes=["gf_bc"])
        P.op("pool", lambda e: e.memset(rs_run[:, :], 0.0), writes=["rs_run"])
        P.op("pool", lambda e: e.memset(state[:, :, :], 0.0), writes=["state"])
        P.op("pool", lambda e: e.memset(state_bf[:, :, :], 0.0), writes=["state_bf"])

        for g in range(NG):
            row0 = b * S + g * 512
            dma(xs[:, :, :], x_d[row0:row0 + 512, :].rearrange("(t p) d -> p t d", p=128), "xld",
                writes=[("xs", t) for t in range(G)])
            dma(ropeg[:, :, :, :], rope_d[:, :, g * G:(g + 1) * G, :].rearrange("r p t i -> p r t i"), "rope",
                writes=["ropeg"])

            gi = b * NG + g
            if gi == 0:
                for stg_ in prefetch_B(0):
                    stg_()
            hT_cur, hkey = HB[gi % 2], HK[gi % 2]
            hT_oth, okey = HB[(gi + 1) % 2], HK[(gi + 1) % 2]

            first = True
            for t in range(G):
                for c in range(8):
                    P.op("pe", lambda e, c=c, t=t, hT_cur=hT_cur, first=first: e.matmul(
                        PS[7][:, t * 8:(t + 1) * 8], lhsT=hT_cur[:, c, t * 128:(t + 1) * 128],
                        rhs=wfg[:, c, :], start=first, stop=(c == 7), skip_group_check=True),
                        reads=[(hkey, c), "wfg"], writes=PK(7))
                    first = False
            P.op("dve", lambda e: e.tensor_tensor(out=fz[:, 0, :].rearrange("p (t h) -> p t h", h=8),
                                                  in0=PS[7][:, 0:32].rearrange("p (t h) -> p t h", h=8),
                                                  in1=bcmid(bfg_bc[:, :], G), op=ALU.add),
                 reads=PK(7) + ["bfg"], writes=[("fz", 0)])
            P.op("act", lambda e: e.activation(out=fz[:, 1, :], in_=fz[:, 0, :], func=AF.Exp, scale=-1.0),
                 reads=[("fz", 0)], writes=[("fz", 1)])
            P.op("act", lambda e: e.activation(out=fz[:, 2, :], in_=fz[:, 1, :], func=AF.Ln, bias=1.0),
                 reads=[("fz", 1)], writes=[("fz", 2)])
            lall = fz[:, 2, :].rearrange("p (t h) -> p t h", h=8)
            P.op("dve", lambda e: e.tensor_copy(out=pre[:, 0, :], in_=rs_run[:, :]), reads=["rs_run"], writes=["pre"])
            for t in range(1, G):
                P.op("dve", lambda e, t=t: e.tensor_tensor(out=pre[:, t, :], in0=pre[:, t - 1, :], in1=lall[:, t - 1, :],
                                                           op=ALU.add), reads=["pre", ("fz", 2)], writes=["pre"])
            P.op("dve", lambda e: e.tensor_tensor(out=rs_run[:, :], in0=pre[:, G - 1, :], in1=lall[:, G - 1, :], op=ALU.add),
                 reads=["pre", ("fz", 2)], writes=["rs_run"])

            def cum_finish():
                P.op("pe", lambda e: e.matmul(PS[7][:, 32:64], lhsT=trif[:, :], rhs=fz[:, 2, :], start=True, stop=False),
                     reads=[("fz", 2), "trif"], writes=PK(7))
                P.op("pe", lambda e: e.matmul(PS[7][:, 32:64], lhsT=onesf[:, :], rhs=pre[:, :, :].rearrange("p t h -> p (t h)"),
                                              start=False, stop=True), reads=["pre", "onesf"], writes=PK(7))
                ncum = PS[7][:, 32:64].rearrange("p (t h) -> p t h", h=8)
                ck = [("cumsp", t) for t in range(G)]
                cr0 = cr[:, 0, :].rearrange("p (t h) -> p t h", h=8)
                cr1 = cr[:, 1, :].rearrange("p (t h) -> p t h", h=8)
                P.op("dve", lambda e: e.tensor_copy(out=cumsp[:, :, :, 0], in_=ncum), reads=PK(7), writes=ck)
                P.op("dve", lambda e: e.tensor_tensor(out=cr0, in0=ncum, in1=cumsp[:, :, :, 0], op=ALU.subtract),
                     reads=PK(7) + ck, writes=[("cr", 0)])
                P.op("dve", lambda e: e.tensor_copy(out=cumsp[:, :, :, 1], in_=cr0), reads=[("cr", 0)], writes=ck)
                P.op("dve", lambda e: e.tensor_tensor(out=cr1, in0=cr0, in1=cumsp[:, :, :, 1], op=ALU.subtract),
                     reads=[("cr", 0)] + ck, writes=[("cr", 1)])
                P.op("dve", lambda e: e.tensor_copy(out=cumsp[:, :, :, 2], in_=cr1), reads=[("cr", 1)], writes=ck)

            pipe = []

            def pipe_tick():
                keep = []
                for it in list(pipe):
                    it[0] += 1
                    a = it[0]
                    if a - 1 < len(it[1]) and it[1][a - 1] is not None:
                        it[1][a - 1]()
                    if a < len(it[1]):
                        keep.append(it)
                pipe[:] = keep

            def pipe_push(stages):
                pipe_tick()
                pipe.append([0, stages])

            def push_deferred(fn):
                pipe_push([None, fn])

            def flush_deferred():
                while pipe:
                    pipe_tick()

            slot, ci = next_chunk(3)
            for t in range(G):
                bank = next_proj_bank()
                proj_mm(hT_cur, t, slot, bank, hkey)
                tmp, tkey = next_tmp()
                P.op("act", lambda e, tmp=tmp, bank=bank: e.activation(out=tmp[:, :], in_=PS[bank][:, :], func=AF.Sigmoid),
                     reads=PK(bank), writes=[tkey])
                P.op("pool", lambda e, t=t, tmp=tmp: e.tensor_tensor(out=gates_f[:, t, :], in0=tmp[:, :], in1=fxg_bc[:, :],
                                                                     op=ALU.mult),
                     reads=[tkey, "fxg"], writes=[("gates_f", t)])
            after_chunk(ci)
            slot, ci = next_chunk(7)
            for t in range(G):
                bank = next_proj_bank()
                proj_mm(hT_cur, t, slot, bank, hkey)
                tmp, tkey = next_tmp()
                tmp2, tkey2 = next_tmp()
                P.op("act", lambda e, tmp=tmp, bank=bank: e.activation(out=tmp[:, :], in_=PS[bank][:, :], func=AF.Sigmoid),
                     reads=PK(bank), writes=[tkey])
                P.op("dve", lambda e, tmp=tmp, tmp2=tmp2, bank=bank: e.tensor_tensor(out=tmp2[:, :], in0=PS[bank][:, :],
                                                                                    in1=tmp[:, :], op=ALU.mult),
                     reads=[tkey] + PK(bank), writes=[tkey2])
                P.op("pool", lambda e, t=t, tmp2=tmp2: e.tensor_tensor(out=gates_r[:, t, :], in0=tmp2[:, :], in1=rtg_bc[:, :],
                                                                       op=ALU.mult),
                     reads=[tkey2, "rtg"], writes=[("gates_r", t)])
            after_chunk(ci)

            cum_finish()

            def qk_step(t, slot, is_q):
                bank = next_proj_bank()
                proj_mm(hT_cur, t, slot, bank, hkey)
                if is_q:
                    par = rr["qa"] % 3
                    rr["qa"] += 1
                    aug, akey, gcol, gkeys = qaug[par], ("qaug", par), qg_col, ["qg", "qg1"]
                else:
                    par = rr["ka"] % 3
                    rr["ka"] += 1
                    aug, akey, gcol, gkeys = kaug[par], ("kaug", par), kg_col, ["kg", "kg1"]
                tmp, tkey = next_tmp()
                par2 = rr["qkp"] % 2
                rr["qkp"] += 1
                cs, cr_ = 64 + 16 * par2, 72 + 16 * par2
                P.op("act", lambda e: e.activation(out=tmp[:, :], in_=PS[bank][:, :], func=AF.Square),
                     reads=PK(bank), writes=[tkey])
                P.op("dve", lambda e: e.tensor_reduce(out=st[:, cs:cs + 8], in_=tmp[:, :].rearrange("p (h i) -> p h i", i=64),
                                                      axis=AX.X, op=ALU.add), reads=[tkey], writes=[("st", cs)])

                def stage_b():
                    act_rstd(cs, cr_, 8, 1.0 / 64)
                    P.op("dve", lambda e: e.tensor_tensor(out=aug[:, :, 0:64],
                                                          in0=PS[bank][:, :].rearrange("p (h i) -> p h i", i=64),
                                                          in1=bc3(st[:, cr_:cr_ + 8], 64), op=ALU.mult),
                         reads=PK(bank) + [("st", cr_)], writes=[akey])
                    if is_q:
                        P.op("pool", lambda e: e.tensor_scalar(out=aug[:, :, 64:67], in0=cumsp[:, t, :, :], scalar1=-1.0,
                                                               scalar2=None, op0=ALU.mult),
                             reads=[("cumsp", t)], writes=[akey])
                    else:
                        P.op("pool", lambda e: e.tensor_copy(out=aug[:, :, 67:70], in_=cumsp[:, t, :, :]),
                             reads=[("cumsp", t)], writes=[akey])

                def deferred():
                    bq = 5 + rr["tq"] % 2
                    rr["tq"] += 1
                    for h in range(8):
                        P.op("pe", lambda e, h=h: e.transpose(out=Pb(bq)[0:70, h * 128:(h + 1) * 128], in_=aug[:, h, :],
                                                              identity=identb[:, :]),
                             reads=[akey, "identb"], writes=PK(bq))
                    srcv = Pb(bq)[0:70, :].rearrange("p (h n) -> p h n", n=128)
                    if is_q:
                        dst, wk = QT_v[:, :, t * 128:(t + 1) * 128], UK(0, 8)
                    else:
                        blk_i = g * G + t
                        dst, wk = KT[:, :, blk_i * 128:(blk_i + 1) * 128], [("KT", blk_i)]
                    P.op("dve", lambda e: e.tensor_scalar(out=dst, in0=srcv, scalar1=gcol[:, 0:1], scalar2=None,
                                                          op0=ALU.mult),
                         reads=PK(bq) + gkeys, writes=wk)
                pipe_push([stage_b, deferred])

            for is_q, ck_ in ((True, 0), (False, 1)):
                slot, ci = next_chunk(ck_)
                for t in range(G):
                    qk_step(t, slot, is_q)
                after_chunk(ci)

            slot, ci = next_chunk(2)
            for t in range(G):
                bank = next_proj_bank()
                proj_mm(hT_cur, t, slot, bank, hkey)
                blk_i = g * G + t
                act_copy(Vaug[:, blk_i, :, 0:64], PS[bank][:, :].rearrange("p (h i) -> p h i", i=64), PK(bank),
                         [("Vaug", blk_i)])
                pipe_push([])
            after_chunk(ci)
            flush_deferred()

            def side_bank():
                bk = (5, 7)[rr["proj"] % 2]
                rr["proj"] += 1
                return bk

            def rope_step(t, slot, is_q):
                bank = side_bank()
                proj_mm(hT_cur, t, slot, bank, hkey)
                r0 = 0 if is_q else 3
                cosv, sinv, nsinv = ropeg[:, r0, t, :], ropeg[:, r0 + 1, t, :], ropeg[:, r0 + 2, t, :]
                ta, tak = next_tmp()
                tb, tbk = next_tmp()
                pv = PS[bank][:, :].rearrange("p (h w i) -> p h w i", h=4, w=2)
                ta4 = ta[:, :].rearrange("p (h w i) -> p h w i", h=4, w=2)
                tb4 = tb[:, :].rearrange("p (h w i) -> p h w i", h=4, w=2)
                cos4 = cosv.unsqueeze(1).unsqueeze(1).to_broadcast([128, 4, 2, 64])
                P.op("dve", lambda e: e.tensor_tensor(out=ta4, in0=pv, in1=cos4, op=ALU.mult),
                     reads=PK(bank) + ["ropeg"], writes=[tak])
                P.op("dve", lambda e: e.tensor_tensor(out=tb4[:, :, 0, :], in0=pv[:, :, 1, :], in1=bcmid(nsinv, 4),
                                                      op=ALU.mult), reads=PK(bank) + ["ropeg"], writes=[tbk])
                P.op("dve", lambda e: e.tensor_tensor(out=tb4[:, :, 1, :], in0=pv[:, :, 0, :], in1=bcmid(sinv, 4),
                                                      op=ALU.mult), reads=PK(bank) + ["ropeg"], writes=[tbk])
                if is_q:
                    i = rr["rq"] % 3
                    rr["rq"] += 1
                    rt, rtk = rqt_v[i], ("U", rqt_g[i])
                else:
                    i = rr["rk"] % 3
                    rr["rk"] += 1
                    rt, rtk = rkt_v[i], ("U", rkt_g[i])
                P.op("pool", lambda e: e.tensor_tensor(out=rt, in0=ta[:, :], in1=tb[:, :], op=ALU.add),
                     reads=[tak, tbk], writes=[rtk])
                if not is_q:
                    P.op("pool", lambda e: e.tensor_tensor(out=Kz_v[:, t, :].rearrange("p (h i) -> p h i", i=128),
                                                           in0=rt.rearrange("p (h i) -> p h i", i=128),
                                                           in1=bc3(zeta[:, :], 128), op=ALU.mult),
                         reads=[rtk, "zeta"], writes=[("U", 20 + t)])

                def deferred():
                    bq = 6
                    for h in range(4):
                        P.op("pe", lambda e, h=h: e.transpose(out=Pb(bq)[:, h * 128:(h + 1) * 128],
                                                              in_=rt[:, h * 128:(h + 1) * 128], identity=identb[:, :]),
                             reads=[rtk, "identb"], writes=PK(bq))
                    srcv = Pb(bq)[:, 0:512].rearrange("p (h n) -> p h n", n=128)
                    tc_ = slice(t * 128, (t + 1) * 128)
                    if is_q:
                        P.op("dve", lambda e: e.tensor_tensor(out=QTr_v[:, :, tc_], in0=srcv, in1=xibc[:, :, :], op=ALU.mult),
                             reads=PK(bq) + ["xibc"], writes=UK(8, 12))
                    else:
                        P.op("dve", lambda e: e.tensor_tensor(out=KTr_v[:, :, tc_], in0=srcv, in1=ixibc[:, :, :], op=ALU.mult),
                             reads=PK(bq) + ["ixibc"], writes=UK(16, 20))
                return deferred

            def rv_step(t, slot):
                bank = side_bank()
                proj_mm(hT_cur, t, slot, bank, hkey)
                dve_copy(Vr_v[:, t, :], PS[bank][:, :], PK(bank), [("U", 24 + t)])
                return lambda: None

            chunk_state = {}

            def side_steps():
                units = []
                for kind, ck_ in (("rq", 4), ("rk", 5), ("rv", 6)):
                    for t in range(G):
                        units.append((kind, ck_, t))
                return units

            units = side_steps()

            def run_unit(u):
                kind, ck_, t = u
                if t == 0:
                    chunk_state["cur"] = next_chunk(ck_)
                slot, ci = chunk_state["cur"]
                if kind == "rq":
                    push_deferred(rope_step(t, slot, True))
                elif kind == "rk":
                    push_deferred(rope_step(t, slot, False))
                else:
                    push_deferred(rv_step(t, slot))
                if t == G - 1:
                    after_chunk(ci)

            nkb = 4 * g + 4
            tasks = [(h, kb) for h in range(8) for kb in range(nkb)]
            mixed_all = [("mixed", t) for t in range(G)]

            def fox_qk(i):
                h, kb = tasks[i]
                jlo = max(0, kb - 4 * g)
                n = (4 - jlo) * 128
                bank = i % 3
                pt = PT[i % 3]
                diag = kb >= 4 * g
                P.op("pe", lambda e: e.matmul(PS[bank][:, 0:n], lhsT=KT[:, h, kb * 128:(kb + 1) * 128],
                                              rhs=QT_v[:, h, jlo * 128:512], start=True, stop=not diag),
                     reads=[("KT", kb), ("U", h)], writes=PK(bank))
                if diag:
                    P.op("pe", lambda e: e.matmul(PS[bank][:, 0:128], lhsT=identb[:, :], rhs=negm[:, :],
                                                  start=False, stop=True, skip_group_check=True),
                         reads=["identb", "negm"], writes=PK(bank))
                P.op("act", lambda e: e.activation(out=pt[:, 0:n], in_=PS[bank][:, 0:n], func=AF.Exp),
                     reads=PK(bank), writes=[("PT", i % 3)])

            def fox_pv(i):
                h, kb = tasks[i]
                jlo = max(0, kb - 4 * g)
                ob = 3 + (h % 2)
                pt = PT[i % 3]
                for j in range(jlo, 4):
                    P.op("pe", lambda e, j=j, last=(kb == 4 * g + j): e.matmul(PS[ob][:, j * 65:(j + 1) * 65],
                                                       lhsT=pt[:, (j - jlo) * 128:(j - jlo + 1) * 128],
                                                       rhs=Vaug[:, kb, h, :], start=(kb == 0 and j == 0),
                                                       stop=last, skip_group_check=True),
                         reads=[("PT", i % 3), ("Vaug", kb), "Vaug_ones"], writes=PK(ob))
                if kb == nkb - 1:
                    fox_epilogue(h, ob, i + 2)

            def at(idx, fn):
                sched.setdefault(idx, []).append(fn)

            def fox_epilogue(h, ob, i_now):
                O = PS[ob][:, 0:260].rearrange("p (j e) -> p j e", e=65)
                p_ = h % 2
                oz, ozk = (TE[0], ("TE", 0)) if p_ == 0 else (TE[3], ("TE", 3))
                c_ss, c_rs = 96 + 16 * p_, 104 + 16 * p_
                P.op("dve", lambda e: e.reciprocal(out=st[:, 40:44], in_=O[:, :, 64]), reads=PK(ob), writes=[("st", 40)])
                P.op("dve", lambda e: e.tensor_tensor(out=oz[:, :, :], in0=O[:, :, 0:64], in1=bc3(st[:, 40:44], 64),
                                                      op=ALU.mult), reads=PK(ob) + [("st", 40)], writes=[ozk])
                P.op("pool", lambda e: e.tensor_tensor(out=TE[1][:, :, :], in0=oz[:, :, :], in1=oz[:, :, :], op=ALU.mult),
                     reads=[ozk], writes=[("TE", 1)])
                P.op("dve", lambda e: e.tensor_reduce(out=st[:, c_ss:c_ss + 4], in_=TE[1][:, :, :], axis=AX.X, op=ALU.add),
                     reads=[("TE", 1)], writes=[("st", c_ss)])

                def e1():
                    act_rstd(c_ss, c_rs, 4, 1.0 / 64)

                def e2():
                    P.op("dve", lambda e: e.tensor_tensor(out=TE[2][:, :, :], in0=oz[:, :, :], in1=bc3(st[:, c_rs:c_rs + 4], 64),
                                                          op=ALU.mult), reads=[ozk, ("st", c_rs)], writes=[("TE", 2)])
                    P.op("pool", lambda e: e.tensor_tensor(out=mixed[:, :, h * 64:(h + 1) * 64], in0=TE[2][:, :, :],
                                                           in1=gates_f[:, :, h * 64:(h + 1) * 64], op=ALU.mult),
                         reads=[("TE", 2)] + [("gates_f", t) for t in range(G)], writes=mixed_all)
                at(i_now + 2, e1)
                at(i_now + 3, e2)

            def ret_stage1(j):
                jc = slice(j * 128, (j + 1) * 128)
                for h in range(4):
                    P.op("pe", lambda e, h=h: e.matmul(PS[5][:, h * 128:(h + 1) * 128], lhsT=KTr_v[:, h, jc],
                                                       rhs=QTr_v[:, h, jc], start=(h == 0), stop=True,
                                                       skip_group_check=True),
                         reads=UK(16, 20) + UK(8, 12), writes=PK(5))
                for h in range(4):
                    hc = slice(h * 128, (h + 1) * 128)
                    P.op("pe", lambda e, hc=hc, h=h: e.matmul(PS[7][:, hc], lhsT=Kz_v[:, j, hc], rhs=Vr_v[:, j, hc],
                                                              start=(h == 0), stop=True, skip_group_check=True),
                         reads=[("U", 20 + j), ("U", 24 + j)], writes=PK(7))
                sj = j % 2
                P.op("dve", lambda e: e.tensor_tensor(out=STr[sj][:, :, :],
                                                      in0=PS[5][:, :].rearrange("p (h n) -> p h n", n=128),
                                                      in1=bcmid(trib[:, :], 4), op=ALU.mult),
                     reads=PK(5) + ["trib"], writes=[("STr", sj)])

            def ret_stage2(j):
                jc = slice(j * 128, (j + 1) * 128)
                sj = j % 2
                for h in range(4):
                    hc = slice(h * 128, (h + 1) * 128)
                    P.op("pe", lambda e, hc=hc, h=h: e.matmul(PS[6][:, hc], lhsT=STr[sj][:, h, :], rhs=Vr_v[:, j, hc],
                                                              start=(h == 0), stop=False, skip_group_check=True),
                         reads=[("STr", sj), ("U", 24 + j)], writes=PK(6))
                    P.op("pe", lambda e, hc=hc, h=h: e.matmul(PS[6][:, hc], lhsT=QTr_v[:, h, jc], rhs=state_bf[:, h, :],
                                                              start=False, stop=True, skip_group_check=True),
                         reads=UK(8, 12) + ["state_bf"], writes=PK(6))
                for h in range(4):
                    hc = slice(h * 128, (h + 1) * 128)
                    P.op("dve", lambda e, hc=hc, h=h: e.scalar_tensor_tensor(
                        out=state[:, h, :], in0=state[:, h, :], scalar=g_chunk[h], in1=PS[7][:, hc],
                        op0=ALU.mult, op1=ALU.add), reads=["state"] + PK(7), writes=["state"])
                P.op("pool", lambda e: e.tensor_copy(out=state_bf[:, :, :], in_=state[:, :, :]),
                     reads=["state"], writes=["state_bf"])
                pj = j % 2
                c_ss, c_rs = 16 + 8 * pj, 20 + 8 * pj
                dve_copy(TR[:, :], PS[6][:, :], PK(6), ["TR"])
                tmp, tkey = next_tmp()
                P.op("pool", lambda e: e.tensor_tensor(out=tmp[:, :], in0=TR[:, :], in1=TR[:, :], op=ALU.mult),
                     reads=["TR"], writes=[tkey])
                P.op("dve", lambda e: e.tensor_reduce(out=st[:, c_ss:c_ss + 4], in_=tmp[:, :].rearrange("p (h i) -> p h i", i=128),
                                                      axis=AX.X, op=ALU.add), reads=[tkey], writes=[("st", c_ss)])

                def r2c():
                    act_rstd(c_ss, c_rs, 4, 1.0 / 128)
                    tmp2, tkey2 = next_tmp()
                    P.op("dve", lambda e: e.tensor_tensor(out=tmp2[:, :].rearrange("p (h i) -> p h i", i=128),
                                                          in0=TR[:, :].rearrange("p (h i) -> p h i", i=128),
                                                          in1=bc3(st[:, c_rs:c_rs + 4], 128), op=ALU.mult),
                         reads=["TR", ("st", c_rs)], writes=[tkey2])
                    P.op("pool", lambda e: e.tensor_tensor(out=mixed[:, j, 512:1024], in0=tmp2[:, :], in1=gates_r[:, j, :],
                                                           op=ALU.mult),
                         reads=[tkey2, ("gates_r", j)], writes=[("mixed", j)])
                at(cur_i[0] + 2, r2c)

            ntask = len(tasks)
            sched = {}
            nun = len(units)
            span = max(nun, int(ntask * 0.55))
            for ui, u in enumerate(units):
                sched.setdefault(min(ntask - 1, ui * span // nun), []).append(lambda u=u: run_unit(u))
            sched.setdefault(min(ntask - 1, span), []).append(flush_deferred)
            rem0 = min(ntask - 1, span + 1)
            for j in range(4):
                p1 = rem0 + (2 * j) * (ntask - rem0) // 8
                p2 = rem0 + (2 * j + 1) * (ntask - rem0) // 8
                sched.setdefault(min(ntask - 1, p1), []).append(lambda j=j: ret_stage1(j))
                sched.setdefault(min(ntask - 1, p2), []).append(lambda j=j: ret_stage2(j))
            cur_i = [0]
            for i in range(ntask + 2):
                cur_i[0] = i
                for f in sched.pop(i, []):
                    f()
                if i < ntask:
                    fox_qk(i)
                if i >= 2:
                    fox_pv(i - 2)
            while sched:
                k_ = min(sched)
                cur_i[0] = k_
                for f in sched.pop(k_):
                    f()

            for c in (0, 1, 2, 4, 5, 6, 7, 3):
                bank = rr["tp"] % 2
                rr["tp"] += 1
                for t in range(G):
                    P.op("pe", lambda e, c=c, t=t, bank=bank: e.transpose(
                        out=Pb(bank)[:, t * 128:(t + 1) * 128], in_=mixed[:, t, c * 128:(c + 1) * 128],
                        identity=identb[:, :]), reads=[("mixed", t), "identb"], writes=PK(bank))
                if c % 2 == 0:
                    act_copy(hT_oth[:, c, :], Pb(bank)[:, 0:512], PK(bank), [(okey, c)])
                else:
                    dve_copy(hT_oth[:, c, :], Pb(bank)[:, 0:512], PK(bank), [(okey, c)])

            for q in range(2):
                slot, ci = next_chunk(8 + q)
                for t in range(G):
                    bank = next_proj_bank()
                    proj_mm(hT_oth, t, slot, bank, okey)
                    tmp, tkey = next_tmp()
                    qc = slice(q * 512, (q + 1) * 512)
                    P.op("dve", lambda e, tmp=tmp, bank=bank, qc=qc: e.tensor_tensor(out=tmp[:, :], in0=PS[bank][:, :],
                                                                                    in1=gm_bc[:, qc], op=ALU.mult),
                         reads=PK(bank) + ["gm_bc"], writes=[tkey])
                    P.op("pool" if t % 2 else "dve", lambda e, tmp=tmp, t=t, qc=qc: e.tensor_tensor(
                        out=xs[:, t, qc], in0=xs[:, t, qc], in1=tmp[:, :], op=ALU.add),
                         reads=[tkey, ("xs", t)], writes=[("xs", t)])
                after_chunk(ci)

            xn2 = u_f32(16, 16).rearrange("p (t d) -> p t d", t=G)
            for stg_ in rms_chain(b, xs, [[("xs", t)] for t in range(G)], xn2, [UK(16 + 4 * t, 20 + 4 * t) for t in range(G)],
                                  hT_cur, hkey, opm_f, sh_f, 4, 3, [0, 1]):
                stg_()

            pstages = prefetch_B(gi + 1) if gi + 1 < NSEQ * NG else []
            for j in range(8):
                slot, ci = next_chunk(10 + j)
                for fc in range(4):
                    step_ = 4 * j + fc
                    if pstages and step_ >= 2 and step_ % 2 == 0 and (step_ - 2) // 2 < len(pstages):
                        pstages[(step_ - 2) // 2]()
                    bank = rr["mi"] % 4
                    rr["mi"] += 1
                    tmp, tkey = next_tmp()
                    for c in range(8):
                        P.op("pe", lambda e, c=c, fc=fc, bank=bank, slot=slot, hT_cur=hT_cur: e.matmul(
                            PS[bank][:, :], lhsT=wbuf[slot][:, c * 512 + fc * 128: c * 512 + (fc + 1) * 128],
                            rhs=hT_cur[:, c, :], start=(c == 0), stop=(c == 7)),
                            reads=[(hkey, c), ("wbuf", slot)], writes=PK(bank))
                    P.op("act", lambda e, tmp=tmp, bank=bank: e.activation(out=tmp[:, :], in_=PS[bank][:, :], func=AF.Relu),
                         reads=PK(bank), writes=[tkey])
                    uc = u_chunk(4 * j + fc)
                    P.op("pool", lambda e, tmp=tmp, uc=uc: e.tensor_tensor(out=uc, in0=tmp[:, :], in1=tmp[:, :], op=ALU.mult),
                         reads=[tkey], writes=[("U", 4 * j + fc)])
                after_chunk(ci)

            for j in range(8):
                slot, ci = next_chunk(18 + j)
                for t in range(G):
                    for hf in range(2):
                        bank = t * 2 + hf
                        for fc in range(4):
                            uc = u_chunk(4 * j + fc)
                            P.op("pe", lambda e, uc=uc, t=t, hf=hf, fc=fc, bank=bank, slot=slot, j=j: e.matmul(
                                PS[bank][:, :], lhsT=uc[:, t * 128:(t + 1) * 128],
                                rhs=wbuf[slot][:, fc * 1024 + hf * 512: fc * 1024 + (hf + 1) * 512],
                                start=(j == 0 and fc == 0), stop=(j == 7 and fc == 3)),
                                reads=[("U", 4 * j + fc), ("wbuf", slot)], writes=PK(bank))
                after_chunk(ci)
            done_half = {}
            for (t, hf) in ((3, 1), (1, 0), (1, 1), (2, 0), (0, 0), (0, 1), (2, 1), (3, 0)):
                if True:
                    bank = t * 2 + hf
                    tmp, tkey = next_tmp()
                    qc = slice(hf * 512, (hf + 1) * 512)
                    P.op("dve", lambda e, tmp=tmp, bank=bank, qc=qc: e.tensor_tensor(out=tmp[:, :], in0=PS[bank][:, :],
                                                                                    in1=gf_bc[:, qc], op=ALU.mult),
                         reads=PK(bank) + ["gf_bc"], writes=[tkey])
                    P.op("pool" if hf else "dve", lambda e, tmp=tmp, t=t, qc=qc: e.tensor_tensor(
                        out=xs[:, t, qc], in0=xs[:, t, qc], in1=tmp[:, :], op=ALU.add),
                         reads=[tkey, ("xs", t)], writes=[("xs", t)])
                done_half[t] = done_half.get(t, 0) + 1
                if done_half[t] == 2:
                    tok = dma(y_d[row0 + t * 128: row0 + (t + 1) * 128, :], xs[:, t, :], "yst", reads=[("xs", t)],
                              writes=[("y", row0 + t * 128)])
                    store_toks.append(tok)

    P.wait_all("sp", [max(store_toks, key=lambda tk: tk[1])])
    return nc, P, es, consts


_BUILT = None


def _get_built():
    global _BUILT
    if _BUILT is None:
        nc, P, es, consts = build_program()
        P.emit(nc, es)
        es.close()
        _BUILT = (nc, consts)
    return _BUILT


def kernel(x, c, w_ada, b_ada, w_in, b_forget, q_norm_gain, k_norm_gain, fox_out_gain, ret_out_gain,
           w_out, w_mlp_in, w_mlp_out):
    nc, consts = _get_built()
    f = np.float32
    x = np.asarray(x, f)
    c = np.asarray(c, f)
    shared = {
        "w_ada": np.ascontiguousarray(np.asarray(w_ada, f)[0]),
        "b_ada": np.ascontiguousarray(np.asarray(b_ada, f)[0].reshape(1, -1)),
        "w_in": np.ascontiguousarray(np.asarray(w_in, f)[0]),
        "b_forget": np.ascontiguousarray(np.asarray(b_forget, f)[0].reshape(1, 8)),
        "q_gain": np.ascontiguousarray(np.asarray(q_norm_gain, f)[0].reshape(1, 64)),
        "k_gain": np.ascontiguousarray(np.asarray(k_norm_gain, f)[0].reshape(1, 64)),
        "fox_gain": np.ascontiguousarray(np.asarray(fox_out_gain, f)[0].reshape(1, 512)),
        "ret_gain": np.ascontiguousarray(np.asarray(ret_out_gain, f)[0].reshape(1, 512)),
        "w_out": np.ascontiguousarray(np.asarray(w_out, f)[0]),
        "w1": np.ascontiguousarray(np.asarray(w_mlp_in, f)[0]),
        "w2": np.ascontiguousarray(np.asarray(w_mlp_out, f)[0]),
    }
    for k in ("identf", "identb", "trib", "negm", "trif", "onesf", "ixi_bc", "xi_bc", "zeta_t", "rope"):
        shared[k] = consts[k]
    in_maps = []
    for i in range(NCORES):
        m = dict(shared)
        m["x"] = np.ascontiguousarray(x[i * NSEQ:(i + 1) * NSEQ].reshape(NSEQ * S, D))
        m["c"] = np.ascontiguousarray(c[i * NSEQ:(i + 1) * NSEQ])
        in_maps.append(m)
    res = run_bass_kernel_spmd(nc, in_maps, core_ids=list(range(NCORES)))
    out = np.concatenate([np.asarray(r["y"], f).reshape(NSEQ, S, D) for r in res.results], axis=0)
    return out
```

```python
import numpy as np
import ml_dtypes
from contextlib import ExitStack
import concourse.bass as bass
import concourse.mybir as mybir
from concourse.bass_utils import run_bass_kernel_spmd

F32 = mybir.dt.float32
BF16 = mybir.dt.bfloat16
AF = mybir.ActivationFunctionType
ALU = mybir.AluOpType
AX = mybir.AxisListType

NCORES = 8
D = 1024
S = 2048
NSEQ = 4
NT = 16
G = 4
NG = NT // G
DFF = 4096
EPS = 1e-6
IN_COLS = 4104
NCHUNK = 26

ENGS = ("pe", "act", "dve", "pool", "sp")
import os
STRICT = bool(int(os.environ.get("KSTRICT", "0")))


class _Op:
    __slots__ = ("fn", "waits", "sig", "dma", "tok")


class Prog:
    def __init__(self):
        self.ops = {e: [] for e in ENGS}
        self.res = {}
        self.known = {e: {} for e in ENGS}
        self.clock = {}
        self.dma_count = {}
        self.needed = set()

    def _deps(self, eng, reads, writes):
        deps = set()
        for k in reads:
            r = self.res.get(k)
            if r is not None and r[0] is not None:
                deps.add(r[0])
        for k in writes:
            r = self.res.get(k)
            if r is not None:
                if r[0] is not None and (STRICT or r[0][0] != eng):
                    deps.add(r[0])
                for src, v in r[1].items():
                    if STRICT or src != eng:
                        deps.add((src, v))
        return deps

    def _commit(self, tok, reads, writes):
        for k in reads:
            r = self.res.get(k)
            if r is None:
                r = [None, {}]
                self.res[k] = r
            if r[1].get(tok[0], 0) < tok[1]:
                r[1][tok[0]] = tok[1]
        for k in writes:
            self.res[k] = [tok, {}]

    def op(self, eng, fn, reads=(), writes=(), dma=None):
        deps = self._deps(eng if dma is None else "dma:" + dma, reads, writes)
        kn = self.known[eng]
        waits = []
        best = {}
        for (src, v) in deps:
            if best.get(src, 0) < v:
                best[src] = v
        deps = set(best.items())
        for (src, v) in sorted(deps, key=lambda t: (str(t[0]), t[1])):
            if kn.get(src, 0) >= v:
                continue
            waits.append((src, v))
            self.needed.add((src, v))
            for s2, v2 in self.clock[(src, v)].items():
                if kn.get(s2, 0) < v2:
                    kn[s2] = v2
        o = _Op()
        o.fn = fn
        o.waits = waits
        o.dma = dma
        self.ops[eng].append(o)
        if dma is None:
            tok = (eng, len(self.ops[eng]))
        else:
            src = "dma:" + dma
            self.dma_count[src] = self.dma_count.get(src, 0) + 16
            tok = (src, self.dma_count[src])
        o.tok = tok
        ck = dict(kn)
        ck[tok[0]] = tok[1]
        self.clock[tok] = ck
        self._commit(tok, reads, writes)
        return tok

    def wait_all(self, eng, toks):
        kn = self.known[eng]
        waits = []
        for (src, v) in toks:
            if kn.get(src, 0) >= v:
                continue
            waits.append((src, v))
            self.needed.add((src, v))
            kn[src] = v
        o = _Op()
        o.fn = None
        o.waits = waits
        o.dma = None
        o.tok = None
        self.ops[eng].append(o)

    def emit(self, nc, es):
        sems = {}
        for e in ("pe", "act", "dve", "pool"):
            sems[e] = es.enter_context(nc.semaphore("sem_" + e))
        for src in self.dma_count:
            sems[src] = es.enter_context(nc.semaphore("sem_" + src.replace(":", "_")))
        sigval = {}
        for e in ("pe", "act", "dve", "pool"):
            cnt = 0
            for i, o in enumerate(self.ops[e]):
                o.sig = False
                if o.fn is not None and o.dma is None and (e, i + 1) in self.needed:
                    cnt += 1
                    o.sig = True
                    sigval[(e, i + 1)] = cnt
        blk = es.enter_context(nc.Block())

        def run(e, name):
            for o in self.ops[name]:
                for (src, v) in o.waits:
                    val = v if src.startswith("dma:") else sigval[(src, v)]
                    e.wait_ge(sems[src], val)
                if o.fn is None:
                    continue
                ins = o.fn(e)
                if o.dma is not None:
                    ins.then_inc(sems["dma:" + o.dma], 16)
                elif o.sig:
                    ins.then_inc(sems[name], 1)

        @blk.tensor
        def _(e):
            run(e, "pe")

        @blk.scalar
        def _(e):
            run(e, "act")

        @blk.vector
        def _(e):
            run(e, "dve")

        @blk.gpsimd
        def _(e):
            run(e, "pool")

        @blk.sync
        def _(e):
            run(e, "sp")


def _constants():
    f = np.float32
    n = np.arange(128, dtype=f)
    ident = np.eye(128, dtype=f)
    tri = (n[:, None] <= n[None, :]).astype(f)
    ones = np.ones((128, 128), f)
    h = np.arange(4, dtype=f)
    log_g = np.log(f(1.0) - f(2.0) ** (f(-5.0) - h)).astype(f)
    diff = n[None, :] - n[:, None]
    maskT = np.where(diff[None] >= 0, np.exp(np.maximum(diff, 0.0)[None] * log_g[:, None, None]), 0.0).astype(f)
    xi = np.exp((n[None, :] + 1.0) * log_g[:, None]).astype(f)
    xi_bc = np.ascontiguousarray(np.broadcast_to(xi[None], (128, 4, 128))).astype(f)
    ixi = np.exp(-(n[None, :] + 1.0) * log_g[:, None]).astype(f)
    ixi_bc = np.ascontiguousarray(np.broadcast_to(ixi[None], (128, 4, 128))).astype(f)
    zeta = np.exp((128 - 1.0 - n[None, :]) * log_g[:, None]).astype(f)
    zeta_t = np.ascontiguousarray(zeta.T)
    g_chunk = np.exp(f(128.0) * log_g).astype(f)
    pos = np.arange(S, dtype=f)
    inv_freq = (f(10000.0) ** (-np.arange(0, 128, 2, dtype=f) / f(128))).astype(f)
    ang = (pos[:, None] * inv_freq[None, :]).astype(f)
    cos = np.cos(ang).astype(f)
    sin = np.sin(ang).astype(f)
    ks = f(128.0 ** -0.5)

    def lay(a):
        return np.ascontiguousarray(a.reshape(16, 128, 64).transpose(1, 0, 2))

    rope = np.stack([lay(cos), lay(sin), lay(-sin), lay(cos * ks), lay(sin * ks), lay(-sin * ks)], 0)
    sel = np.zeros((4, 4, 128), f)
    for b in range(4):
        sel[b, b, :] = 1.0
    return dict(
        identf=ident, identb=ident.astype(ml_dtypes.bfloat16), trib=tri.astype(ml_dtypes.bfloat16),
        negm=((1.0 - tri) * -30000.0).astype(ml_dtypes.bfloat16),
        trif=tri, onesf=ones, ixi_bc=ixi_bc, xi_bc=xi_bc, zeta_t=zeta_t,
        rope=np.ascontiguousarray(rope), g_chunk=g_chunk,
    )


_CONST = None


def build_program():
    consts = _constants()
    g_chunk = [float(v) for v in consts["g_chunk"]]
    nc = bass.Bass("TRN2", target_bir_lowering=False)
    P = Prog()
    es = ExitStack()

    def din(name, shape, dt=F32):
        return nc.dram_tensor(name, list(shape), dt, kind="ExternalInput").ap()

    x_d = din("x", [NSEQ * S, D])
    c_d = din("c", [NSEQ, D])
    wada_d = din("w_ada", [D, 6 * D])
    bada_d = din("b_ada", [1, 6 * D])
    win_d = din("w_in", [D, IN_COLS])
    bfg_d = din("b_forget", [1, 8])
    qg_d = din("q_gain", [1, 64])
    kg_d = din("k_gain", [1, 64])
    fxg_d = din("fox_gain", [1, 512])
    rtg_d = din("ret_gain", [1, 512])
    wout_d = din("w_out", [D, D])
    w1_d = din("w1", [D, DFF])
    w2_d = din("w2", [DFF, D])
    identf_d = din("identf", [128, 128])
    identb_d = din("identb", [128, 128], BF16)
    trib_d = din("trib", [128, 128], BF16)
    negm_d = din("negm", [128, 128], BF16)
    trif_d = din("trif", [128, 128])
    onesf_d = din("onesf", [128, 128])
    ixibc_d = din("ixi_bc", [128, 4, 128])
    xibc_d = din("xi_bc", [128, 4, 128])
    zeta_d = din("zeta_t", [128, 4])
    rope_d = din("rope", [6, 128, 16, 64])
    y_d = nc.dram_tensor("y", [NSEQ * S, D], F32, kind="ExternalOutput").ap()
    wbf_d = nc.dram_tensor("wbf", [NCHUNK, 128, 4096], BF16, kind="Internal").ap()
    gates_d = nc.dram_tensor("gates_scr", [4, 2048], F32, kind="Internal").ap()

    def sb(name, shape, dt=F32):
        return es.enter_context(nc.sbuf_tensor(name, list(shape), dt))

    wbuf = [sb(f"wbuf{i}", [128, 4096], BF16) for i in range(3)]
    xs = sb("xs", [128, G, D])
    hTA = sb("hTA", [128, 8, 512], BF16)
    hTB = sb("hTB", [128, 8, 512], BF16)
    U = sb("U", [128, 16384], BF16)
    KT = sb("KT", [70, 8, S], BF16)
    Vaug = sb("Vaug", [128, NT, 8, 65], BF16)
    qaug = [sb(f"qaug{i}", [128, 8, 70], BF16) for i in range(3)]
    kaug = [sb(f"kaug{i}", [128, 8, 70], BF16) for i in range(3)]
    state = sb("state", [128, 4, 128])
    state_bf = sb("state_bf", [128, 4, 128], BF16)
    MG = sb("MG", [128, 8192], BF16)
    mixed = MG[:, 0:4096].rearrange("p (t d) -> p t d", d=1024)
    gates_f = MG[:, 4096:6144].rearrange("p (t d) -> p t d", d=512)
    gates_r = MG[:, 6144:8192].rearrange("p (t d) -> p t d", d=512)
    xnext = MG[:, :].bitcast(F32).rearrange("p (t d) -> p t d", d=1024)
    XNK = [[("mixed", 0), ("mixed", 1)], [("mixed", 2), ("mixed", 3)],
           [("gates_f", t) for t in range(G)], [("gates_r", t) for t in range(G)]]
    PT = [sb(f"PT{i}", [128, 512], BF16) for i in range(3)]
    STr = [sb(f"STr{i}", [128, 4, 128], BF16) for i in range(2)]
    TA = [sb(f"TA{i}", [128, 512]) for i in range(2)]
    TB = [sb(f"TB{i}", [128, 512]) for i in range(2)]
    TE = [sb(f"TE{i}", [128, 4, 64]) for i in range(4)]
    TR = sb("TR", [128, 512])
    ropeg = sb("ropeg", [128, 6, G, 64])
    gm_bc = sb("gm_bc", [128, D])
    gf_bc = sb("gf_bc", [128, D])
    opm_m = sb("opm_m", [128, 8, 4])
    sh_m = sb("sh_m", [128, 8, 4])
    opm_f = sb("opm_f", [128, 8, 4])
    sh_f = sb("sh_f", [128, 8, 4])
    identf = sb("identf_s", [128, 128])
    identb = sb("identb_s", [128, 128], BF16)
    trib = sb("trib_s", [128, 128], BF16)
    negm = sb("negm_s", [128, 128], BF16)
    trif = sb("trif_s", [128, 128])
    onesf = sb("onesf_s", [128, 128])
    ixibc = sb("ixibc_s", [128, 4, 128])
    xibc = sb("xibc_s", [128, 4, 128])
    zeta = sb("zeta_s", [128, 4])
    qg_col = sb("qg_col", [70, 1])
    kg_col = sb("kg_col", [70, 1])
    fxg_bc = sb("fxg_bc", [128, 512])
    rtg_bc = sb("rtg_bc", [128, 512])
    bfg_bc = sb("bfg_bc", [128, 8])
    wfg = sb("wfg", [128, 8, 8], BF16)
    wfg32 = sb("wfg32", [128, 8, 8])
    epsc = sb("epsc", [128, 1])
    st = sb("st", [128, 128])
    rs_run = sb("rs_run", [128, 8])
    fz = sb("fz", [128, 3, 32])
    cr = sb("cr", [128, 2, 32])
    pre = sb("pre", [128, G, 8])
    cumsp = sb("cumsp", [128, G, 8, 3], BF16)
    cactT = sb("cactT", [128, 8, 4])
    ctmp = sb("ctmp", [128, 8, 4])
    ones14 = sb("ones14", [1, 4])

    c4 = xs[0:4, 0, :]
    badar = [TB[0][0:1, :], TB[1][0:1, :]]
    grow = xs[0:4, 1:3, :].rearrange("p t d -> p (t d)")
    PS = [es.enter_context(nc.psum_tensor(f"P{i}", [128, 512], F32)) for i in range(8)]

    def UK(lo, hi):
        return [("U", i) for i in range(lo, hi)]

    def u_chunk(fc):
        return U[:, fc * 512:(fc + 1) * 512]

    def u_f32(lo_gran, n_gran):
        return U[:, lo_gran * 512:(lo_gran + n_gran) * 512].bitcast(F32)

    QT_v = U[0:70, 0:4096].rearrange("p (h n) -> p h n", n=512)
    QTr_v = U[:, 4096:6144].rearrange("p (h n) -> p h n", n=512)
    QxT_v = U[:, 6144:8192].rearrange("p (h n) -> p h n", n=512)
    KTr_v = U[:, 8192:10240].rearrange("p (h n) -> p h n", n=512)
    Kz_v = U[:, 10240:12288].rearrange("p (t n) -> p t n", n=512)
    Vr_v = U[:, 12288:14336].rearrange("p (t n) -> p t n", n=512)
    rqt_v = [U[:, 14336:14848], U[:, 14848:15360], U[:, 6144:6656]]
    rkt_v = [U[:, 15360:15872], U[:, 15872:16384], U[:, 6656:7168]]
    rqt_g = [28, 29, 12]
    rkt_g = [30, 31, 13]

    def PK(i):
        return [("P", i)]

    def Pb(i):
        return PS[i][:, :].bitcast(BF16)

    def dma(out, in_, sem, reads=(), writes=(), eng="sp"):
        return P.op(eng, lambda e, o=out, i=in_: e.dma_start(out=o, in_=i), reads=reads, writes=writes, dma=sem)

    dma(identf[:, :], identf_d, "c0", writes=["identf"])
    dma(identb[:, :], identb_d, "c0", writes=["identb"])
    dma(trib[:, :], trib_d, "c0", writes=["trib"])
    dma(negm[:, :], negm_d, "c0", writes=["negm"])
    dma(trif[:, :], trif_d, "c0", writes=["trif"])
    dma(onesf[:, :], onesf_d, "c0", writes=["onesf"])
    dma(ixibc[:, :, :], ixibc_d, "c0", writes=["ixibc"])
    dma(xibc[:, :, :], xibc_d, "c0", writes=["xibc"])
    dma(zeta[:, :], zeta_d, "c0", writes=["zeta"])
    dma(qg_col[0:64, :], qg_d.rearrange("o d -> d o"), "c0", writes=["qg"])
    dma(kg_col[0:64, :], kg_d.rearrange("o d -> d o"), "c0", writes=["kg"])
    dma(fxg_bc[:, :], fxg_d.partition_broadcast(128), "c0", writes=["fxg"])
    dma(rtg_bc[:, :], rtg_d.partition_broadcast(128), "c0", writes=["rtg"])
    dma(bfg_bc[:, :], bfg_d.partition_broadcast(128), "c0", writes=["bfg"])
    dma(c4, c_d, "c0", writes=["c4", ("xs", 0)])
    dma(wfg32[:, :, :], win_d[:, 2048:2056].rearrange("(c p) n -> p c n", p=128), "c0", writes=["wfg32"])
    c0_total = P.dma_count["dma:c0"]
    for k in ["identf", "identb", "trib", "negm", "trif", "onesf", "ixibc", "xibc", "zeta", "qg", "kg", "fxg",
              "rtg", "bfg", "c4", "wfg32"]:
        P.res[k][0] = ("dma:c0", c0_total)
    P.clock[("dma:c0", c0_total)] = {"dma:c0": c0_total}

    P.op("pool", lambda e: e.memset(epsc[:, :], EPS), writes=["epsc"])
    P.op("pool", lambda e: e.memset(ones14[:, :], 1.0), writes=["ones14"])
    P.op("pool", lambda e: e.memset(Vaug[:, :, :, 64:65], 1.0), writes=["Vaug_ones"])
    for i in range(3):
        P.op("pool", lambda e, i=i: e.memset(qaug[i][:, :, 67:70], 1.0), writes=[("qaug", i)])
        P.op("pool", lambda e, i=i: e.memset(kaug[i][:, :, 64:67], 1.0), writes=[("kaug", i)])
    P.op("dve", lambda e: e.tensor_copy(out=wfg[:, :, :], in_=wfg32[:, :, :]), reads=["wfg32"], writes=["wfg"])
    P.op("pool", lambda e: e.memset(qg_col[64:70, :], 1.0), writes=["qg1"])
    P.op("pool", lambda e: e.memset(kg_col[64:70, :], 1.0), writes=["kg1"])
    P.op("dve", lambda e: e.tensor_scalar(out=qg_col[0:64, :], in0=qg_col[0:64, :], scalar1=0.125, scalar2=None,
                                          op0=ALU.mult), reads=["qg"], writes=["qg"])

    def chunk_src(k):
        if k < 8:
            col0 = [0, 512, 1024, 1536, 2056, 2568, 3080, 3592][k]
            return win_d[:, col0:col0 + 512].rearrange("(c p) n -> p c n", p=128)
        if k < 10:
            q = k - 8
            return wout_d[:, q * 512:(q + 1) * 512].rearrange("(c p) n -> p c n", p=128)
        if k < 18:
            j = k - 10
            return w1_d[:, j * 512:(j + 1) * 512].rearrange("(c p) n -> p c n", p=128)
        j = k - 18
        return w2_d[j * 512:(j + 1) * 512, :].rearrange("(c p) n -> p c n", p=128)

    CAST_ORDER = [3, 7, 0, 1, 2, 4, 5, 6] + list(range(8, NCHUNK))

    def cast_chunk(k, extra_reads=()):
        src_ap = chunk_src(k)
        dst = wbf_d[k].rearrange("p (c n) -> p c n", c=src_ap.shape[1])
        dma(dst, src_ap, f"cst{k}", reads=list(extra_reads), writes=[("wbf", k)], eng="pool")

    for k in CAST_ORDER[:5]:
        cast_chunk(k)

    c4k = [("xs", 0)]
    growk = [("xs", 1), ("xs", 2)]
    mrow = xs[0:4, 3, 0:512]
    mrowk = [("xs", 3)]
    for cc in range(8):
        P.op("pe", lambda e, cc=cc: e.transpose(out=PS[0][:, cc * 4:(cc + 1) * 4], in_=c4[:, cc * 128:(cc + 1) * 128],
                                               identity=identf[0:4, 0:4]),
             reads=["c4", "identf"] + c4k, writes=PK(0))
    P.op("act", lambda e: e.activation(out=ctmp[:, :, :], in_=PS[0][:, 0:32].rearrange("p (c b) -> p c b", b=4),
                                       func=AF.Exp, scale=-1.0), reads=PK(0), writes=["ctmp"])
    P.op("dve", lambda e: e.tensor_scalar(out=ctmp[:, :, :], in0=ctmp[:, :, :], scalar1=1.0, scalar2=None, op0=ALU.add),
         reads=["ctmp"], writes=["ctmp"])
    P.op("dve", lambda e: e.reciprocal(out=ctmp[:, :, :], in_=ctmp[:, :, :]), reads=["ctmp"], writes=["ctmp"])
    P.op("dve", lambda e: e.tensor_tensor(out=cactT[:, :, :], in0=ctmp[:, :, :],
                                          in1=PS[0][:, 0:32].rearrange("p (c b) -> p c b", b=4), op=ALU.mult),
         reads=["ctmp"] + PK(0), writes=["cactT"])

    modT_dst = {0: (sh_m, False), 1: (opm_m, True), 3: (sh_f, False), 4: (opm_f, True)}
    for kb in range(12):
        s = kb % 2
        v = kb // 2
        half = kb % 2
        stg = u_f32(16 * s, 16).rearrange("p (c n) -> p c n", c=8)
        dma(stg, wada_d[:, kb * 512:(kb + 1) * 512].rearrange("(c p) n -> p c n", p=128), f"stg{s}",
            writes=UK(16 * s, 16 * s + 16) + [("wada_blk", kb)])
        rk = UK(16 * s, 16 * s + 16)
        bd = badar[kb % 2]
        bdk = ("TB", kb % 2)
        dma(bd, bada_d[0:1, kb * 512:(kb + 1) * 512], f"bd{kb % 2}", writes=[bdk])
        bank = 3 + (kb % 2)
        for dc in range(8):
            P.op("pe", lambda e, bank=bank, dc=dc, stg=stg: e.matmul(
                PS[bank][0:4, :], lhsT=cactT[:, dc, :], rhs=stg[:, dc, :], start=(dc == 0), stop=False),
                reads=rk + ["cactT"], writes=PK(bank))
        P.op("pe", lambda e, bank=bank, bd=bd: e.matmul(
            PS[bank][0:4, :], lhsT=ones14[0:1, :], rhs=bd[0:1, :],
            start=False, stop=True), reads=[bdk, "ones14"], writes=PK(bank))
        if v in modT_dst:
            dst, plus1 = modT_dst[v]
            P.op("dve", lambda e, bank=bank: e.tensor_copy(out=mrow, in_=PS[bank][0:4, :]), reads=PK(bank), writes=mrowk)
            tb_ = 1 + (kb % 2)
            for ec in range(4):
                P.op("pe", lambda e, tb_=tb_, ec=ec: e.transpose(out=PS[tb_][:, ec * 4:(ec + 1) * 4],
                                                                 in_=mrow[:, ec * 128:(ec + 1) * 128],
                                                                 identity=identf[0:4, 0:4]),
                     reads=mrowk + ["identf"], writes=PK(tb_))
            src_v = PS[tb_][:, 0:16].rearrange("p (c b) -> p c b", b=4)
            dst_v = dst[:, half * 4:(half + 1) * 4, :]
            if plus1:
                P.op("dve", lambda e, o=dst_v, i=src_v: e.tensor_scalar(out=o, in0=i, scalar1=1.0, scalar2=None,
                                                                        op0=ALU.add),
                     reads=PK(tb_), writes=[("modT", v, half)])
            else:
                P.op("dve", lambda e, o=dst_v, i=src_v: e.tensor_copy(out=o, in_=i),
                     reads=PK(tb_), writes=[("modT", v, half)])
        else:
            gi = 0 if v == 2 else 1
            P.op("dve", lambda e, bank=bank, gi=gi, half=half: e.tensor_copy(
                out=grow[:, gi * 1024 + half * 512: gi * 1024 + (half + 1) * 512], in_=PS[bank][0:4, :]),
                reads=PK(bank), writes=growk)
    dma(gates_d, grow, "gsc", reads=growk, writes=["gates_d"])
    for k in CAST_ORDER[5:]:
        cast_chunk(k, extra_reads=[("wada_blk", 10), ("wada_blk", 11)])

    from collections import deque
    GROUP_ORDER = [3, 7, 0, 1, 2, 4, 5, 6] + list(range(8, NCHUNK))
    stream = [k for _ in range(NSEQ * NG) for k in GROUP_ORDER]
    pf = {"next": 0, "slots": {}, "cons": 0}

    def prefetch_upto(n):
        while pf["next"] < min(n, len(stream)):
            i = pf["next"]
            slot = i % 3
            dma(wbuf[slot][:, :], wbf_d[stream[i]], f"wld{slot}", reads=[("wbf", stream[i])], writes=[("wbuf", slot)])
            pf["slots"][i] = slot
            pf["next"] += 1

    def next_chunk(expect):
        i = pf["cons"]
        assert stream[i] == expect, (stream[i], expect)
        prefetch_upto(i + 2)
        pf["cons"] += 1
        return pf["slots"][i], i

    def after_chunk(i):
        prefetch_upto(i + 3)

    rr = {"proj": 0, "tp": 0, "tq": 0, "qa": 0, "ka": 0, "mi": 0, "tmp": 0, "rq": 0, "rk": 0, "qkp": 0}
    store_toks = []
    tmps = [(TA[0], ("TA", 0)), (TB[0], ("TB", 0)), (TA[1], ("TA", 1)), (TB[1], ("TB", 1))]

    def next_tmp():
        r = tmps[rr["tmp"] % 4]
        rr["tmp"] += 1
        return r

    def bc3(ap2, n):
        return ap2.unsqueeze(2).to_broadcast([128, ap2.shape[1], n])

    def bcmid(ap2, m):
        return ap2.unsqueeze(1).to_broadcast([128, m, ap2.shape[1]])

    def act_rstd(c_in, c_out, n, inv):
        P.op("act", lambda e: e.activation(out=st[:, c_out:c_out + n], in_=st[:, c_in:c_in + n], func=AF.Ln,
                                           scale=inv, bias=epsc[:, 0:1]),
             reads=[("st", c_in), "epsc"], writes=[("st", c_out)])
        P.op("act", lambda e: e.activation(out=st[:, c_out:c_out + n], in_=st[:, c_out:c_out + n], func=AF.Exp,
                                           scale=-0.5),
             reads=[("st", c_out)], writes=[("st", c_out)])

    def act_copy(out, in_, reads, writes):
        P.op("act", lambda e: e.activation(out=out, in_=in_, func=AF.Copy), reads=reads, writes=writes)

    def dve_copy(out, in_, reads, writes):
        P.op("dve", lambda e: e.tensor_copy(out=out, in_=in_), reads=reads, writes=writes)

    HB = [hTA, hTB]
    HK = ["hTA", "hTB"]

    def rms_chain(b_, src_t, src_keys, xn, xn_keys, hT, hkey, opm, shf, vs, vh, banks):
        junk = hT[:, 0:2, :].rearrange("p a n -> p (a n)")
        jk = [(hkey, 0), (hkey, 1)]

        def mk_sq(t):
            def f():
                P.op("act", lambda e: e.activation(out=junk, in_=src_t[:, t, :], func=AF.Square,
                                                   accum_out=st[:, t:t + 1]),
                     reads=src_keys[t], writes=jk + [("st", 0)])
            return f

        def norm():
            act_rstd(0, 8, 4, 1.0 / D)
            for t in range(G):
                P.op("dve", lambda e, t=t: e.tensor_scalar(out=xn[:, t, :], in0=src_t[:, t, :], scalar1=st[:, 8 + t:9 + t],
                                                           scalar2=None, op0=ALU.mult),
                     reads=src_keys[t] + [("st", 8)], writes=xn_keys[t])

        def mk(c):
            def stage():
                bank = banks[c % len(banks)]
                for t in range(G):
                    P.op("pe", lambda e, t=t: e.transpose(
                        out=PS[bank][:, t * 128:(t + 1) * 128], in_=xn[:, t, c * 128:(c + 1) * 128],
                        identity=identf[:, :]),
                        reads=xn_keys[t] + ["identf"], writes=PK(bank))
                if c % 2 == 0:
                    P.op("act", lambda e: e.activation(
                        out=hT[:, c, :], in_=PS[bank][:, :], func=AF.Identity,
                        scale=opm[:, c, b_:b_ + 1], bias=shf[:, c, b_:b_ + 1]),
                        reads=PK(bank) + [("modT", vs, c // 4), ("modT", vh, c // 4)], writes=[(hkey, c)])
                else:
                    P.op("dve", lambda e: e.tensor_scalar(
                        out=hT[:, c, :], in0=PS[bank][:, :], scalar1=opm[:, c, b_:b_ + 1],
                        scalar2=shf[:, c, b_:b_ + 1], op0=ALU.mult, op1=ALU.add),
                        reads=PK(bank) + [("modT", vs, c // 4), ("modT", vh, c // 4)], writes=[(hkey, c)])
            return stage
        return [mk_sq(t) for t in range(G)] + [norm] + [mk(c) for c in range(8)]

    def prefetch_B(gi_n):
        b_n, g_n = gi_n // NG, gi_n % NG
        r0 = b_n * S + g_n * 512
        dma(xnext[:, :, :], x_d[r0:r0 + 512, :].rearrange("(t p) d -> p t d", p=128), "xnl",
            writes=[k for ks in XNK for k in ks])
        return rms_chain(b_n, xnext, XNK, xnext, XNK, HB[gi_n % 2], HK[gi_n % 2], opm_m, sh_m, 1, 0, [4, 5, 6, 7])

    def keep_warm(n, bank):
        for _ in range(n):
            P.op("pe", lambda e: e.matmul(PS[bank][:, :], lhsT=identb[:, :], rhs=PT[0][:, :], start=True, stop=True),
                 reads=["identb", ("PT", 0)], writes=PK(bank))

    def proj_mm(src, t, slot, bank, key):
        for c in range(8):
            P.op("pe", lambda e, c=c: e.matmul(
                PS[bank][:, :], lhsT=src[:, c, t * 128:(t + 1) * 128],
                rhs=wbuf[slot][:, c * 512:(c + 1) * 512], start=(c == 0), stop=(c == 7)),
                reads=[(key, c), ("wbuf", slot)], writes=PK(bank))

    def next_proj_bank():
        bk = 2 + rr["proj"] % 3
        rr["proj"] += 1
        return bk

    for b in range(NSEQ):
        dma(gm_bc[:, :], gates_d[b:b + 1, 0:1024].partition_broadcast(128), "gbm", reads=["gates_d"], writes=["gm_bc"])
        dma(gf_bc[:, :], gates_d[b:b + 1, 1024:2048].partition_broadcast(128), "gbf", reads=["gates_d"], writes=["gf_bc"])
        P.op("pool", lambda e: e.memset(rs_run[:, :], 0.0), writes=["rs_run"])
        P.op("pool", lambda e: e.memset(state[:, :, :], 0.0), writes=["state"])
        P.op("pool", lambda e: e.memset(state_bf[:, :, :], 0.0), writes=["state_bf"])

        for g in range(NG):
            row0 = b * S + g * 512
            dma(xs[:, :, :], x_d[row0:row0 + 512, :].rearrange("(t p) d -> p t d", p=128), "xld",
                writes=[("xs", t) for t in range(G)])
            dma(ropeg[:, :, :, :], rope_d[:, :, g * G:(g + 1) * G, :].rearrange("r p t i -> p r t i"), "rope",
                writes=["ropeg"])

            gi = b * NG + g
            if gi == 0:
                for stg_ in prefetch_B(0):
                    stg_()
            hT_cur, hkey = HB[gi % 2], HK[gi % 2]
            hT_oth, okey = HB[(gi + 1) % 2], HK[(gi + 1) % 2]

            first = True
            for t in range(G):
                for c in range(8):
                    P.op("pe", lambda e, c=c, t=t, hT_cur=hT_cur, first=first: e.matmul(
                        PS[7][:, t * 8:(t + 1) * 8], lhsT=hT_cur[:, c, t * 128:(t + 1) * 128],
                        rhs=wfg[:, c, :], start=first, stop=(c == 7), skip_group_check=True),
                        reads=[(hkey, c), "wfg"], writes=PK(7))
                    first = False
            P.op("dve", lambda e: e.tensor_tensor(out=fz[:, 0, :].rearrange("p (t h) -> p t h", h=8),
                                                  in0=PS[7][:, 0:32].rearrange("p (t h) -> p t h", h=8),
                                                  in1=bcmid(bfg_bc[:, :], G), op=ALU.add),
                 reads=PK(7) + ["bfg"], writes=[("fz", 0)])
            P.op("act", lambda e: e.activation(out=fz[:, 1, :], in_=fz[:, 0, :], func=AF.Exp, scale=-1.0),
                 reads=[("fz", 0)], writes=[("fz", 1)])
            P.op("act", lambda e: e.activation(out=fz[:, 2, :], in_=fz[:, 1, :], func=AF.Ln, bias=1.0),
                 reads=[("fz", 1)], writes=[("fz", 2)])
            lall = fz[:, 2, :].rearrange("p (t h) -> p t h", h=8)
            P.op("dve", lambda e: e.tensor_copy(out=pre[:, 0, :], in_=rs_run[:, :]), reads=["rs_run"], writes=["pre"])
            for t in range(1, G):
                P.op("dve", lambda e, t=t: e.tensor_tensor(out=pre[:, t, :], in0=pre[:, t - 1, :], in1=lall[:, t - 1, :],
                                                           op=ALU.add), reads=["pre", ("fz", 2)], writes=["pre"])
            P.op("dve", lambda e: e.tensor_tensor(out=rs_run[:, :], in0=pre[:, G - 1, :], in1=lall[:, G - 1, :], op=ALU.add),
                 reads=["pre", ("fz", 2)], writes=["rs_run"])

            def cum_finish():
                P.op("pe", lambda e: e.matmul(PS[7][:, 32:64], lhsT=trif[:, :], rhs=fz[:, 2, :], start=True, stop=False),
                     reads=[("fz", 2), "trif"], writes=PK(7))
                P.op("pe", lambda e: e.matmul(PS[7][:, 32:64], lhsT=onesf[:, :], rhs=pre[:, :, :].rearrange("p t h -> p (t h)"),
                                              start=False, stop=True), reads=["pre", "onesf"], writes=PK(7))
                ncum = PS[7][:, 32:64].rearrange("p (t h) -> p t h", h=8)
                ck = [("cumsp", t) for t in range(G)]
                cr0 = cr[:, 0, :].rearrange("p (t h) -> p t h", h=8)
                cr1 = cr[:, 1, :].rearrange("p (t h) -> p t h", h=8)
                P.op("dve", lambda e: e.tensor_copy(out=cumsp[:, :, :, 0], in_=ncum), reads=PK(7), writes=ck)
                P.op("dve", lambda e: e.tensor_tensor(out=cr0, in0=ncum, in1=cumsp[:, :, :, 0], op=ALU.subtract),
                     reads=PK(7) + ck, writes=[("cr", 0)])
                P.op("dve", lambda e: e.tensor_copy(out=cumsp[:, :, :, 1], in_=cr0), reads=[("cr", 0)], writes=ck)
                P.op("dve", lambda e: e.tensor_tensor(out=cr1, in0=cr0, in1=cumsp[:, :, :, 1], op=ALU.subtract),
                     reads=[("cr", 0)] + ck, writes=[("cr", 1)])
                P.op("dve", lambda e: e.tensor_copy(out=cumsp[:, :, :, 2], in_=cr1), reads=[("cr", 1)], writes=ck)

            pipe = []

            def pipe_tick():
                keep = []
                for it in list(pipe):
                    it[0] += 1
                    a = it[0]
                    if a - 1 < len(it[1]) and it[1][a - 1] is not None:
                        it[1][a - 1]()
                    if a < len(it[1]):
                        keep.append(it)
                pipe[:] = keep

            def pipe_push(stages):
                pipe_tick()
                pipe.append([0, stages])

            def push_deferred(fn):
                pipe_push([None, fn])

            def flush_deferred():
                while pipe:
                    pipe_tick()

            slot, ci = next_chunk(3)
            for t in range(G):
                bank = next_proj_bank()
                proj_mm(hT_cur, t, slot, bank, hkey)
                tmp, tkey = next_tmp()
                P.op("act", lambda e, tmp=tmp, bank=bank: e.activation(out=tmp[:, :], in_=PS[bank][:, :], func=AF.Sigmoid),
                     reads=PK(bank), writes=[tkey])
                P.op("pool", lambda e, t=t, tmp=tmp: e.tensor_tensor(out=gates_f[:, t, :], in0=tmp[:, :], in1=fxg_bc[:, :],
                                                                     op=ALU.mult),
                     reads=[tkey, "fxg"], writes=[("gates_f", t)])
            after_chunk(ci)
            slot, ci = next_chunk(7)
            for t in range(G):
                bank = next_proj_bank()
                proj_mm(hT_cur, t, slot, bank, hkey)
                tmp, tkey = next_tmp()
                tmp2, tkey2 = next_tmp()
                P.op("act", lambda e, tmp=tmp, bank=bank: e.activation(out=tmp[:, :], in_=PS[bank][:, :], func=AF.Sigmoid),
                     reads=PK(bank), writes=[tkey])
                P.op("dve", lambda e, tmp=tmp, tmp2=tmp2, bank=bank: e.tensor_tensor(out=tmp2[:, :], in0=PS[bank][:, :],
                                                                                    in1=tmp[:, :], op=ALU.mult),
                     reads=[tkey] + PK(bank), writes=[tkey2])
                P.op("pool", lambda e, t=t, tmp2=tmp2: e.tensor_tensor(out=gates_r[:, t, :], in0=tmp2[:, :], in1=rtg_bc[:, :],
                                                                       op=ALU.mult),
                     reads=[tkey2, "rtg"], writes=[("gates_r", t)])
            after_chunk(ci)

            cum_finish()

            def qk_step(t, slot, is_q):
                bank = next_proj_bank()
                proj_mm(hT_cur, t, slot, bank, hkey)
                if is_q:
                    par = rr["qa"] % 3
                    rr["qa"] += 1
                    aug, akey, gcol, gkeys = qaug[par], ("qaug", par), qg_col, ["qg", "qg1"]
                else:
                    par = rr["ka"] % 3
                    rr["ka"] += 1
                    aug, akey, gcol, gkeys = kaug[par], ("kaug", par), kg_col, ["kg", "kg1"]
                tmp, tkey = next_tmp()
                par2 = rr["qkp"] % 2
                rr["qkp"] += 1
                cs, cr_ = 64 + 16 * par2, 72 + 16 * par2
                P.op("act", lambda e: e.activation(out=tmp[:, :], in_=PS[bank][:, :], func=AF.Square),
                     reads=PK(bank), writes=[tkey])
                P.op("dve", lambda e: e.tensor_reduce(out=st[:, cs:cs + 8], in_=tmp[:, :].rearrange("p (h i) -> p h i", i=64),
                                                      axis=AX.X, op=ALU.add), reads=[tkey], writes=[("st", cs)])

                def stage_b():
                    act_rstd(cs, cr_, 8, 1.0 / 64)
                    P.op("dve", lambda e: e.tensor_tensor(out=aug[:, :, 0:64],
                                                          in0=PS[bank][:, :].rearrange("p (h i) -> p h i", i=64),
                                                          in1=bc3(st[:, cr_:cr_ + 8], 64), op=ALU.mult),
                         reads=PK(bank) + [("st", cr_)], writes=[akey])
                    if is_q:
                        P.op("pool", lambda e: e.tensor_scalar(out=aug[:, :, 64:67], in0=cumsp[:, t, :, :], scalar1=-1.0,
                                                               scalar2=None, op0=ALU.mult),
                             reads=[("cumsp", t)], writes=[akey])
                    else:
                        P.op("pool", lambda e: e.tensor_copy(out=aug[:, :, 67:70], in_=cumsp[:, t, :, :]),
                             reads=[("cumsp", t)], writes=[akey])

                def deferred():
                    bq = 5 + rr["tq"] % 2
                    rr["tq"] += 1
                    for h in range(8):
                        P.op("pe", lambda e, h=h: e.transpose(out=Pb(bq)[0:70, h * 128:(h + 1) * 128], in_=aug[:, h, :],
                                                              identity=identb[:, :]),
                             reads=[akey, "identb"], writes=PK(bq))
                    srcv = Pb(bq)[0:70, :].rearrange("p (h n) -> p h n", n=128)
                    if is_q:
                        dst, wk = QT_v[:, :, t * 128:(t + 1) * 128], UK(0, 8)
                    else:
                        blk_i = g * G + t
                        dst, wk = KT[:, :, blk_i * 128:(blk_i + 1) * 128], [("KT", blk_i)]
                    P.op("dve", lambda e: e.tensor_scalar(out=dst, in0=srcv, scalar1=gcol[:, 0:1], scalar2=None,
                                                          op0=ALU.mult),
                         reads=PK(bq) + gkeys, writes=wk)
                pipe_push([stage_b, deferred])

            for is_q, ck_ in ((True, 0), (False, 1)):
                slot, ci = next_chunk(ck_)
                for t in range(G):
                    qk_step(t, slot, is_q)
                after_chunk(ci)

            slot, ci = next_chunk(2)
            for t in range(G):
                bank = next_proj_bank()
                proj_mm(hT_cur, t, slot, bank, hkey)
                blk_i = g * G + t
                act_copy(Vaug[:, blk_i, :, 0:64], PS[bank][:, :].rearrange("p (h i) -> p h i", i=64), PK(bank),
                         [("Vaug", blk_i)])
                pipe_push([])
            after_chunk(ci)
            flush_deferred()

            def side_bank():
                bk = (5, 7)[rr["proj"] % 2]
                rr["proj"] += 1
                return bk

            def rope_step(t, slot, is_q):
                bank = side_bank()
                proj_mm(hT_cur, t, slot, bank, hkey)
                r0 = 0 if is_q else 3
                cosv, sinv, nsinv = ropeg[:, r0, t, :], ropeg[:, r0 + 1, t, :], ropeg[:, r0 + 2, t, :]
                ta, tak = next_tmp()
                tb, tbk = next_tmp()
                pv = PS[bank][:, :].rearrange("p (h w i) -> p h w i", h=4, w=2)
                ta4 = ta[:, :].rearrange("p (h w i) -> p h w i", h=4, w=2)
                tb4 = tb[:, :].rearrange("p (h w i) -> p h w i", h=4, w=2)
                cos4 = cosv.unsqueeze(1).unsqueeze(1).to_broadcast([128, 4, 2, 64])
                P.op("dve", lambda e: e.tensor_tensor(out=ta4, in0=pv, in1=cos4, op=ALU.mult),
                     reads=PK(bank) + ["ropeg"], writes=[tak])
                P.op("dve", lambda e: e.tensor_tensor(out=tb4[:, :, 0, :], in0=pv[:, :, 1, :], in1=bcmid(nsinv, 4),
                                                      op=ALU.mult), reads=PK(bank) + ["ropeg"], writes=[tbk])
                P.op("dve", lambda e: e.tensor_tensor(out=tb4[:, :, 1, :], in0=pv[:, :, 0, :], in1=bcmid(sinv, 4),
                                                      op=ALU.mult), reads=PK(bank) + ["ropeg"], writes=[tbk])
                if is_q:
                    i = rr["rq"] % 3
                    rr["rq"] += 1
                    rt, rtk = rqt_v[i], ("U", rqt_g[i])
                else:
                    i = rr["rk"] % 3
                    rr["rk"] += 1
                    rt, rtk = rkt_v[i], ("U", rkt_g[i])
                P.op("pool", lambda e: e.tensor_tensor(out=rt, in0=ta[:, :], in1=tb[:, :], op=ALU.add),
                     reads=[tak, tbk], writes=[rtk])
                if not is_q:
                    P.op("pool", lambda e: e.tensor_tensor(out=Kz_v[:, t, :].rearrange("p (h i) -> p h i", i=128),
                                                           in0=rt.rearrange("p (h i) -> p h i", i=128),
                                                           in1=bc3(zeta[:, :], 128), op=ALU.mult),
                         reads=[rtk, "zeta"], writes=[("U", 20 + t)])

                def deferred():
                    bq = 6
                    for h in range(4):
                        P.op("pe", lambda e, h=h: e.transpose(out=Pb(bq)[:, h * 128:(h + 1) * 128],
                                                              in_=rt[:, h * 128:(h + 1) * 128], identity=identb[:, :]),
                             reads=[rtk, "identb"], writes=PK(bq))
                    srcv = Pb(bq)[:, 0:512].rearrange("p (h n) -> p h n", n=128)
                    tc_ = slice(t * 128, (t + 1) * 128)
                    if is_q:
                        P.op("dve", lambda e: e.tensor_tensor(out=QTr_v[:, :, tc_], in0=srcv, in1=xibc[:, :, :], op=ALU.mult),
                             reads=PK(bq) + ["xibc"], writes=UK(8, 12))
                    else:
                        P.op("dve", lambda e: e.tensor_tensor(out=KTr_v[:, :, tc_], in0=srcv, in1=ixibc[:, :, :], op=ALU.mult),
                             reads=PK(bq) + ["ixibc"], writes=UK(16, 20))
                return deferred

            def rv_step(t, slot):
                bank = side_bank()
                proj_mm(hT_cur, t, slot, bank, hkey)
                dve_copy(Vr_v[:, t, :], PS[bank][:, :], PK(bank), [("U", 24 + t)])
                return lambda: None

            chunk_state = {}

            def side_steps():
                units = []
                for kind, ck_ in (("rq", 4), ("rk", 5), ("rv", 6)):
                    for t in range(G):
                        units.append((kind, ck_, t))
                return units

            units = side_steps()

            def run_unit(u):
                kind, ck_, t = u
                if t == 0:
                    chunk_state["cur"] = next_chunk(ck_)
                slot, ci = chunk_state["cur"]
                if kind == "rq":
                    push_deferred(rope_step(t, slot, True))
                elif kind == "rk":
                    push_deferred(rope_step(t, slot, False))
                else:
                    push_deferred(rv_step(t, slot))
                if t == G - 1:
                    after_chunk(ci)

            nkb = 4 * g + 4
            tasks = [(h, kb) for h in range(8) for kb in range(nkb)]
            mixed_all = [("mixed", t) for t in range(G)]

            def fox_qk(i):
                h, kb = tasks[i]
                jlo = max(0, kb - 4 * g)
                n = (4 - jlo) * 128
                bank = i % 3
                pt = PT[i % 3]
                diag = kb >= 4 * g
                P.op("pe", lambda e: e.matmul(PS[bank][:, 0:n], lhsT=KT[:, h, kb * 128:(kb + 1) * 128],
                                              rhs=QT_v[:, h, jlo * 128:512], start=True, stop=True),
                     reads=[("KT", kb), ("U", h)], writes=PK(bank))
                if diag:
                    P.op("pe", lambda e: e.matmul(PS[bank][:, 0:128], lhsT=identb[:, :], rhs=negm[:, :],
                                                  start=False, stop=True, skip_group_check=True),
                         reads=["identb", "negm"], writes=PK(bank))
                P.op("act", lambda e: e.activation(out=pt[:, 0:n], in_=PS[bank][:, 0:n], func=AF.Exp),
                     reads=PK(bank), writes=[("PT", i % 3)])

            def fox_pv(i):
                h, kb = tasks[i]
                jlo = max(0, kb - 4 * g)
                ob = 3 + (h % 2)
                pt = PT[i % 3]
                for j in range(jlo, 4):
                    P.op("pe", lambda e, j=j, last=(kb == 4 * g + j): e.matmul(PS[ob][:, j * 65:(j + 1) * 65],
                                                       lhsT=pt[:, (j - jlo) * 128:(j - jlo + 1) * 128],
                                                       rhs=Vaug[:, kb, h, :], start=(kb == 0 and j == 0),
                                                       stop=last, skip_group_check=True),
                         reads=[("PT", i % 3), ("Vaug", kb), "Vaug_ones"], writes=PK(ob))
                if kb == nkb - 1:
                    fox_epilogue(h, ob, i + 2)

            def at(idx, fn):
                sched.setdefault(idx, []).append(fn)

            def fox_epilogue(h, ob, i_now):
                O = PS[ob][:, 0:260].rearrange("p (j e) -> p j e", e=65)
                p_ = h % 2
                oz, ozk = (TE[0], ("TE", 0)) if p_ == 0 else (TE[3], ("TE", 3))
                c_ss, c_rs = 96 + 16 * p_, 104 + 16 * p_
                P.op("dve", lambda e: e.reciprocal(out=st[:, 40:44], in_=O[:, :, 64]), reads=PK(ob), writes=[("st", 40)])
                P.op("dve", lambda e: e.tensor_tensor(out=oz[:, :, :], in0=O[:, :, 0:64], in1=bc3(st[:, 40:44], 64),
                                                      op=ALU.mult), reads=PK(ob) + [("st", 40)], writes=[ozk])
                P.op("pool", lambda e: e.tensor_tensor(out=TE[1][:, :, :], in0=oz[:, :, :], in1=oz[:, :, :], op=ALU.mult),
                     reads=[ozk], writes=[("TE", 1)])
                P.op("dve", lambda e: e.tensor_reduce(out=st[:, c_ss:c_ss + 4], in_=TE[1][:, :, :], axis=AX.X, op=ALU.add),
                     reads=[("TE", 1)], writes=[("st", c_ss)])

                def e1():
                    act_rstd(c_ss, c_rs, 4, 1.0 / 64)

                def e2():
                    P.op("dve", lambda e: e.tensor_tensor(out=TE[2][:, :, :], in0=oz[:, :, :], in1=bc3(st[:, c_rs:c_rs + 4], 64),
                                                          op=ALU.mult), reads=[ozk, ("st", c_rs)], writes=[("TE", 2)])
                    P.op("pool", lambda e: e.tensor_tensor(out=mixed[:, :, h * 64:(h + 1) * 64], in0=TE[2][:, :, :],
                                                           in1=gates_f[:, :, h * 64:(h + 1) * 64], op=ALU.mult),
                         reads=[("TE", 2)] + [("gates_f", t) for t in range(G)], writes=mixed_all)
                at(i_now + 2, e1)
                at(i_now + 3, e2)

            def ret_stage1(j):
                jc = slice(j * 128, (j + 1) * 128)
                for h in range(4):
                    P.op("pe", lambda e, h=h: e.matmul(PS[5][:, h * 128:(h + 1) * 128], lhsT=KTr_v[:, h, jc],
                                                       rhs=QTr_v[:, h, jc], start=(h == 0), stop=True,
                                                       skip_group_check=True),
                         reads=UK(16, 20) + UK(8, 12), writes=PK(5))
                for h in range(4):
                    hc = slice(h * 128, (h + 1) * 128)
                    P.op("pe", lambda e, hc=hc, h=h: e.matmul(PS[7][:, hc], lhsT=Kz_v[:, j, hc], rhs=Vr_v[:, j, hc],
                                                              start=(h == 0), stop=True, skip_group_check=True),
                         reads=[("U", 20 + j), ("U", 24 + j)], writes=PK(7))
                sj = j % 2
                P.op("dve", lambda e: e.tensor_tensor(out=STr[sj][:, :, :],
                                                      in0=PS[5][:, :].rearrange("p (h n) -> p h n", n=128),
                                                      in1=bcmid(trib[:, :], 4), op=ALU.mult),
                     reads=PK(5) + ["trib"], writes=[("STr", sj)])

            def ret_stage2(j):
                jc = slice(j * 128, (j + 1) * 128)
                sj = j % 2
                for h in range(4):
                    hc = slice(h * 128, (h + 1) * 128)
                    P.op("pe", lambda e, hc=hc, h=h: e.matmul(PS[6][:, hc], lhsT=STr[sj][:, h, :], rhs=Vr_v[:, j, hc],
                                                              start=(h == 0), stop=False, skip_group_check=True),
                         reads=[("STr", sj), ("U", 24 + j)], writes=PK(6))
                    P.op("pe", lambda e, hc=hc, h=h: e.matmul(PS[6][:, hc], lhsT=QTr_v[:, h, jc], rhs=state_bf[:, h, :],
                                                              start=False, stop=True, skip_group_check=True),
                         reads=UK(8, 12) + ["state_bf"], writes=PK(6))
                for h in range(4):
                    hc = slice(h * 128, (h + 1) * 128)
                    P.op("dve", lambda e, hc=hc, h=h: e.scalar_tensor_tensor(
                        out=state[:, h, :], in0=state[:, h, :], scalar=g_chunk[h], in1=PS[7][:, hc],
                        op0=ALU.mult, op1=ALU.add), reads=["state"] + PK(7), writes=["state"])
                P.op("pool", lambda e: e.tensor_copy(out=state_bf[:, :, :], in_=state[:, :, :]),
                     reads=["state"], writes=["state_bf"])
                pj = j % 2
                c_ss, c_rs = 16 + 8 * pj, 20 + 8 * pj
                dve_copy(TR[:, :], PS[6][:, :], PK(6), ["TR"])
                tmp, tkey = next_tmp()
                P.op("pool", lambda e: e.tensor_tensor(out=tmp[:, :], in0=TR[:, :], in1=TR[:, :], op=ALU.mult),
                     reads=["TR"], writes=[tkey])
                P.op("dve", lambda e: e.tensor_reduce(out=st[:, c_ss:c_ss + 4], in_=tmp[:, :].rearrange("p (h i) -> p h i", i=128),
                                                      axis=AX.X, op=ALU.add), reads=[tkey], writes=[("st", c_ss)])

                def r2c():
                    act_rstd(c_ss, c_rs, 4, 1.0 / 128)
                    tmp2, tkey2 = next_tmp()
                    P.op("dve", lambda e: e.tensor_tensor(out=tmp2[:, :].rearrange("p (h i) -> p h i", i=128),
                                                          in0=TR[:, :].rearrange("p (h i) -> p h i", i=128),
                                                          in1=bc3(st[:, c_rs:c_rs + 4], 128), op=ALU.mult),
                         reads=["TR", ("st", c_rs)], writes=[tkey2])
                    P.op("pool", lambda e: e.tensor_tensor(out=mixed[:, j, 512:1024], in0=tmp2[:, :], in1=gates_r[:, j, :],
                                                           op=ALU.mult),
                         reads=[tkey2, ("gates_r", j)], writes=[("mixed", j)])
                at(cur_i[0] + 2, r2c)

            ntask = len(tasks)
            sched = {}
            nun = len(units)
            span = max(nun, int(ntask * 0.55))
            for ui, u in enumerate(units):
                sched.setdefault(min(ntask - 1, ui * span // nun), []).append(lambda u=u: run_unit(u))
            sched.setdefault(min(ntask - 1, span), []).append(flush_deferred)
            rem0 = min(ntask - 1, span + 1)
            for j in range(4):
                p1 = rem0 + (2 * j) * (ntask - rem0) // 8
                p2 = rem0 + (2 * j + 1) * (ntask - rem0) // 8
                sched.setdefault(min(ntask - 1, p1), []).append(lambda j=j: ret_stage1(j))
                sched.setdefault(min(ntask - 1, p2), []).append(lambda j=j: ret_stage2(j))
            cur_i = [0]
            for i in range(ntask + 2):
                cur_i[0] = i
                for f in sched.pop(i, []):
                    f()
                if i < ntask:
                    fox_qk(i)
                if i >= 2:
                    fox_pv(i - 2)
            while sched:
                k_ = min(sched)
                cur_i[0] = k_
                for f in sched.pop(k_):
                    f()

            keep_warm(14, 2)
            for c in (0, 1, 2, 4, 5, 6, 7, 3):
                bank = rr["tp"] % 2
                rr["tp"] += 1
                for t in range(G):
                    P.op("pe", lambda e, c=c, t=t, bank=bank: e.transpose(
                        out=Pb(bank)[:, t * 128:(t + 1) * 128], in_=mixed[:, t, c * 128:(c + 1) * 128],
                        identity=identb[:, :]), reads=[("mixed", t), "identb"], writes=PK(bank))
                if c % 2 == 0:
                    act_copy(hT_oth[:, c, :], Pb(bank)[:, 0:512], PK(bank), [(okey, c)])
                else:
                    dve_copy(hT_oth[:, c, :], Pb(bank)[:, 0:512], PK(bank), [(okey, c)])

            for q in range(2):
                slot, ci = next_chunk(8 + q)
                for t in range(G):
                    bank = next_proj_bank()
                    proj_mm(hT_oth, t, slot, bank, okey)
                    tmp, tkey = next_tmp()
                    qc = slice(q * 512, (q + 1) * 512)
                    P.op("dve", lambda e, tmp=tmp, bank=bank, qc=qc: e.tensor_tensor(out=tmp[:, :], in0=PS[bank][:, :],
                                                                                    in1=gm_bc[:, qc], op=ALU.mult),
                         reads=PK(bank) + ["gm_bc"], writes=[tkey])
                    P.op("pool" if t % 2 else "dve", lambda e, tmp=tmp, t=t, qc=qc: e.tensor_tensor(
                        out=xs[:, t, qc], in0=xs[:, t, qc], in1=tmp[:, :], op=ALU.add),
                         reads=[tkey, ("xs", t)], writes=[("xs", t)])
                after_chunk(ci)

            keep_warm(16, 7)
            xn2 = u_f32(16, 16).rearrange("p (t d) -> p t d", t=G)
            for stg_ in rms_chain(b, xs, [[("xs", t)] for t in range(G)], xn2, [UK(16 + 4 * t, 20 + 4 * t) for t in range(G)],
                                  hT_cur, hkey, opm_f, sh_f, 4, 3, [0, 1]):
                stg_()
                keep_warm(2, 7)

            pstages = prefetch_B(gi + 1) if gi + 1 < NSEQ * NG else []
            for j in range(8):
                slot, ci = next_chunk(10 + j)
                for fc in range(4):
                    step_ = 4 * j + fc
                    if pstages and step_ >= 2 and step_ % 2 == 0 and (step_ - 2) // 2 < len(pstages):
                        pstages[(step_ - 2) // 2]()
                    bank = rr["mi"] % 4
                    rr["mi"] += 1
                    tmp, tkey = next_tmp()
                    for c in range(8):
                        P.op("pe", lambda e, c=c, fc=fc, bank=bank, slot=slot, hT_cur=hT_cur: e.matmul(
                            PS[bank][:, :], lhsT=wbuf[slot][:, c * 512 + fc * 128: c * 512 + (fc + 1) * 128],
                            rhs=hT_cur[:, c, :], start=(c == 0), stop=(c == 7)),
                            reads=[(hkey, c), ("wbuf", slot)], writes=PK(bank))
                    P.op("act", lambda e, tmp=tmp, bank=bank: e.activation(out=tmp[:, :], in_=PS[bank][:, :], func=AF.Relu),
                         reads=PK(bank), writes=[tkey])
                    uc = u_chunk(4 * j + fc)
                    P.op("pool", lambda e, tmp=tmp, uc=uc: e.tensor_tensor(out=uc, in0=tmp[:, :], in1=tmp[:, :], op=ALU.mult),
                         reads=[tkey], writes=[("U", 4 * j + fc)])
                after_chunk(ci)

            for j in range(8):
                slot, ci = next_chunk(18 + j)
                for t in range(G):
                    for hf in range(2):
                        bank = t * 2 + hf
                        for fc in range(4):
                            uc = u_chunk(4 * j + fc)
                            P.op("pe", lambda e, uc=uc, t=t, hf=hf, fc=fc, bank=bank, slot=slot, j=j: e.matmul(
                                PS[bank][:, :], lhsT=uc[:, t * 128:(t + 1) * 128],
                                rhs=wbuf[slot][:, fc * 1024 + hf * 512: fc * 1024 + (hf + 1) * 512],
                                start=(j == 0 and fc == 0), stop=(j == 7 and fc == 3)),
                                reads=[("U", 4 * j + fc), ("wbuf", slot)], writes=PK(bank))
                after_chunk(ci)
            done_half = {}
            for (t, hf) in ((3, 1), (1, 0), (1, 1), (2, 0), (0, 0), (0, 1), (2, 1), (3, 0)):
                if True:
                    bank = t * 2 + hf
                    tmp, tkey = next_tmp()
                    qc = slice(hf * 512, (hf + 1) * 512)
                    P.op("dve", lambda e, tmp=tmp, bank=bank, qc=qc: e.tensor_tensor(out=tmp[:, :], in0=PS[bank][:, :],
                                                                                    in1=gf_bc[:, qc], op=ALU.mult),
                         reads=PK(bank) + ["gf_bc"], writes=[tkey])
                    P.op("pool" if hf else "dve", lambda e, tmp=tmp, t=t, qc=qc: e.tensor_tensor(
                        out=xs[:, t, qc], in0=xs[:, t, qc], in1=tmp[:, :], op=ALU.add),
                         reads=[tkey, ("xs", t)], writes=[("xs", t)])
                done_half[t] = done_half.get(t, 0) + 1
                if done_half[t] == 2:
                    tok = dma(y_d[row0 + t * 128: row0 + (t + 1) * 128, :], xs[:, t, :], "yst", reads=[("xs", t)],
                              writes=[("y", row0 + t * 128)])
                    store_toks.append(tok)

    P.wait_all("sp", [max(store_toks, key=lambda tk: tk[1])])
    return nc, P, es, consts


_BUILT = None


def _get_built():
    global _BUILT
    if _BUILT is None:
        nc, P, es, consts = build_program()
        P.emit(nc, es)
        es.close()
        _BUILT = (nc, consts)
    return _BUILT


def kernel(x, c, w_ada, b_ada, w_in, b_forget, q_norm_gain, k_norm_gain, fox_out_gain, ret_out_gain,
           w_out, w_mlp_in, w_mlp_out):
    nc, consts = _get_built()
    f = np.float32
    x = np.asarray(x, f)
    c = np.asarray(c, f)
    shared = {
        "w_ada": np.ascontiguousarray(np.asarray(w_ada, f)[0]),
        "b_ada": np.ascontiguousarray(np.asarray(b_ada, f)[0].reshape(1, -1)),
        "w_in": np.ascontiguousarray(np.asarray(w_in, f)[0]),
        "b_forget": np.ascontiguousarray(np.asarray(b_forget, f)[0].reshape(1, 8)),
        "q_gain": np.ascontiguousarray(np.asarray(q_norm_gain, f)[0].reshape(1, 64)),
        "k_gain": np.ascontiguousarray(np.asarray(k_norm_gain, f)[0].reshape(1, 64)),
        "fox_gain": np.ascontiguousarray(np.asarray(fox_out_gain, f)[0].reshape(1, 512)),
        "ret_gain": np.ascontiguousarray(np.asarray(ret_out_gain, f)[0].reshape(1, 512)),
        "w_out": np.ascontiguousarray(np.asarray(w_out, f)[0]),
        "w1": np.ascontiguousarray(np.asarray(w_mlp_in, f)[0]),
        "w2": np.ascontiguousarray(np.asarray(w_mlp_out, f)[0]),
    }
    for k in ("identf", "identb", "trib", "negm", "trif", "onesf", "ixi_bc", "xi_bc", "zeta_t", "rope"):
        shared[k] = consts[k]
    in_maps = []
    for i in range(NCORES):
        m = dict(shared)
        m["x"] = np.ascontiguousarray(x[i * NSEQ:(i + 1) * NSEQ].reshape(NSEQ * S, D))
        m["c"] = np.ascontiguousarray(c[i * NSEQ:(i + 1) * NSEQ])
        in_maps.append(m)
    res = run_bass_kernel_spmd(nc, in_maps, core_ids=list(range(NCORES)))
    out = np.concatenate([np.asarray(r["y"], f).reshape(NSEQ, S, D) for r in res.results], axis=0)
    return out
```

```python
import numpy as np
import ml_dtypes
from contextlib import ExitStack
import concourse.bass as bass
import concourse.mybir as mybir
from concourse.bass_utils import run_bass_kernel_spmd

F32 = mybir.dt.float32
BF16 = mybir.dt.bfloat16
AF = mybir.ActivationFunctionType
ALU = mybir.AluOpType
AX = mybir.AxisListType

NCORES = 8
D = 1024
S = 2048
NSEQ = 4
NT = 16
G = 4
NG = NT // G
DFF = 4096
EPS = 1e-6
IN_COLS = 4104
NCHUNK = 26

ENGS = ("pe", "act", "dve", "pool", "sp")
import os
STRICT = bool(int(os.environ.get("KSTRICT", "0")))


class _Op:
    __slots__ = ("fn", "waits", "sig", "dma", "tok")


class Prog:
    def __init__(self):
        self.ops = {e: [] for e in ENGS}
        self.res = {}
        self.known = {e: {} for e in ENGS}
        self.clock = {}
        self.dma_count = {}
        self.needed = set()

    def _deps(self, eng, reads, writes):
        deps = set()
        for k in reads:
            r = self.res.get(k)
            if r is not None and r[0] is not None:
                deps.add(r[0])
        for k in writes:
            r = self.res.get(k)
            if r is not None:
                if r[0] is not None and (STRICT or r[0][0] != eng):
                    deps.add(r[0])
                for src, v in r[1].items():
                    if STRICT or src != eng:
                        deps.add((src, v))
        return deps

    def _commit(self, tok, reads, writes):
        for k in reads:
            r = self.res.get(k)
            if r is None:
                r = [None, {}]
                self.res[k] = r
            if r[1].get(tok[0], 0) < tok[1]:
                r[1][tok[0]] = tok[1]
        for k in writes:
            self.res[k] = [tok, {}]

    def op(self, eng, fn, reads=(), writes=(), dma=None):
        deps = self._deps(eng if dma is None else "dma:" + dma, reads, writes)
        kn = self.known[eng]
        waits = []
        best = {}
        for (src, v) in deps:
            if best.get(src, 0) < v:
                best[src] = v
        deps = set(best.items())
        for (src, v) in sorted(deps, key=lambda t: (str(t[0]), t[1])):
            if kn.get(src, 0) >= v:
                continue
            waits.append((src, v))
            self.needed.add((src, v))
            for s2, v2 in self.clock[(src, v)].items():
                if kn.get(s2, 0) < v2:
                    kn[s2] = v2
        o = _Op()
        o.fn = fn
        o.waits = waits
        o.dma = dma
        self.ops[eng].append(o)
        if dma is None:
            tok = (eng, len(self.ops[eng]))
        else:
            src = "dma:" + dma
            self.dma_count[src] = self.dma_count.get(src, 0) + 16
            tok = (src, self.dma_count[src])
        o.tok = tok
        ck = dict(kn)
        ck[tok[0]] = tok[1]
        self.clock[tok] = ck
        self._commit(tok, reads, writes)
        return tok

    def wait_all(self, eng, toks):
        kn = self.known[eng]
        waits = []
        for (src, v) in toks:
            if kn.get(src, 0) >= v:
                continue
            waits.append((src, v))
            self.needed.add((src, v))
            kn[src] = v
        o = _Op()
        o.fn = None
        o.waits = waits
        o.dma = None
        o.tok = None
        self.ops[eng].append(o)

    def emit(self, nc, es):
        sems = {}
        for e in ("pe", "act", "dve", "pool"):
            sems[e] = es.enter_context(nc.semaphore("sem_" + e))
        for src in self.dma_count:
            sems[src] = es.enter_context(nc.semaphore("sem_" + src.replace(":", "_")))
        sigval = {}
        for e in ("pe", "act", "dve", "pool"):
            cnt = 0
            for i, o in enumerate(self.ops[e]):
                o.sig = False
                if o.fn is not None and o.dma is None and (e, i + 1) in self.needed:
                    cnt += 1
                    o.sig = True
                    sigval[(e, i + 1)] = cnt
        blk = es.enter_context(nc.Block())

        def run(e, name):
            for o in self.ops[name]:
                for (src, v) in o.waits:
                    val = v if src.startswith("dma:") else sigval[(src, v)]
                    e.wait_ge(sems[src], val)
                if o.fn is None:
                    continue
                ins = o.fn(e)
                if o.dma is not None:
                    ins.then_inc(sems["dma:" + o.dma], 16)
                elif o.sig:
                    ins.then_inc(sems[name], 1)

        @blk.tensor
        def _(e):
            run(e, "pe")

        @blk.scalar
        def _(e):
            run(e, "act")

        @blk.vector
        def _(e):
            run(e, "dve")

        @blk.gpsimd
        def _(e):
            run(e, "pool")

        @blk.sync
        def _(e):
            run(e, "sp")


def _constants():
    f = np.float32
    n = np.arange(128, dtype=f)
    ident = np.eye(128, dtype=f)
    tri = (n[:, None] <= n[None, :]).astype(f)
    ones = np.ones((128, 128), f)
    h = np.arange(4, dtype=f)
    log_g = np.log(f(1.0) - f(2.0) ** (f(-5.0) - h)).astype(f)
    diff = n[None, :] - n[:, None]
    maskT = np.where(diff[None] >= 0, np.exp(np.maximum(diff, 0.0)[None] * log_g[:, None, None]), 0.0).astype(f)
    xi = np.exp((n[None, :] + 1.0) * log_g[:, None]).astype(f)
    xi_bc = np.ascontiguousarray(np.broadcast_to(xi[None], (128, 4, 128))).astype(f)
    ixi = np.exp(-(n[None, :] + 1.0) * log_g[:, None]).astype(f)
    ixi_bc = np.ascontiguousarray(np.broadcast_to(ixi[None], (128, 4, 128))).astype(f)
    zeta = np.exp((128 - 1.0 - n[None, :]) * log_g[:, None]).astype(f)
    zeta_t = np.ascontiguousarray(zeta.T)
    g_chunk = np.exp(f(128.0) * log_g).astype(f)
    pos = np.arange(S, dtype=f)
    inv_freq = (f(10000.0) ** (-np.arange(0, 128, 2, dtype=f) / f(128))).astype(f)
    ang = (pos[:, None] * inv_freq[None, :]).astype(f)
    cos = np.cos(ang).astype(f)
    sin = np.sin(ang).astype(f)
    ks = f(128.0 ** -0.5)

    def lay(a):
        return np.ascontiguousarray(a.reshape(16, 128, 64).transpose(1, 0, 2))

    rope = np.stack([lay(cos), lay(sin), lay(-sin), lay(cos * ks), lay(sin * ks), lay(-sin * ks)], 0)
    sel = np.zeros((4, 4, 128), f)
    for b in range(4):
        sel[b, b, :] = 1.0
    return dict(
        identf=ident, identb=ident.astype(ml_dtypes.bfloat16), trib=tri.astype(ml_dtypes.bfloat16),
        negm=((1.0 - tri) * -30000.0).astype(ml_dtypes.bfloat16),
        trif=tri, onesf=ones, ixi_bc=ixi_bc, xi_bc=xi_bc, zeta_t=zeta_t,
        rope=np.ascontiguousarray(rope), g_chunk=g_chunk,
    )


_CONST = None


def build_program():
    consts = _constants()
    g_chunk = [float(v) for v in consts["g_chunk"]]
    nc = bass.Bass("TRN2", target_bir_lowering=False)
    P = Prog()
    es = ExitStack()

    def din(name, shape, dt=F32):
        return nc.dram_tensor(name, list(shape), dt, kind="ExternalInput").ap()

    x_d = din("x", [NSEQ * S, D])
    c_d = din("c", [NSEQ, D])
    wada_d = din("w_ada", [D, 6 * D])
    bada_d = din("b_ada", [1, 6 * D])
    win_d = din("w_in", [D, IN_COLS])
    bfg_d = din("b_forget", [1, 8])
    qg_d = din("q_gain", [1, 64])
    kg_d = din("k_gain", [1, 64])
    fxg_d = din("fox_gain", [1, 512])
    rtg_d = din("ret_gain", [1, 512])
    wout_d = din("w_out", [D, D])
    w1_d = din("w1", [D, DFF])
    w2_d = din("w2", [DFF, D])
    identf_d = din("identf", [128, 128])
    identb_d = din("identb", [128, 128], BF16)
    trib_d = din("trib", [128, 128], BF16)
    negm_d = din("negm", [128, 128], BF16)
    trif_d = din("trif", [128, 128])
    onesf_d = din("onesf", [128, 128])
    ixibc_d = din("ixi_bc", [128, 4, 128])
    xibc_d = din("xi_bc", [128, 4, 128])
    zeta_d = din("zeta_t", [128, 4])
    rope_d = din("rope", [6, 128, 16, 64])
    y_d = nc.dram_tensor("y", [NSEQ * S, D], F32, kind="ExternalOutput").ap()
    wbf_d = nc.dram_tensor("wbf", [NCHUNK, 128, 4096], BF16, kind="Internal").ap()
    gates_d = nc.dram_tensor("gates_scr", [4, 2048], F32, kind="Internal").ap()

    def sb(name, shape, dt=F32):
        return es.enter_context(nc.sbuf_tensor(name, list(shape), dt))

    wbuf = [sb(f"wbuf{i}", [128, 4096], BF16) for i in range(3)]
    xs = sb("xs", [128, G, D])
    hTA = sb("hTA", [128, 8, 512], BF16)
    hTB = sb("hTB", [128, 8, 512], BF16)
    U = sb("U", [128, 16384], BF16)
    KT = sb("KT", [70, 8, S], BF16)
    Vaug = sb("Vaug", [128, NT, 8, 65], BF16)
    qaug = [sb(f"qaug{i}", [128, 8, 70], BF16) for i in range(3)]
    kaug = [sb(f"kaug{i}", [128, 8, 70], BF16) for i in range(3)]
    state = sb("state", [128, 4, 128])
    state_bf = sb("state_bf", [128, 4, 128], BF16)
    MG = sb("MG", [128, 8192], BF16)
    mixed = MG[:, 0:4096].rearrange("p (t d) -> p t d", d=1024)
    gates_f = MG[:, 4096:6144].rearrange("p (t d) -> p t d", d=512)
    gates_r = MG[:, 6144:8192].rearrange("p (t d) -> p t d", d=512)
    xnext = MG[:, :].bitcast(F32).rearrange("p (t d) -> p t d", d=1024)
    XNK = [[("mixed", 0), ("mixed", 1)], [("mixed", 2), ("mixed", 3)],
           [("gates_f", t) for t in range(G)], [("gates_r", t) for t in range(G)]]
    PT = [sb(f"PT{i}", [128, 512], BF16) for i in range(3)]
    STr = [sb(f"STr{i}", [128, 4, 128], BF16) for i in range(2)]
    TA = [sb(f"TA{i}", [128, 512]) for i in range(2)]
    TB = [sb(f"TB{i}", [128, 512]) for i in range(2)]
    TE = [sb(f"TE{i}", [128, 4, 64]) for i in range(4)]
    TR = sb("TR", [128, 512])
    ropeg = sb("ropeg", [128, 6, G, 64])
    gm_bc = sb("gm_bc", [128, D])
    gf_bc = sb("gf_bc", [128, D])
    opm_m = sb("opm_m", [128, 8, 4])
    sh_m = sb("sh_m", [128, 8, 4])
    opm_f = sb("opm_f", [128, 8, 4])
    sh_f = sb("sh_f", [128, 8, 4])
    identf = sb("identf_s", [128, 128])
    identb = sb("identb_s", [128, 128], BF16)
    trib = sb("trib_s", [128, 128], BF16)
    negm = sb("negm_s", [128, 128], BF16)
    trif = sb("trif_s", [128, 128])
    onesf = sb("onesf_s", [128, 128])
    ixibc = sb("ixibc_s", [128, 4, 128])
    xibc = sb("xibc_s", [128, 4, 128])
    zeta = sb("zeta_s", [128, 4])
    qg_col = sb("qg_col", [70, 1])
    kg_col = sb("kg_col", [70, 1])
    fxg_bc = sb("fxg_bc", [128, 512])
    rtg_bc = sb("rtg_bc", [128, 512])
    bfg_bc = sb("bfg_bc", [128, 8])
    wfg = sb("wfg", [128, 8, 8], BF16)
    wfg32 = sb("wfg32", [128, 8, 8])
    epsc = sb("epsc", [128, 1])
    st = sb("st", [128, 128])
    rs_run = sb("rs_run", [128, 8])
    fz = sb("fz", [128, 3, 32])
    cr = sb("cr", [128, 2, 32])
    pre = sb("pre", [128, G, 8])
    cumsp = sb("cumsp", [128, G, 8, 3], BF16)
    cactT = sb("cactT", [128, 8, 4])
    ctmp = sb("ctmp", [128, 8, 4])
    ones14 = sb("ones14", [1, 4])

    c4 = xs[0:4, 0, :]
    badar = [TB[0][0:1, :], TB[1][0:1, :]]
    grow = xs[0:4, 1:3, :].rearrange("p t d -> p (t d)")
    PS = [es.enter_context(nc.psum_tensor(f"P{i}", [128, 512], F32)) for i in range(8)]

    def UK(lo, hi):
        return [("U", i) for i in range(lo, hi)]

    def u_chunk(fc):
        return U[:, fc * 512:(fc + 1) * 512]

    def u_f32(lo_gran, n_gran):
        return U[:, lo_gran * 512:(lo_gran + n_gran) * 512].bitcast(F32)

    QT_v = U[0:70, 0:4096].rearrange("p (h n) -> p h n", n=512)
    QTr_v = U[:, 4096:6144].rearrange("p (h n) -> p h n", n=512)
    QxT_v = U[:, 6144:8192].rearrange("p (h n) -> p h n", n=512)
    KTr_v = U[:, 8192:10240].rearrange("p (h n) -> p h n", n=512)
    Kz_v = U[:, 10240:12288].rearrange("p (t n) -> p t n", n=512)
    Vr_v = U[:, 12288:14336].rearrange("p (t n) -> p t n", n=512)
    rqt_v = [U[:, 14336:14848], U[:, 14848:15360], U[:, 6144:6656]]
    rkt_v = [U[:, 15360:15872], U[:, 15872:16384], U[:, 6656:7168]]
    rqt_g = [28, 29, 12]
    rkt_g = [30, 31, 13]

    def PK(i):
        return [("P", i)]

    def Pb(i):
        return PS[i][:, :].bitcast(BF16)

    def dma(out, in_, sem, reads=(), writes=(), eng="sp"):
        return P.op(eng, lambda e, o=out, i=in_: e.dma_start(out=o, in_=i), reads=reads, writes=writes, dma=sem)

    dma(identf[:, :], identf_d, "c0", writes=["identf"])
    dma(identb[:, :], identb_d, "c0", writes=["identb"])
    dma(trib[:, :], trib_d, "c0", writes=["trib"])
    dma(negm[:, :], negm_d, "c0", writes=["negm"])
    dma(trif[:, :], trif_d, "c0", writes=["trif"])
    dma(onesf[:, :], onesf_d, "c0", writes=["onesf"])
    dma(ixibc[:, :, :], ixibc_d, "c0", writes=["ixibc"])
    dma(xibc[:, :, :], xibc_d, "c0", writes=["xibc"])
    dma(zeta[:, :], zeta_d, "c0", writes=["zeta"])
    dma(qg_col[0:64, :], qg_d.rearrange("o d -> d o"), "c0", writes=["qg"])
    dma(kg_col[0:64, :], kg_d.rearrange("o d -> d o"), "c0", writes=["kg"])
    dma(fxg_bc[:, :], fxg_d.partition_broadcast(128), "c0", writes=["fxg"])
    dma(rtg_bc[:, :], rtg_d.partition_broadcast(128), "c0", writes=["rtg"])
    dma(bfg_bc[:, :], bfg_d.partition_broadcast(128), "c0", writes=["bfg"])
    dma(c4, c_d, "c0", writes=["c4", ("xs", 0)])
    dma(wfg32[:, :, :], win_d[:, 2048:2056].rearrange("(c p) n -> p c n", p=128), "c0", writes=["wfg32"])
    c0_total = P.dma_count["dma:c0"]
    for k in ["identf", "identb", "trib", "negm", "trif", "onesf", "ixibc", "xibc", "zeta", "qg", "kg", "fxg",
              "rtg", "bfg", "c4", "wfg32"]:
        P.res[k][0] = ("dma:c0", c0_total)
    P.clock[("dma:c0", c0_total)] = {"dma:c0": c0_total}

    P.op("pool", lambda e: e.memset(epsc[:, :], EPS), writes=["epsc"])
    P.op("pool", lambda e: e.memset(ones14[:, :], 1.0), writes=["ones14"])
    P.op("pool", lambda e: e.memset(Vaug[:, :, :, 64:65], 1.0), writes=["Vaug_ones"])
    for i in range(3):
        P.op("pool", lambda e, i=i: e.memset(qaug[i][:, :, 67:70], 1.0), writes=[("qaug", i)])
        P.op("pool", lambda e, i=i: e.memset(kaug[i][:, :, 64:67], 1.0), writes=[("kaug", i)])
    P.op("dve", lambda e: e.tensor_copy(out=wfg[:, :, :], in_=wfg32[:, :, :]), reads=["wfg32"], writes=["wfg"])
    P.op("pool", lambda e: e.memset(qg_col[64:70, :], 1.0), writes=["qg1"])
    P.op("pool", lambda e: e.memset(kg_col[64:70, :], 1.0), writes=["kg1"])
    P.op("dve", lambda e: e.tensor_scalar(out=qg_col[0:64, :], in0=qg_col[0:64, :], scalar1=0.125, scalar2=None,
                                          op0=ALU.mult), reads=["qg"], writes=["qg"])

    def chunk_src(k):
        if k < 8:
            col0 = [0, 512, 1024, 1536, 2056, 2568, 3080, 3592][k]
            return win_d[:, col0:col0 + 512].rearrange("(c p) n -> p c n", p=128)
        if k < 10:
            q = k - 8
            return wout_d[:, q * 512:(q + 1) * 512].rearrange("(c p) n -> p c n", p=128)
        if k < 18:
            j = k - 10
            return w1_d[:, j * 512:(j + 1) * 512].rearrange("(c p) n -> p c n", p=128)
        j = k - 18
        return w2_d[j * 512:(j + 1) * 512, :].rearrange("(c p) n -> p c n", p=128)

    CAST_ORDER = [3, 7, 0, 1, 2, 4, 5, 6] + list(range(8, NCHUNK))

    def cast_chunk(k, extra_reads=()):
        src_ap = chunk_src(k)
        dst = wbf_d[k].rearrange("p (c n) -> p c n", c=src_ap.shape[1])
        dma(dst, src_ap, f"cst{k}", reads=list(extra_reads), writes=[("wbf", k)], eng="pool")

    for k in CAST_ORDER[:5]:
        cast_chunk(k)

    c4k = [("xs", 0)]
    growk = [("xs", 1), ("xs", 2)]
    mrow = xs[0:4, 3, 0:512]
    mrowk = [("xs", 3)]
    for cc in range(8):
        P.op("pe", lambda e, cc=cc: e.transpose(out=PS[0][:, cc * 4:(cc + 1) * 4], in_=c4[:, cc * 128:(cc + 1) * 128],
                                               identity=identf[0:4, 0:4]),
             reads=["c4", "identf"] + c4k, writes=PK(0))
    P.op("act", lambda e: e.activation(out=ctmp[:, :, :], in_=PS[0][:, 0:32].rearrange("p (c b) -> p c b", b=4),
                                       func=AF.Exp, scale=-1.0), reads=PK(0), writes=["ctmp"])
    P.op("dve", lambda e: e.tensor_scalar(out=ctmp[:, :, :], in0=ctmp[:, :, :], scalar1=1.0, scalar2=None, op0=ALU.add),
         reads=["ctmp"], writes=["ctmp"])
    P.op("dve", lambda e: e.reciprocal(out=ctmp[:, :, :], in_=ctmp[:, :, :]), reads=["ctmp"], writes=["ctmp"])
    P.op("dve", lambda e: e.tensor_tensor(out=cactT[:, :, :], in0=ctmp[:, :, :],
                                          in1=PS[0][:, 0:32].rearrange("p (c b) -> p c b", b=4), op=ALU.mult),
         reads=["ctmp"] + PK(0), writes=["cactT"])

    modT_dst = {0: (sh_m, False), 1: (opm_m, True), 3: (sh_f, False), 4: (opm_f, True)}
    for kb in range(12):
        s = kb % 2
        v = kb // 2
        half = kb % 2
        stg = u_f32(16 * s, 16).rearrange("p (c n) -> p c n", c=8)
        dma(stg, wada_d[:, kb * 512:(kb + 1) * 512].rearrange("(c p) n -> p c n", p=128), f"stg{s}",
            writes=UK(16 * s, 16 * s + 16) + [("wada_blk", kb)])
        rk = UK(16 * s, 16 * s + 16)
        bd = badar[kb % 2]
        bdk = ("TB", kb % 2)
        dma(bd, bada_d[0:1, kb * 512:(kb + 1) * 512], f"bd{kb % 2}", writes=[bdk])
        bank = 3 + (kb % 2)
        for dc in range(8):
            P.op("pe", lambda e, bank=bank, dc=dc, stg=stg: e.matmul(
                PS[bank][0:4, :], lhsT=cactT[:, dc, :], rhs=stg[:, dc, :], start=(dc == 0), stop=False),
                reads=rk + ["cactT"], writes=PK(bank))
        P.op("pe", lambda e, bank=bank, bd=bd: e.matmul(
            PS[bank][0:4, :], lhsT=ones14[0:1, :], rhs=bd[0:1, :],
            start=False, stop=True), reads=[bdk, "ones14"], writes=PK(bank))
        if v in modT_dst:
            dst, plus1 = modT_dst[v]
            P.op("dve", lambda e, bank=bank: e.tensor_copy(out=mrow, in_=PS[bank][0:4, :]), reads=PK(bank), writes=mrowk)
            tb_ = 1 + (kb % 2)
            for ec in range(4):
                P.op("pe", lambda e, tb_=tb_, ec=ec: e.transpose(out=PS[tb_][:, ec * 4:(ec + 1) * 4],
                                                                 in_=mrow[:, ec * 128:(ec + 1) * 128],
                                                                 identity=identf[0:4, 0:4]),
                     reads=mrowk + ["identf"], writes=PK(tb_))
            src_v = PS[tb_][:, 0:16].rearrange("p (c b) -> p c b", b=4)
            dst_v = dst[:, half * 4:(half + 1) * 4, :]
            if plus1:
                P.op("dve", lambda e, o=dst_v, i=src_v: e.tensor_scalar(out=o, in0=i, scalar1=1.0, scalar2=None,
                                                                        op0=ALU.add),
                     reads=PK(tb_), writes=[("modT", v, half)])
            else:
                P.op("dve", lambda e, o=dst_v, i=src_v: e.tensor_copy(out=o, in_=i),
                     reads=PK(tb_), writes=[("modT", v, half)])
        else:
            gi = 0 if v == 2 else 1
            P.op("dve", lambda e, bank=bank, gi=gi, half=half: e.tensor_copy(
                out=grow[:, gi * 1024 + half * 512: gi * 1024 + (half + 1) * 512], in_=PS[bank][0:4, :]),
                reads=PK(bank), writes=growk)
    dma(gates_d, grow, "gsc", reads=growk, writes=["gates_d"])
    for k in CAST_ORDER[5:]:
        cast_chunk(k, extra_reads=[("wada_blk", 10), ("wada_blk", 11)])

    from collections import deque
    GROUP_ORDER = [3, 7, 0, 1, 2, 4, 5, 6] + list(range(8, NCHUNK))
    stream = [k for _ in range(NSEQ * NG) for k in GROUP_ORDER]
    pf = {"next": 0, "slots": {}, "cons": 0}

    def prefetch_upto(n):
        while pf["next"] < min(n, len(stream)):
            i = pf["next"]
            slot = i % 3
            dma(wbuf[slot][:, :], wbf_d[stream[i]], f"wld{slot}", reads=[("wbf", stream[i])], writes=[("wbuf", slot)])
            pf["slots"][i] = slot
            pf["next"] += 1

    def next_chunk(expect):
        i = pf["cons"]
        assert stream[i] == expect, (stream[i], expect)
        prefetch_upto(i + 2)
        pf["cons"] += 1
        return pf["slots"][i], i

    def after_chunk(i):
        prefetch_upto(i + 3)

    rr = {"proj": 0, "tp": 0, "tq": 0, "qa": 0, "ka": 0, "mi": 0, "tmp": 0, "rq": 0, "rk": 0, "qkp": 0}
    store_toks = []
    tmps = [(TA[0], ("TA", 0)), (TB[0], ("TB", 0)), (TA[1], ("TA", 1)), (TB[1], ("TB", 1))]

    def next_tmp():
        r = tmps[rr["tmp"] % 4]
        rr["tmp"] += 1
        return r

    def bc3(ap2, n):
        return ap2.unsqueeze(2).to_broadcast([128, ap2.shape[1], n])

    def bcmid(ap2, m):
        return ap2.unsqueeze(1).to_broadcast([128, m, ap2.shape[1]])

    def act_rstd(c_in, c_out, n, inv):
        P.op("act", lambda e: e.activation(out=st[:, c_out:c_out + n], in_=st[:, c_in:c_in + n], func=AF.Ln,
                                           scale=inv, bias=epsc[:, 0:1]),
             reads=[("st", c_in), "epsc"], writes=[("st", c_out)])
        P.op("act", lambda e: e.activation(out=st[:, c_out:c_out + n], in_=st[:, c_out:c_out + n], func=AF.Exp,
                                           scale=-0.5),
             reads=[("st", c_out)], writes=[("st", c_out)])

    def act_copy(out, in_, reads, writes):
        P.op("act", lambda e: e.activation(out=out, in_=in_, func=AF.Copy), reads=reads, writes=writes)

    def dve_copy(out, in_, reads, writes):
        P.op("dve", lambda e: e.tensor_copy(out=out, in_=in_), reads=reads, writes=writes)

    HB = [hTA, hTB]
    HK = ["hTA", "hTB"]

    def rms_chain(b_, src_t, src_keys, xn, xn_keys, hT, hkey, opm, shf, vs, vh, banks):
        junk = hT[:, 0:2, :].rearrange("p a n -> p (a n)")
        jk = [(hkey, 0), (hkey, 1)]

        def mk_sq(t):
            def f():
                P.op("act", lambda e: e.activation(out=junk, in_=src_t[:, t, :], func=AF.Square,
                                                   accum_out=st[:, t:t + 1]),
                     reads=src_keys[t], writes=jk + [("st", 0)])
            return f

        def norm():
            act_rstd(0, 8, 4, 1.0 / D)
            for t in range(G):
                P.op("dve", lambda e, t=t: e.tensor_scalar(out=xn[:, t, :], in0=src_t[:, t, :], scalar1=st[:, 8 + t:9 + t],
                                                           scalar2=None, op0=ALU.mult),
                     reads=src_keys[t] + [("st", 8)], writes=xn_keys[t])

        def mk(c):
            def stage():
                bank = banks[c % len(banks)]
                for t in range(G):
                    P.op("pe", lambda e, t=t: e.transpose(
                        out=PS[bank][:, t * 128:(t + 1) * 128], in_=xn[:, t, c * 128:(c + 1) * 128],
                        identity=identf[:, :]),
                        reads=xn_keys[t] + ["identf"], writes=PK(bank))
                if c % 2 == 0:
                    P.op("act", lambda e: e.activation(
                        out=hT[:, c, :], in_=PS[bank][:, :], func=AF.Identity,
                        scale=opm[:, c, b_:b_ + 1], bias=shf[:, c, b_:b_ + 1]),
                        reads=PK(bank) + [("modT", vs, c // 4), ("modT", vh, c // 4)], writes=[(hkey, c)])
                else:
                    P.op("dve", lambda e: e.tensor_scalar(
                        out=hT[:, c, :], in0=PS[bank][:, :], scalar1=opm[:, c, b_:b_ + 1],
                        scalar2=shf[:, c, b_:b_ + 1], op0=ALU.mult, op1=ALU.add),
                        reads=PK(bank) + [("modT", vs, c // 4), ("modT", vh, c // 4)], writes=[(hkey, c)])
            return stage
        return [mk_sq(t) for t in range(G)] + [norm] + [mk(c) for c in range(8)]

    def prefetch_B(gi_n):
        b_n, g_n = gi_n // NG, gi_n % NG
        r0 = b_n * S + g_n * 512
        dma(xnext[:, :, :], x_d[r0:r0 + 512, :].rearrange("(t p) d -> p t d", p=128), "xnl",
            writes=[k for ks in XNK for k in ks])
        return rms_chain(b_n, xnext, XNK, xnext, XNK, HB[gi_n % 2], HK[gi_n % 2], opm_m, sh_m, 1, 0, [4, 5, 6, 7])

    def proj_mm(src, t, slot, bank, key):
        for c in range(8):
            P.op("pe", lambda e, c=c: e.matmul(
                PS[bank][:, :], lhsT=src[:, c, t * 128:(t + 1) * 128],
                rhs=wbuf[slot][:, c * 512:(c + 1) * 512], start=(c == 0), stop=(c == 7)),
                reads=[(key, c), ("wbuf", slot)], writes=PK(bank))

    def next_proj_bank():
        bk = 2 + rr["proj"] % 3
        rr["proj"] += 1
        return bk

    for b in range(NSEQ):
        dma(gm_bc[:, :], gates_d[b:b + 1, 0:1024].partition_broadcast(128), "gbm", reads=["gates_d"], writes=["gm_bc"])
        dma(gf_bc[:, :], gates_d[b:b + 1, 1024:2048].partition_broadcast(128), "gbf", reads=["gates_d"], writes=["gf_bc"])
        P.op("pool", lambda e: e.memset(rs_run[:, :], 0.0), writes=["rs_run"])
        P.op("pool", lambda e: e.memset(state[:, :, :], 0.0), writes=["state"])
        P.op("pool", lambda e: e.memset(state_bf[:, :, :], 0.0), writes=["state_bf"])

        for g in range(NG):
            row0 = b * S + g * 512
            dma(xs[:, :, :], x_d[row0:row0 + 512, :].rearrange("(t p) d -> p t d", p=128), "xld",
                writes=[("xs", t) for t in range(G)])
            dma(ropeg[:, :, :, :], rope_d[:, :, g * G:(g + 1) * G, :].rearrange("r p t i -> p r t i"), "rope",
                writes=["ropeg"])

            gi = b * NG + g
            if gi == 0:
                for stg_ in prefetch_B(0):
                    stg_()
            hT_cur, hkey = HB[gi % 2], HK[gi % 2]
            hT_oth, okey = HB[(gi + 1) % 2], HK[(gi + 1) % 2]

            first = True
            for t in range(G):
                for c in range(8):
                    P.op("pe", lambda e, c=c, t=t, hT_cur=hT_cur, first=first: e.matmul(
                        PS[7][:, t * 8:(t + 1) * 8], lhsT=hT_cur[:, c, t * 128:(t + 1) * 128],
                        rhs=wfg[:, c, :], start=first, stop=(c == 7), skip_group_check=True),
                        reads=[(hkey, c), "wfg"], writes=PK(7))
                    first = False
            P.op("dve", lambda e: e.tensor_tensor(out=fz[:, 0, :].rearrange("p (t h) -> p t h", h=8),
                                                  in0=PS[7][:, 0:32].rearrange("p (t h) -> p t h", h=8),
                                                  in1=bcmid(bfg_bc[:, :], G), op=ALU.add),
                 reads=PK(7) + ["bfg"], writes=[("fz", 0)])
            P.op("act", lambda e: e.activation(out=fz[:, 1, :], in_=fz[:, 0, :], func=AF.Exp, scale=-1.0),
                 reads=[("fz", 0)], writes=[("fz", 1)])
            P.op("act", lambda e: e.activation(out=fz[:, 2, :], in_=fz[:, 1, :], func=AF.Ln, bias=1.0),
                 reads=[("fz", 1)], writes=[("fz", 2)])
            lall = fz[:, 2, :].rearrange("p (t h) -> p t h", h=8)
            P.op("dve", lambda e: e.tensor_copy(out=pre[:, 0, :], in_=rs_run[:, :]), reads=["rs_run"], writes=["pre"])
            for t in range(1, G):
                P.op("dve", lambda e, t=t: e.tensor_tensor(out=pre[:, t, :], in0=pre[:, t - 1, :], in1=lall[:, t - 1, :],
                                                           op=ALU.add), reads=["pre", ("fz", 2)], writes=["pre"])
            P.op("dve", lambda e: e.tensor_tensor(out=rs_run[:, :], in0=pre[:, G - 1, :], in1=lall[:, G - 1, :], op=ALU.add),
                 reads=["pre", ("fz", 2)], writes=["rs_run"])

            def cum_finish():
                P.op("pe", lambda e: e.matmul(PS[7][:, 32:64], lhsT=trif[:, :], rhs=fz[:, 2, :], start=True, stop=False),
                     reads=[("fz", 2), "trif"], writes=PK(7))
                P.op("pe", lambda e: e.matmul(PS[7][:, 32:64], lhsT=onesf[:, :], rhs=pre[:, :, :].rearrange("p t h -> p (t h)"),
                                              start=False, stop=True), reads=["pre", "onesf"], writes=PK(7))
                ncum = PS[7][:, 32:64].rearrange("p (t h) -> p t h", h=8)
                ck = [("cumsp", t) for t in range(G)]
                cr0 = cr[:, 0, :].rearrange("p (t h) -> p t h", h=8)
                cr1 = cr[:, 1, :].rearrange("p (t h) -> p t h", h=8)
                P.op("dve", lambda e: e.tensor_copy(out=cumsp[:, :, :, 0], in_=ncum), reads=PK(7), writes=ck)
                P.op("dve", lambda e: e.tensor_tensor(out=cr0, in0=ncum, in1=cumsp[:, :, :, 0], op=ALU.subtract),
                     reads=PK(7) + ck, writes=[("cr", 0)])
                P.op("dve", lambda e: e.tensor_copy(out=cumsp[:, :, :, 1], in_=cr0), reads=[("cr", 0)], writes=ck)
                P.op("dve", lambda e: e.tensor_tensor(out=cr1, in0=cr0, in1=cumsp[:, :, :, 1], op=ALU.subtract),
                     reads=[("cr", 0)] + ck, writes=[("cr", 1)])
                P.op("dve", lambda e: e.tensor_copy(out=cumsp[:, :, :, 2], in_=cr1), reads=[("cr", 1)], writes=ck)

            pipe = []

            def pipe_tick():
                keep = []
                for it in list(pipe):
                    it[0] += 1
                    a = it[0]
                    if a - 1 < len(it[1]) and it[1][a - 1] is not None:
                        it[1][a - 1]()
                    if a < len(it[1]):
                        keep.append(it)
                pipe[:] = keep

            def pipe_push(stages):
                pipe_tick()
                pipe.append([0, stages])

            def push_deferred(fn):
                pipe_push([None, fn])

            def flush_deferred():
                while pipe:
                    pipe_tick()

            slot, ci = next_chunk(3)
            for t in range(G):
                bank = next_proj_bank()
                proj_mm(hT_cur, t, slot, bank, hkey)
                tmp, tkey = next_tmp()
                P.op("act", lambda e, tmp=tmp, bank=bank: e.activation(out=tmp[:, :], in_=PS[bank][:, :], func=AF.Sigmoid),
                     reads=PK(bank), writes=[tkey])
                P.op("pool", lambda e, t=t, tmp=tmp: e.tensor_tensor(out=gates_f[:, t, :], in0=tmp[:, :], in1=fxg_bc[:, :],
                                                                     op=ALU.mult),
                     reads=[tkey, "fxg"], writes=[("gates_f", t)])
            after_chunk(ci)
            slot, ci = next_chunk(7)
            for t in range(G):
                bank = next_proj_bank()
                proj_mm(hT_cur, t, slot, bank, hkey)
                tmp, tkey = next_tmp()
                tmp2, tkey2 = next_tmp()
                P.op("act", lambda e, tmp=tmp, bank=bank: e.activation(out=tmp[:, :], in_=PS[bank][:, :], func=AF.Sigmoid),
                     reads=PK(bank), writes=[tkey])
                P.op("dve", lambda e, tmp=tmp, tmp2=tmp2, bank=bank: e.tensor_tensor(out=tmp2[:, :], in0=PS[bank][:, :],
                                                                                    in1=tmp[:, :], op=ALU.mult),
                     reads=[tkey] + PK(bank), writes=[tkey2])
                P.op("pool", lambda e, t=t, tmp2=tmp2: e.tensor_tensor(out=gates_r[:, t, :], in0=tmp2[:, :], in1=rtg_bc[:, :],
                                                                       op=ALU.mult),
                     reads=[tkey2, "rtg"], writes=[("gates_r", t)])
            after_chunk(ci)

            cum_finish()

            def qk_step(t, slot, is_q):
                bank = next_proj_bank()
                proj_mm(hT_cur, t, slot, bank, hkey)
                if is_q:
                    par = rr["qa"] % 3
                    rr["qa"] += 1
                    aug, akey, gcol, gkeys = qaug[par], ("qaug", par), qg_col, ["qg", "qg1"]
                else:
                    par = rr["ka"] % 3
                    rr["ka"] += 1
                    aug, akey, gcol, gkeys = kaug[par], ("kaug", par), kg_col, ["kg", "kg1"]
                tmp, tkey = next_tmp()
                par2 = rr["qkp"] % 2
                rr["qkp"] += 1
                cs, cr_ = 64 + 16 * par2, 72 + 16 * par2
                P.op("act", lambda e: e.activation(out=tmp[:, :], in_=PS[bank][:, :], func=AF.Square),
                     reads=PK(bank), writes=[tkey])
                P.op("dve", lambda e: e.tensor_reduce(out=st[:, cs:cs + 8], in_=tmp[:, :].rearrange("p (h i) -> p h i", i=64),
                                                      axis=AX.X, op=ALU.add), reads=[tkey], writes=[("st", cs)])

                def stage_b():
                    act_rstd(cs, cr_, 8, 1.0 / 64)
                    P.op("dve", lambda e: e.tensor_tensor(out=aug[:, :, 0:64],
                                                          in0=PS[bank][:, :].rearrange("p (h i) -> p h i", i=64),
                                                          in1=bc3(st[:, cr_:cr_ + 8], 64), op=ALU.mult),
                         reads=PK(bank) + [("st", cr_)], writes=[akey])
                    if is_q:
                        P.op("pool", lambda e: e.tensor_scalar(out=aug[:, :, 64:67], in0=cumsp[:, t, :, :], scalar1=-1.0,
                                                               scalar2=None, op0=ALU.mult),
                             reads=[("cumsp", t)], writes=[akey])
                    else:
                        P.op("pool", lambda e: e.tensor_copy(out=aug[:, :, 67:70], in_=cumsp[:, t, :, :]),
                             reads=[("cumsp", t)], writes=[akey])

                def deferred():
                    bq = 5 + rr["tq"] % 2
                    rr["tq"] += 1
                    for h in range(8):
                        P.op("pe", lambda e, h=h: e.transpose(out=Pb(bq)[0:70, h * 128:(h + 1) * 128], in_=aug[:, h, :],
                                                              identity=identb[:, :]),
                             reads=[akey, "identb"], writes=PK(bq))
                    srcv = Pb(bq)[0:70, :].rearrange("p (h n) -> p h n", n=128)
                    if is_q:
                        dst, wk = QT_v[:, :, t * 128:(t + 1) * 128], UK(0, 8)
                    else:
                        blk_i = g * G + t
                        dst, wk = KT[:, :, blk_i * 128:(blk_i + 1) * 128], [("KT", blk_i)]
                    P.op("dve", lambda e: e.tensor_scalar(out=dst, in0=srcv, scalar1=gcol[:, 0:1], scalar2=None,
                                                          op0=ALU.mult),
                         reads=PK(bq) + gkeys, writes=wk)
                pipe_push([stage_b, deferred])

            for is_q, ck_ in ((True, 0), (False, 1)):
                slot, ci = next_chunk(ck_)
                for t in range(G):
                    qk_step(t, slot, is_q)
                after_chunk(ci)

            slot, ci = next_chunk(2)
            for t in range(G):
                bank = next_proj_bank()
                proj_mm(hT_cur, t, slot, bank, hkey)
                blk_i = g * G + t
                act_copy(Vaug[:, blk_i, :, 0:64], PS[bank][:, :].rearrange("p (h i) -> p h i", i=64), PK(bank),
                         [("Vaug", blk_i)])
                pipe_push([])
            after_chunk(ci)
            flush_deferred()

            def side_bank():
                bk = (5, 7)[rr["proj"] % 2]
                rr["proj"] += 1
                return bk

            def rope_step(t, slot, is_q):
                bank = side_bank()
                proj_mm(hT_cur, t, slot, bank, hkey)
                r0 = 0 if is_q else 3
                cosv, sinv, nsinv = ropeg[:, r0, t, :], ropeg[:, r0 + 1, t, :], ropeg[:, r0 + 2, t, :]
                ta, tak = next_tmp()
                tb, tbk = next_tmp()
                pv = PS[bank][:, :].rearrange("p (h w i) -> p h w i", h=4, w=2)
                ta4 = ta[:, :].rearrange("p (h w i) -> p h w i", h=4, w=2)
                tb4 = tb[:, :].rearrange("p (h w i) -> p h w i", h=4, w=2)
                cos4 = cosv.unsqueeze(1).unsqueeze(1).to_broadcast([128, 4, 2, 64])
                P.op("dve", lambda e: e.tensor_tensor(out=ta4, in0=pv, in1=cos4, op=ALU.mult),
                     reads=PK(bank) + ["ropeg"], writes=[tak])
                P.op("dve", lambda e: e.tensor_tensor(out=tb4[:, :, 0, :], in0=pv[:, :, 1, :], in1=bcmid(nsinv, 4),
                                                      op=ALU.mult), reads=PK(bank) + ["ropeg"], writes=[tbk])
                P.op("dve", lambda e: e.tensor_tensor(out=tb4[:, :, 1, :], in0=pv[:, :, 0, :], in1=bcmid(sinv, 4),
                                                      op=ALU.mult), reads=PK(bank) + ["ropeg"], writes=[tbk])
                if is_q:
                    i = rr["rq"] % 3
                    rr["rq"] += 1
                    rt, rtk = rqt_v[i], ("U", rqt_g[i])
                else:
                    i = rr["rk"] % 3
                    rr["rk"] += 1
                    rt, rtk = rkt_v[i], ("U", rkt_g[i])
                P.op("pool", lambda e: e.tensor_tensor(out=rt, in0=ta[:, :], in1=tb[:, :], op=ALU.add),
                     reads=[tak, tbk], writes=[rtk])
                if not is_q:
                    P.op("pool", lambda e: e.tensor_tensor(out=Kz_v[:, t, :].rearrange("p (h i) -> p h i", i=128),
                                                           in0=rt.rearrange("p (h i) -> p h i", i=128),
                                                           in1=bc3(zeta[:, :], 128), op=ALU.mult),
                         reads=[rtk, "zeta"], writes=[("U", 20 + t)])

                def deferred():
                    bq = 6
                    for h in range(4):
                        P.op("pe", lambda e, h=h: e.transpose(out=Pb(bq)[:, h * 128:(h + 1) * 128],
                                                              in_=rt[:, h * 128:(h + 1) * 128], identity=identb[:, :]),
                             reads=[rtk, "identb"], writes=PK(bq))
                    srcv = Pb(bq)[:, 0:512].rearrange("p (h n) -> p h n", n=128)
                    tc_ = slice(t * 128, (t + 1) * 128)
                    if is_q:
                        P.op("dve", lambda e: e.tensor_tensor(out=QTr_v[:, :, tc_], in0=srcv, in1=xibc[:, :, :], op=ALU.mult),
                             reads=PK(bq) + ["xibc"], writes=UK(8, 12))
                    else:
                        P.op("dve", lambda e: e.tensor_tensor(out=KTr_v[:, :, tc_], in0=srcv, in1=ixibc[:, :, :], op=ALU.mult),
                             reads=PK(bq) + ["ixibc"], writes=UK(16, 20))
                return deferred

            def rv_step(t, slot):
                bank = side_bank()
                proj_mm(hT_cur, t, slot, bank, hkey)
                dve_copy(Vr_v[:, t, :], PS[bank][:, :], PK(bank), [("U", 24 + t)])
                return lambda: None

            chunk_state = {}

            def side_steps():
                units = []
                for kind, ck_ in (("rq", 4), ("rk", 5), ("rv", 6)):
                    for t in range(G):
                        units.append((kind, ck_, t))
                return units

            units = side_steps()

            def run_unit(u):
                kind, ck_, t = u
                if t == 0:
                    chunk_state["cur"] = next_chunk(ck_)
                slot, ci = chunk_state["cur"]
                if kind == "rq":
                    push_deferred(rope_step(t, slot, True))
                elif kind == "rk":
                    push_deferred(rope_step(t, slot, False))
                else:
                    push_deferred(rv_step(t, slot))
                if t == G - 1:
                    after_chunk(ci)

            nkb = 4 * g + 4
            tasks = [(h, kb) for h in range(8) for kb in range(nkb)]
            mixed_all = [("mixed", t) for t in range(G)]

            def fox_qk(i):
                h, kb = tasks[i]
                jlo = max(0, kb - 4 * g)
                n = (4 - jlo) * 128
                bank = i % 3
                pt = PT[i % 3]
                diag = kb >= 4 * g
                P.op("pe", lambda e: e.matmul(PS[bank][:, 0:n], lhsT=KT[:, h, kb * 128:(kb + 1) * 128],
                                              rhs=QT_v[:, h, jlo * 128:512], start=True, stop=True),
                     reads=[("KT", kb), ("U", h)], writes=PK(bank))
                if diag:
                    P.op("pe", lambda e: e.matmul(PS[bank][:, 0:128], lhsT=identb[:, :], rhs=negm[:, :],
                                                  start=False, stop=True, skip_group_check=True),
                         reads=["identb", "negm"], writes=PK(bank))
                P.op("act", lambda e: e.activation(out=pt[:, 0:n], in_=PS[bank][:, 0:n], func=AF.Exp),
                     reads=PK(bank), writes=[("PT", i % 3)])

            def fox_pv(i):
                h, kb = tasks[i]
                jlo = max(0, kb - 4 * g)
                ob = 3 + (h % 2)
                pt = PT[i % 3]
                for j in range(jlo, 4):
                    P.op("pe", lambda e, j=j, last=(kb == 4 * g + j): e.matmul(PS[ob][:, j * 65:(j + 1) * 65],
                                                       lhsT=pt[:, (j - jlo) * 128:(j - jlo + 1) * 128],
                                                       rhs=Vaug[:, kb, h, :], start=(kb == 0 and j == 0),
                                                       stop=last, skip_group_check=True),
                         reads=[("PT", i % 3), ("Vaug", kb), "Vaug_ones"], writes=PK(ob))
                if kb == nkb - 1:
                    fox_epilogue(h, ob, i + 2)

            def at(idx, fn):
                sched.setdefault(idx, []).append(fn)

            def fox_epilogue(h, ob, i_now):
                O = PS[ob][:, 0:260].rearrange("p (j e) -> p j e", e=65)
                p_ = h % 2
                oz, ozk = (TE[0], ("TE", 0)) if p_ == 0 else (TE[3], ("TE", 3))
                c_ss, c_rs = 96 + 16 * p_, 104 + 16 * p_
                P.op("dve", lambda e: e.reciprocal(out=st[:, 40:44], in_=O[:, :, 64]), reads=PK(ob), writes=[("st", 40)])
                P.op("dve", lambda e: e.tensor_tensor(out=oz[:, :, :], in0=O[:, :, 0:64], in1=bc3(st[:, 40:44], 64),
                                                      op=ALU.mult), reads=PK(ob) + [("st", 40)], writes=[ozk])
                P.op("pool", lambda e: e.tensor_tensor(out=TE[1][:, :, :], in0=oz[:, :, :], in1=oz[:, :, :], op=ALU.mult),
                     reads=[ozk], writes=[("TE", 1)])
                P.op("dve", lambda e: e.tensor_reduce(out=st[:, c_ss:c_ss + 4], in_=TE[1][:, :, :], axis=AX.X, op=ALU.add),
                     reads=[("TE", 1)], writes=[("st", c_ss)])

                def e1():
                    act_rstd(c_ss, c_rs, 4, 1.0 / 64)

                def e2():
                    P.op("dve", lambda e: e.tensor_tensor(out=TE[2][:, :, :], in0=oz[:, :, :], in1=bc3(st[:, c_rs:c_rs + 4], 64),
                                                          op=ALU.mult), reads=[ozk, ("st", c_rs)], writes=[("TE", 2)])
                    P.op("pool", lambda e: e.tensor_tensor(out=mixed[:, :, h * 64:(h + 1) * 64], in0=TE[2][:, :, :],
                                                           in1=gates_f[:, :, h * 64:(h + 1) * 64], op=ALU.mult),
                         reads=[("TE", 2)] + [("gates_f", t) for t in range(G)], writes=mixed_all)
                at(i_now + 2, e1)
                at(i_now + 3, e2)

            def ret_stage1(j):
                jc = slice(j * 128, (j + 1) * 128)
                for h in range(4):
                    P.op("pe", lambda e, h=h: e.matmul(PS[5][:, h * 128:(h + 1) * 128], lhsT=KTr_v[:, h, jc],
                                                       rhs=QTr_v[:, h, jc], start=(h == 0), stop=True,
                                                       skip_group_check=True),
                         reads=UK(16, 20) + UK(8, 12), writes=PK(5))
                for h in range(4):
                    hc = slice(h * 128, (h + 1) * 128)
                    P.op("pe", lambda e, hc=hc, h=h: e.matmul(PS[7][:, hc], lhsT=Kz_v[:, j, hc], rhs=Vr_v[:, j, hc],
                                                              start=(h == 0), stop=True, skip_group_check=True),
                         reads=[("U", 20 + j), ("U", 24 + j)], writes=PK(7))
                sj = j % 2
                P.op("dve", lambda e: e.tensor_tensor(out=STr[sj][:, :, :],
                                                      in0=PS[5][:, :].rearrange("p (h n) -> p h n", n=128),
                                                      in1=bcmid(trib[:, :], 4), op=ALU.mult),
                     reads=PK(5) + ["trib"], writes=[("STr", sj)])

            def ret_stage2(j):
                jc = slice(j * 128, (j + 1) * 128)
                sj = j % 2
                for h in range(4):
                    hc = slice(h * 128, (h + 1) * 128)
                    P.op("pe", lambda e, hc=hc, h=h: e.matmul(PS[6][:, hc], lhsT=STr[sj][:, h, :], rhs=Vr_v[:, j, hc],
                                                              start=(h == 0), stop=False, skip_group_check=True),
                         reads=[("STr", sj), ("U", 24 + j)], writes=PK(6))
                    P.op("pe", lambda e, hc=hc, h=h: e.matmul(PS[6][:, hc], lhsT=QTr_v[:, h, jc], rhs=state_bf[:, h, :],
                                                              start=False, stop=True, skip_group_check=True),
                         reads=UK(8, 12) + ["state_bf"], writes=PK(6))
                for h in range(4):
                    hc = slice(h * 128, (h + 1) * 128)
                    P.op("dve", lambda e, hc=hc, h=h: e.scalar_tensor_tensor(
                        out=state[:, h, :], in0=state[:, h, :], scalar=g_chunk[h], in1=PS[7][:, hc],
                        op0=ALU.mult, op1=ALU.add), reads=["state"] + PK(7), writes=["state"])
                P.op("pool", lambda e: e.tensor_copy(out=state_bf[:, :, :], in_=state[:, :, :]),
                     reads=["state"], writes=["state_bf"])
                pj = j % 2
                c_ss, c_rs = 16 + 8 * pj, 20 + 8 * pj
                dve_copy(TR[:, :], PS[6][:, :], PK(6), ["TR"])
                tmp, tkey = next_tmp()
                P.op("pool", lambda e: e.tensor_tensor(out=tmp[:, :], in0=TR[:, :], in1=TR[:, :], op=ALU.mult),
                     reads=["TR"], writes=[tkey])
                P.op("dve", lambda e: e.tensor_reduce(out=st[:, c_ss:c_ss + 4], in_=tmp[:, :].rearrange("p (h i) -> p h i", i=128),
                                                      axis=AX.X, op=ALU.add), reads=[tkey], writes=[("st", c_ss)])

                def r2c():
                    act_rstd(c_ss, c_rs, 4, 1.0 / 128)
                    tmp2, tkey2 = next_tmp()
                    P.op("dve", lambda e: e.tensor_tensor(out=tmp2[:, :].rearrange("p (h i) -> p h i", i=128),
                                                          in0=TR[:, :].rearrange("p (h i) -> p h i", i=128),
                                                          in1=bc3(st[:, c_rs:c_rs + 4], 128), op=ALU.mult),
                         reads=["TR", ("st", c_rs)], writes=[tkey2])
                    P.op("pool", lambda e: e.tensor_tensor(out=mixed[:, j, 512:1024], in0=tmp2[:, :], in1=gates_r[:, j, :],
                                                           op=ALU.mult),
                         reads=[tkey2, ("gates_r", j)], writes=[("mixed", j)])
                at(cur_i[0] + 2, r2c)

            ntask = len(tasks)
            sched = {}
            nun = len(units)
            span = max(nun, int(ntask * 0.55))
            for ui, u in enumerate(units):
                sched.setdefault(min(ntask - 1, ui * span // nun), []).append(lambda u=u: run_unit(u))
            sched.setdefault(min(ntask - 1, span), []).append(flush_deferred)
            rem0 = min(ntask - 1, span + 1)
            for j in range(4):
                p1 = rem0 + (2 * j) * (ntask - rem0) // 8
                p2 = rem0 + (2 * j + 1) * (ntask - rem0) // 8
                sched.setdefault(min(ntask - 1, p1), []).append(lambda j=j: ret_stage1(j))
                sched.setdefault(min(ntask - 1, p2), []).append(lambda j=j: ret_stage2(j))
            cur_i = [0]
            for i in range(ntask + 2):
                cur_i[0] = i
                for f in sched.pop(i, []):
                    f()
                if i < ntask:
                    fox_qk(i)
                if i >= 2:
                    fox_pv(i - 2)
            while sched:
                k_ = min(sched)
                cur_i[0] = k_
                for f in sched.pop(k_):
                    f()

            for c in (0, 1, 2, 4, 5, 6, 7, 3):
                bank = rr["tp"] % 2
                rr["tp"] += 1
                for t in range(G):
                    P.op("pe", lambda e, c=c, t=t, bank=bank: e.transpose(
                        out=Pb(bank)[:, t * 128:(t + 1) * 128], in_=mixed[:, t, c * 128:(c + 1) * 128],
                        identity=identb[:, :]), reads=[("mixed", t), "identb"], writes=PK(bank))
                if c % 2 == 0:
                    act_copy(hT_oth[:, c, :], Pb(bank)[:, 0:512], PK(bank), [(okey, c)])
                else:
                    dve_copy(hT_oth[:, c, :], Pb(bank)[:, 0:512], PK(bank), [(okey, c)])

            for q in range(2):
                slot, ci = next_chunk(8 + q)
                for t in range(G):
                    bank = next_proj_bank()
                    proj_mm(hT_oth, t, slot, bank, okey)
                    tmp, tkey = next_tmp()
                    qc = slice(q * 512, (q + 1) * 512)
                    P.op("dve", lambda e, tmp=tmp, bank=bank, qc=qc: e.tensor_tensor(out=tmp[:, :], in0=PS[bank][:, :],
                                                                                    in1=gm_bc[:, qc], op=ALU.mult),
                         reads=PK(bank) + ["gm_bc"], writes=[tkey])
                    P.op("pool" if t % 2 else "dve", lambda e, tmp=tmp, t=t, qc=qc: e.tensor_tensor(
                        out=xs[:, t, qc], in0=xs[:, t, qc], in1=tmp[:, :], op=ALU.add),
                         reads=[tkey, ("xs", t)], writes=[("xs", t)])
                after_chunk(ci)

            xn2 = u_f32(16, 16).rearrange("p (t d) -> p t d", t=G)
            for stg_ in rms_chain(b, xs, [[("xs", t)] for t in range(G)], xn2, [UK(16 + 4 * t, 20 + 4 * t) for t in range(G)],
                                  hT_cur, hkey, opm_f, sh_f, 4, 3, [0, 1]):
                stg_()

            pstages = prefetch_B(gi + 1) if gi + 1 < NSEQ * NG else []
            for j in range(8):
                slot, ci = next_chunk(10 + j)
                for fc in range(4):
                    step_ = 4 * j + fc
                    if pstages and step_ >= 2 and step_ % 2 == 0 and (step_ - 2) // 2 < len(pstages):
                        pstages[(step_ - 2) // 2]()
                    bank = rr["mi"] % 4
                    rr["mi"] += 1
                    tmp, tkey = next_tmp()
                    for c in range(8):
                        P.op("pe", lambda e, c=c, fc=fc, bank=bank, slot=slot, hT_cur=hT_cur: e.matmul(
                            PS[bank][:, :], lhsT=wbuf[slot][:, c * 512 + fc * 128: c * 512 + (fc + 1) * 128],
                            rhs=hT_cur[:, c, :], start=(c == 0), stop=(c == 7)),
                            reads=[(hkey, c), ("wbuf", slot)], writes=PK(bank))
                    P.op("act", lambda e, tmp=tmp, bank=bank: e.activation(out=tmp[:, :], in_=PS[bank][:, :], func=AF.Relu),
                         reads=PK(bank), writes=[tkey])
                    uc = u_chunk(4 * j + fc)
                    P.op("pool", lambda e, tmp=tmp, uc=uc: e.tensor_tensor(out=uc, in0=tmp[:, :], in1=tmp[:, :], op=ALU.mult),
                         reads=[tkey], writes=[("U", 4 * j + fc)])
                after_chunk(ci)

            for j in range(8):
                slot, ci = next_chunk(18 + j)
                for t in range(G):
                    for hf in range(2):
                        bank = t * 2 + hf
                        for fc in range(4):
                            uc = u_chunk(4 * j + fc)
                            P.op("pe", lambda e, uc=uc, t=t, hf=hf, fc=fc, bank=bank, slot=slot, j=j: e.matmul(
                                PS[bank][:, :], lhsT=uc[:, t * 128:(t + 1) * 128],
                                rhs=wbuf[slot][:, fc * 1024 + hf * 512: fc * 1024 + (hf + 1) * 512],
                                start=(j == 0 and fc == 0), stop=(j == 7 and fc == 3)),
                                reads=[("U", 4 * j + fc), ("wbuf", slot)], writes=PK(bank))
                after_chunk(ci)
            done_half = {}
            for (t, hf) in ((3, 1), (1, 0), (1, 1), (2, 0), (0, 0), (0, 1), (2, 1), (3, 0)):
                if True:
                    bank = t * 2 + hf
                    tmp, tkey = next_tmp()
                    qc = slice(hf * 512, (hf + 1) * 512)
                    P.op("dve", lambda e, tmp=tmp, bank=bank, qc=qc: e.tensor_tensor(out=tmp[:, :], in0=PS[bank][:, :],
                                                                                    in1=gf_bc[:, qc], op=ALU.mult),
                         reads=PK(bank) + ["gf_bc"], writes=[tkey])
                    P.op("pool" if hf else "dve", lambda e, tmp=tmp, t=t, qc=qc: e.tensor_tensor(
                        out=xs[:, t, qc], in0=xs[:, t, qc], in1=tmp[:, :], op=ALU.add),
                         reads=[tkey, ("xs", t)], writes=[("xs", t)])
                done_half[t] = done_half.get(t, 0) + 1
                if done_half[t] == 2:
                    tok = dma(y_d[row0 + t * 128: row0 + (t + 1) * 128, :], xs[:, t, :], "yst", reads=[("xs", t)],
                              writes=[("y", row0 + t * 128)])
                    store_toks.append(tok)

    P.wait_all("sp", [max(store_toks, key=lambda tk: tk[1])])
    return nc, P, es, consts


_BUILT = None


def _get_built():
    global _BUILT
    if _BUILT is None:
        nc, P, es, consts = build_program()
        P.emit(nc, es)
        es.close()
        _BUILT = (nc, consts)
    return _BUILT


def kernel(x, c, w_ada, b_ada, w_in, b_forget, q_norm_gain, k_norm_gain, fox_out_gain, ret_out_gain,
           w_out, w_mlp_in, w_mlp_out):
    nc, consts = _get_built()
    f = np.float32
    x = np.asarray(x, f)
    c = np.asarray(c, f)
    shared = {
        "w_ada": np.ascontiguousarray(np.asarray(w_ada, f)[0]),
        "b_ada": np.ascontiguousarray(np.asarray(b_ada, f)[0].reshape(1, -1)),
        "w_in": np.ascontiguousarray(np.asarray(w_in, f)[0]),
        "b_forget": np.ascontiguousarray(np.asarray(b_forget, f)[0].reshape(1, 8)),
        "q_gain": np.ascontiguousarray(np.asarray(q_norm_gain, f)[0].reshape(1, 64)),
        "k_gain": np.ascontiguousarray(np.asarray(k_norm_gain, f)[0].reshape(1, 64)),
        "fox_gain": np.ascontiguousarray(np.asarray(fox_out_gain, f)[0].reshape(1, 512)),
        "ret_gain": np.ascontiguousarray(np.asarray(ret_out_gain, f)[0].reshape(1, 512)),
        "w_out": np.ascontiguousarray(np.asarray(w_out, f)[0]),
        "w1": np.ascontiguousarray(np.asarray(w_mlp_in, f)[0]),
        "w2": np.ascontiguousarray(np.asarray(w_mlp_out, f)[0]),
    }
    for k in ("identf", "identb", "trib", "negm", "trif", "onesf", "ixi_bc", "xi_bc", "zeta_t", "rope"):
        shared[k] = consts[k]
    in_maps = []
    for i in range(NCORES):
        m = dict(shared)
        m["x"] = np.ascontiguousarray(x[i * NSEQ:(i + 1) * NSEQ].reshape(NSEQ * S, D))
        m["c"] = np.ascontiguousarray(c[i * NSEQ:(i + 1) * NSEQ])
        in_maps.append(m)
    res = run_bass_kernel_spmd(nc, in_maps, core_ids=list(range(NCORES)))
    out = np.concatenate([np.asarray(r["y"], f).reshape(NSEQ, S, D) for r in res.results], axis=0)
    return out
```
